# Optimizing a Trainium2 kernel written in Bass

```python
import math
import jax, jax.numpy as jnp
from jax import lax
import numpy as np

D_MODEL = 2048
BATCH = 4
SEQ = 2048
DEPTH = 2
DEC_BATCH = 8
DEC_SEQ = 4096
PAST_LEN = 128

RW_HEADS = 12
RW_HEAD_DIM = 64
RW_WIDTH = RW_HEADS * RW_HEAD_DIM
DECAY_LORA = 64
ICLR_LORA = 64
GATE_LORA = 128
N_DIR = 2
GN_EPS = 64e-5
MLA_HEADS = 8
MLA_NOPE = 64
MLA_ROPE = 32
MLA_V = 64
MLA_Q_LORA = 512
MLA_KV_LORA = 256
MLA_WIDTH = MLA_HEADS * MLA_V
DF_HEADS = 6
DF_HEAD_DIM = 64
DF_WIDTH = DF_HEADS * 2 * DF_HEAD_DIM
DF_EPS = 1e-5
N_BRANCH = 3
N_EXPERTS = 16
N_GROUPS = 4
EXPERTS_PER_GROUP = N_EXPERTS // N_GROUPS
TOP_K = 2
D_EXPERT = 1024
ROPE_THETA = 10000.0
Q_BLOCK = 128
LN_EPS = 1e-5
RMS_EPS = 1e-6
ALPHA = (2 * DEPTH) ** 0.25
BETA = (8 * DEPTH) ** -0.25

RW_IN = 3 * RW_WIDTH + N_DIR * DECAY_LORA + N_DIR * ICLR_LORA + GATE_LORA
MLA_IN = MLA_Q_LORA + MLA_KV_LORA + MLA_ROPE
DF_IN = 3 * DF_WIDTH
GATE_IN = N_BRANCH * D_MODEL
IN_WIDTH = RW_IN + MLA_IN + DF_IN + GATE_IN
SPLIT_POINTS = [RW_IN, RW_IN + MLA_IN, RW_IN + MLA_IN + DF_IN]
RW_SPLIT_POINTS = [RW_WIDTH, 2 * RW_WIDTH, 3 * RW_WIDTH,
                   3 * RW_WIDTH + N_DIR * DECAY_LORA,
                   3 * RW_WIDTH + N_DIR * DECAY_LORA + N_DIR * ICLR_LORA]

kernel_name = 'hybrid_rwkv7_mla_diffattn_moe_encoder'

F32 = jnp.float32


def _layer_norm(x, g, b):
    xf = x.astype(F32)
    mu = jnp.mean(xf, -1, keepdims=True)
    var = jnp.mean(jnp.square(xf - mu), -1, keepdims=True)
    return ((xf - mu) * lax.rsqrt(var + LN_EPS) * g + b).astype(x.dtype)


def _rms_norm(x, g, eps):
    xf = x.astype(F32)
    return (xf * lax.rsqrt(jnp.mean(xf * xf, -1, keepdims=True) + eps) * g).astype(x.dtype)


def _rope(x):
    S, d = x.shape[1], x.shape[-1]
    half = d // 2
    inv_freq = jnp.power(ROPE_THETA, -jnp.arange(half, dtype=F32) * (2.0 / d))
    ang = jnp.arange(S, dtype=F32)[:, None] * inv_freq[None, :]
    shape = (1, S) + (1,) * (x.ndim - 3) + (half,)
    cos = jnp.cos(ang).reshape(shape)
    sin = jnp.sin(ang).reshape(shape)
    xf = x.astype(F32)
    x1, x2 = xf[..., :half], xf[..., half:]
    return jnp.concatenate([x1 * cos - x2 * sin, x2 * cos + x1 * sin], -1).astype(x.dtype)


def _sweep_query_blocks(block_fn, q):
    B, S, H, dq = q.shape
    nb = S // Q_BLOCK
    qb = jnp.moveaxis(q.reshape(B, nb, Q_BLOCK, H, dq), 1, 0)
    out = lax.map(block_fn, qb)
    return jnp.moveaxis(out, 0, 1).reshape(B, S, H, out.shape[-1])


def _centred_shift(z, mu_prev, mu_next):
    prev = jnp.pad(z, ((0, 0), (1, 0), (0, 0)))[:, :-1]
    nxt = jnp.pad(z, ((0, 0), (0, 1), (0, 0)))[:, 1:]
    return z + mu_prev * (prev - z) + mu_next * (nxt - z)


def _wkv7_scan(r, w, k, v, kk, a, reverse):
    B, S, H, N = r.shape

    def step(state, inp):
        r_t, w_t, k_t, v_t, kk_t, a_t = inp
        sa = jnp.einsum('bhvk,bhk->bhv', state, -kk_t)
        state = (state * w_t[:, :, None, :]
                 + sa[..., None] * (kk_t * a_t)[:, :, None, :]
                 + v_t[..., None] * k_t[:, :, None, :])
        return state, jnp.einsum('bhvk,bhk->bhv', state, r_t)

    xs = tuple(jnp.moveaxis(t, 1, 0) for t in (r, w, k, v, kk, a))
    s0 = jnp.zeros((B, H, N, N), F32)
    _, ys = lax.scan(step, s0, xs, reverse=reverse)
    return jnp.moveaxis(ys, 0, 1)


def _rwkv7_mixer(z, mu_prev, mu_next, w0, w2, a0, a2, g2, k_k, k_a, r_k, lnx_g, lnx_b):
    dtype = z.dtype
    z = _centred_shift(z.astype(F32), mu_prev, mu_next)
    B, S, _ = z.shape
    H, N, C = RW_HEADS, RW_HEAD_DIM, RW_WIDTH
    r, k, v, wl, al, gl = jnp.split(z, RW_SPLIT_POINTS, axis=-1)
    wl = wl.reshape(B, S, N_DIR, DECAY_LORA)
    al = al.reshape(B, S, N_DIR, ICLR_LORA)
    w_raw = w0 + jnp.einsum('bsdr,drc->bsdc', jnp.tanh(wl), w2)
    decay = jnp.exp(-jnp.exp(-jax.nn.softplus(-w_raw) - 0.5)).reshape(B, S, N_DIR, H, N)
    a = jax.nn.sigmoid(a0 + jnp.einsum('bsdr,drc->bsdc', al, a2)).reshape(B, S, N_DIR, H, N)
    g = jnp.einsum('bsr,rc->bsc', jax.nn.sigmoid(gl), g2)
    r = r.reshape(B, S, H, N)
    k = k.reshape(B, S, H, N)
    v = v.reshape(B, S, H, N)
    kk = k * k_k.reshape(H, N)
    kk = kk * lax.rsqrt(jnp.sum(kk * kk, -1, keepdims=True) + 1e-12)
    k_dir = k[:, :, None] * (1.0 + (a - 1.0) * k_a.reshape(H, N))
    y = (_wkv7_scan(r, decay[:, :, 0], k_dir[:, :, 0], v, kk, a[:, :, 0], reverse=False)
         + _wkv7_scan(r, decay[:, :, 1], k_dir[:, :, 1], v, kk, a[:, :, 1], reverse=True))
    mu = jnp.mean(y, -1, keepdims=True)
    var = jnp.mean(jnp.square(y - mu), -1, keepdims=True)
    y = ((y - mu) * lax.rsqrt(var + GN_EPS)).reshape(B, S, C) * lnx_g + lnx_b
    bonus = jnp.sum(jnp.sum(r[:, :, None] * k_dir * r_k, -1, keepdims=True) * v[:, :, None], axis=2)
    return ((y + bonus.reshape(B, S, C)) * g).astype(dtype)


def _mla_mixer(z, q_norm_g, kv_norm_g, w_uq, w_ukv):
    B, S, _ = z.shape
    H = MLA_HEADS
    c_q, c_kv, k_rope = jnp.split(z, [MLA_Q_LORA, MLA_Q_LORA + MLA_KV_LORA], axis=-1)
    c_q = _rms_norm(c_q, q_norm_g, RMS_EPS)
    c_kv = _rms_norm(c_kv, kv_norm_g, RMS_EPS)
    q = jnp.einsum('bsr,re->bse', c_q, w_uq).reshape(B, S, H, MLA_NOPE + MLA_ROPE)
    q = jnp.concatenate([q[..., :MLA_NOPE], _rope(q[..., MLA_NOPE:])], -1)
    kv = jnp.einsum('bsr,re->bse', c_kv, w_ukv).reshape(B, S, H, MLA_NOPE + MLA_V)
    k_nope, v = kv[..., :MLA_NOPE], kv[..., MLA_NOPE:]
    k_rope = jnp.broadcast_to(_rope(k_rope)[:, :, None, :], (B, S, H, MLA_ROPE))
    k = jnp.concatenate([k_nope, k_rope], -1)
    scale = (MLA_NOPE + MLA_ROPE) ** -0.5

    def block(qb):
        s = jnp.einsum('bqhd,bkhd->bhqk', qb, k).astype(F32) * scale
        p = jax.nn.softmax(s, axis=-1).astype(v.dtype)
        return jnp.einsum('bhqk,bkhd->bqhd', p, v)

    return _sweep_query_blocks(block, q).reshape(B, S, MLA_WIDTH)


def _diff_mixer(z, lq1, lk1, lq2, lk2, subln_g, lambda_init):
    B, S, _ = z.shape
    H, dh = DF_HEADS, DF_HEAD_DIM
    q, k, v = jnp.split(z, [DF_WIDTH, 2 * DF_WIDTH], axis=-1)
    q = _rope(q.reshape(B, S, H, 2, dh)).reshape(B, S, H, 2 * dh)
    k = _rope(k.reshape(B, S, H, 2, dh))
    k1, k2 = k[..., 0, :], k[..., 1, :]
    v = v.reshape(B, S, H, 2 * dh)
    lam = (jnp.exp(jnp.sum(lq1.astype(F32) * lk1.astype(F32)))
           - jnp.exp(jnp.sum(lq2.astype(F32) * lk2.astype(F32))) + lambda_init)
    scale = dh ** -0.5

    def block(qb):
        s1 = jnp.einsum('bqhd,bkhd->bhqk', qb[..., :dh], k1).astype(F32) * scale
        s2 = jnp.einsum('bqhd,bkhd->bhqk', qb[..., dh:], k2).astype(F32) * scale
        p = jax.nn.softmax(s1, axis=-1) - lam * jax.nn.softmax(s2, axis=-1)
        return jnp.einsum('bhqk,bkhd->bqhd', p.astype(v.dtype), v)

    o = _sweep_query_blocks(block, q)
    o = _rms_norm(o, subln_g, DF_EPS) * (1.0 - lambda_init)
    return o.reshape(B, S, DF_WIDTH)


def _moe(x, router_w, router_bias, w_gate, w_up, w_down):
    B, S, D = x.shape
    xf = x.reshape(B * S, D)
    scores = jax.nn.sigmoid(jnp.einsum('nd,de->ne', xf, router_w).astype(F32))
    sel = (scores + router_bias.astype(F32)).reshape(-1, N_GROUPS, EXPERTS_PER_GROUP)
    group_score = jnp.sum(lax.top_k(sel, TOP_K)[0], -1)
    best = jnp.argmax(group_score, -1)
    in_group = best[:, None] == jnp.arange(N_GROUPS)[None, :]
    masked = jnp.where(in_group[..., None], sel, -jnp.inf).reshape(-1, N_EXPERTS)
    _, idx = lax.top_k(masked, TOP_K)
    wts = jnp.take_along_axis(scores, idx, -1)
    wts = wts / jnp.sum(wts, -1, keepdims=True)
    gates = jnp.einsum('nk,nke->ne', wts, jax.nn.one_hot(idx, N_EXPERTS, dtype=F32))
    y = jnp.zeros((B * S, D), F32)
    for e in range(N_EXPERTS):
        h = jax.nn.silu(xf @ w_gate[e]) * (xf @ w_up[e])
        y = y + gates[:, e:e + 1] * (h @ w_down[e]).astype(F32)
    return y.astype(x.dtype).reshape(B, S, D)


def _trunk(x, w_in, shift_prev, shift_next, rw_w0, rw_w2, rw_a0, rw_a2, rw_g2, rw_k_k, rw_k_a,
           rw_r_k, rw_lnx_g, rw_lnx_b, mla_q_norm, mla_kv_norm, mla_w_uq, mla_w_ukv,
           df_lq1, df_lk1, df_lq2, df_lk2, df_subln, w_up_rw, w_up_mla, w_up_df, w_o,
           ln1_g, ln1_b, ln2_g, ln2_b, router_w, router_bias, ex_w_gate, ex_w_up, ex_w_down):
    B, S, D = x.shape
    for l in range(DEPTH):
        lambda_init = 0.8 - 0.6 * math.exp(-0.3 * l)
        z = jnp.einsum('bsd,de->bse', x, w_in[l])
        z_rw, z_mla, z_df, z_gate = jnp.split(z, SPLIT_POINTS, axis=-1)
        o_rw = _rwkv7_mixer(z_rw, shift_prev[l], shift_next[l], rw_w0[l], rw_w2[l], rw_a0[l],
                            rw_a2[l], rw_g2[l], rw_k_k[l], rw_k_a[l], rw_r_k[l],
                            rw_lnx_g[l], rw_lnx_b[l])
        o_mla = _mla_mixer(z_mla, mla_q_norm[l], mla_kv_norm[l], mla_w_uq[l], mla_w_ukv[l])
        o_df = _diff_mixer(z_df, df_lq1[l], df_lk1[l], df_lq2[l], df_lk2[l], df_subln[l],
                           lambda_init)
        g = jax.nn.sigmoid(z_gate.astype(F32)).reshape(B, S, N_BRANCH, D)
        merged = (g[:, :, 0] * jnp.einsum('bsc,cd->bsd', o_rw, w_up_rw[l])
                  + g[:, :, 1] * jnp.einsum('bsc,cd->bsd', o_mla, w_up_mla[l])
                  + g[:, :, 2] * jnp.einsum('bsc,cd->bsd', o_df, w_up_df[l]))
        mix = jnp.einsum('bsd,de->bse', merged.astype(x.dtype), w_o[l])
        x = _layer_norm(ALPHA * x + mix, ln1_g[l], ln1_b[l])
        ffn = _moe(x, router_w, router_bias, ex_w_gate[l], ex_w_up[l], ex_w_down[l])
        x = _layer_norm(ALPHA * x + ffn, ln2_g[l], ln2_b[l])
    return x


def setup_inputs(seed: int = 0) -> dict:
    key = jax.random.key(seed)
    ks = jax.random.split(key, 40)

    def nrm(i, shape, scale):
        return scale * jax.random.normal(ks[i], shape, F32)

    def unif(i, shape, lo, hi):
        return jax.random.uniform(ks[i], shape, F32, lo, hi)

    L, D, C = DEPTH, D_MODEL, RW_WIDTH
    return {
        'x_prompt': nrm(0, (BATCH, SEQ, D), 1.0),
        'x_sample': nrm(1, (DEC_BATCH, DEC_SEQ, D), 1.0),
        'w_in': nrm(2, (L, D, IN_WIDTH), D ** -0.5),
        'shift_prev': unif(3, (L, RW_IN), 0.05, 0.5),
        'shift_next': unif(4, (L, RW_IN), 0.05, 0.5),
        'rw_w0': unif(5, (L, N_DIR, C), -6.0, -1.0),
        'rw_w2': nrm(6, (L, N_DIR, DECAY_LORA, C), 0.1 * DECAY_LORA ** -0.5),
        'rw_a0': nrm(7, (L, N_DIR, C), 0.1),
        'rw_a2': nrm(8, (L, N_DIR, ICLR_LORA, C), 0.1 * ICLR_LORA ** -0.5),
        'rw_g2': nrm(9, (L, GATE_LORA, C), GATE_LORA ** -0.5),
        'rw_k_k': 0.85 + nrm(10, (L, C), 0.05),
        'rw_k_a': 1.0 + nrm(11, (L, C), 0.05),
        'rw_r_k': nrm(12, (L, RW_HEADS, RW_HEAD_DIM), 0.1),
        'rw_lnx_g': 1.0 + nrm(13, (L, C), 0.05),
        'rw_lnx_b': nrm(14, (L, C), 0.01),
        'mla_q_norm': 1.0 + nrm(15, (L, MLA_Q_LORA), 0.05),
        'mla_kv_norm': 1.0 + nrm(16, (L, MLA_KV_LORA), 0.05),
        'mla_w_uq': nrm(17, (L, MLA_Q_LORA, MLA_HEADS * (MLA_NOPE + MLA_ROPE)), MLA_Q_LORA ** -0.5),
        'mla_w_ukv': nrm(18, (L, MLA_KV_LORA, MLA_HEADS * (MLA_NOPE + MLA_V)), MLA_KV_LORA ** -0.5),
        'df_lq1': nrm(19, (L, DF_HEAD_DIM), 0.1),
        'df_lk1': nrm(20, (L, DF_HEAD_DIM), 0.1),
        'df_lq2': nrm(21, (L, DF_HEAD_DIM), 0.1),
        'df_lk2': nrm(22, (L, DF_HEAD_DIM), 0.1),
        'df_subln': 1.0 + nrm(23, (L, 2 * DF_HEAD_DIM), 0.05),
        'w_up_rw': nrm(24, (L, RW_WIDTH, D), RW_WIDTH ** -0.5),
        'w_up_mla': nrm(25, (L, MLA_WIDTH, D), MLA_WIDTH ** -0.5),
        'w_up_df': nrm(26, (L, DF_WIDTH, D), DF_WIDTH ** -0.5),
        'w_o': nrm(27, (L, D, D), BETA * D ** -0.5),
        'ln1_g': 1.0 + nrm(28, (L, D), 0.05),
        'ln1_b': nrm(29, (L, D), 0.01),
        'ln2_g': 1.0 + nrm(30, (L, D), 0.05),
        'ln2_b': nrm(31, (L, D), 0.01),
        'router_w': nrm(32, (D, N_EXPERTS), D ** -0.5),
        'router_bias': nrm(33, (N_EXPERTS,), 0.01),
        'ex_w_gate': nrm(34, (L, N_EXPERTS, D, D_EXPERT), D ** -0.5),
        'ex_w_up': nrm(35, (L, N_EXPERTS, D, D_EXPERT), D ** -0.5),
        'ex_w_down': nrm(36, (L, N_EXPERTS, D_EXPERT, D), BETA * D_EXPERT ** -0.5),
    }


def reference(x_prompt, x_sample, w_in, shift_prev, shift_next, rw_w0, rw_w2, rw_a0, rw_a2,
              rw_g2, rw_k_k, rw_k_a, rw_r_k, rw_lnx_g, rw_lnx_b, mla_q_norm, mla_kv_norm,
              mla_w_uq, mla_w_ukv, df_lq1, df_lk1, df_lq2, df_lk2, df_subln, w_up_rw, w_up_mla,
              w_up_df, w_o, ln1_g, ln1_b, ln2_g, ln2_b, router_w, router_bias, ex_w_gate,
              ex_w_up, ex_w_down):
    params = (w_in, shift_prev, shift_next, rw_w0, rw_w2, rw_a0, rw_a2, rw_g2, rw_k_k, rw_k_a,
              rw_r_k, rw_lnx_g, rw_lnx_b, mla_q_norm, mla_kv_norm, mla_w_uq, mla_w_ukv,
              df_lq1, df_lk1, df_lq2, df_lk2, df_subln, w_up_rw, w_up_mla, w_up_df, w_o,
              ln1_g, ln1_b, ln2_g, ln2_b, router_w, router_bias, ex_w_gate, ex_w_up, ex_w_down)
    y_prompt = _trunk(x_prompt, *params)
    y_sample = _trunk(x_sample, *params)
    return (y_prompt, y_sample)
```

```python
import contextlib
import numpy as np
import ml_dtypes
import concourse.bass as bass
import concourse.mybir as mybir
from concourse.bass_utils import run_bass_kernel_spmd

F32 = mybir.dt.float32
BF16 = mybir.dt.bfloat16
AF = mybir.ActivationFunctionType
ALU = mybir.AluOpType
AX = mybir.AxisListType

ENGS = ("pe", "act", "dve", "pool", "sp")

D = 2048
DEPTH = 2
IN_W = 11936
RW_IN, MLA_IN, DF_IN, GATE_IN = 2688, 800, 2304, 6144
C_RW = 768
NE, DE = 16, 1024
ALPHA = (2 * DEPTH) ** 0.25
LN_EPS = 1e-5
RMS_EPS = 1e-6
DF_EPS = 1e-5
GN_EPS = 64e-5
TT = 512
import os as _os
NOSELF = _os.environ.get("NOSELF", "0") == "1"


class _Op:
    __slots__ = ("eng", "fn", "reads", "writes", "dma", "semkey", "deps", "needed", "token")


class Prog:
    def __init__(self, nc):
        self.nc = nc
        self.ops = []
        self.st = contextlib.ExitStack()
        self.esem = {e: self.st.enter_context(nc.semaphore("s_" + e)) for e in ENGS}
        self.dsem = {}
        self.ecount = {e: 0 for e in ENGS}
        self.dcount = {}
        self.seen = {e: {} for e in ENGS}
        self.n_total = 0

    def add(self, eng, fn, reads=(), writes=(), dma=False, semkey=None):
        op = _Op()
        if eng != "pe":
            extra = [r for r in reads if isinstance(r, tuple) and r and r[0] == "ps" and r not in writes]
            if extra:
                writes = tuple(writes) + tuple(extra)
        op.eng, op.fn, op.reads, op.writes = eng, fn, tuple(reads), tuple(writes)
        op.dma, op.semkey = dma, semkey
        op.deps, op.needed, op.token = set(), False, None
        if dma and semkey not in self.dsem:
            self.dsem[semkey] = self.st.enter_context(self.nc.semaphore("d_%d" % len(self.dsem)))
            self.dcount[semkey] = 0
        self.ops.append(op)
        return op

    def barrier(self):
        for e in ENGS:
            self.add(e, None, reads=("__all__",))

    def _analyze(self):
        ops = self.ops
        last_w, readers, last_of_eng, last_dma = {}, {}, {}, {}
        for i, op in enumerate(ops):
            deps = set()
            if op.reads == ("__all__",):
                deps.update(last_of_eng.values())
                deps.update(last_dma.values())
            else:
                for r in op.reads:
                    if r in last_w:
                        deps.add(last_w[r])
                for w in op.writes:
                    if w in last_w:
                        deps.add(last_w[w])
                    deps.update(readers.get(w, ()))
                for r in op.reads:
                    readers.setdefault(r, []).append(i)
                for w in op.writes:
                    last_w[w] = i
                    readers[w] = []
            deps.discard(i)
            op.deps = deps
            if op.fn is not None:
                if op.dma:
                    last_dma[op.semkey] = i
                else:
                    last_of_eng[op.eng] = i
        for op in ops:
            for d in op.deps:
                p = ops[d]
                if p.dma:
                    continue
                if p.eng == op.eng and not op.dma and (p.eng == "pe" or NOSELF):
                    continue
                p.needed = True

    def flush(self):
        self.barrier()
        self._analyze()
        ops = self.ops
        waits = []
        for op in ops:
            w = {}
            for d in op.deps:
                p = ops[d]
                if p.dma:
                    s = ("d", p.semkey)
                    v = self.dcount[p.semkey]
                else:
                    if p.eng == op.eng and not op.dma and (p.eng == "pe" or NOSELF):
                        continue
                    s = ("e", p.eng)
                    v = p.token
                if v > w.get(s, 0):
                    w[s] = v
            wl = []
            for s, v in w.items():
                if self.seen[op.eng].get(s, 0) >= v:
                    continue
                self.seen[op.eng][s] = v
                wl.append((self.dsem[s[1]] if s[0] == "d" else self.esem[s[1]], v))
            waits.append(wl)
            if op.fn is not None:
                if op.dma:
                    self.dcount[op.semkey] += 16
                    op.token = self.dcount[op.semkey]
                elif op.needed:
                    self.ecount[op.eng] += 1
                    op.token = self.ecount[op.eng]
        dsem, esem = self.dsem, self.esem

        def run(ename, eng):
            for op, wl in zip(ops, waits):
                if op.eng != ename:
                    continue
                for s, v in wl:
                    eng.wait_ge(s, v)
                if op.fn is None:
                    continue
                ins = op.fn(eng)
                if op.dma:
                    ins.then_inc(dsem[op.semkey], 16)
                elif op.needed:
                    ins.then_inc(esem[op.eng], 1)

        with self.nc.Block() as block:
            @block.tensor
            def _(eng):
                run("pe", eng)

            @block.scalar
            def _(eng):
                run("act", eng)

            @block.vector
            def _(eng):
                run("dve", eng)

            @block.gpsimd
            def _(eng):
                run("pool", eng)

            @block.sync
            def _(eng):
                run("sp", eng)
        self.n_total += len(ops)
        self.ops = []

    def close(self):
        self.st.close()


def _rope_tables(d, S):
    half = d // 2
    inv = np.power(np.float32(10000.0), -np.arange(half, dtype=np.float32) * np.float32(2.0 / d)).astype(np.float32)
    ang = np.arange(S, dtype=np.float32)[None, :] * inv[:, None]
    return np.cos(ang).astype(np.float32), np.sin(ang).astype(np.float32)


def make_consts(Tmax):
    c = {}
    cos64, sin64 = _rope_tables(64, Tmax)
    cos32, sin32 = _rope_tables(32, Tmax)
    c["cosdf"] = np.concatenate([cos64, cos64, cos64, cos64], 0)
    c["sindf"] = np.concatenate([-sin64, sin64, -sin64, sin64], 0)
    perm = np.zeros((128, 128), np.float32)
    for m in range(128):
        blk, j = divmod(m, 64)
        perm[blk * 64 + (j + 32) % 64, m] = 1.0
    c["permdf"] = perm
    c["cos96"] = np.concatenate([np.ones((64, Tmax), np.float32), cos32, cos32], 0)
    c["sin96"] = np.concatenate([np.zeros((64, Tmax), np.float32), -sin32, sin32], 0)
    p96 = np.zeros((96, 96), np.float32)
    for m in range(96):
        if m < 64:
            p96[m, m] = 1.0
        else:
            j = m - 64
            p96[64 + (j + 16) % 32, m] = 1.0
    c["perm96"] = p96
    p32 = np.zeros((128, 32), np.float32)
    for m in range(32):
        p32[(m + 16) % 32, m] = 1.0
    c["perm32"] = p32
    permq = np.zeros((768, 128), np.float32)
    cosq = np.ones((768, Tmax), np.float32)
    sinq = np.zeros((768, Tmax), np.float32)
    for g in range(768):
        j, m = divmod(g, 128)
        h, d_ = divmod(g, 96)
        if d_ < 64:
            permq[j * 128 + m, m] = 1.0
        else:
            jj = d_ - 64
            g2 = h * 96 + 64 + (jj + 16) % 32
            assert g2 // 128 == j
            permq[j * 128 + (g2 - j * 128), m] = 1.0
            cosq[g] = cos32[jj % 16]
            sinq[g] = -sin32[jj] if jj < 16 else sin32[jj - 16]
    c["permq"], c["cosq"], c["sinq"] = permq, cosq, sinq
    permk = np.zeros((128, 128), np.float32)
    for m in range(32):
        permk[(m + 16) % 32, m] = 1.0
    c["permk"] = permk
    cosk = np.zeros((128, Tmax), np.float32)
    sink = np.zeros((128, Tmax), np.float32)
    cosk[:32] = np.concatenate([cos32, cos32], 0)
    sink[:32] = np.concatenate([-sin32, sin32], 0)
    c["cosk"], c["sink"] = cosk, sink
    c["ident"] = np.eye(128, dtype=np.float32)
    c["ones"] = np.ones((128, 128), np.float32)
    s = np.arange(128)[:, None]
    t = np.arange(128)[None, :]
    c["m_su"] = (s < t).astype(np.float32)
    c["m_iu"] = (s <= t).astype(np.float32)
    c["m_sl"] = (s > t).astype(np.float32)
    c["m_il"] = (s >= t).astype(np.float32)
    return c


CONST_SHAPES = lambda Tmax: {
    "cosdf": [128, Tmax], "sindf": [128, Tmax], "permdf": [128, 128],
    "cos96": [96, Tmax], "sin96": [96, Tmax], "perm96": [96, 96], "perm32": [128, 32],
    "ident": [128, 128], "ones": [128, 128],
    "permq": [768, 128], "cosq": [768, Tmax], "sinq": [768, Tmax], "permk": [128, 128], "cosk": [128, Tmax], "sink": [128, Tmax],
    "m_su": [128, 128], "m_iu": [128, 128], "m_sl": [128, 128], "m_il": [128, 128],
}

WEIGHT_SHAPES = {
    "w_in": [2, 2048, 11936], "shift_prev": [2, 2688], "shift_next": [2, 2688],
    "rw_w0": [2, 2, 768], "rw_w2": [2, 2, 64, 768], "rw_a0": [2, 2, 768], "rw_a2": [2, 2, 64, 768],
    "rw_g2": [2, 128, 768], "rw_k_k": [2, 768], "rw_k_a": [2, 768], "rw_r_k": [2, 768],
    "rw_lnx_g": [2, 768], "rw_lnx_b": [2, 768], "mla_q_norm": [2, 512], "mla_kv_norm": [2, 256],
    "mla_w_uq": [2, 512, 768], "mla_w_ukv": [2, 256, 1024], "df_lq1": [2, 64], "df_lk1": [2, 64],
    "df_lq2": [2, 64], "df_lk2": [2, 64], "df_subln": [2, 128], "w_up_rw": [2, 768, 2048],
    "w_up_mla": [2, 512, 2048], "w_up_df": [2, 768, 2048], "w_o": [2, 2048, 2048],
    "ln1_g": [2, 2048], "ln1_b": [2, 2048], "ln2_g": [2, 2048], "ln2_b": [2, 2048],
    "router_w": [2048, 16], "router_bias": [1, 16],
    "ex_w_gate": [2, 16, 2048, 1024], "ex_w_up": [2, 16, 2048, 1024], "ex_w_down": [2, 16, 1024, 2048],
}


class _Lazy:
    def __init__(self, nc, shapes):
        self.nc, self.shapes, self.d = nc, shapes, {}

    def __getitem__(self, k):
        if k not in self.d:
            self.d[k] = self.nc.dram_tensor(k, self.shapes[k], F32, kind="ExternalInput").ap()
        return self.d[k]

    def used(self):
        return list(self.d.keys())


class Builder:
    def __init__(self, seqs, dbg=(), n_layers=DEPTH, phases=None):
        self.seqs = list(seqs)
        self.Tmax = max(seqs)
        self.dbg = set(dbg)
        self.n_layers = n_layers
        self.phases = phases
        nc = self.nc = bass.Bass("TRN2", target_bir_lowering=False)
        self.P = Prog(nc)
        self.W = _Lazy(nc, WEIGHT_SHAPES)
        self.C = _Lazy(nc, CONST_SHAPES(self.Tmax))
        self.xin = [nc.dram_tensor("xs%d" % i, [T, D], F32, kind="ExternalInput").ap() for i, T in enumerate(seqs)]
        self.yout = [nc.dram_tensor("ys%d" % i, [T, D], F32, kind="ExternalOutput").ap() for i, T in enumerate(seqs)]
        Tm = self.Tmax
        self.S = {}
        for name, shape, dt in [
            ("xmid", [Tm, D], F32),
            ("zrw", [Tm, RW_IN], F32),
            ("cqT", [512, Tm], F32), ("ckvT", [256, Tm], F32), ("krT", [32, Tm], F32),
            ("dfqkT", [24 * 64, Tm], BF16),
            ("vdf", [Tm, 768], BF16),
            ("sgT", [GATE_IN, Tm], BF16),
            ("qmT", [8 * 96, Tm], BF16), ("kmT", [8 * 96, Tm], BF16), ("vm", [Tm, 512], BF16),
            ("oT", [D, Tm], BF16),
            ("yrw", [Tm, C_RW], F32),
            ("bon", [Tm, C_RW], F32),
        ]:
            kind = "ExternalOutput" if name in self.dbg else "Internal"
            self.S[name] = nc.dram_tensor("scr_" + name, shape, dt, kind=kind).ap()
        self._bank = 0

    def uid(self):
        self._uid = getattr(self, "_uid", 0) + 1
        return self._uid

    def nb(self):
        b = self._bank
        self._bank = (self._bank + 1) % 8
        return b

    def build(self):
        nc, P = self.nc, self.P
        with contextlib.ExitStack() as gst:
            self.ps = [gst.enter_context(nc.psum_tensor("ps%d" % b, [128, 512], F32)) for b in range(8)]
            for si, T in enumerate(self.seqs):
                for l in range(self.n_layers):
                    xin = self.xin[si] if l == 0 else self.S["xmid"]
                    xout = self.yout[si] if l == self.n_layers - 1 else self.S["xmid"]
                    ph = self.phases
                    if ph is None or "A" in ph:
                        self.phase_inproj(l, xin, T)
                    if ph is None or "B" in ph:
                        self.phase_mla(l, T)
                    if ph is None or "C" in ph:
                        self.phase_diff(l, T)
                    if ph is None or "D" in ph:
                        self.phase_rwkv(l, T)
                    if ph is None or "E" in ph:
                        self.phase_ffn(l, xin, xout, T)
        P.close()
        return nc

    def phase_inproj(self, l, xin, T):
        nc, P, S, C, ps = self.nc, self.P, self.S, self.C, self.ps
        w_in = self.W["w_in"][l].rearrange("(kc p) n -> p kc n", p=128)
        with contextlib.ExitStack() as st:
            sb = lambda name, shape, dt: st.enter_context(nc.sbuf_tensor("A%d_" % self.uid() + name, shape, dt))
            xt = sb("xt", [128, 4, D], BF16)
            xT = sb("xT", [128, 16, TT], BF16)
            wt = [sb("wt%d" % i, [128, 16, 512], BF16) for i in range(2)]
            stgb = [sb("stgb%d" % i, [128, 4, 512], BF16) for i in range(2)]
            stgf = [sb("stgf%d" % i, [128, 4, 512], F32) for i in range(2)]
            zb = [sb("zb%d" % i, [128, 512], BF16) for i in range(2)]
            t1 = [sb("t1_%d" % i, [128, 512], F32) for i in range(2)]
            t2 = [sb("t2_%d" % i, [128, 512], F32) for i in range(2)]
            idn = sb("idn", [128, 128], BF16)
            perm = sb("perm", [128, 128], BF16)
            cosb = sb("cos", [128, TT], F32)
            sinb = sb("sin", [128, TT], F32)
            P.add("pool", lambda e: e.dma_start(out=idn[:], in_=C["ident"]), writes=["idn"], dma=True, semkey="c0")
            P.add("pool", lambda e: e.dma_start(out=perm[:], in_=C["permdf"]), writes=["perm"], dma=True, semkey="c1")
            cnt = {"w": 0, "sb": 0, "sf": 0, "z": 0, "ev": 0}

            def evac_copy(out_ap, in_ap, reads, writes):
                cnt["ev"] += 1
                if cnt["ev"] % 2 == 0:
                    P.add("act", lambda e: e.activation(out_ap, in_ap, AF.Copy), reads=reads, writes=writes)
                else:
                    P.add("dve", lambda e: e.tensor_copy(out_ap, in_ap), reads=reads, writes=writes)

            blocks = []
            for c0 in range(0, RW_IN, 512):
                blocks.append((c0, min(512, RW_IN - c0), "rw"))
            blocks.append((RW_IN, 512, "cq"))
            blocks.append((RW_IN + 512, 288, "ckv"))
            base = RW_IN + MLA_IN
            for which in range(2):
                blocks.append((base + which * 768, 512, "dfqk"))
                blocks.append((base + which * 768 + 512, 256, "dfqk"))
            blocks.append((base + 1536, 512, "dfv"))
            blocks.append((base + 1536 + 512, 256, "dfv"))
            gbase = base + DF_IN
            for c0 in range(0, GATE_IN, 512):
                blocks.append((gbase + c0, 512, "gate"))

            for ti in range(T // TT):
                t0 = ti * TT
                P.add("pool", lambda e, t0=t0: e.dma_start(
                    out=xt[:], in_=xin[t0:t0 + TT, :].rearrange("(s p) d -> p s d", p=128)),
                    writes=["xt"], dma=True, semkey="xt")
                P.add("sp", lambda e, t0=t0: e.dma_start(out=cosb[:], in_=C["cosdf"][:, t0:t0 + TT]),
                      writes=["cos"], dma=True, semkey="cos")
                P.add("sp", lambda e, t0=t0: e.dma_start(out=sinb[:], in_=C["sindf"][:, t0:t0 + TT]),
                      writes=["sin"], dma=True, semkey="sin")
                for g in range(8):
                    b = self.nb()
                    pv = ps[b][:].bitcast(BF16)
                    for kk in range(2):
                        kc = 2 * g + kk
                        for s in range(4):
                            P.add("pe", lambda e, pv=pv, kk=kk, s=s, kc=kc: e.transpose(
                                pv[:, kk * 512 + s * 128: kk * 512 + (s + 1) * 128],
                                xt[:, s, kc * 128:(kc + 1) * 128], idn[:]),
                                reads=["xt", "idn"], writes=[("ps", b)])
                    evac_copy(xT[:, 2 * g:2 * g + 2, :], pv.rearrange("p (a t) -> p a t", a=2),
                              [("ps", b)], [("xT", g)])
                xT_all = [("xT", g) for g in range(8)]

                for (c0, w, kind) in blocks:
                    if getattr(self, "only_kinds", None) is not None and kind not in self.only_kinds:
                        continue
                    ws = cnt["w"] % 2
                    cnt["w"] += 1
                    P.add("pool", lambda e, ws=ws, c0=c0, w=w: e.dma_start(
                        out=wt[ws][:, :, :w], in_=w_in[:, :, c0:c0 + w]),
                        writes=[("wt", ws)], dma=True, semkey="wt%d" % ws)
                    if kind in ("rw", "dfv"):
                        if kind == "rw":
                            ss = cnt["sf"] % 2
                            cnt["sf"] += 1
                            stg, skey = stgf[ss], ("stgf", ss)
                        else:
                            ss = cnt["sb"] % 2
                            cnt["sb"] += 1
                            stg, skey = stgb[ss], ("stgb", ss)
                        for s in range(4):
                            b = self.nb()
                            for kc in range(16):
                                P.add("pe", lambda e, b=b, kc=kc, s=s, ws=ws, w=w: e.matmul(
                                    ps[b][:, :w], xT[:, kc, s * 128:(s + 1) * 128], wt[ws][:, kc, :w],
                                    start=(kc == 0), stop=(kc == 15)),
                                    reads=xT_all + [("wt", ws)], writes=[("ps", b)])
                            evac_copy(stg[:, s, :w], ps[b][:, :w], [("ps", b)], [skey])
                        if kind == "rw":
                            dst = S["zrw"][t0:t0 + TT, c0:c0 + w].rearrange("(s p) n -> p s n", p=128)
                        else:
                            cc = c0 - (base + 1536)
                            dst = S["vdf"][t0:t0 + TT, cc:cc + w].rearrange("(s p) n -> p s n", p=128)
                        P.add("sp", lambda e, dst=dst, stg=stg, w=w: e.dma_start(out=dst, in_=stg[:, :, :w]),
                              reads=[skey], dma=True, semkey="st_%s%d" % (skey[0], ss))
                    else:
                        if kind in ("cq", "ckv"):
                            ss = cnt["sf"] % 2
                            cnt["sf"] += 1
                            stg, skey = stgf[ss], ("stgf", ss)
                        else:
                            ss = cnt["sb"] % 2
                            cnt["sb"] += 1
                            stg, skey = stgb[ss], ("stgb", ss)
                        chunks = [(j, min(128, w - j)) for j in range(0, w, 128)]
                        for ci, (j, cw) in enumerate(chunks):
                            b = self.nb()
                            for kc in range(16):
                                P.add("pe", lambda e, b=b, kc=kc, j=j, cw=cw, ws=ws: e.matmul(
                                    ps[b][:cw, :], wt[ws][:, kc, j:j + cw], xT[:, kc, :],
                                    start=(kc == 0), stop=(kc == 15)),
                                    reads=xT_all + [("wt", ws)], writes=[("ps", b)])
                            if kind == "gate":
                                P.add("act", lambda e, b=b, ci=ci, stg=stg: e.activation(
                                    stg[:, ci, :], ps[b][:], AF.Sigmoid), reads=[("ps", b)], writes=[skey])
                            elif kind == "dfqk":
                                zs = cnt["z"] % 2
                                cnt["z"] += 1
                                b2 = self.nb()
                                P.add("dve", lambda e, b=b, zs=zs: e.tensor_copy(zb[zs][:], ps[b][:]),
                                      reads=[("ps", b)], writes=[("zb", zs)])
                                import os
                                _m = os.environ.get("DFQK_MODE", "")
                                if _m == "1":
                                    b2 = b
                                else:
                                    P.add("pe", lambda e, b2=b2, zs=zs: e.matmul(ps[b2][:], perm[:], zb[zs][:],
                                                                               start=True, stop=True),
                                          reads=[("zb", zs), "perm"], writes=[("ps", b2)])
                                P.add("dve", lambda e, b=b, zs=zs: e.tensor_tensor(t1[zs][:], ps[b][:], cosb[:], ALU.mult),
                                      reads=[("ps", b), "cos"], writes=[("t1", zs)])
                                P.add("dve", lambda e, b2=b2, zs=zs: e.tensor_tensor(t2[zs][:], ps[b2][:], sinb[:], ALU.mult),
                                      reads=[("ps", b2), "sin"], writes=[("t2", zs)])
                                P.add(os.environ.get("ADD_ENG", "pool"), lambda e, zs=zs, ci=ci, stg=stg: e.tensor_tensor(
                                    stg[:, ci, :], t1[zs][:], t2[zs][:], ALU.add),
                                    reads=[("t1", zs), ("t2", zs)], writes=[skey])
                            else:
                                evac_copy(stg[:cw, ci, :], ps[b][:cw, :], [("ps", b)], [skey])
                        stkey = "st_%s%d" % (skey[0], ss)
                        if kind == "gate":
                            r0 = c0 - gbase
                            dst = S["sgT"][r0:r0 + 512, t0:t0 + TT].rearrange("(c p) t -> p c t", p=128)
                            P.add("sp", lambda e, dst=dst, stg=stg: e.dma_start(out=dst, in_=stg[:]),
                                  reads=[skey], dma=True, semkey=stkey)
                        elif kind == "dfqk":
                            r0 = c0 - base
                            nch = len(chunks)
                            dst = S["dfqkT"][r0:r0 + w, t0:t0 + TT].rearrange("(c p) t -> p c t", p=128)
                            P.add("sp", lambda e, dst=dst, stg=stg, nch=nch: e.dma_start(out=dst, in_=stg[:, :nch, :]),
                                  reads=[skey], dma=True, semkey=stkey)
                        elif kind == "cq":
                            dst = S["cqT"][:, t0:t0 + TT].rearrange("(c p) t -> p c t", p=128)
                            P.add("sp", lambda e, dst=dst, stg=stg: e.dma_start(out=dst, in_=stg[:]),
                                  reads=[skey], dma=True, semkey=stkey)
                        else:
                            dst = S["ckvT"][:, t0:t0 + TT].rearrange("(c p) t -> p c t", p=128)
                            P.add("sp", lambda e, dst=dst, stg=stg: e.dma_start(out=dst, in_=stg[:, 0:2, :]),
                                  reads=[skey], dma=True, semkey=stkey)
                            dst2 = S["krT"][:, t0:t0 + TT]
                            P.add("sp", lambda e, dst2=dst2, stg=stg: e.dma_start(out=dst2, in_=stg[:32, 2, :]),
                                  reads=[skey], dma=True, semkey=stkey)
            P.flush()

    def phase_mla(self, l, T):
        nc, P, S, C, W, ps = self.nc, self.P, self.S, self.C, self.W, self.ps
        NT = T // TT
        scale = 96.0 ** -0.5
        with contextlib.ExitStack() as st:
            sb = lambda name, shape, dt: st.enter_context(nc.sbuf_tensor("B%d_" % self.uid() + name, shape, dt))
            onesb = sb("ones", [128, 128], BF16)
            pq = sb("pq", [128, 6, 128], BF16)
            pk = sb("pk", [128, 128], BF16)
            wkv = sb("wkv", [128, 2, 1024], BF16)
            KR = sb("KR", [128, TT], F32)
            cosq = sb("cosq", [128, 6, TT], F32)
            sinq = sb("sinq", [128, 6, TT], F32)
            cosk = sb("cosk", [128, TT], F32)
            sink = sb("sink", [128, TT], F32)
            wuq = sb("wuq", [128, 4, 768], BF16)
            wv = sb("wv", [128, 2, 8, 64], BF16)
            gq = sb("gq", [128, 4], F32)
            gkv = sb("gkv", [128, 2], F32)
            cq = sb("cq", [128, 4, TT], F32)
            ckv = sb("ckv", [128, 2, TT], F32)
            sq = sb("sq", [128, 4, TT], BF16)
            rt = sb("rt", [128, TT], F32)
            rstd = sb("rstd", [128, TT], F32)
            cqn = sb("cqn", [128, 4, TT], BF16)
            ckvn = sb("ckvn", [128, 2, TT], BF16)
            zb = [sb("zb%d" % i, [128, TT], BF16) for i in range(2)]
            for i in range(2):
                P.add("pool", lambda e, i=i: e.memset(zb[i][:], 0.0), writes=[("zb", i)])
            t1 = [sb("t1_%d" % i, [128, TT], F32) for i in range(2)]
            t2 = [sb("t2_%d" % i, [128, TT], F32) for i in range(2)]
            P.add("pool", lambda e: e.memset(KR[:], 0.0), writes=["kr"])
            qstg = [sb("qstg%d" % i, [128, 3, TT], BF16) for i in range(2)]
            kstg = [sb("kstg%d" % i, [128, 4, TT], BF16) for i in range(2)]
            krr = sb("krr", [128, TT], BF16)
            vstg = sb("vstg", [128, 4, 512], BF16)
            ld = lambda eng, out, in_, key: P.add(eng, lambda e: e.dma_start(out=out, in_=in_), writes=[key], dma=True, semkey="L_" + key)
            ld("pool", onesb[:], C["ones"], "ones")
            ld("pool", pq[:], C["permq"].rearrange("(j k) m -> k j m", k=128), "pq")
            ld("pool", pk[:], C["permk"], "pk")
            ld("pool", wkv[:], W["mla_w_ukv"][l].rearrange("(kc p) n -> p kc n", p=128), "wkv")
            ld("pool", wuq[:], W["mla_w_uq"][l].rearrange("(kc p) n -> p kc n", p=128), "wuq")
            wukv = W["mla_w_ukv"][l].rearrange("(kc p) (h c) -> kc p h c", p=128, c=128)
            for kc in range(2):
                P.add("pool", lambda e, kc=kc: e.dma_start(out=wv[:, kc, :, :], in_=wukv[kc, :, :, 64:128]),
                      writes=["wv"], dma=True, semkey="L_wv")
            for kc in range(4):
                P.add("sp", lambda e, kc=kc: e.dma_start(
                    out=gq[:, kc:kc + 1], in_=W["mla_q_norm"][l, kc * 128:(kc + 1) * 128].rearrange("(p o) -> p o", o=1)),
                    writes=["gq"], dma=True, semkey="L_gq")
            for kc in range(2):
                P.add("sp", lambda e, kc=kc: e.dma_start(
                    out=gkv[:, kc:kc + 1], in_=W["mla_kv_norm"][l, kc * 128:(kc + 1) * 128].rearrange("(p o) -> p o", o=1)),
                    writes=["gkv"], dma=True, semkey="L_gkv")
            cnt = {"z": 0, "q": 0, "k": 0}
            for ti in range(NT):
                t0 = ti * TT
                ld("sp", cq[:], S["cqT"][:, t0:t0 + TT].rearrange("(c p) t -> p c t", p=128), "cq")
                ld("sp", ckv[:], S["ckvT"][:, t0:t0 + TT].rearrange("(c p) t -> p c t", p=128), "ckv")
                ld("sp", KR[:32, :], S["krT"][:, t0:t0 + TT], "kr")
                ld("sp", cosq[:], C["cosq"][:, t0:t0 + TT].rearrange("(c p) t -> p c t", p=128), "cosq")
                ld("sp", sinq[:], C["sinq"][:, t0:t0 + TT].rearrange("(c p) t -> p c t", p=128), "sinq")
                ld("sp", cosk[:], C["cosk"][:, t0:t0 + TT], "cosk")
                ld("sp", sink[:], C["sink"][:, t0:t0 + TT], "sink")

                def rmsnorm(src, key, nk, g, dst, dkey, width):
                    b = self.nb()
                    P.add("act", lambda e: e.activation(sq[:, :nk, :], src[:], AF.Square), reads=[key], writes=["sq"])
                    for kc in range(nk):
                        P.add("pe", lambda e, kc=kc: e.matmul(ps[b][:], onesb[:], sq[:, kc, :], start=(kc == 0), stop=(kc == nk - 1)),
                              reads=["sq", "ones"], writes=[("ps", b)])
                    P.add("act", lambda e: e.activation(rt[:], ps[b][:], AF.Sqrt, bias=RMS_EPS, scale=1.0 / width),
                          reads=[("ps", b)], writes=["rt"])
                    P.add("dve", lambda e: e.reciprocal(rstd[:], rt[:]), reads=["rt"], writes=["rstd"])
                    for kc in range(nk):
                        P.add("dve" if kc % 2 == 0 else "pool", lambda e, kc=kc: (
                            e.scalar_tensor_tensor(dst[:, kc, :], src[:, kc, :], g[:, kc:kc + 1], rstd[:], ALU.mult, ALU.mult)
                            if False else e.scalar_tensor_tensor(dst[:, kc, :], src[:, kc, :], g[:, kc:kc + 1], rstd[:], ALU.mult, ALU.mult)),
                            reads=[key, "rstd", "gq", "gkv"], writes=[dkey]) if False else None
                        P.add("dve", lambda e, kc=kc: e.scalar_tensor_tensor(
                            dst[:, kc, :], src[:, kc, :], g[:, kc:kc + 1], rstd[:], ALU.mult, ALU.mult),
                            reads=[key, "rstd", "gq", "gkv"], writes=[dkey])

                rmsnorm(cq, "cq", 4, gq, cqn, "cqn", 512.0)
                rmsnorm(ckv, "ckv", 2, gkv, ckvn, "ckvn", 256.0)
                for j in range(6):
                    b = self.nb()
                    for kc in range(4):
                        P.add("pe", lambda e, b=b, kc=kc, j=j: e.matmul(
                            ps[b][:], wuq[:, kc, j * 128:(j + 1) * 128], cqn[:, kc, :], start=(kc == 0), stop=(kc == 3)),
                            reads=["wuq", "cqn"], writes=[("ps", b)])
                    zs = cnt["z"] % 2
                    cnt["z"] += 1
                    b2 = self.nb()
                    P.add("act", lambda e, b=b, zs=zs: e.activation(zb[zs][:], ps[b][:], AF.Copy),
                          reads=[("ps", b)], writes=[("zb", zs)])
                    P.add("pe", lambda e, b2=b2, zs=zs, j=j: e.matmul(ps[b2][:], pq[:, j, :], zb[zs][:], start=True, stop=True),
                          reads=[("zb", zs), "pq"], writes=[("ps", b2)])
                    P.add("dve", lambda e, b=b, zs=zs, j=j: e.tensor_tensor(t1[zs][:], ps[b][:], cosq[:, j, :], ALU.mult),
                          reads=[("ps", b), "cosq"], writes=[("t1", zs)])
                    P.add("dve", lambda e, b2=b2, zs=zs, j=j: e.tensor_tensor(t2[zs][:], ps[b2][:], sinq[:, j, :], ALU.mult),
                          reads=[("ps", b2), "sinq"], writes=[("t2", zs)])
                    qs = (cnt["q"] // 3) % 2
                    cnt["q"] += 1
                    P.add("pool", lambda e, zs=zs, qs=qs, j=j: e.tensor_tensor(qstg[qs][:, j % 3, :], t1[zs][:], t2[zs][:], ALU.add),
                          reads=[("t1", zs), ("t2", zs)], writes=[("qstg", qs)])
                    if j % 3 == 2:
                        j0 = j - 2
                        dst = S["qmT"][j0 * 128:(j0 + 3) * 128, t0:t0 + TT].rearrange("(c p) t -> p c t", p=128)
                        P.add("sp", lambda e, dst=dst, qs=qs: e.dma_start(out=dst, in_=qstg[qs][:]),
                              reads=[("qstg", qs)], dma=True, semkey="st_q%d" % qs)
                for h in range(8):
                    b = self.nb()
                    for kc in range(2):
                        P.add("pe", lambda e, b=b, kc=kc, h=h: e.matmul(
                            ps[b][:], wkv[:, kc, h * 128:(h + 1) * 128], ckvn[:, kc, :], start=(kc == 0), stop=(kc == 1)),
                            reads=["wkv", "ckvn"], writes=[("ps", b)])
                    ks = (cnt["k"] // 4) % 2
                    cnt["k"] += 1
                    if h % 2 == 0:
                        P.add("act", lambda e, b=b, ks=ks, h=h: e.activation(kstg[ks][:, h % 4, :], ps[b][:], AF.Copy),
                              reads=[("ps", b)], writes=[("kstg", ks)])
                    else:
                        P.add("dve", lambda e, b=b, ks=ks, h=h: e.tensor_copy(kstg[ks][:, h % 4, :], ps[b][:]),
                              reads=[("ps", b)], writes=[("kstg", ks)])
                    dst = S["kmT"][h * 96:h * 96 + 64, t0:t0 + TT]
                    P.add("sp", lambda e, dst=dst, ks=ks, h=h: e.dma_start(out=dst, in_=kstg[ks][:64, h % 4, :]),
                          reads=[("kstg", ks)], dma=True, semkey="st_k%d" % ks)
                zs = cnt["z"] % 2
                cnt["z"] += 1
                b2 = self.nb()
                P.add("act", lambda e, zs=zs: e.activation(zb[zs][:], KR[:], AF.Copy), reads=["kr"], writes=[("zb", zs)])
                P.add("pe", lambda e, b2=b2, zs=zs: e.matmul(ps[b2][:], pk[:], zb[zs][:], start=True, stop=True),
                      reads=[("zb", zs), "pk"], writes=[("ps", b2)])
                P.add("dve", lambda e, zs=zs: e.tensor_tensor(t1[zs][:], KR[:], cosk[:], ALU.mult),
                      reads=["kr", "cosk"], writes=[("t1", zs)])
                P.add("dve", lambda e, b2=b2, zs=zs: e.tensor_tensor(t2[zs][:], ps[b2][:], sink[:], ALU.mult),
                      reads=[("ps", b2), "sink"], writes=[("t2", zs)])
                P.add("pool", lambda e, zs=zs: e.tensor_tensor(krr[:], t1[zs][:], t2[zs][:], ALU.add),
                      reads=[("t1", zs), ("t2", zs)], writes=["krr"])
                for h in range(8):
                    dst = S["kmT"][h * 96 + 64:h * 96 + 96, t0:t0 + TT]
                    P.add("sp", lambda e, dst=dst: e.dma_start(out=dst, in_=krr[:32, :]), reads=["krr"], dma=True, semkey="st_kr")
                for s_ in range(4):
                    b = self.nb()
                    for kc in range(2):
                        P.add("pe", lambda e, b=b, kc=kc, s_=s_: e.matmul(
                            ps[b][:], ckvn[:, kc, s_ * 128:(s_ + 1) * 128], wv[:, kc, :, :].rearrange("p h c -> p (h c)"),
                            start=(kc == 0), stop=(kc == 1)), reads=["wv", "ckvn"], writes=[("ps", b)])
                    if s_ % 2 == 0:
                        P.add("act", lambda e, b=b, s_=s_: e.activation(vstg[:, s_, :], ps[b][:], AF.Copy),
                              reads=[("ps", b)], writes=["vstg"])
                    else:
                        P.add("dve", lambda e, b=b, s_=s_: e.tensor_copy(vstg[:, s_, :], ps[b][:]),
                              reads=[("ps", b)], writes=["vstg"])
                dst = S["vm"][t0:t0 + TT, :].rearrange("(s p) n -> p s n", p=128)
                P.add("sp", lambda e, dst=dst: e.dma_start(out=dst, in_=vstg[:]), reads=["vstg"], dma=True, semkey="st_v")
            P.flush()
        import os
        if "B2" in os.environ.get("SKIP", ""):
            return
        self.attention(T, nheads=8, parts=96, dv=64, scale=scale,
                       kT_rows=lambda h, c: (S["kmT"], h * 96), qT_rows=lambda h, c: (S["qmT"], h * 96),
                       v_src=lambda h: (S["vm"], h * 64), ncomp=1, out_row0=768, l=l)

    def attention(self, T, nheads, parts, dv, scale, kT_rows, qT_rows, v_src, ncomp, out_row0, l):
        nc, P, S, C, W, ps = self.nc, self.P, self.S, self.C, self.W, self.ps
        NT, NK = T // TT, T // 128
        lambda_init = 0.8 - 0.6 * np.exp(-0.3 * l)
        with contextlib.ExitStack() as st:
            sb = lambda name, shape, dt: st.enter_context(nc.sbuf_tensor("At%d_" % self.uid() + name, shape, dt))
            onesb = sb("ones", [128, 128], BF16)
            nrows = parts
            KT = [sb("KT%d" % i, [128, T], BF16) for i in range(2)]
            V = [sb("V%d" % i, [128, NK, dv], BF16) for i in range(2)]
            QT = [[sb("QT%d_%d" % (i, c), [128, TT], BF16) for c in range(ncomp)] for i in range(2)]
            for i in range(2):
                P.add("pool", lambda e, i=i: e.memset(KT[i][:], 0.0), writes=[("KT", i)])
                for c in range(ncomp):
                    P.add("pool", lambda e, i=i, c=c: e.memset(QT[i][c][:], 0.0), writes=[("QT", i, c)])
            pT = [sb("pT%d" % i, [128, TT], BF16) for i in range(3)]
            rD = [sb("rD%d" % i, [dv, TT], F32) for i in range(2)]
            ostg = [sb("ostg%d" % i, [dv, TT], BF16) for i in range(2)]
            P.add("pool", lambda e: e.dma_start(out=onesb[:], in_=C["ones"]), writes=["ones"], dma=True, semkey="L_ones")
            if ncomp == 2:
                lq = [sb("lq%d" % i, [128, 64], F32) for i in range(4)]
                lp = [sb("lp%d" % i, [128, 64], F32) for i in range(2)]
                lsum = sb("lsum", [128, 2], F32)
                lexp = sb("lexp", [128, 2], F32)
                neglam = sb("neglam", [128, 1], F32)
                gs0 = sb("gs0", [128, 1], F32)
                gs = sb("gs", [128, 1], F32)
                o1 = sb("o1", [128, TT], F32)
                o2 = sb("o2", [128, TT], F32)
                oo = sb("oo", [128, TT], F32)
                osq = sb("osq", [128, TT], BF16)
                ort = sb("ort", [128, TT], F32)
                orstd = sb("orstd", [128, TT], F32)
                for i, nm in enumerate(["df_lq1", "df_lk1", "df_lq2", "df_lk2"]):
                    P.add("sp", lambda e, i=i, nm=nm: e.dma_start(out=lq[i][:], in_=W[nm][l].partition_broadcast(128)),
                          writes=[("lq", i)], dma=True, semkey="L_lq%d" % i)
                P.add("sp", lambda e: e.dma_start(out=gs0[:], in_=W["df_subln"][l].rearrange("(p o) -> p o", o=1)),
                      writes=["gs0"], dma=True, semkey="L_gs0")
                for j in range(2):
                    P.add("dve", lambda e, j=j: e.tensor_tensor(lp[j][:], lq[2 * j][:], lq[2 * j + 1][:], ALU.mult),
                          reads=[("lq", 2 * j), ("lq", 2 * j + 1)], writes=[("lp", j)])
                    P.add("pool" if False else "dve", lambda e, j=j: e.tensor_reduce(lsum[:, j:j + 1], lp[j][:], AX.X, ALU.add),
                          reads=[("lp", j)], writes=["lsum"])
                P.add("act", lambda e: e.activation(lexp[:], lsum[:], AF.Exp), reads=["lsum"], writes=["lexp"])
                P.add("dve", lambda e: e.scalar_tensor_tensor(neglam[:], lexp[:, 1:2], -float(lambda_init), lexp[:, 0:1],
                                                              ALU.add, ALU.subtract), reads=["lexp"], writes=["neglam"])
                P.add("pool", lambda e: e.tensor_scalar(gs[:], gs0[:], float(1.0 - lambda_init), None, ALU.mult),
                      reads=["gs0"], writes=["gs"])
            cnt = {"p": 0, "o": 0}
            acc_banks = [3, 4, 5, 6] if ncomp == 2 else [3, 4, 5, 6]
            for h in range(nheads):
                hs = h % 2
                src, r0 = kT_rows(h, 0)
                nk_rows = nrows * ncomp
                P.add("sp", lambda e, hs=hs, src=src, r0=r0, nk_rows=nk_rows: e.dma_start(out=KT[hs][:nk_rows, :], in_=src[r0:r0 + nk_rows, 0:T]),
                      writes=[("KT", hs)], dma=True, semkey="L_KT%d" % hs)
                vsrc, c0 = v_src(h)
                P.add("sp", lambda e, hs=hs, vsrc=vsrc, c0=c0: e.dma_start(
                    out=V[hs][:], in_=vsrc[0:T, c0:c0 + dv].rearrange("(c p) d -> p c d", p=128)),
                    writes=[("V", hs)], dma=True, semkey="L_V%d" % hs)
                for qi in range(NT):
                    t0 = qi * TT
                    qs = (h * NT + qi) % 2
                    for c in range(ncomp):
                        src, r0 = qT_rows(h, c)
                        rr = r0 + c * nrows * (ncomp - 1)
                        pr = c * nrows * (ncomp - 1)
                        P.add("sp", lambda e, qs=qs, c=c, src=src, rr=rr, pr=pr, t0=t0: e.dma_start(
                            out=QT[qs][c][pr:pr + nrows, :], in_=src[rr:rr + nrows, t0:t0 + TT]),
                            writes=[("QT", qs, c)], dma=True, semkey="L_QT%d%d" % (qs, c))
                    accs = []
                    for c in range(ncomp):
                        if ncomp == 1:
                            bo, bd = acc_banks[2 * (cnt["o"] % 2)], acc_banks[2 * (cnt["o"] % 2) + 1]
                        else:
                            bo, bd = acc_banks[2 * c], acc_banks[2 * c + 1]
                        accs.append((bo, bd))
                        for kc in range(NK):
                            b = cnt["p"] % 3
                            pslot = cnt["p"] % 3
                            cnt["p"] += 1
                            P.add("pe", lambda e, b=b, hs=hs, c=c, kc=kc, qs=qs: e.matmul(
                                ps[b][:], KT[hs][:, kc * 128:(kc + 1) * 128], QT[qs][c][:], start=True, stop=True),
                                reads=[("KT", hs), ("QT", qs, c)], writes=[("ps", b)])
                            P.add("act", lambda e, b=b, pslot=pslot: e.activation(pT[pslot][:], ps[b][:], AF.Exp, scale=float(scale)),
                                  reads=[("ps", b)], writes=[("pT", pslot)])
                            P.add("pe", lambda e, bo=bo, hs=hs, kc=kc, pslot=pslot: e.matmul(
                                ps[bo][:dv, :], V[hs][:, kc, :], pT[pslot][:], start=(kc == 0), stop=(kc == NK - 1)),
                                reads=[("V", hs), ("pT", pslot)], writes=[("ps", bo)])
                            P.add("pe", lambda e, bd=bd, kc=kc, pslot=pslot: e.matmul(
                                ps[bd][:dv, :], onesb[:, :dv], pT[pslot][:], start=(kc == 0), stop=(kc == NK - 1)),
                                reads=["ones", ("pT", pslot)], writes=[("ps", bd)])
                    os_ = cnt["o"] % 2
                    cnt["o"] += 1
                    if ncomp == 1:
                        bo, bd = accs[0]
                        P.add("dve", lambda e, bd=bd, os_=os_: e.reciprocal(rD[os_][:], ps[bd][:dv, :]),
                              reads=[("ps", bd)], writes=[("rD", os_)])
                        P.add("dve", lambda e, bo=bo, os_=os_: e.tensor_tensor(ostg[os_][:], ps[bo][:dv, :], rD[os_][:], ALU.mult),
                              reads=[("ps", bo), ("rD", os_)], writes=[("ostg", os_)])
                    else:
                        (bo1, bd1), (bo2, bd2) = accs
                        P.add("dve", lambda e, bd1=bd1: e.reciprocal(rD[0][:], ps[bd1][:]), reads=[("ps", bd1)], writes=[("rD", 0)])
                        P.add("dve", lambda e, bd2=bd2: e.reciprocal(rD[1][:], ps[bd2][:]), reads=[("ps", bd2)], writes=[("rD", 1)])
                        P.add("dve", lambda e, bo1=bo1: e.tensor_tensor(o1[:], ps[bo1][:], rD[0][:], ALU.mult),
                              reads=[("ps", bo1), ("rD", 0)], writes=["o1"])
                        P.add("dve", lambda e, bo2=bo2: e.tensor_tensor(o2[:], ps[bo2][:], rD[1][:], ALU.mult),
                              reads=[("ps", bo2), ("rD", 1)], writes=["o2"])
                        P.add("dve", lambda e: e.scalar_tensor_tensor(oo[:], o2[:], neglam[:], o1[:], ALU.mult, ALU.add),
                              reads=["o1", "o2", "neglam"], writes=["oo"])
                        P.add("act", lambda e: e.activation(osq[:], oo[:], AF.Square), reads=["oo"], writes=["osq"])
                        bs = 7
                        P.add("pe", lambda e: e.matmul(ps[bs][:], onesb[:], osq[:], start=True, stop=True),
                              reads=["osq", "ones"], writes=[("ps", bs)])
                        P.add("act", lambda e: e.activation(ort[:], ps[bs][:], AF.Sqrt, bias=DF_EPS, scale=1.0 / 128.0),
                              reads=[("ps", bs)], writes=["ort"])
                        P.add("dve", lambda e: e.reciprocal(orstd[:], ort[:]), reads=["ort"], writes=["orstd"])
                        P.add("dve", lambda e, os_=os_: e.scalar_tensor_tensor(ostg[os_][:], oo[:], gs[:], orstd[:], ALU.mult, ALU.mult),
                              reads=["oo", "gs", "orstd"], writes=[("ostg", os_)])
                    dst = S["oT"][out_row0 + h * dv:out_row0 + (h + 1) * dv, t0:t0 + TT]
                    P.add("sp", lambda e, dst=dst, os_=os_: e.dma_start(out=dst, in_=ostg[os_][:]),
                          reads=[("ostg", os_)], dma=True, semkey="st_o%d" % os_)
            P.flush()

    def phase_diff(self, l, T):
        S = self.S
        self.attention(T, nheads=6, parts=64, dv=128, scale=64.0 ** -0.5,
                       kT_rows=lambda h, c: (S["dfqkT"], (6 + h) * 128), qT_rows=lambda h, c: (S["dfqkT"], h * 128),
                       v_src=lambda h: (S["vdf"], h * 128), ncomp=2, out_row0=1280, l=l)

    def phase_rwkv(self, l, T):
        nc, P, S, C, W, ps = self.nc, self.P, self.S, self.C, self.W, self.ps
        NCH = T // 128
        CL = -float(np.exp(-0.5))
        u = self.uid()
        with contextlib.ExitStack() as st:
            sb = lambda name, shape, dt: st.enter_context(nc.sbuf_tensor("D%d_" % u + name, shape, dt))
            f768 = lambda name: sb(name, [128, 768], F32)
            b768 = lambda name: sb(name, [128, 768], BF16)
            mup, mun = sb("mup", [128, RW_IN], F32), sb("mun", [128, RW_IN], F32)
            kkbc, kabc, omka, rkbc, lgbc, lbbc = [f768(n) for n in ("kkbc", "kabc", "omka", "rkbc", "lgbc", "lbbc")]
            _w0 = f768("w0bc"); w0bc = [_w0, _w0]
            _a0 = f768("a0bc"); a0bc = [_a0, _a0]
            _w2 = sb("w2b", [128, 768], BF16); w2b = [_w2, _w2]
            _a2 = sb("a2b", [128, 768], BF16); a2b = [_a2, _a2]
            g2b = sb("g2b", [128, 768], BF16)
            _mk = sb("MK2", [128, 2, 2, 128], F32); MK2 = [_mk, _mk]
            _mn = sb("MN4", [128, 4, 128], F32); MN4 = [_mn, _mn]
            _tr = sb("TRI", [128, 128], F32); TRI = [_tr, _tr]
            onesf = sb("onesf", [128, 128], F32)
            idb = sb("idb", [128, 128], BF16)
            zc, zp, zn = [sb(n, [128, RW_IN], F32) for n in ("zc", "zp", "zn")]
            lorab = sb("lorab", [128, 256], BF16)
            lT = sb("lT", [128, 384], BF16)
            Ssg, a_, g_, kk, akk, kd, tA, tB, tC, E1, E2, bon, y_ = [f768(n) for n in (
                "Ssg", "a_", "g_", "kk", "akk", "kd", "tA", "tB", "tC", "E1", "E2", "bon", "y_")]
            yf, bf_ = E1, E2
            s12 = sb("s12", [128, 64], F32)
            gC = sb("gC", [64, 24], F32)
            Rt, Kt, At, Bt, Kcb, Bcb, Vb, Wb, Ub, ob = [b768(n) for n in ("Rt", "Kt", "At", "Bt", "Kcb", "Bcb", "Vb", "Wb", "Ub", "ob")]
            ART = sb("ART", [128, 12, 2, 128], BF16)
            KBT = sb("KBT", [128, 12, 2, 128], BF16)
            AKRK = sb("AKRK", [128, 12, 2, 128], BF16)
            ABRB = sb("ABRB", [128, 12, 2, 128], BF16)
            Pn = [sb("Pn%d" % i, [128, 12, 128], BF16) for i in range(2)]
            PnT = [sb("PnT%d" % i, [128, 12, 128], BF16) for i in range(2)]
            TTb = [sb("TTb%d" % i, [128, 12, 128], BF16) for i in range(2)]
            Hf = sb("Hf", [64, 12, 64], F32)
            Hb = sb("Hb", [128, 12, 64], BF16)
            ost = sb("ost", [128, 6, 128], BF16)

            def ld(eng, out, in_, key):
                P.add(eng, lambda e: e.dma_start(out=out, in_=in_), writes=[key], dma=True, semkey="L_" + key)

            def TT_(eng, out, a, b, op, rd, wr):
                P.add(eng, lambda e: e.tensor_tensor(out, a, b, op), reads=rd, writes=wr)

            def ACT_(out, in_, func, rd, wr, **kw):
                P.add("act", lambda e: e.activation(out, in_, func, **kw), reads=rd, writes=wr)

            ld("sp", mup[:], W["shift_prev"][l].partition_broadcast(128), "mup")
            ld("sp", mun[:], W["shift_next"][l].partition_broadcast(128), "mun")
            ld("sp", kkbc[:], W["rw_k_k"][l].partition_broadcast(128), "kkbc")
            ld("sp", kabc[:], W["rw_k_a"][l].partition_broadcast(128), "kabc")
            ld("sp", rkbc[:], W["rw_r_k"][l].partition_broadcast(128), "rkbc")
            ld("sp", lgbc[:], W["rw_lnx_g"][l].partition_broadcast(128), "lgbc")
            ld("sp", lbbc[:], W["rw_lnx_b"][l].partition_broadcast(128), "lbbc")
            P.add("pool", lambda e: e.tensor_scalar(omka[:], kabc[:], -1.0, 1.0, ALU.mult, ALU.add), reads=["kabc"], writes=["omka"])
            def load_dir_consts(d):
                ld("sp", w0bc[d][:], W["rw_w0"][l, d].partition_broadcast(128), "w0bc%d" % d)
                ld("sp", a0bc[d][:], W["rw_a0"][l, d].partition_broadcast(128), "a0bc%d" % d)
                ld("pool", w2b[d][:64, :], W["rw_w2"][l, d], "w2b%d" % d)
                ld("pool", a2b[d][:64, :], W["rw_a2"][l, d], "a2b%d" % d)
                strict, incl, nmask = ("m_su", "m_iu", "m_sl") if d == 0 else ("m_sl", "m_il", "m_su")
                for hh in range(2):
                    ld("sp", MK2[d][:, hh, 0, :], C[strict], "MK2_%d" % d)
                    ld("sp", MK2[d][:, hh, 1, :], C[incl], "MK2_%d" % d)
                for hh in range(4):
                    ld("sp", MN4[d][:, hh, :], C[nmask], "MN4_%d" % d)
                ld("sp", TRI[d][:], C[incl], "TRI%d" % d)
            ld("pool", g2b[:], W["rw_g2"][l], "g2b")
            for (tl, nm) in ((_w2, "w2b0"), (_a2, "a2b0"), (lT, "lT"), (ART, "ART"), (KBT, "KBT")):
                P.add("pool", lambda e, tl=tl: e.memset(tl[:], 0.0), writes=[nm])
            P.flush()
            ld("sp", onesf[:], C["ones"], "onesf")
            ld("pool", idb[:], C["ident"], "idb")

            for d in range(2):
                load_dir_consts(d)
                P.add("pool", lambda e: e.memset(Hf[:], 0.0), writes=["Hf"])
                P.add("pool", lambda e: e.memset(Hb[:], 0.0), writes=["Hb"])
                order = list(range(NCH)) if d == 0 else list(range(NCH - 1, -1, -1))
                for ci in order:
                    t0 = ci * 128
                    ld("sp", zc[:], S["zrw"][t0:t0 + 128, :], "zc")
                    if t0 == 0:
                        P.add("pool", lambda e: e.memset(zp[:], 0.0), writes=["zp"])
                        ld("sp", zp[1:128, :], S["zrw"][0:127, :], "zp")
                    else:
                        ld("sp", zp[:], S["zrw"][t0 - 1:t0 + 127, :], "zp")
                    if t0 + 128 == T:
                        P.add("pool", lambda e: e.memset(zn[:], 0.0), writes=["zn"])
                        ld("sp", zn[0:127, :], S["zrw"][t0 + 1:t0 + 128, :], "zn")
                    else:
                        ld("sp", zn[:], S["zrw"][t0 + 1:t0 + 129, :], "zn")
                    TT_("pool", zp[:], zp[:], zc[:], ALU.subtract, ["zp", "zc"], ["zp"])
                    TT_("pool", zp[:], zp[:], mup[:], ALU.mult, ["zp", "mup"], ["zp"])
                    TT_("dve", zn[:], zn[:], zc[:], ALU.subtract, ["zn", "zc"], ["zn"])
                    TT_("dve", zn[:], zn[:], mun[:], ALU.mult, ["zn", "mun"], ["zn"])
                    TT_("dve", zc[:], zc[:], zp[:], ALU.add, ["zp", "zc"], ["zc"])
                    TT_("dve", zc[:], zc[:], zn[:], ALU.add, ["zn", "zc"], ["zc"])
                    r_, k_, v_ = zc[:, 0:768], zc[:, 768:1536], zc[:, 1536:2304]
                    wl = zc[:, 2304 + d * 64:2304 + (d + 1) * 64]
                    al = zc[:, 2432 + d * 64:2432 + (d + 1) * 64]
                    gl = zc[:, 2560:2688]
                    ACT_(lorab[:, 0:64], wl, AF.Tanh, ["zc"], ["lorab"])
                    ACT_(lorab[:, 64:128], al, AF.Copy, ["zc"], ["lorab"])
                    ACT_(lorab[:, 128:256], gl, AF.Sigmoid, ["zc"], ["lorab"])
                    bt = self.nb()
                    pv = ps[bt][:].bitcast(BF16)
                    P.add("pe", lambda e, pv=pv: e.transpose(pv[:64, 0:128], lorab[:, 0:64], idb[:]), reads=["lorab", "idb"], writes=[("ps", bt)])
                    P.add("pe", lambda e, pv=pv: e.transpose(pv[:64, 128:256], lorab[:, 64:128], idb[:]), reads=["lorab", "idb"], writes=[("ps", bt)])
                    P.add("pe", lambda e, pv=pv: e.transpose(pv[:, 256:384], lorab[:, 128:256], idb[:]), reads=["lorab", "idb"], writes=[("ps", bt)])
                    ACT_(lT[:64, 0:256], pv[:64, 0:256], AF.Copy, [("ps", bt)], ["lT"])
                    P.add("dve", lambda e, pv=pv: e.tensor_copy(lT[:, 256:384], pv[:, 256:384]), reads=[("ps", bt)], writes=["lT"])
                    halves = ((0, 512), (512, 256))
                    bw = [self.nb(), self.nb()]
                    ba = [self.nb(), self.nb()]
                    for hi_, (c0, cw) in enumerate(halves):
                        P.add("pe", lambda e, hi_=hi_, c0=c0, cw=cw, bw=bw, d=d: e.matmul(ps[bw[hi_]][:, :cw], lT[:, 0:128], w2b[d][:, c0:c0 + cw], start=True, stop=True),
                              reads=["lT", "w2b%d" % d], writes=[("ps", bw[hi_])])
                        P.add("pe", lambda e, hi_=hi_, c0=c0, cw=cw, ba=ba, d=d: e.matmul(ps[ba[hi_]][:, :cw], lT[:, 128:256], a2b[d][:, c0:c0 + cw], start=True, stop=True),
                              reads=["lT", "a2b%d" % d], writes=[("ps", ba[hi_])])
                    for hi_, (c0, cw) in enumerate(halves):
                        TT_("dve", Ssg[:, c0:c0 + cw], ps[bw[hi_]][:, :cw], w0bc[d][:, c0:c0 + cw], ALU.add, [("ps", bw[hi_]), "w0bc%d" % d], ["Ssg"])
                        TT_("dve", a_[:, c0:c0 + cw], ps[ba[hi_]][:, :cw], a0bc[d][:, c0:c0 + cw], ALU.add, [("ps", ba[hi_]), "a0bc%d" % d], ["a_"])
                    ACT_(Ssg[:], Ssg[:], AF.Sigmoid, ["Ssg"], ["Ssg"])
                    ACT_(a_[:], a_[:], AF.Sigmoid, ["a_"], ["a_"])
                    if d == 1:
                        bg = [self.nb(), self.nb()]
                        for hi_, (c0, cw) in enumerate(halves):
                            P.add("pe", lambda e, hi_=hi_, c0=c0, cw=cw, bg=bg: e.matmul(ps[bg[hi_]][:, :cw], lT[:, 256:384], g2b[:, c0:c0 + cw], start=True, stop=True),
                                  reads=["lT", "g2b"], writes=[("ps", bg[hi_])])
                            ACT_(g_[:, c0:c0 + cw], ps[bg[hi_]][:, :cw], AF.Copy, [("ps", bg[hi_])], ["g_"])
                    h3 = lambda ap: ap.rearrange("p (h n) -> p h n", n=64)
                    bc12 = lambda ap: ap.rearrange("p (h o) -> p h o", o=1).to_broadcast([128, 12, 64])
                    TT_("pool", kk[:], k_, kkbc[:], ALU.mult, ["zc", "kkbc"], ["kk"])
                    TT_("pool", tA[:], kk[:], kk[:], ALU.mult, ["kk"], ["tA"])
                    P.add("dve", lambda e: e.tensor_reduce(s12[:, 0:12], h3(tA[:]), AX.X, ALU.add), reads=["tA"], writes=["s12a"])
                    ACT_(s12[:, 0:12], s12[:, 0:12], AF.Sqrt, ["s12a"], ["s12a"], bias=1e-12)
                    P.add("dve", lambda e: e.reciprocal(s12[:, 0:12], s12[:, 0:12]), reads=["s12a"], writes=["s12a"])
                    TT_("pool", h3(kk[:]), h3(kk[:]), bc12(s12[:, 0:12]), ALU.mult, ["kk", "s12a"], ["kk"])
                    TT_("pool", akk[:], a_[:], kk[:], ALU.mult, ["a_", "kk"], ["akk"])
                    TT_("pool", tA[:], a_[:], kabc[:], ALU.mult, ["a_", "kabc"], ["tA"])
                    TT_("pool", tA[:], tA[:], omka[:], ALU.add, ["tA", "omka"], ["tA"])
                    TT_("pool", kd[:], k_, tA[:], ALU.mult, ["zc", "tA"], ["kd"])
                    bc_ = [self.nb(), self.nb()]
                    for hi_, (c0, cw) in enumerate(halves):
                        P.add("pe", lambda e, hi_=hi_, c0=c0, cw=cw, bc_=bc_, d=d: e.matmul(ps[bc_[hi_]][:, :cw], TRI[d][:], Ssg[:, c0:c0 + cw], start=True, stop=True),
                              reads=["Ssg", "TRI%d" % d], writes=[("ps", bc_[hi_])])
                    for hi_, (c0, cw) in enumerate(halves):
                        kps = ("ps", bc_[hi_])
                        ACT_(E1[:, c0:c0 + cw], ps[bc_[hi_]][:, :cw], AF.Exp, [kps], ["E1"], scale=CL)
                        ACT_(E2[:, c0:c0 + cw], ps[bc_[hi_]][:, :cw], AF.Exp, [kps], ["E2"], scale=-CL)
                        TT_("dve", tB[:, c0:c0 + cw], ps[bc_[hi_]][:, :cw], Ssg[:, c0:c0 + cw], ALU.subtract, [kps, "Ssg"], ["tB"])
                        P.add("dve", lambda e, hi_=hi_, c0=c0, cw=cw, bc_=bc_: e.tensor_copy(tC[:, c0:c0 + cw], ps[bc_[hi_]][:, :cw]), reads=[kps], writes=["tC"])
                    ACT_(tB[:], tB[:], AF.Exp, ["tB"], ["tB"], scale=CL)
                    bt_ = [self.nb(), self.nb()]
                    for hi_, (c0, cw) in enumerate(halves):
                        P.add("pe", lambda e, hi_=hi_, c0=c0, cw=cw, bt_=bt_: e.matmul(ps[bt_[hi_]][:, :cw], onesf[:], Ssg[:, c0:c0 + cw], start=True, stop=True),
                              reads=["Ssg", "onesf"], writes=[("ps", bt_[hi_])])
                        TT_("dve", tC[:, c0:c0 + cw], ps[bt_[hi_]][:, :cw], tC[:, c0:c0 + cw], ALU.subtract, [("ps", bt_[hi_]), "tC"], ["tC"])
                    ACT_(tC[:], tC[:], AF.Exp, ["tC"], ["tC"], scale=CL)
                    bgc = self.nb()
                    for h in range(12):
                        P.add("pe", lambda e, h=h, bgc=bgc: e.matmul(ps[bgc][:64, 2 * h:2 * h + 2], Ssg[:, h * 64:(h + 1) * 64], onesf[:, 0:2], start=True, stop=True),
                              reads=["Ssg", "onesf"], writes=[("ps", bgc)])
                    ACT_(gC[:], ps[bgc][:64, 0:24], AF.Exp, [("ps", bgc)], ["gC"], scale=CL)
                    TT_("pool", Rt[:], r_, E1[:], ALU.mult, ["zc", "E1"], ["Rt"])
                    TT_("pool", Kt[:], kd[:], E2[:], ALU.mult, ["kd", "E2"], ["Kt"])
                    TT_("pool", At[:], kk[:], tB[:], ALU.mult, ["kk", "tB"], ["At"])
                    P.add("dve", lambda e: e.scalar_tensor_tensor(Bt[:], akk[:], -1.0, E2[:], ALU.mult, ALU.mult), reads=["akk", "E2"], writes=["Bt"])
                    TT_("pool", Kcb[:], kd[:], tC[:], ALU.mult, ["kd", "tC"], ["Kcb"])
                    P.add("dve", lambda e: e.scalar_tensor_tensor(Bcb[:], akk[:], -1.0, tC[:], ALU.mult, ALU.mult), reads=["akk", "tC"], writes=["Bcb"])
                    ACT_(Vb[:], v_, AF.Copy, ["zc"], ["Vb"])
                    TT_("pool", tA[:], r_, kd[:], ALU.mult, ["zc", "kd"], ["tA"])
                    TT_("pool", tA[:], tA[:], rkbc[:], ALU.mult, ["tA", "rkbc"], ["tA"])
                    P.add("dve", lambda e: e.tensor_reduce(s12[:, 16:28], h3(tA[:]), AX.X, ALU.add), reads=["tA"], writes=["s12b"])
                    TT_("pool", h3(bon[:]), h3(v_), bc12(s12[:, 16:28]), ALU.mult, ["zc", "s12b"], ["bon"])
                    for (X0, X1, xk0, xk1, DST, dk_) in ((At, Rt, "At", "Rt", ART, "ART"), (Kt, Bt, "Kt", "Bt", KBT, "KBT")):
                        for j in range(3):
                            b = self.nb()
                            pv = ps[b][:].bitcast(BF16)
                            for hh in range(4):
                                h = 4 * j + hh
                                for wi, (X, xk) in enumerate(((X0, xk0), (X1, xk1))):
                                    off = (hh * 2 + wi) * 128
                                    P.add("pe", lambda e, pv=pv, off=off, X=X, h=h: e.transpose(pv[:64, off:off + 128], X[:, h * 64:(h + 1) * 64], idb[:]),
                                          reads=[xk, "idb"], writes=[("ps", b)])
                            dsl = DST[:64, 4 * j:4 * j + 4, :, :].rearrange("p h a t -> p (h a t)")
                            if j % 2 == 0:
                                ACT_(dsl, pv[:64, :], AF.Copy, [("ps", b)], [dk_])
                            else:
                                P.add("dve", lambda e, dsl=dsl, pv=pv: e.tensor_copy(dsl, pv[:64, :]), reads=[("ps", b)], writes=[dk_])
                    mk2 = MK2[d][:].rearrange("p h a t -> p (h a t)")
                    mn4 = MN4[d][:].rearrange("p h t -> p (h t)")
                    for (wi, DST, dk_) in ((0, AKRK, "AKRK"), (1, ABRB, "ABRB")):
                        for hp in range(6):
                            b = self.nb()
                            for hh in range(2):
                                h = 2 * hp + hh
                                P.add("pe", lambda e, b=b, hh=hh, h=h, wi=wi: e.matmul(
                                    ps[b][:, hh * 256:(hh + 1) * 256], KBT[:, h, wi, :], ART[:, h, :, :].rearrange("p a t -> p (a t)"),
                                    start=True, stop=True), reads=["KBT", "ART"], writes=[("ps", b)])
                            dsl = DST[:, 2 * hp:2 * hp + 2, :, :].rearrange("p h a t -> p (h a t)")
                            TT_("dve", dsl, ps[b][:], mk2, ALU.mult, [("ps", b), "MK2_%d" % d], [dk_])
                    for j in range(3):
                        b = self.nb()
                        for hh in range(4):
                            h = 4 * j + hh
                            P.add("pe", lambda e, b=b, hh=hh, h=h: e.matmul(
                                ps[b][:, hh * 128:(hh + 1) * 128], ART[:, h, 0, :], KBT[:, h, 1, :], start=True, stop=True),
                                reads=["KBT", "ART"], writes=[("ps", b)])
                        TT_("dve", Pn[0][:, 4 * j:4 * j + 4, :].rearrange("p h t -> p (h t)"), ps[b][:], mn4, ALU.mult,
                            [("ps", b), "MN4_%d" % d], ["Pn0"])
                    TT_("pool", TTb[0][:], ABRB[:, :, 0, :], idb[:].rearrange("p (o t) -> p o t", o=1).to_broadcast([128, 12, 128]), ALU.add,
                        ["ABRB", "idb"], ["TTb0"])
                    for k in range(6):
                        cur, nxt = k % 2, (k + 1) % 2
                        Pc = Pn[cur]
                        PTc = (lambda h: ABRB[:, h, 0, :]) if k == 0 else (lambda h, cur=cur: PnT[cur][:, h, :])
                        ptkey = "ABRB" if k == 0 else "PnT%d" % cur
                        for j in range(3):
                            b = self.nb()
                            for hh in range(4):
                                h = 4 * j + hh
                                P.add("pe", lambda e, b=b, hh=hh, h=h, Pc=Pc, PTc=PTc: e.matmul(
                                    ps[b][:, hh * 128:(hh + 1) * 128], PTc(h), Pc[:, h, :], start=True, stop=True),
                                    reads=[ptkey, "Pn%d" % cur], writes=[("ps", b)])
                            ACT_(Pn[nxt][:, 4 * j:4 * j + 4, :].rearrange("p h t -> p (h t)"), ps[b][:], AF.Copy, [("ps", b)], ["Pn%d" % nxt])
                        if k < 5:
                            for j in range(3):
                                b = self.nb()
                                for hh in range(4):
                                    h = 4 * j + hh
                                    P.add("pe", lambda e, b=b, hh=hh, h=h, Pc=Pc, PTc=PTc: e.matmul(
                                        ps[b][:, hh * 128:(hh + 1) * 128], Pc[:, h, :], PTc(h), start=True, stop=True),
                                        reads=[ptkey, "Pn%d" % cur], writes=[("ps", b)])
                                P.add("dve", lambda e, b=b, j=j, nxt=nxt: e.tensor_copy(
                                    PnT[nxt][:, 4 * j:4 * j + 4, :].rearrange("p h t -> p (h t)"), ps[b][:]),
                                    reads=[("ps", b)], writes=["PnT%d" % nxt])
                        for j in range(3):
                            b = self.nb()
                            for hh in range(4):
                                h = 4 * j + hh
                                P.add("pe", lambda e, b=b, hh=hh, h=h, nxt=nxt, cur=cur: e.matmul(
                                    ps[b][:, hh * 128:(hh + 1) * 128], Pn[nxt][:, h, :], TTb[cur][:, h, :], start=True, stop=True),
                                    reads=["Pn%d" % nxt, "TTb%d" % cur], writes=[("ps", b)])
                            TT_("dve", TTb[nxt][:, 4 * j:4 * j + 4, :].rearrange("p h t -> p (h t)"), ps[b][:],
                                TTb[cur][:, 4 * j:4 * j + 4, :].rearrange("p h t -> p (h t)"), ALU.add,
                                [("ps", b), "TTb%d" % cur], ["TTb%d" % nxt])
                    TTf = TTb[0]
                    hsl = lambda h: slice(h * 64, (h + 1) * 64)
                    bank_of = lambda bb, h: (bb[0], (h * 64)) if h < 8 else (bb[1], (h - 8) * 64)
                    bW = [self.nb(), self.nb()]
                    for h in range(12):
                        b, o = bank_of(bW, h)
                        P.add("pe", lambda e, b=b, o=o, h=h: e.matmul(ps[b][:, o:o + 64], ART[:, h, 0, :], Hb[:, h, :], start=True, stop=False),
                              reads=["ART", "Hb"], writes=[("ps", b)])
                        P.add("pe", lambda e, b=b, o=o, h=h: e.matmul(ps[b][:, o:o + 64], AKRK[:, h, 0, :], Vb[:, hsl(h)], start=False, stop=True),
                              reads=["AKRK", "Vb"], writes=[("ps", b)])
                    ACT_(Wb[:, 0:512], ps[bW[0]][:], AF.Copy, [("ps", bW[0])], ["Wb"])
                    P.add("dve", lambda e, bW=bW: e.tensor_copy(Wb[:, 512:768], ps[bW[1]][:, 0:256]), reads=[("ps", bW[1])], writes=["Wb"])
                    bU = [self.nb(), self.nb()]
                    for h in range(12):
                        b, o = bank_of(bU, h)
                        P.add("pe", lambda e, b=b, o=o, h=h: e.matmul(ps[b][:, o:o + 64], TTf[:, h, :], Wb[:, hsl(h)], start=True, stop=True),
                              reads=["TTb0", "Wb"], writes=[("ps", b)])
                    ACT_(Ub[:, 0:512], ps[bU[0]][:], AF.Copy, [("ps", bU[0])], ["Ub"])
                    P.add("dve", lambda e, bU=bU: e.tensor_copy(Ub[:, 512:768], ps[bU[1]][:, 0:256]), reads=[("ps", bU[1])], writes=["Ub"])
                    bY = [self.nb(), self.nb()]
                    for h in range(12):
                        b, o = bank_of(bY, h)
                        P.add("pe", lambda e, b=b, o=o, h=h: e.matmul(ps[b][:, o:o + 64], ART[:, h, 1, :], Hb[:, h, :], start=True, stop=False),
                              reads=["ART", "Hb"], writes=[("ps", b)])
                        P.add("pe", lambda e, b=b, o=o, h=h: e.matmul(ps[b][:, o:o + 64], AKRK[:, h, 1, :], Vb[:, hsl(h)], start=False, stop=False),
                              reads=["AKRK", "Vb"], writes=[("ps", b)])
                        P.add("pe", lambda e, b=b, o=o, h=h: e.matmul(ps[b][:, o:o + 64], ABRB[:, h, 1, :], Ub[:, hsl(h)], start=False, stop=True),
                              reads=["ABRB", "Ub"], writes=[("ps", b)])
                    bH = [self.nb(), self.nb()]
                    for h in range(12):
                        b, o = bank_of(bH, h)
                        P.add("pe", lambda e, b=b, o=o, h=h: e.matmul(ps[b][:64, o:o + 64], Kcb[:, hsl(h)], Vb[:, hsl(h)], start=True, stop=False),
                              reads=["Kcb", "Vb"], writes=[("ps", b)])
                        P.add("pe", lambda e, b=b, o=o, h=h: e.matmul(ps[b][:64, o:o + 64], Bcb[:, hsl(h)], Ub[:, hsl(h)], start=False, stop=True),
                              reads=["Bcb", "Ub"], writes=[("ps", b)])
                    for h in range(12):
                        b, o = bank_of(bH, h)
                        P.add("dve", lambda e, b=b, o=o, h=h: e.scalar_tensor_tensor(
                            Hf[:, h, :], Hf[:, h, :], gC[:, 2 * h:2 * h + 1], ps[b][:64, o:o + 64], ALU.mult, ALU.add),
                            reads=["Hf", "gC", ("ps", b)], writes=["Hf"])
                    ACT_(Hb[:64, :, :], Hf[:], AF.Copy, ["Hf"], ["Hb"])
                    if d == 0:
                        ACT_(y_[:, 0:512], ps[bY[0]][:], AF.Copy, [("ps", bY[0])], ["y_"])
                        P.add("dve", lambda e, bY=bY: e.tensor_copy(y_[:, 512:768], ps[bY[1]][:, 0:256]), reads=[("ps", bY[1])], writes=["y_"])
                        P.add("sp", lambda e, t0=t0: e.dma_start(out=S["yrw"][t0:t0 + 128, :], in_=y_[:]), reads=["y_"], dma=True, semkey="st_y")
                        P.add("sp", lambda e, t0=t0: e.dma_start(out=S["bon"][t0:t0 + 128, :], in_=bon[:]), reads=["bon"], dma=True, semkey="st_bon")
                    else:
                        ld("sp", yf[:], S["yrw"][t0:t0 + 128, :], "E1")
                        ld("sp", bf_[:], S["bon"][t0:t0 + 128, :], "E2")
                        TT_("dve", y_[:, 0:512], ps[bY[0]][:], yf[:, 0:512], ALU.add, [("ps", bY[0]), "E1"], ["y_"])
                        TT_("dve", y_[:, 512:768], ps[bY[1]][:, 0:256], yf[:, 512:768], ALU.add, [("ps", bY[1]), "E1"], ["y_"])
                        P.add("dve", lambda e: e.tensor_reduce(s12[:, 32:44], h3(y_[:]), AX.X, ALU.add), reads=["y_"], writes=["s12c"])
                        P.add("pool", lambda e: e.tensor_scalar(s12[:, 32:44], s12[:, 32:44], -1.0 / 64, None, ALU.mult), reads=["s12c"], writes=["s12c"])
                        TT_("pool", h3(y_[:]), h3(y_[:]), bc12(s12[:, 32:44]), ALU.add, ["y_", "s12c"], ["y_"])
                        TT_("pool", tA[:], y_[:], y_[:], ALU.mult, ["y_"], ["tA"])
                        P.add("dve", lambda e: e.tensor_reduce(s12[:, 48:60], h3(tA[:]), AX.X, ALU.add), reads=["tA"], writes=["s12d"])
                        ACT_(s12[:, 48:60], s12[:, 48:60], AF.Sqrt, ["s12d"], ["s12d"], bias=GN_EPS, scale=1.0 / 64)
                        P.add("dve", lambda e: e.reciprocal(s12[:, 48:60], s12[:, 48:60]), reads=["s12d"], writes=["s12d"])
                        TT_("pool", h3(y_[:]), h3(y_[:]), bc12(s12[:, 48:60]), ALU.mult, ["y_", "s12d"], ["y_"])
                        TT_("pool", y_[:], y_[:], lgbc[:], ALU.mult, ["y_", "lgbc"], ["y_"])
                        TT_("pool", y_[:], y_[:], lbbc[:], ALU.add, ["y_", "lbbc"], ["y_"])
                        TT_("pool", y_[:], y_[:], bon[:], ALU.add, ["y_", "bon"], ["y_"])
                        TT_("pool", y_[:], y_[:], bf_[:], ALU.add, ["y_", "E2"], ["y_"])
                        TT_("dve", ob[:], y_[:], g_[:], ALU.mult, ["y_", "g_"], ["ob"])
                        b = self.nb()
                        pv = ps[b][:].bitcast(BF16)
                        for c in range(6):
                            P.add("pe", lambda e, pv=pv, c=c: e.transpose(pv[:, c * 128:(c + 1) * 128], ob[:, c * 128:(c + 1) * 128], idb[:]),
                                  reads=["ob", "idb"], writes=[("ps", b)])
                        ACT_(ost[:].rearrange("p c t -> p (c t)"), pv[:, 0:768], AF.Copy, [("ps", b)], ["ost"])
                        dst = S["oT"][0:768, t0:t0 + 128].rearrange("(c p) t -> p c t", p=128)
                        P.add("sp", lambda e, dst=dst: e.dma_start(out=dst, in_=ost[:]), reads=["ost"], dma=True, semkey="st_o")
                P.flush()

    def phase_ffn(self, l, xin, xout, T):
        nc, P, S, C, W, ps = self.nc, self.P, self.S, self.C, self.W, self.ps
        NT = T // TT
        with contextlib.ExitStack() as st0:
            u0 = self.uid()
            sb0 = lambda name, shape, dt: st0.enter_context(nc.sbuf_tensor("E%d_" % u0 + name, shape, dt))
            x1 = sb0("x1", [128, 4, D], F32)
            x1T = sb0("x1T", [128, 16, TT], BF16)
            gates = sb0("gates", [128, 4, NE], F32)
            idf = sb0("idf", [128, 128], F32)
            P.add("sp", lambda e: e.dma_start(out=idf[:], in_=C["ident"]), writes=["idf"], dma=True, semkey="L_idf")
            P.flush()
            for ti in range(NT):
                t0 = ti * TT
                with contextlib.ExitStack() as st:
                    u = self.uid()
                    sb = lambda name, shape, dt: st.enter_context(nc.sbuf_tensor("E%d_" % u + name, shape, dt))
                    oT = sb("oT", [128, 16, TT], BF16)
                    mT = sb("mT", [128, 16, TT], BF16)
                    sg = [sb("sg%d" % i, [128, 3, 4, TT], BF16) for i in range(2)]
                    wt = [sb("wt%d" % i, [128, 16, 512], BF16) for i in range(2)]
                    gbc = sb("gbc", [128, D], F32)
                    bbc = sb("bbc", [128, D], F32)
                    ta = [sb("ta%d" % i, [128, TT], F32) for i in range(2)]
                    tb = [sb("tb%d" % i, [128, TT], F32) for i in range(2)]
                    tcc = [sb("tc%d" % i, [128, TT], F32) for i in range(2)]
                    ts1 = [sb("ts%d" % i, [128, TT], F32) for i in range(2)]
                    xf = [sb("xf%d" % i, [128, TT], F32) for i in range(2)]
                    rw = sb("rw", [128, 16, NE], F32)
                    rb = sb("rb", [128, NE], F32)
                    junk = sb("junk", [128, D], BF16)
                    st4 = sb("st4", [128, 8], F32)
                    rt_ = sb("rt_", [128, 64], F32)
                    ld = lambda eng, out, in_, key: P.add(eng, lambda e: e.dma_start(out=out, in_=in_), writes=[key], dma=True, semkey="L_" + key)
                    ld("sp", oT[:], S["oT"][:, t0:t0 + TT].rearrange("(c p) t -> p c t", p=128), "oT")
                    ld("sp", x1[:], xin[t0:t0 + TT, :].rearrange("(s p) d -> p s d", p=128), "x1")
                    ld("sp", gbc[:], W["ln1_g"][l].partition_broadcast(128), "gbc")
                    ld("sp", bbc[:], W["ln1_b"][l].partition_broadcast(128), "bbc")
                    ld("sp", rw[:], W["router_w"].rearrange("(c p) e -> p c e", p=128), "rw")
                    ld("sp", rb[:], W["router_bias"][0].partition_broadcast(128), "rb")
                    wcnt = 0
                    for nbk in range(4):
                        ws = wcnt % 2
                        wcnt += 1
                        for (nm, k0, nk) in (("w_up_rw", 0, 6), ("w_up_mla", 6, 4), ("w_up_df", 10, 6)):
                            P.add("pool", lambda e, ws=ws, nm=nm, k0=k0, nk=nk, nbk=nbk: e.dma_start(
                                out=wt[ws][:, k0:k0 + nk, :],
                                in_=W[nm][l].rearrange("(c p) n -> p c n", p=128)[:, :, nbk * 512:(nbk + 1) * 512]),
                                writes=[("wt", ws)], dma=True, semkey="L_wt%d" % ws)
                        sgs = nbk % 2
                        for br in range(3):
                            P.add("sp", lambda e, sgs=sgs, br=br, nbk=nbk: e.dma_start(
                                out=sg[sgs][:, br, :, :],
                                in_=S["sgT"][br * D + nbk * 512:br * D + (nbk + 1) * 512, t0:t0 + TT].rearrange("(c p) t -> p c t", p=128)),
                                writes=[("sg", sgs)], dma=True, semkey="L_sg%d" % sgs)
                        for c in range(4):
                            n = nbk * 4 + c
                            q = n % 2
                            bks = []
                            for br, (k0, nk) in enumerate(((0, 6), (6, 4), (10, 6))):
                                b = self.nb()
                                bks.append(b)
                                for kk in range(nk):
                                    kc = k0 + kk
                                    P.add("pe", lambda e, b=b, ws=ws, kc=kc, c=c, kk=kk, nk=nk: e.matmul(
                                        ps[b][:], wt[ws][:, kc, c * 128:(c + 1) * 128], oT[:, kc, :],
                                        start=(kk == 0), stop=(kk == nk - 1)),
                                        reads=[("wt", ws), "oT"], writes=[("ps", b)])
                            P.add("dve", lambda e, q=q, b=bks[0], sgs=sgs, c=c: e.tensor_tensor(ta[q][:], ps[b][:], sg[sgs][:, 0, c, :], ALU.mult),
                                  reads=[("ps", bks[0]), ("sg", sgs)], writes=[("ta", q)])
                            P.add("dve", lambda e, q=q, b=bks[1], sgs=sgs, c=c: e.tensor_tensor(tb[q][:], ps[b][:], sg[sgs][:, 1, c, :], ALU.mult),
                                  reads=[("ps", bks[1]), ("sg", sgs)], writes=[("tb", q)])
                            P.add("dve", lambda e, q=q, b=bks[2], sgs=sgs, c=c: e.tensor_tensor(tcc[q][:], ps[b][:], sg[sgs][:, 2, c, :], ALU.mult),
                                  reads=[("ps", bks[2]), ("sg", sgs)], writes=[("tc", q)])
                            P.add("pool", lambda e, q=q: e.tensor_tensor(ts1[q][:], ta[q][:], tb[q][:], ALU.add),
                                  reads=[("ta", q), ("tb", q)], writes=[("ts", q)])
                            P.add("pool", lambda e, q=q, n=n: e.tensor_tensor(mT[:, n, :], ts1[q][:], tcc[q][:], ALU.add),
                                  reads=[("ts", q), ("tc", q)], writes=[("mT", n)])
                    mT_all = [("mT", n) for n in range(16)]
                    for nbk in range(4):
                        ws = wcnt % 2
                        wcnt += 1
                        P.add("pool", lambda e, ws=ws, nbk=nbk: e.dma_start(
                            out=wt[ws][:], in_=W["w_o"][l].rearrange("(c p) n -> p c n", p=128)[:, :, nbk * 512:(nbk + 1) * 512]),
                            writes=[("wt", ws)], dma=True, semkey="L_wt%d" % ws)
                        for s_ in range(4):
                            b = self.nb()
                            for kc in range(16):
                                P.add("pe", lambda e, b=b, ws=ws, kc=kc, s_=s_: e.matmul(
                                    ps[b][:], mT[:, kc, s_ * 128:(s_ + 1) * 128], wt[ws][:, kc, :],
                                    start=(kc == 0), stop=(kc == 15)),
                                    reads=mT_all + [("wt", ws)], writes=[("ps", b)])
                            P.add("dve", lambda e, b=b, s_=s_, nbk=nbk: e.scalar_tensor_tensor(
                                x1[:, s_, nbk * 512:(nbk + 1) * 512], x1[:, s_, nbk * 512:(nbk + 1) * 512], float(ALPHA), ps[b][:],
                                ALU.mult, ALU.add), reads=[("ps", b), "x1"], writes=[("x1s", s_)])
                    for s_ in range(4):
                        self.layer_norm(x1[:, s_, :], ("x1s", s_), gbc, bbc, junk, st4, s_)
                    rbanks = [4, 5, 6, 7]
                    for kc in range(16):
                        b = kc % 4
                        q = kc % 2
                        for s_ in range(4):
                            P.add("pe", lambda e, b=b, kc=kc, s_=s_: e.transpose(
                                ps[b][:, s_ * 128:(s_ + 1) * 128], x1[:, s_, kc * 128:(kc + 1) * 128], idf[:]),
                                reads=[("x1s", s_), "idf"], writes=[("ps", b)])
                        P.add("act", lambda e, b=b, kc=kc: e.activation(x1T[:, kc, :], ps[b][:], AF.Copy),
                              reads=[("ps", b)], writes=[("x1T", kc)])
                        P.add("dve", lambda e, b=b, q=q: e.tensor_copy(xf[q][:], ps[b][:]), reads=[("ps", b)], writes=[("xf", q)])
                        for s_ in range(4):
                            P.add("pe", lambda e, kc=kc, s_=s_, q=q: e.matmul(
                                ps[rbanks[s_]][:, :NE], xf[q][:, s_ * 128:(s_ + 1) * 128], rw[:, kc, :],
                                start=(kc == 0), stop=(kc == 15)),
                                reads=[("xf", q), "rw"], writes=[("ps", rbanks[s_])])
                    for s_ in range(4):
                        self.routing(ps[rbanks[s_]][:, :NE], ("ps", rbanks[s_]), rb, rt_, gates[:, s_, :], s_)
                    P.flush()
                with contextlib.ExitStack() as st:
                    u = self.uid()
                    sb = lambda name, shape, dt: st.enter_context(nc.sbuf_tensor("E%d_" % u + name, shape, dt))
                    wsl = [sb("w%d" % i, [128, 16, 512], BF16) for i in range(3)]
                    hT = sb("hT", [128, 8, TT], BF16)
                    yacc = sb("yacc", [128, 4, D], F32)
                    sgt = [sb("sgt%d" % i, [128, TT], F32) for i in range(2)]
                    gbc = sb("gbc", [128, D], F32)
                    bbc = sb("bbc", [128, D], F32)
                    junk = sb("junk", [128, D], BF16)
                    st4 = sb("st4", [128, 8], F32)
                    ld = lambda eng, out, in_, key: P.add(eng, lambda e: e.dma_start(out=out, in_=in_), writes=[key], dma=True, semkey="L_" + key)
                    ld("sp", gbc[:], W["ln2_g"][l].partition_broadcast(128), "gbc")
                    ld("sp", bbc[:], W["ln2_b"][l].partition_broadcast(128), "bbc")
                    x1T_all = [("x1T", kc) for kc in range(16)]
                    wcnt = 0
                    hcnt = 0
                    for ex in range(NE):
                        for jb in range(2):
                            wsg = wcnt % 3
                            wsu = (wcnt + 1) % 3
                            wcnt += 2
                            for (nm, wsx) in (("ex_w_gate", wsg), ("ex_w_up", wsu)):
                                P.add("pool", lambda e, nm=nm, wsx=wsx, ex=ex, jb=jb: e.dma_start(
                                    out=wsl[wsx][:], in_=W[nm][l, ex].rearrange("(c p) n -> p c n", p=128)[:, :, jb * 512:(jb + 1) * 512]),
                                    writes=[("w", wsx)], dma=True, semkey="L_w%d" % wsx)
                            for jj in range(4):
                                j = jb * 4 + jj
                                bg, bu = self.nb(), self.nb()
                                for kc in range(16):
                                    P.add("pe", lambda e, bg=bg, wsg=wsg, kc=kc, jj=jj: e.matmul(
                                        ps[bg][:], wsl[wsg][:, kc, jj * 128:(jj + 1) * 128], x1T[:, kc, :],
                                        start=(kc == 0), stop=(kc == 15)), reads=x1T_all + [("w", wsg)], writes=[("ps", bg)])
                                for kc in range(16):
                                    P.add("pe", lambda e, bu=bu, wsu=wsu, kc=kc, jj=jj: e.matmul(
                                        ps[bu][:], wsl[wsu][:, kc, jj * 128:(jj + 1) * 128], x1T[:, kc, :],
                                        start=(kc == 0), stop=(kc == 15)), reads=x1T_all + [("w", wsu)], writes=[("ps", bu)])
                                q = hcnt % 2
                                hcnt += 1
                                P.add("act", lambda e, bg=bg, q=q: e.activation(sgt[q][:], ps[bg][:], AF.Silu),
                                      reads=[("ps", bg)], writes=[("sgt", q)])
                                P.add("dve", lambda e, bu=bu, q=q, j=j: e.tensor_tensor(hT[:, j, :], ps[bu][:], sgt[q][:], ALU.mult),
                                      reads=[("ps", bu), ("sgt", q)], writes=[("hT", j)])
                        hT_all = [("hT", j) for j in range(8)]
                        for nbk in range(4):
                            wsd = wcnt % 3
                            wcnt += 1
                            P.add("pool", lambda e, wsd=wsd, ex=ex, nbk=nbk: e.dma_start(
                                out=wsl[wsd][:, 0:8, :],
                                in_=W["ex_w_down"][l, ex].rearrange("(c p) n -> p c n", p=128)[:, :, nbk * 512:(nbk + 1) * 512]),
                                writes=[("w", wsd)], dma=True, semkey="L_w%d" % wsd)
                            for s_ in range(4):
                                b = self.nb()
                                for j in range(8):
                                    P.add("pe", lambda e, b=b, wsd=wsd, j=j, s_=s_: e.matmul(
                                        ps[b][:], hT[:, j, s_ * 128:(s_ + 1) * 128], wsl[wsd][:, j, :],
                                        start=(j == 0), stop=(j == 7)), reads=hT_all + [("w", wsd)], writes=[("ps", b)])
                                ysl = yacc[:, s_, nbk * 512:(nbk + 1) * 512]
                                if ex == 0:
                                    P.add("dve", lambda e, b=b, ysl=ysl, s_=s_, ex=ex: e.tensor_scalar(
                                        ysl, ps[b][:], gates[:, s_, ex:ex + 1], None, ALU.mult),
                                        reads=[("ps", b), "gates"], writes=[("y", s_, nbk)])
                                else:
                                    P.add("dve", lambda e, b=b, ysl=ysl, s_=s_, ex=ex: e.scalar_tensor_tensor(
                                        ysl, ps[b][:], gates[:, s_, ex:ex + 1], ysl, ALU.mult, ALU.add),
                                        reads=[("ps", b), "gates", ("y", s_, nbk)], writes=[("y", s_, nbk)])
                    for s_ in range(4):
                        yk = [("y", s_, nbk) for nbk in range(4)]
                        P.add("dve", lambda e, s_=s_: e.scalar_tensor_tensor(
                            yacc[:, s_, :], x1[:, s_, :], float(ALPHA), yacc[:, s_, :], ALU.mult, ALU.add),
                            reads=yk + [("x1s", s_)], writes=[("ys", s_)])
                        self.layer_norm(yacc[:, s_, :], ("ys", s_), gbc, bbc, junk, st4, s_)
                    dst = xout[t0:t0 + TT, :].rearrange("(s p) d -> p s d", p=128)
                    P.add("sp", lambda e, dst=dst: e.dma_start(out=dst, in_=yacc[:]),
                          reads=[("ys", s_) for s_ in range(4)], dma=True, semkey="st_y")
                    P.flush()

    def layer_norm(self, xap, key, gbc, bbc, junk, st4, s_):
        P = self.P
        k = "st4"
        P.add("dve", lambda e: e.tensor_reduce(st4[:, 0:1], xap, AX.X, ALU.add), reads=[key], writes=[k])
        P.add("pool", lambda e: e.tensor_scalar(st4[:, 1:2], st4[:, 0:1], -1.0 / D, None, ALU.mult), reads=[k], writes=[(k, 1)])
        P.add("act", lambda e: e.activation(xap, xap, AF.Identity, bias=st4[:, 1:2]), reads=[(k, 1), key], writes=[key])
        P.add("act", lambda e: e.activation(junk[:], xap, AF.Square, accum_out=st4[:, 2:3]), reads=[key], writes=["junk", (k, 2)])
        P.add("act", lambda e: e.activation(st4[:, 3:4], st4[:, 2:3], AF.Sqrt, bias=LN_EPS, scale=1.0 / D), reads=[(k, 2)], writes=[(k, 3)])
        P.add("dve", lambda e: e.reciprocal(st4[:, 4:5], st4[:, 3:4]), reads=[(k, 3)], writes=[(k, 4)])
        P.add("dve", lambda e: e.scalar_tensor_tensor(xap, xap, st4[:, 4:5], gbc[:], ALU.mult, ALU.mult),
              reads=[key, (k, 4), "gbc"], writes=[key])
        P.add("pool", lambda e: e.tensor_tensor(xap, xap, bbc[:], ALU.add), reads=[key, "bbc"], writes=[key])

    def routing(self, logits, lkey, rb, rt_, gout, s_):
        P = self.P
        sc, sel, M, N_ = rt_[:, 0:16], rt_[:, 16:32], rt_[:, 32:40], rt_[:, 40:48]
        sel3 = sel.rearrange("p (g e) -> p g e", e=4)
        M3 = M.rearrange("p (g e) -> p g e", e=2)
        N3 = N_.rearrange("p (g e) -> p g e", e=2)
        hi, lo, nn, gsc = rt_[:, 48:52], rt_[:, 52:56], rt_[:, 56:60], rt_[:, 60:64]
        k = "rt"
        seq = []
        A = lambda eng, fn: seq.append((eng, fn))
        A("act", lambda e: e.activation(sc, logits, AF.Sigmoid))
        A("dve", lambda e: e.tensor_tensor(sel, sc, rb[:], ALU.add))
        A("dve", lambda e: e.tensor_tensor(M3, sel3[:, :, 0:2], sel3[:, :, 2:4], ALU.max))
        A("dve", lambda e: e.tensor_tensor(N3, sel3[:, :, 0:2], sel3[:, :, 2:4], ALU.min))
        A("dve", lambda e: e.tensor_tensor(hi.rearrange("p (g o) -> p g o", o=1), M3[:, :, 0:1], M3[:, :, 1:2], ALU.max))
        A("dve", lambda e: e.tensor_tensor(lo.rearrange("p (g o) -> p g o", o=1), M3[:, :, 0:1], M3[:, :, 1:2], ALU.min))
        A("dve", lambda e: e.tensor_tensor(nn.rearrange("p (g o) -> p g o", o=1), N3[:, :, 0:1], N3[:, :, 1:2], ALU.max))
        A("dve", lambda e: e.tensor_tensor(lo, lo, nn, ALU.max))
        A("dve", lambda e: e.tensor_tensor(gsc, hi, lo, ALU.add))
        gm = M[:, 0:1]
        A("dve", lambda e: e.tensor_reduce(gm, gsc, AX.X, ALU.max))
        A("dve", lambda e: e.tensor_scalar(hi, gsc, gm, None, ALU.is_equal))
        A("dve", lambda e: e.tensor_scalar(hi, hi, -1.0, 1e30, ALU.add, ALU.mult))
        for ei in range(4):
            A("dve", lambda e, ei=ei: e.tensor_tensor(sel3[:, :, ei:ei + 1], sel3[:, :, ei:ei + 1],
                                                     hi.rearrange("p (g o) -> p g o", o=1), ALU.add))
        m1 = M[:, 1:2]
        eq = rt_[:, 32:48]
        A("dve", lambda e: e.tensor_reduce(m1, sel, AX.X, ALU.max))
        eq1 = rt_[:, 40:56]
        A("dve", lambda e: e.tensor_scalar(eq1, sel, m1, None, ALU.is_equal))
        A("dve", lambda e: e.scalar_tensor_tensor(sel, eq1, -1e30, sel, ALU.mult, ALU.add))
        m2 = M[:, 2:3]
        A("dve", lambda e: e.tensor_reduce(m2, sel, AX.X, ALU.max))
        A("dve", lambda e: e.scalar_tensor_tensor(sel, sel, m2, eq1, ALU.is_equal, ALU.add))
        A("dve", lambda e: e.tensor_tensor(sel, sel, sc, ALU.mult))
        den = M[:, 3:4]
        A("dve", lambda e: e.tensor_reduce(den, sel, AX.X, ALU.add))
        A("dve", lambda e: e.reciprocal(den, den))
        A("dve", lambda e: e.tensor_scalar(gout, sel, den, None, ALU.mult))
        for i, (eng, fn) in enumerate(seq):
            rd = [k, "rb"] + ([lkey] if i == 0 else [])
            wr = [k] + (["gates"] if i == len(seq) - 1 else [])
            P.add(eng, fn, reads=rd, writes=wr)


def host_weights(W):
    out = {}
    for k, shp in WEIGHT_SHAPES.items():
        out[k] = np.ascontiguousarray(np.asarray(W[k], dtype=np.float32).reshape(shp))
    return out


_CACHE = {}


def _get_program(seqs):
    key = tuple(seqs)
    if key not in _CACHE:
        b = Builder(seqs)
        nc = b.build()
        _CACHE[key] = (b, nc)
    return _CACHE[key]


def kernel(**inputs):
    x_prompt = np.asarray(inputs["x_prompt"], dtype=np.float32)
    x_sample = np.asarray(inputs["x_sample"], dtype=np.float32)
    NB_P, S_P = x_prompt.shape[0], x_prompt.shape[1]
    NB_S, S_S = x_sample.shape[0], x_sample.shape[1]
    n_cores = 8
    assert NB_S == n_cores and n_cores % NB_P == 0
    seqs = [S_S, S_P]
    b, nc = _get_program(seqs)
    hw = host_weights(inputs)
    consts = make_consts(max(seqs))
    base = {k: hw[k] for k in b.W.used()}
    base.update({k: consts[k] for k in b.C.used()})
    in_maps = []
    for c in range(n_cores):
        d = dict(base)
        d["xs0"] = np.ascontiguousarray(x_sample[c])
        d["xs1"] = np.ascontiguousarray(x_prompt[c % NB_P])
        in_maps.append(d)
    res = run_bass_kernel_spmd(nc, in_maps, core_ids=list(range(n_cores)))
    y_sample = np.stack([np.asarray(res.results[c]["ys0"], dtype=np.float32) for c in range(n_cores)], 0)
    y_prompt = np.stack([np.asarray(res.results[c]["ys1"], dtype=np.float32) for c in range(NB_P)], 0)
    return (y_prompt, y_sample)
```

```python
import contextlib
import numpy as np
import ml_dtypes
import concourse.bass as bass
import concourse.mybir as mybir
from concourse.bass_utils import run_bass_kernel_spmd

F32 = mybir.dt.float32
BF16 = mybir.dt.bfloat16
AF = mybir.ActivationFunctionType
ALU = mybir.AluOpType
AX = mybir.AxisListType

ENGS = ("pe", "act", "dve", "pool", "sp")

D = 2048
DEPTH = 2
IN_W = 11936
RW_IN, MLA_IN, DF_IN, GATE_IN = 2688, 800, 2304, 6144
C_RW = 768
NE, DE = 16, 1024
ALPHA = (2 * DEPTH) ** 0.25
LN_EPS = 1e-5
RMS_EPS = 1e-6
DF_EPS = 1e-5
GN_EPS = 64e-5
TT = 512
import os as _os
NOSELF = _os.environ.get("NOSELF", "0") == "1"


class _Op:
    __slots__ = ("eng", "fn", "reads", "writes", "dma", "semkey", "deps", "needed", "token")


class Prog:
    def __init__(self, nc):
        self.nc = nc
        self.ops = []
        self.st = contextlib.ExitStack()
        self.esem = {e: self.st.enter_context(nc.semaphore("s_" + e)) for e in ENGS}
        self.dsem = {}
        self.ecount = {e: 0 for e in ENGS}
        self.dcount = {}
        self.seen = {e: {} for e in ENGS}
        self.n_total = 0
        self.phase_keys = {}

    def add(self, eng, fn, reads=(), writes=(), dma=False, semkey=None):
        op = _Op()
        if eng != "pe":
            extra = [r for r in reads if isinstance(r, tuple) and r and r[0] == "ps" and r not in writes]
            if extra:
                writes = tuple(writes) + tuple(extra)
        op.eng, op.fn, op.reads, op.writes = eng, fn, tuple(reads), tuple(writes)
        op.dma, op.semkey = dma, semkey
        op.deps, op.needed, op.token = set(), False, None
        if dma:
            pk = (eng, semkey)
            if pk not in self.phase_keys:
                idx = (eng, sum(1 for k in self.phase_keys if k[0] == eng))
                self.phase_keys[pk] = idx
                if idx not in self.dsem:
                    self.dsem[idx] = self.st.enter_context(self.nc.semaphore("d_%s%d" % idx))
                    self.dcount[idx] = 0
            op.semkey = self.phase_keys[pk]
        self.ops.append(op)
        return op

    def barrier(self):
        for e in ENGS:
            self.add(e, None, reads=("__all__",))

    def _analyze(self):
        ops = self.ops
        last_w, readers, last_of_eng, last_dma = {}, {}, {}, {}
        for i, op in enumerate(ops):
            deps = set()
            if op.reads == ("__all__",):
                deps.update(last_of_eng.values())
                deps.update(last_dma.values())
            else:
                for r in op.reads:
                    if r in last_w:
                        deps.add(last_w[r])
                for w in op.writes:
                    if w in last_w:
                        deps.add(last_w[w])
                    deps.update(readers.get(w, ()))
                for r in op.reads:
                    readers.setdefault(r, []).append(i)
                for w in op.writes:
                    last_w[w] = i
                    readers[w] = []
            deps.discard(i)
            op.deps = deps
            if op.fn is not None:
                if op.dma:
                    last_dma[op.semkey] = i
                else:
                    last_of_eng[op.eng] = i
        for op in ops:
            for d in op.deps:
                p = ops[d]
                if p.dma:
                    continue
                if p.eng == op.eng and not op.dma and (p.eng == "pe" or NOSELF):
                    continue
                p.needed = True

    def flush(self):
        self.barrier()
        self._analyze()
        ops = self.ops
        waits = []
        for op in ops:
            w = {}
            for d in op.deps:
                p = ops[d]
                if p.dma:
                    s = ("d", p.semkey)
                    v = self.dcount[p.semkey]
                else:
                    if p.eng == op.eng and not op.dma and (p.eng == "pe" or NOSELF):
                        continue
                    s = ("e", p.eng)
                    v = p.token
                if v > w.get(s, 0):
                    w[s] = v
            wl = []
            for s, v in w.items():
                if self.seen[op.eng].get(s, 0) >= v:
                    continue
                self.seen[op.eng][s] = v
                wl.append((self.dsem[s[1]] if s[0] == "d" else self.esem[s[1]], v))
            waits.append(wl)
            if op.fn is not None:
                if op.dma:
                    self.dcount[op.semkey] += 16
                    op.token = self.dcount[op.semkey]
                elif op.needed:
                    self.ecount[op.eng] += 1
                    op.token = self.ecount[op.eng]
        dsem, esem = self.dsem, self.esem

        def run(ename, eng):
            for op, wl in zip(ops, waits):
                if op.eng != ename:
                    continue
                for s, v in wl:
                    eng.wait_ge(s, v)
                if op.fn is None:
                    continue
                ins = op.fn(eng)
                if op.dma:
                    ins.then_inc(dsem[op.semkey], 16)
                elif op.needed:
                    ins.then_inc(esem[op.eng], 1)

        with self.nc.Block() as block:
            @block.tensor
            def _(eng):
                run("pe", eng)

            @block.scalar
            def _(eng):
                run("act", eng)

            @block.vector
            def _(eng):
                run("dve", eng)

            @block.gpsimd
            def _(eng):
                run("pool", eng)

            @block.sync
            def _(eng):
                run("sp", eng)
        self.n_total += len(ops)
        self.ops = []
        self.phase_keys = {}

    def close(self):
        self.st.close()


def _rope_tables(d, S):
    half = d // 2
    inv = np.power(np.float32(10000.0), -np.arange(half, dtype=np.float32) * np.float32(2.0 / d)).astype(np.float32)
    ang = np.arange(S, dtype=np.float32)[None, :] * inv[:, None]
    return np.cos(ang).astype(np.float32), np.sin(ang).astype(np.float32)


def make_consts(Tmax):
    c = {}
    cos64, sin64 = _rope_tables(64, Tmax)
    cos32, sin32 = _rope_tables(32, Tmax)
    c["cosdf"] = np.concatenate([cos64, cos64, cos64, cos64], 0)
    c["sindf"] = np.concatenate([-sin64, sin64, -sin64, sin64], 0)
    perm = np.zeros((128, 128), np.float32)
    for m in range(128):
        blk, j = divmod(m, 64)
        perm[blk * 64 + (j + 32) % 64, m] = 1.0
    c["permdf"] = perm
    c["cos96"] = np.concatenate([np.ones((64, Tmax), np.float32), cos32, cos32], 0)
    c["sin96"] = np.concatenate([np.zeros((64, Tmax), np.float32), -sin32, sin32], 0)
    p96 = np.zeros((96, 96), np.float32)
    for m in range(96):
        if m < 64:
            p96[m, m] = 1.0
        else:
            j = m - 64
            p96[64 + (j + 16) % 32, m] = 1.0
    c["perm96"] = p96
    p32 = np.zeros((128, 32), np.float32)
    for m in range(32):
        p32[(m + 16) % 32, m] = 1.0
    c["perm32"] = p32
    permq = np.zeros((768, 128), np.float32)
    cosq = np.ones((768, Tmax), np.float32)
    sinq = np.zeros((768, Tmax), np.float32)
    for g in range(768):
        j, m = divmod(g, 128)
        h, d_ = divmod(g, 96)
        if d_ < 64:
            permq[j * 128 + m, m] = 1.0
        else:
            jj = d_ - 64
            g2 = h * 96 + 64 + (jj + 16) % 32
            assert g2 // 128 == j
            permq[j * 128 + (g2 - j * 128), m] = 1.0
            cosq[g] = cos32[jj % 16]
            sinq[g] = -sin32[jj] if jj < 16 else sin32[jj - 16]
    c["permq"], c["cosq"], c["sinq"] = permq, cosq, sinq
    permk = np.zeros((128, 128), np.float32)
    for m in range(32):
        permk[(m + 16) % 32, m] = 1.0
    c["permk"] = permk
    cosk = np.zeros((128, Tmax), np.float32)
    sink = np.zeros((128, Tmax), np.float32)
    cosk[:32] = np.concatenate([cos32, cos32], 0)
    sink[:32] = np.concatenate([-sin32, sin32], 0)
    c["cosk"], c["sink"] = cosk, sink
    c["ident"] = np.eye(128, dtype=np.float32)
    c["ones"] = np.ones((128, 128), np.float32)
    s = np.arange(128)[:, None]
    t = np.arange(128)[None, :]
    c["m_su"] = (s < t).astype(np.float32)
    c["m_iu"] = (s <= t).astype(np.float32)
    c["m_sl"] = (s > t).astype(np.float32)
    c["m_il"] = (s >= t).astype(np.float32)
    return c


CONST_SHAPES = lambda Tmax: {
    "cosdf": [128, Tmax], "sindf": [128, Tmax], "permdf": [128, 128],
    "cos96": [96, Tmax], "sin96": [96, Tmax], "perm96": [96, 96], "perm32": [128, 32],
    "ident": [128, 128], "ones": [128, 128],
    "permq": [768, 128], "cosq": [768, Tmax], "sinq": [768, Tmax], "permk": [128, 128], "cosk": [128, Tmax], "sink": [128, Tmax],
    "m_su": [128, 128], "m_iu": [128, 128], "m_sl": [128, 128], "m_il": [128, 128],
}

WEIGHT_SHAPES = {
    "w_in": [2, 2048, 11936], "shift_prev": [2, 2688], "shift_next": [2, 2688],
    "rw_w0": [2, 2, 768], "rw_w2": [2, 2, 64, 768], "rw_a0": [2, 2, 768], "rw_a2": [2, 2, 64, 768],
    "rw_g2": [2, 128, 768], "rw_k_k": [2, 768], "rw_k_a": [2, 768], "rw_r_k": [2, 768],
    "rw_lnx_g": [2, 768], "rw_lnx_b": [2, 768], "mla_q_norm": [2, 512], "mla_kv_norm": [2, 256],
    "mla_w_uq": [2, 512, 768], "mla_w_ukv": [2, 256, 1024], "df_lq1": [2, 64], "df_lk1": [2, 64],
    "df_lq2": [2, 64], "df_lk2": [2, 64], "df_subln": [2, 128], "w_up_rw": [2, 768, 2048],
    "w_up_mla": [2, 512, 2048], "w_up_df": [2, 768, 2048], "w_o": [2, 2048, 2048],
    "ln1_g": [2, 2048], "ln1_b": [2, 2048], "ln2_g": [2, 2048], "ln2_b": [2, 2048],
    "router_w": [2048, 16], "router_bias": [1, 16],
    "ex_w_gate": [2, 16, 2048, 1024], "ex_w_up": [2, 16, 2048, 1024], "ex_w_down": [2, 16, 1024, 2048],
}


class _Lazy:
    def __init__(self, nc, shapes):
        self.nc, self.shapes, self.d = nc, shapes, {}

    def __getitem__(self, k):
        if k not in self.d:
            self.d[k] = self.nc.dram_tensor(k, self.shapes[k], F32, kind="ExternalInput").ap()
        return self.d[k]

    def used(self):
        return list(self.d.keys())


class Builder:
    def __init__(self, seqs, dbg=(), n_layers=DEPTH, phases=None):
        self.seqs = list(seqs)
        self.Tmax = max(seqs)
        self.dbg = set(dbg)
        self.n_layers = n_layers
        self.phases = phases
        nc = self.nc = bass.Bass("TRN2", target_bir_lowering=False)
        self.P = Prog(nc)
        self.W = _Lazy(nc, WEIGHT_SHAPES)
        self.C = _Lazy(nc, CONST_SHAPES(self.Tmax))
        self.xin = [nc.dram_tensor("xs%d" % i, [T, D], F32, kind="ExternalInput").ap() for i, T in enumerate(seqs)]
        self.yout = [nc.dram_tensor("ys%d" % i, [T, D], F32, kind="ExternalOutput").ap() for i, T in enumerate(seqs)]
        Tm = self.Tmax
        self.S = {}
        for name, shape, dt in [
            ("xmid", [Tm, D], F32),
            ("zrw", [Tm, RW_IN], F32),
            ("cqT", [512, Tm], F32), ("ckvT", [256, Tm], F32), ("krT", [32, Tm], F32),
            ("dfqkT", [24 * 64, Tm], BF16),
            ("vdf", [Tm, 768], BF16),
            ("sgT", [GATE_IN, Tm], BF16),
            ("qmT", [8 * 96, Tm], BF16), ("kmT", [8 * 96, Tm], BF16), ("vm", [Tm, 512], BF16),
            ("oT", [D, Tm], BF16),
            ("yrw", [Tm, C_RW], F32),
            ("bon", [Tm, C_RW], F32),
        ]:
            kind = "ExternalOutput" if name in self.dbg else "Internal"
            self.S[name] = nc.dram_tensor("scr_" + name, shape, dt, kind=kind).ap()
        self._bank = 0
        self.WB = {}

    def precast_weights(self):
        nc, P, W = self.nc, self.P, self.W
        L = self.n_layers
        def mk(name, shape):
            self.WB[name] = nc.dram_tensor("wb_" + name, shape, BF16, kind="Internal").ap()
        mk("w_in", [2, 2048, IN_W]); mk("w_o", [2, 2048, 2048])
        mk("w_up_rw", [2, 768, 2048]); mk("w_up_mla", [2, 512, 2048]); mk("w_up_df", [2, 768, 2048])
        mk("ex_w_gate", [2, 16, 2048, 1024]); mk("ex_w_up", [2, 16, 2048, 1024]); mk("ex_w_down", [2, 16, 1024, 2048])
        n = 0
        def cp(dst, src):
            nonlocal n
            P.add("pool", lambda e: e.dma_start(out=dst, in_=src), dma=True, semkey="pc%d" % (n % 4))
            n += 1
        for l in range(L):
            for r0 in range(0, 2048, 256):
                cp(self.WB["w_in"][l, r0:r0 + 256, :], W["w_in"][l, r0:r0 + 256, :])
            for r0 in range(0, 2048, 512):
                cp(self.WB["w_o"][l, r0:r0 + 512, :], W["w_o"][l, r0:r0 + 512, :])
            for nm in ("w_up_rw", "w_up_mla", "w_up_df"):
                cp(self.WB[nm][l], W[nm][l])
            for ex in range(NE):
                for nm in ("ex_w_gate", "ex_w_up", "ex_w_down"):
                    rows = WEIGHT_SHAPES[nm][2]
                    for r0 in range(0, rows, 512):
                        cp(self.WB[nm][l, ex, r0:r0 + 512, :], W[nm][l, ex, r0:r0 + 512, :])
        P.flush()

    def uid(self):
        self._uid = getattr(self, "_uid", 0) + 1
        return self._uid

    def nb(self):
        b = self._bank
        self._bank = (self._bank + 1) % 8
        return b

    def build(self):
        nc, P = self.nc, self.P
        with contextlib.ExitStack() as gst:
            self.ps = [gst.enter_context(nc.psum_tensor("ps%d" % b, [128, 512], F32)) for b in range(8)]
            if self.phases is None or "A" in self.phases or "E" in self.phases:
                self.precast_weights()
            for si, T in enumerate(self.seqs):
                for l in range(self.n_layers):
                    xin = self.xin[si] if l == 0 else self.S["xmid"]
                    xout = self.yout[si] if l == self.n_layers - 1 else self.S["xmid"]
                    ph = self.phases
                    if ph is None or "A" in ph:
                        self.phase_inproj(l, xin, T)
                    if ph is None or "B" in ph:
                        self.phase_mla(l, T)
                    if ph is None or "C" in ph:
                        self.phase_diff(l, T)
                    if ph is None or "D" in ph:
                        self.phase_rwkv(l, T)
                    if ph is None or "E" in ph:
                        self.phase_ffn(l, xin, xout, T)
        P.close()
        return nc

    def phase_inproj(self, l, xin, T):
        nc, P, S, C, ps = self.nc, self.P, self.S, self.C, self.ps
        w_in = self.WB["w_in"][l].rearrange("(kc p) n -> p kc n", p=128)
        with contextlib.ExitStack() as st:
            sb = lambda name, shape, dt: st.enter_context(nc.sbuf_tensor("A%d_" % self.uid() + name, shape, dt))
            xt = sb("xt", [128, 4, D], BF16)
            xT = sb("xT", [128, 16, TT], BF16)
            wt = [sb("wt%d" % i, [128, 16, 512], BF16) for i in range(2)]
            stgb = [sb("stgb%d" % i, [128, 4, 512], BF16) for i in range(2)]
            stgf = [sb("stgf%d" % i, [128, 4, 512], F32) for i in range(2)]
            zb = [sb("zb%d" % i, [128, 512], BF16) for i in range(2)]
            t1 = [sb("t1_%d" % i, [128, 512], F32) for i in range(2)]
            t2 = [sb("t2_%d" % i, [128, 512], F32) for i in range(2)]
            idn = sb("idn", [128, 128], BF16)
            perm = sb("perm", [128, 128], BF16)
            cosb = sb("cos", [128, TT], F32)
            sinb = sb("sin", [128, TT], F32)
            P.add("pool", lambda e: e.dma_start(out=idn[:], in_=C["ident"]), writes=["idn"], dma=True, semkey="c0")
            P.add("pool", lambda e: e.dma_start(out=perm[:], in_=C["permdf"]), writes=["perm"], dma=True, semkey="c1")
            cnt = {"w": 0, "sb": 0, "sf": 0, "z": 0, "ev": 0}

            def evac_copy(out_ap, in_ap, reads, writes):
                cnt["ev"] += 1
                if cnt["ev"] % 2 == 0:
                    P.add("act", lambda e: e.activation(out_ap, in_ap, AF.Copy), reads=reads, writes=writes)
                else:
                    P.add("dve", lambda e: e.tensor_copy(out_ap, in_ap), reads=reads, writes=writes)

            blocks = []
            for c0 in range(0, RW_IN, 512):
                blocks.append((c0, min(512, RW_IN - c0), "rw"))
            blocks.append((RW_IN, 512, "cq"))
            blocks.append((RW_IN + 512, 288, "ckv"))
            base = RW_IN + MLA_IN
            for which in range(2):
                blocks.append((base + which * 768, 512, "dfqk"))
                blocks.append((base + which * 768 + 512, 256, "dfqk"))
            blocks.append((base + 1536, 512, "dfv"))
            blocks.append((base + 1536 + 512, 256, "dfv"))
            gbase = base + DF_IN
            for c0 in range(0, GATE_IN, 512):
                blocks.append((gbase + c0, 512, "gate"))

            for ti in range(T // TT):
                t0 = ti * TT
                P.add("pool", lambda e, t0=t0: e.dma_start(
                    out=xt[:], in_=xin[t0:t0 + TT, :].rearrange("(s p) d -> p s d", p=128)),
                    writes=["xt"], dma=True, semkey="xt")
                P.add("sp", lambda e, t0=t0: e.dma_start(out=cosb[:], in_=C["cosdf"][:, t0:t0 + TT]),
                      writes=["cos"], dma=True, semkey="cos")
                P.add("sp", lambda e, t0=t0: e.dma_start(out=sinb[:], in_=C["sindf"][:, t0:t0 + TT]),
                      writes=["sin"], dma=True, semkey="sin")
                for g in range(8):
                    b = self.nb()
                    pv = ps[b][:].bitcast(BF16)
                    for kk in range(2):
                        kc = 2 * g + kk
                        for s in range(4):
                            P.add("pe", lambda e, pv=pv, kk=kk, s=s, kc=kc: e.transpose(
                                pv[:, kk * 512 + s * 128: kk * 512 + (s + 1) * 128],
                                xt[:, s, kc * 128:(kc + 1) * 128], idn[:]),
                                reads=["xt", "idn"], writes=[("ps", b)])
                    evac_copy(xT[:, 2 * g:2 * g + 2, :], pv.rearrange("p (a t) -> p a t", a=2),
                              [("ps", b)], [("xT", g)])
                xT_all = [("xT", g) for g in range(8)]

                for (c0, w, kind) in blocks:
                    if getattr(self, "only_kinds", None) is not None and kind not in self.only_kinds:
                        continue
                    ws = cnt["w"] % 2
                    cnt["w"] += 1
                    P.add("pool", lambda e, ws=ws, c0=c0, w=w: e.dma_start(
                        out=wt[ws][:, :, :w], in_=w_in[:, :, c0:c0 + w]),
                        writes=[("wt", ws)], dma=True, semkey="wt%d" % ws)
                    if kind in ("rw", "dfv"):
                        if kind == "rw":
                            ss = cnt["sf"] % 2
                            cnt["sf"] += 1
                            stg, skey = stgf[ss], ("stgf", ss)
                        else:
                            ss = cnt["sb"] % 2
                            cnt["sb"] += 1
                            stg, skey = stgb[ss], ("stgb", ss)
                        for s in range(4):
                            b = self.nb()
                            for kc in range(16):
                                P.add("pe", lambda e, b=b, kc=kc, s=s, ws=ws, w=w: e.matmul(
                                    ps[b][:, :w], xT[:, kc, s * 128:(s + 1) * 128], wt[ws][:, kc, :w],
                                    start=(kc == 0), stop=(kc == 15)),
                                    reads=xT_all + [("wt", ws)], writes=[("ps", b)])
                            evac_copy(stg[:, s, :w], ps[b][:, :w], [("ps", b)], [skey])
                        if kind == "rw":
                            dst = S["zrw"][t0:t0 + TT, c0:c0 + w].rearrange("(s p) n -> p s n", p=128)
                        else:
                            cc = c0 - (base + 1536)
                            dst = S["vdf"][t0:t0 + TT, cc:cc + w].rearrange("(s p) n -> p s n", p=128)
                        P.add("sp", lambda e, dst=dst, stg=stg, w=w: e.dma_start(out=dst, in_=stg[:, :, :w]),
                              reads=[skey], dma=True, semkey="st_%s%d" % (skey[0], ss))
                    else:
                        if kind in ("cq", "ckv"):
                            ss = cnt["sf"] % 2
                            cnt["sf"] += 1
                            stg, skey = stgf[ss], ("stgf", ss)
                        else:
                            ss = cnt["sb"] % 2
                            cnt["sb"] += 1
                            stg, skey = stgb[ss], ("stgb", ss)
                        chunks = [(j, min(128, w - j)) for j in range(0, w, 128)]
                        for ci, (j, cw) in enumerate(chunks):
                            b = self.nb()
                            for kc in range(16):
                                P.add("pe", lambda e, b=b, kc=kc, j=j, cw=cw, ws=ws: e.matmul(
                                    ps[b][:cw, :], wt[ws][:, kc, j:j + cw], xT[:, kc, :],
                                    start=(kc == 0), stop=(kc == 15)),
                                    reads=xT_all + [("wt", ws)], writes=[("ps", b)])
                            if kind == "gate":
                                P.add("act", lambda e, b=b, ci=ci, stg=stg: e.activation(
                                    stg[:, ci, :], ps[b][:], AF.Sigmoid), reads=[("ps", b)], writes=[skey])
                            elif kind == "dfqk":
                                zs = cnt["z"] % 2
                                cnt["z"] += 1
                                b2 = self.nb()
                                P.add("dve", lambda e, b=b, zs=zs: e.tensor_copy(zb[zs][:], ps[b][:]),
                                      reads=[("ps", b)], writes=[("zb", zs)])
                                import os
                                _m = os.environ.get("DFQK_MODE", "")
                                if _m == "1":
                                    b2 = b
                                else:
                                    P.add("pe", lambda e, b2=b2, zs=zs: e.matmul(ps[b2][:], perm[:], zb[zs][:],
                                                                               start=True, stop=True),
                                          reads=[("zb", zs), "perm"], writes=[("ps", b2)])
                                P.add("dve", lambda e, b=b, zs=zs: e.tensor_tensor(t1[zs][:], ps[b][:], cosb[:], ALU.mult),
                                      reads=[("ps", b), "cos"], writes=[("t1", zs)])
                                P.add("dve", lambda e, b2=b2, zs=zs: e.tensor_tensor(t2[zs][:], ps[b2][:], sinb[:], ALU.mult),
                                      reads=[("ps", b2), "sin"], writes=[("t2", zs)])
                                P.add(os.environ.get("ADD_ENG", "pool"), lambda e, zs=zs, ci=ci, stg=stg: e.tensor_tensor(
                                    stg[:, ci, :], t1[zs][:], t2[zs][:], ALU.add),
                                    reads=[("t1", zs), ("t2", zs)], writes=[skey])
                            else:
                                evac_copy(stg[:cw, ci, :], ps[b][:cw, :], [("ps", b)], [skey])
                        stkey = "st_%s%d" % (skey[0], ss)
                        if kind == "gate":
                            r0 = c0 - gbase
                            dst = S["sgT"][r0:r0 + 512, t0:t0 + TT].rearrange("(c p) t -> p c t", p=128)
                            P.add("sp", lambda e, dst=dst, stg=stg: e.dma_start(out=dst, in_=stg[:]),
                                  reads=[skey], dma=True, semkey=stkey)
                        elif kind == "dfqk":
                            r0 = c0 - base
                            nch = len(chunks)
                            dst = S["dfqkT"][r0:r0 + w, t0:t0 + TT].rearrange("(c p) t -> p c t", p=128)
                            P.add("sp", lambda e, dst=dst, stg=stg, nch=nch: e.dma_start(out=dst, in_=stg[:, :nch, :]),
                                  reads=[skey], dma=True, semkey=stkey)
                        elif kind == "cq":
                            dst = S["cqT"][:, t0:t0 + TT].rearrange("(c p) t -> p c t", p=128)
                            P.add("sp", lambda e, dst=dst, stg=stg: e.dma_start(out=dst, in_=stg[:]),
                                  reads=[skey], dma=True, semkey=stkey)
                        else:
                            dst = S["ckvT"][:, t0:t0 + TT].rearrange("(c p) t -> p c t", p=128)
                            P.add("sp", lambda e, dst=dst, stg=stg: e.dma_start(out=dst, in_=stg[:, 0:2, :]),
                                  reads=[skey], dma=True, semkey=stkey)
                            dst2 = S["krT"][:, t0:t0 + TT]
                            P.add("sp", lambda e, dst2=dst2, stg=stg: e.dma_start(out=dst2, in_=stg[:32, 2, :]),
                                  reads=[skey], dma=True, semkey=stkey)
            P.flush()

    def phase_mla(self, l, T):
        nc, P, S, C, W, ps = self.nc, self.P, self.S, self.C, self.W, self.ps
        NT = T // TT
        scale = 96.0 ** -0.5
        with contextlib.ExitStack() as st:
            sb = lambda name, shape, dt: st.enter_context(nc.sbuf_tensor("B%d_" % self.uid() + name, shape, dt))
            onesb = sb("ones", [128, 128], BF16)
            pq = sb("pq", [128, 6, 128], BF16)
            pk = sb("pk", [128, 128], BF16)
            wkv = sb("wkv", [128, 2, 1024], BF16)
            KR = sb("KR", [128, TT], F32)
            cosq = sb("cosq", [128, 6, TT], F32)
            sinq = sb("sinq", [128, 6, TT], F32)
            cosk = sb("cosk", [128, TT], F32)
            sink = sb("sink", [128, TT], F32)
            wuq = sb("wuq", [128, 4, 768], BF16)
            wv = sb("wv", [128, 2, 8, 64], BF16)
            gq = sb("gq", [128, 4], F32)
            gkv = sb("gkv", [128, 2], F32)
            cq = sb("cq", [128, 4, TT], F32)
            ckv = sb("ckv", [128, 2, TT], F32)
            sq = sb("sq", [128, 4, TT], BF16)
            rt = sb("rt", [128, TT], F32)
            rstd = sb("rstd", [128, TT], F32)
            cqn = sb("cqn", [128, 4, TT], BF16)
            ckvn = sb("ckvn", [128, 2, TT], BF16)
            zb = [sb("zb%d" % i, [128, TT], BF16) for i in range(2)]
            for i in range(2):
                P.add("pool", lambda e, i=i: e.memset(zb[i][:], 0.0), writes=[("zb", i)])
            t1 = [sb("t1_%d" % i, [128, TT], F32) for i in range(2)]
            t2 = [sb("t2_%d" % i, [128, TT], F32) for i in range(2)]
            P.add("pool", lambda e: e.memset(KR[:], 0.0), writes=["kr"])
            qstg = [sb("qstg%d" % i, [128, 3, TT], BF16) for i in range(2)]
            kstg = [sb("kstg%d" % i, [128, 4, TT], BF16) for i in range(2)]
            krr = sb("krr", [128, TT], BF16)
            vstg = sb("vstg", [128, 4, 512], BF16)
            ld = lambda eng, out, in_, key: P.add(eng, lambda e: e.dma_start(out=out, in_=in_), writes=[key], dma=True, semkey="L_" + key)
            ld("pool", onesb[:], C["ones"], "ones")
            ld("pool", pq[:], C["permq"].rearrange("(j k) m -> k j m", k=128), "pq")
            ld("pool", pk[:], C["permk"], "pk")
            ld("pool", wkv[:], W["mla_w_ukv"][l].rearrange("(kc p) n -> p kc n", p=128), "wkv")
            ld("pool", wuq[:], W["mla_w_uq"][l].rearrange("(kc p) n -> p kc n", p=128), "wuq")
            wukv = W["mla_w_ukv"][l].rearrange("(kc p) (h c) -> kc p h c", p=128, c=128)
            for kc in range(2):
                P.add("pool", lambda e, kc=kc: e.dma_start(out=wv[:, kc, :, :], in_=wukv[kc, :, :, 64:128]),
                      writes=["wv"], dma=True, semkey="L_wv")
            for kc in range(4):
                P.add("sp", lambda e, kc=kc: e.dma_start(
                    out=gq[:, kc:kc + 1], in_=W["mla_q_norm"][l, kc * 128:(kc + 1) * 128].rearrange("(p o) -> p o", o=1)),
                    writes=["gq"], dma=True, semkey="L_gq")
            for kc in range(2):
                P.add("sp", lambda e, kc=kc: e.dma_start(
                    out=gkv[:, kc:kc + 1], in_=W["mla_kv_norm"][l, kc * 128:(kc + 1) * 128].rearrange("(p o) -> p o", o=1)),
                    writes=["gkv"], dma=True, semkey="L_gkv")
            cnt = {"z": 0, "q": 0, "k": 0}
            for ti in range(NT):
                t0 = ti * TT
                ld("sp", cq[:], S["cqT"][:, t0:t0 + TT].rearrange("(c p) t -> p c t", p=128), "cq")
                ld("sp", ckv[:], S["ckvT"][:, t0:t0 + TT].rearrange("(c p) t -> p c t", p=128), "ckv")
                ld("sp", KR[:32, :], S["krT"][:, t0:t0 + TT], "kr")
                ld("sp", cosq[:], C["cosq"][:, t0:t0 + TT].rearrange("(c p) t -> p c t", p=128), "cosq")
                ld("sp", sinq[:], C["sinq"][:, t0:t0 + TT].rearrange("(c p) t -> p c t", p=128), "sinq")
                ld("sp", cosk[:], C["cosk"][:, t0:t0 + TT], "cosk")
                ld("sp", sink[:], C["sink"][:, t0:t0 + TT], "sink")

                def rmsnorm(src, key, nk, g, dst, dkey, width):
                    b = self.nb()
                    P.add("act", lambda e: e.activation(sq[:, :nk, :], src[:], AF.Square), reads=[key], writes=["sq"])
                    for kc in range(nk):
                        P.add("pe", lambda e, kc=kc: e.matmul(ps[b][:], onesb[:], sq[:, kc, :], start=(kc == 0), stop=(kc == nk - 1)),
                              reads=["sq", "ones"], writes=[("ps", b)])
                    P.add("act", lambda e: e.activation(rt[:], ps[b][:], AF.Sqrt, bias=RMS_EPS, scale=1.0 / width),
                          reads=[("ps", b)], writes=["rt"])
                    P.add("dve", lambda e: e.reciprocal(rstd[:], rt[:]), reads=["rt"], writes=["rstd"])
                    for kc in range(nk):
                        P.add("dve" if kc % 2 == 0 else "pool", lambda e, kc=kc: (
                            e.scalar_tensor_tensor(dst[:, kc, :], src[:, kc, :], g[:, kc:kc + 1], rstd[:], ALU.mult, ALU.mult)
                            if False else e.scalar_tensor_tensor(dst[:, kc, :], src[:, kc, :], g[:, kc:kc + 1], rstd[:], ALU.mult, ALU.mult)),
                            reads=[key, "rstd", "gq", "gkv"], writes=[dkey]) if False else None
                        P.add("dve", lambda e, kc=kc: e.scalar_tensor_tensor(
                            dst[:, kc, :], src[:, kc, :], g[:, kc:kc + 1], rstd[:], ALU.mult, ALU.mult),
                            reads=[key, "rstd", "gq", "gkv"], writes=[dkey])

                rmsnorm(cq, "cq", 4, gq, cqn, "cqn", 512.0)
                rmsnorm(ckv, "ckv", 2, gkv, ckvn, "ckvn", 256.0)
                for j in range(6):
                    b = self.nb()
                    for kc in range(4):
                        P.add("pe", lambda e, b=b, kc=kc, j=j: e.matmul(
                            ps[b][:], wuq[:, kc, j * 128:(j + 1) * 128], cqn[:, kc, :], start=(kc == 0), stop=(kc == 3)),
                            reads=["wuq", "cqn"], writes=[("ps", b)])
                    zs = cnt["z"] % 2
                    cnt["z"] += 1
                    b2 = self.nb()
                    P.add("act", lambda e, b=b, zs=zs: e.activation(zb[zs][:], ps[b][:], AF.Copy),
                          reads=[("ps", b)], writes=[("zb", zs)])
                    P.add("pe", lambda e, b2=b2, zs=zs, j=j: e.matmul(ps[b2][:], pq[:, j, :], zb[zs][:], start=True, stop=True),
                          reads=[("zb", zs), "pq"], writes=[("ps", b2)])
                    P.add("dve", lambda e, b=b, zs=zs, j=j: e.tensor_tensor(t1[zs][:], ps[b][:], cosq[:, j, :], ALU.mult),
                          reads=[("ps", b), "cosq"], writes=[("t1", zs)])
                    P.add("dve", lambda e, b2=b2, zs=zs, j=j: e.tensor_tensor(t2[zs][:], ps[b2][:], sinq[:, j, :], ALU.mult),
                          reads=[("ps", b2), "sinq"], writes=[("t2", zs)])
                    qs = (cnt["q"] // 3) % 2
                    cnt["q"] += 1
                    P.add("pool", lambda e, zs=zs, qs=qs, j=j: e.tensor_tensor(qstg[qs][:, j % 3, :], t1[zs][:], t2[zs][:], ALU.add),
                          reads=[("t1", zs), ("t2", zs)], writes=[("qstg", qs)])
                    if j % 3 == 2:
                        j0 = j - 2
                        dst = S["qmT"][j0 * 128:(j0 + 3) * 128, t0:t0 + TT].rearrange("(c p) t -> p c t", p=128)
                        P.add("sp", lambda e, dst=dst, qs=qs: e.dma_start(out=dst, in_=qstg[qs][:]),
                              reads=[("qstg", qs)], dma=True, semkey="st_q%d" % qs)
                for h in range(8):
                    b = self.nb()
                    for kc in range(2):
                        P.add("pe", lambda e, b=b, kc=kc, h=h: e.matmul(
                            ps[b][:], wkv[:, kc, h * 128:(h + 1) * 128], ckvn[:, kc, :], start=(kc == 0), stop=(kc == 1)),
                            reads=["wkv", "ckvn"], writes=[("ps", b)])
                    ks = (cnt["k"] // 4) % 2
                    cnt["k"] += 1
                    if h % 2 == 0:
                        P.add("act", lambda e, b=b, ks=ks, h=h: e.activation(kstg[ks][:, h % 4, :], ps[b][:], AF.Copy),
                              reads=[("ps", b)], writes=[("kstg", ks)])
                    else:
                        P.add("dve", lambda e, b=b, ks=ks, h=h: e.tensor_copy(kstg[ks][:, h % 4, :], ps[b][:]),
                              reads=[("ps", b)], writes=[("kstg", ks)])
                    dst = S["kmT"][h * 96:h * 96 + 64, t0:t0 + TT]
                    P.add("sp", lambda e, dst=dst, ks=ks, h=h: e.dma_start(out=dst, in_=kstg[ks][:64, h % 4, :]),
                          reads=[("kstg", ks)], dma=True, semkey="st_k%d" % ks)
                zs = cnt["z"] % 2
                cnt["z"] += 1
                b2 = self.nb()
                P.add("act", lambda e, zs=zs: e.activation(zb[zs][:], KR[:], AF.Copy), reads=["kr"], writes=[("zb", zs)])
                P.add("pe", lambda e, b2=b2, zs=zs: e.matmul(ps[b2][:], pk[:], zb[zs][:], start=True, stop=True),
                      reads=[("zb", zs), "pk"], writes=[("ps", b2)])
                P.add("dve", lambda e, zs=zs: e.tensor_tensor(t1[zs][:], KR[:], cosk[:], ALU.mult),
                      reads=["kr", "cosk"], writes=[("t1", zs)])
                P.add("dve", lambda e, b2=b2, zs=zs: e.tensor_tensor(t2[zs][:], ps[b2][:], sink[:], ALU.mult),
                      reads=[("ps", b2), "sink"], writes=[("t2", zs)])
                P.add("pool", lambda e, zs=zs: e.tensor_tensor(krr[:], t1[zs][:], t2[zs][:], ALU.add),
                      reads=[("t1", zs), ("t2", zs)], writes=["krr"])
                for h in range(8):
                    dst = S["kmT"][h * 96 + 64:h * 96 + 96, t0:t0 + TT]
                    P.add("sp", lambda e, dst=dst: e.dma_start(out=dst, in_=krr[:32, :]), reads=["krr"], dma=True, semkey="st_kr")
                for s_ in range(4):
                    b = self.nb()
                    for kc in range(2):
                        P.add("pe", lambda e, b=b, kc=kc, s_=s_: e.matmul(
                            ps[b][:], ckvn[:, kc, s_ * 128:(s_ + 1) * 128], wv[:, kc, :, :].rearrange("p h c -> p (h c)"),
                            start=(kc == 0), stop=(kc == 1)), reads=["wv", "ckvn"], writes=[("ps", b)])
                    if s_ % 2 == 0:
                        P.add("act", lambda e, b=b, s_=s_: e.activation(vstg[:, s_, :], ps[b][:], AF.Copy),
                              reads=[("ps", b)], writes=["vstg"])
                    else:
                        P.add("dve", lambda e, b=b, s_=s_: e.tensor_copy(vstg[:, s_, :], ps[b][:]),
                              reads=[("ps", b)], writes=["vstg"])
                dst = S["vm"][t0:t0 + TT, :].rearrange("(s p) n -> p s n", p=128)
                P.add("sp", lambda e, dst=dst: e.dma_start(out=dst, in_=vstg[:]), reads=["vstg"], dma=True, semkey="st_v")
            P.flush()
        import os
        if "B2" in os.environ.get("SKIP", ""):
            return
        self.attention(T, nheads=8, parts=96, dv=64, scale=scale,
                       kT_rows=lambda h, c: (S["kmT"], h * 96), qT_rows=lambda h, c: (S["qmT"], h * 96),
                       v_src=lambda h: (S["vm"], h * 64), ncomp=1, out_row0=768, l=l)

    def attention(self, T, nheads, parts, dv, scale, kT_rows, qT_rows, v_src, ncomp, out_row0, l):
        nc, P, S, C, W, ps = self.nc, self.P, self.S, self.C, self.W, self.ps
        NT, NK = T // TT, T // 128
        lambda_init = 0.8 - 0.6 * np.exp(-0.3 * l)
        with contextlib.ExitStack() as st:
            sb = lambda name, shape, dt: st.enter_context(nc.sbuf_tensor("At%d_" % self.uid() + name, shape, dt))
            onesb = sb("ones", [128, 128], BF16)
            nrows = parts
            KT = [sb("KT%d" % i, [128, T], BF16) for i in range(2)]
            V = [sb("V%d" % i, [128, NK, dv], BF16) for i in range(2)]
            QT = [[sb("QT%d_%d" % (i, c), [128, TT], BF16) for c in range(ncomp)] for i in range(2)]
            for i in range(2):
                P.add("pool", lambda e, i=i: e.memset(KT[i][:], 0.0), writes=[("KT", i)])
                for c in range(ncomp):
                    P.add("pool", lambda e, i=i, c=c: e.memset(QT[i][c][:], 0.0), writes=[("QT", i, c)])
            pT = [sb("pT%d" % i, [128, TT], BF16) for i in range(3)]
            rD = [sb("rD%d" % i, [dv, TT], F32) for i in range(2)]
            ostg = [sb("ostg%d" % i, [dv, TT], BF16) for i in range(2)]
            P.add("pool", lambda e: e.dma_start(out=onesb[:], in_=C["ones"]), writes=["ones"], dma=True, semkey="L_ones")
            if ncomp == 2:
                lq = [sb("lq%d" % i, [128, 64], F32) for i in range(4)]
                lp = [sb("lp%d" % i, [128, 64], F32) for i in range(2)]
                lsum = sb("lsum", [128, 2], F32)
                lexp = sb("lexp", [128, 2], F32)
                neglam = sb("neglam", [128, 1], F32)
                gs0 = sb("gs0", [128, 1], F32)
                gs = sb("gs", [128, 1], F32)
                o1 = sb("o1", [128, TT], F32)
                o2 = sb("o2", [128, TT], F32)
                oo = sb("oo", [128, TT], F32)
                osq = sb("osq", [128, TT], BF16)
                ort = sb("ort", [128, TT], F32)
                orstd = sb("orstd", [128, TT], F32)
                for i, nm in enumerate(["df_lq1", "df_lk1", "df_lq2", "df_lk2"]):
                    P.add("sp", lambda e, i=i, nm=nm: e.dma_start(out=lq[i][:], in_=W[nm][l].partition_broadcast(128)),
                          writes=[("lq", i)], dma=True, semkey="L_lq%d" % i)
                P.add("sp", lambda e: e.dma_start(out=gs0[:], in_=W["df_subln"][l].rearrange("(p o) -> p o", o=1)),
                      writes=["gs0"], dma=True, semkey="L_gs0")
                for j in range(2):
                    P.add("dve", lambda e, j=j: e.tensor_tensor(lp[j][:], lq[2 * j][:], lq[2 * j + 1][:], ALU.mult),
                          reads=[("lq", 2 * j), ("lq", 2 * j + 1)], writes=[("lp", j)])
                    P.add("pool" if False else "dve", lambda e, j=j: e.tensor_reduce(lsum[:, j:j + 1], lp[j][:], AX.X, ALU.add),
                          reads=[("lp", j)], writes=["lsum"])
                P.add("act", lambda e: e.activation(lexp[:], lsum[:], AF.Exp), reads=["lsum"], writes=["lexp"])
                P.add("dve", lambda e: e.scalar_tensor_tensor(neglam[:], lexp[:, 1:2], -float(lambda_init), lexp[:, 0:1],
                                                              ALU.add, ALU.subtract), reads=["lexp"], writes=["neglam"])
                P.add("pool", lambda e: e.tensor_scalar(gs[:], gs0[:], float(1.0 - lambda_init), None, ALU.mult),
                      reads=["gs0"], writes=["gs"])
            cnt = {"p": 0, "o": 0}
            acc_banks = [3, 4, 5, 6] if ncomp == 2 else [3, 4, 5, 6]
            for h in range(nheads):
                hs = h % 2
                src, r0 = kT_rows(h, 0)
                nk_rows = nrows * ncomp
                P.add("sp", lambda e, hs=hs, src=src, r0=r0, nk_rows=nk_rows: e.dma_start(out=KT[hs][:nk_rows, :], in_=src[r0:r0 + nk_rows, 0:T]),
                      writes=[("KT", hs)], dma=True, semkey="L_KT%d" % hs)
                vsrc, c0 = v_src(h)
                P.add("sp", lambda e, hs=hs, vsrc=vsrc, c0=c0: e.dma_start(
                    out=V[hs][:], in_=vsrc[0:T, c0:c0 + dv].rearrange("(c p) d -> p c d", p=128)),
                    writes=[("V", hs)], dma=True, semkey="L_V%d" % hs)
                for qi in range(NT):
                    t0 = qi * TT
                    qs = (h * NT + qi) % 2
                    for c in range(ncomp):
                        src, r0 = qT_rows(h, c)
                        rr = r0 + c * nrows * (ncomp - 1)
                        pr = c * nrows * (ncomp - 1)
                        P.add("sp", lambda e, qs=qs, c=c, src=src, rr=rr, pr=pr, t0=t0: e.dma_start(
                            out=QT[qs][c][pr:pr + nrows, :], in_=src[rr:rr + nrows, t0:t0 + TT]),
                            writes=[("QT", qs, c)], dma=True, semkey="L_QT%d%d" % (qs, c))
                    accs = []
                    for c in range(ncomp):
                        if ncomp == 1:
                            bo, bd = acc_banks[2 * (cnt["o"] % 2)], acc_banks[2 * (cnt["o"] % 2) + 1]
                        else:
                            bo, bd = acc_banks[2 * c], acc_banks[2 * c + 1]
                        accs.append((bo, bd))
                        for kc in range(NK):
                            b = cnt["p"] % 3
                            pslot = cnt["p"] % 3
                            cnt["p"] += 1
                            P.add("pe", lambda e, b=b, hs=hs, c=c, kc=kc, qs=qs: e.matmul(
                                ps[b][:], KT[hs][:, kc * 128:(kc + 1) * 128], QT[qs][c][:], start=True, stop=True),
                                reads=[("KT", hs), ("QT", qs, c)], writes=[("ps", b)])
                            P.add("act", lambda e, b=b, pslot=pslot: e.activation(pT[pslot][:], ps[b][:], AF.Exp, scale=float(scale)),
                                  reads=[("ps", b)], writes=[("pT", pslot)])
                            P.add("pe", lambda e, bo=bo, hs=hs, kc=kc, pslot=pslot: e.matmul(
                                ps[bo][:dv, :], V[hs][:, kc, :], pT[pslot][:], start=(kc == 0), stop=(kc == NK - 1)),
                                reads=[("V", hs), ("pT", pslot)], writes=[("ps", bo)])
                            P.add("pe", lambda e, bd=bd, kc=kc, pslot=pslot: e.matmul(
                                ps[bd][:dv, :], onesb[:, :dv], pT[pslot][:], start=(kc == 0), stop=(kc == NK - 1)),
                                reads=["ones", ("pT", pslot)], writes=[("ps", bd)])
                    os_ = cnt["o"] % 2
                    cnt["o"] += 1
                    if ncomp == 1:
                        bo, bd = accs[0]
                        P.add("dve", lambda e, bd=bd, os_=os_: e.reciprocal(rD[os_][:], ps[bd][:dv, :]),
                              reads=[("ps", bd)], writes=[("rD", os_)])
                        P.add("dve", lambda e, bo=bo, os_=os_: e.tensor_tensor(ostg[os_][:], ps[bo][:dv, :], rD[os_][:], ALU.mult),
                              reads=[("ps", bo), ("rD", os_)], writes=[("ostg", os_)])
                    else:
                        (bo1, bd1), (bo2, bd2) = accs
                        P.add("dve", lambda e, bd1=bd1: e.reciprocal(rD[0][:], ps[bd1][:]), reads=[("ps", bd1)], writes=[("rD", 0)])
                        P.add("dve", lambda e, bd2=bd2: e.reciprocal(rD[1][:], ps[bd2][:]), reads=[("ps", bd2)], writes=[("rD", 1)])
                        P.add("dve", lambda e, bo1=bo1: e.tensor_tensor(o1[:], ps[bo1][:], rD[0][:], ALU.mult),
                              reads=[("ps", bo1), ("rD", 0)], writes=["o1"])
                        P.add("dve", lambda e, bo2=bo2: e.tensor_tensor(o2[:], ps[bo2][:], rD[1][:], ALU.mult),
                              reads=[("ps", bo2), ("rD", 1)], writes=["o2"])
                        P.add("dve", lambda e: e.scalar_tensor_tensor(oo[:], o2[:], neglam[:], o1[:], ALU.mult, ALU.add),
                              reads=["o1", "o2", "neglam"], writes=["oo"])
                        P.add("act", lambda e: e.activation(osq[:], oo[:], AF.Square), reads=["oo"], writes=["osq"])
                        bs = 7
                        P.add("pe", lambda e: e.matmul(ps[bs][:], onesb[:], osq[:], start=True, stop=True),
                              reads=["osq", "ones"], writes=[("ps", bs)])
                        P.add("act", lambda e: e.activation(ort[:], ps[bs][:], AF.Sqrt, bias=DF_EPS, scale=1.0 / 128.0),
                              reads=[("ps", bs)], writes=["ort"])
                        P.add("dve", lambda e: e.reciprocal(orstd[:], ort[:]), reads=["ort"], writes=["orstd"])
                        P.add("dve", lambda e, os_=os_: e.scalar_tensor_tensor(ostg[os_][:], oo[:], gs[:], orstd[:], ALU.mult, ALU.mult),
                              reads=["oo", "gs", "orstd"], writes=[("ostg", os_)])
                    dst = S["oT"][out_row0 + h * dv:out_row0 + (h + 1) * dv, t0:t0 + TT]
                    P.add("sp", lambda e, dst=dst, os_=os_: e.dma_start(out=dst, in_=ostg[os_][:]),
                          reads=[("ostg", os_)], dma=True, semkey="st_o%d" % os_)
            P.flush()

    def phase_diff(self, l, T):
        S = self.S
        self.attention(T, nheads=6, parts=64, dv=128, scale=64.0 ** -0.5,
                       kT_rows=lambda h, c: (S["dfqkT"], (6 + h) * 128), qT_rows=lambda h, c: (S["dfqkT"], h * 128),
                       v_src=lambda h: (S["vdf"], h * 128), ncomp=2, out_row0=1280, l=l)

    def phase_rwkv(self, l, T):
        nc, P, S, C, W, ps = self.nc, self.P, self.S, self.C, self.W, self.ps
        NCH = T // 128
        CL = -float(np.exp(-0.5))
        u = self.uid()
        with contextlib.ExitStack() as st:
            sb = lambda name, shape, dt: st.enter_context(nc.sbuf_tensor("D%d_" % u + name, shape, dt))
            f768 = lambda name: sb(name, [128, 768], F32)
            b768 = lambda name: sb(name, [128, 768], BF16)
            mup, mun = sb("mup", [128, RW_IN], F32), sb("mun", [128, RW_IN], F32)
            kkbc, kabc, omka, rkbc, lgbc, lbbc = [f768(n) for n in ("kkbc", "kabc", "omka", "rkbc", "lgbc", "lbbc")]
            _w0 = f768("w0bc"); w0bc = [_w0, _w0]
            _a0 = f768("a0bc"); a0bc = [_a0, _a0]
            _w2 = sb("w2b", [128, 768], BF16); w2b = [_w2, _w2]
            _a2 = sb("a2b", [128, 768], BF16); a2b = [_a2, _a2]
            g2b = sb("g2b", [128, 768], BF16)
            _mk = sb("MK2", [128, 2, 2, 128], F32); MK2 = [_mk, _mk]
            _mn = sb("MN4", [128, 4, 128], F32); MN4 = [_mn, _mn]
            _tr = sb("TRI", [128, 128], F32); TRI = [_tr, _tr]
            onesf = sb("onesf", [128, 128], F32)
            idb = sb("idb", [128, 128], BF16)
            zc, zp, zn = [sb(n, [128, RW_IN], F32) for n in ("zc", "zp", "zn")]
            lorab = sb("lorab", [128, 256], BF16)
            lT = sb("lT", [128, 384], BF16)
            Ssg, a_, g_, kk, akk, kd, tA, tB, tC, E1, E2, bon, y_ = [f768(n) for n in (
                "Ssg", "a_", "g_", "kk", "akk", "kd", "tA", "tB", "tC", "E1", "E2", "bon", "y_")]
            yf, bf_ = E1, E2
            s12 = sb("s12", [128, 64], F32)
            gC = sb("gC", [64, 24], F32)
            Rt, Kt, At, Bt, Kcb, Bcb, Vb, Wb, Ub, ob = [b768(n) for n in ("Rt", "Kt", "At", "Bt", "Kcb", "Bcb", "Vb", "Wb", "Ub", "ob")]
            ART = sb("ART", [128, 12, 2, 128], BF16)
            KBT = sb("KBT", [128, 12, 2, 128], BF16)
            AKRK = sb("AKRK", [128, 12, 2, 128], BF16)
            ABRB = sb("ABRB", [128, 12, 2, 128], BF16)
            Pn = [sb("Pn%d" % i, [128, 12, 128], BF16) for i in range(2)]
            PnT = [sb("PnT%d" % i, [128, 12, 128], BF16) for i in range(2)]
            TTb = [sb("TTb%d" % i, [128, 12, 128], BF16) for i in range(2)]
            Hf = sb("Hf", [64, 12, 64], F32)
            Hb = sb("Hb", [128, 12, 64], BF16)
            ost = sb("ost", [128, 6, 128], BF16)

            def ld(eng, out, in_, key):
                P.add(eng, lambda e: e.dma_start(out=out, in_=in_), writes=[key], dma=True, semkey="L_" + key)

            def TT_(eng, out, a, b, op, rd, wr):
                P.add(eng, lambda e: e.tensor_tensor(out, a, b, op), reads=rd, writes=wr)

            def ACT_(out, in_, func, rd, wr, **kw):
                P.add("act", lambda e: e.activation(out, in_, func, **kw), reads=rd, writes=wr)

            ld("sp", mup[:], W["shift_prev"][l].partition_broadcast(128), "mup")
            ld("sp", mun[:], W["shift_next"][l].partition_broadcast(128), "mun")
            ld("sp", kkbc[:], W["rw_k_k"][l].partition_broadcast(128), "kkbc")
            ld("sp", kabc[:], W["rw_k_a"][l].partition_broadcast(128), "kabc")
            ld("sp", rkbc[:], W["rw_r_k"][l].partition_broadcast(128), "rkbc")
            ld("sp", lgbc[:], W["rw_lnx_g"][l].partition_broadcast(128), "lgbc")
            ld("sp", lbbc[:], W["rw_lnx_b"][l].partition_broadcast(128), "lbbc")
            P.add("pool", lambda e: e.tensor_scalar(omka[:], kabc[:], -1.0, 1.0, ALU.mult, ALU.add), reads=["kabc"], writes=["omka"])
            def load_dir_consts(d):
                ld("sp", w0bc[d][:], W["rw_w0"][l, d].partition_broadcast(128), "w0bc%d" % d)
                ld("sp", a0bc[d][:], W["rw_a0"][l, d].partition_broadcast(128), "a0bc%d" % d)
                ld("pool", w2b[d][:64, :], W["rw_w2"][l, d], "w2b%d" % d)
                ld("pool", a2b[d][:64, :], W["rw_a2"][l, d], "a2b%d" % d)
                strict, incl, nmask = ("m_su", "m_iu", "m_sl") if d == 0 else ("m_sl", "m_il", "m_su")
                for hh in range(2):
                    ld("sp", MK2[d][:, hh, 0, :], C[strict], "MK2_%d" % d)
                    ld("sp", MK2[d][:, hh, 1, :], C[incl], "MK2_%d" % d)
                for hh in range(4):
                    ld("sp", MN4[d][:, hh, :], C[nmask], "MN4_%d" % d)
                ld("sp", TRI[d][:], C[incl], "TRI%d" % d)
            ld("pool", g2b[:], W["rw_g2"][l], "g2b")
            for (tl, nm) in ((_w2, "w2b0"), (_a2, "a2b0"), (lT, "lT"), (ART, "ART"), (KBT, "KBT")):
                P.add("pool", lambda e, tl=tl: e.memset(tl[:], 0.0), writes=[nm])
            P.flush()
            ld("sp", onesf[:], C["ones"], "onesf")
            ld("pool", idb[:], C["ident"], "idb")

            for d in range(2):
                load_dir_consts(d)
                P.add("pool", lambda e: e.memset(Hf[:], 0.0), writes=["Hf"])
                P.add("pool", lambda e: e.memset(Hb[:], 0.0), writes=["Hb"])
                order = list(range(NCH)) if d == 0 else list(range(NCH - 1, -1, -1))
                for ci in order:
                    t0 = ci * 128
                    ld("sp", zc[:], S["zrw"][t0:t0 + 128, :], "zc")
                    if t0 == 0:
                        P.add("pool", lambda e: e.memset(zp[:], 0.0), writes=["zp"])
                        ld("sp", zp[1:128, :], S["zrw"][0:127, :], "zp")
                    else:
                        ld("sp", zp[:], S["zrw"][t0 - 1:t0 + 127, :], "zp")
                    if t0 + 128 == T:
                        P.add("pool", lambda e: e.memset(zn[:], 0.0), writes=["zn"])
                        ld("sp", zn[0:127, :], S["zrw"][t0 + 1:t0 + 128, :], "zn")
                    else:
                        ld("sp", zn[:], S["zrw"][t0 + 1:t0 + 129, :], "zn")
                    TT_("pool", zp[:], zp[:], zc[:], ALU.subtract, ["zp", "zc"], ["zp"])
                    TT_("pool", zp[:], zp[:], mup[:], ALU.mult, ["zp", "mup"], ["zp"])
                    TT_("dve", zn[:], zn[:], zc[:], ALU.subtract, ["zn", "zc"], ["zn"])
                    TT_("dve", zn[:], zn[:], mun[:], ALU.mult, ["zn", "mun"], ["zn"])
                    TT_("dve", zc[:], zc[:], zp[:], ALU.add, ["zp", "zc"], ["zc"])
                    TT_("dve", zc[:], zc[:], zn[:], ALU.add, ["zn", "zc"], ["zc"])
                    r_, k_, v_ = zc[:, 0:768], zc[:, 768:1536], zc[:, 1536:2304]
                    wl = zc[:, 2304 + d * 64:2304 + (d + 1) * 64]
                    al = zc[:, 2432 + d * 64:2432 + (d + 1) * 64]
                    gl = zc[:, 2560:2688]
                    ACT_(lorab[:, 0:64], wl, AF.Tanh, ["zc"], ["lorab"])
                    ACT_(lorab[:, 64:128], al, AF.Copy, ["zc"], ["lorab"])
                    ACT_(lorab[:, 128:256], gl, AF.Sigmoid, ["zc"], ["lorab"])
                    bt = self.nb()
                    pv = ps[bt][:].bitcast(BF16)
                    P.add("pe", lambda e, pv=pv: e.transpose(pv[:64, 0:128], lorab[:, 0:64], idb[:]), reads=["lorab", "idb"], writes=[("ps", bt)])
                    P.add("pe", lambda e, pv=pv: e.transpose(pv[:64, 128:256], lorab[:, 64:128], idb[:]), reads=["lorab", "idb"], writes=[("ps", bt)])
                    P.add("pe", lambda e, pv=pv: e.transpose(pv[:, 256:384], lorab[:, 128:256], idb[:]), reads=["lorab", "idb"], writes=[("ps", bt)])
                    ACT_(lT[:64, 0:256], pv[:64, 0:256], AF.Copy, [("ps", bt)], ["lT"])
                    P.add("dve", lambda e, pv=pv: e.tensor_copy(lT[:, 256:384], pv[:, 256:384]), reads=[("ps", bt)], writes=["lT"])
                    halves = ((0, 512), (512, 256))
                    bw = [self.nb(), self.nb()]
                    ba = [self.nb(), self.nb()]
                    for hi_, (c0, cw) in enumerate(halves):
                        P.add("pe", lambda e, hi_=hi_, c0=c0, cw=cw, bw=bw, d=d: e.matmul(ps[bw[hi_]][:, :cw], lT[:, 0:128], w2b[d][:, c0:c0 + cw], start=True, stop=True),
                              reads=["lT", "w2b%d" % d], writes=[("ps", bw[hi_])])
                        P.add("pe", lambda e, hi_=hi_, c0=c0, cw=cw, ba=ba, d=d: e.matmul(ps[ba[hi_]][:, :cw], lT[:, 128:256], a2b[d][:, c0:c0 + cw], start=True, stop=True),
                              reads=["lT", "a2b%d" % d], writes=[("ps", ba[hi_])])
                    for hi_, (c0, cw) in enumerate(halves):
                        TT_("dve", Ssg[:, c0:c0 + cw], ps[bw[hi_]][:, :cw], w0bc[d][:, c0:c0 + cw], ALU.add, [("ps", bw[hi_]), "w0bc%d" % d], ["Ssg"])
                        TT_("dve", a_[:, c0:c0 + cw], ps[ba[hi_]][:, :cw], a0bc[d][:, c0:c0 + cw], ALU.add, [("ps", ba[hi_]), "a0bc%d" % d], ["a_"])
                    ACT_(Ssg[:], Ssg[:], AF.Sigmoid, ["Ssg"], ["Ssg"])
                    ACT_(a_[:], a_[:], AF.Sigmoid, ["a_"], ["a_"])
                    if d == 1:
                        bg = [self.nb(), self.nb()]
                        for hi_, (c0, cw) in enumerate(halves):
                            P.add("pe", lambda e, hi_=hi_, c0=c0, cw=cw, bg=bg: e.matmul(ps[bg[hi_]][:, :cw], lT[:, 256:384], g2b[:, c0:c0 + cw], start=True, stop=True),
                                  reads=["lT", "g2b"], writes=[("ps", bg[hi_])])
                            ACT_(g_[:, c0:c0 + cw], ps[bg[hi_]][:, :cw], AF.Copy, [("ps", bg[hi_])], ["g_"])
                    h3 = lambda ap: ap.rearrange("p (h n) -> p h n", n=64)
                    bc12 = lambda ap: ap.rearrange("p (h o) -> p h o", o=1).to_broadcast([128, 12, 64])
                    TT_("pool", kk[:], k_, kkbc[:], ALU.mult, ["zc", "kkbc"], ["kk"])
                    TT_("pool", tA[:], kk[:], kk[:], ALU.mult, ["kk"], ["tA"])
                    P.add("dve", lambda e: e.tensor_reduce(s12[:, 0:12], h3(tA[:]), AX.X, ALU.add), reads=["tA"], writes=["s12a"])
                    ACT_(s12[:, 0:12], s12[:, 0:12], AF.Sqrt, ["s12a"], ["s12a"], bias=1e-12)
                    P.add("dve", lambda e: e.reciprocal(s12[:, 0:12], s12[:, 0:12]), reads=["s12a"], writes=["s12a"])
                    TT_("pool", h3(kk[:]), h3(kk[:]), bc12(s12[:, 0:12]), ALU.mult, ["kk", "s12a"], ["kk"])
                    TT_("pool", akk[:], a_[:], kk[:], ALU.mult, ["a_", "kk"], ["akk"])
                    TT_("pool", tA[:], a_[:], kabc[:], ALU.mult, ["a_", "kabc"], ["tA"])
                    TT_("pool", tA[:], tA[:], omka[:], ALU.add, ["tA", "omka"], ["tA"])
                    TT_("pool", kd[:], k_, tA[:], ALU.mult, ["zc", "tA"], ["kd"])
                    bc_ = [self.nb(), self.nb()]
                    for hi_, (c0, cw) in enumerate(halves):
                        P.add("pe", lambda e, hi_=hi_, c0=c0, cw=cw, bc_=bc_, d=d: e.matmul(ps[bc_[hi_]][:, :cw], TRI[d][:], Ssg[:, c0:c0 + cw], start=True, stop=True),
                              reads=["Ssg", "TRI%d" % d], writes=[("ps", bc_[hi_])])
                    for hi_, (c0, cw) in enumerate(halves):
                        kps = ("ps", bc_[hi_])
                        ACT_(E1[:, c0:c0 + cw], ps[bc_[hi_]][:, :cw], AF.Exp, [kps], ["E1"], scale=CL)
                        ACT_(E2[:, c0:c0 + cw], ps[bc_[hi_]][:, :cw], AF.Exp, [kps], ["E2"], scale=-CL)
                        TT_("dve", tB[:, c0:c0 + cw], ps[bc_[hi_]][:, :cw], Ssg[:, c0:c0 + cw], ALU.subtract, [kps, "Ssg"], ["tB"])
                        P.add("dve", lambda e, hi_=hi_, c0=c0, cw=cw, bc_=bc_: e.tensor_copy(tC[:, c0:c0 + cw], ps[bc_[hi_]][:, :cw]), reads=[kps], writes=["tC"])
                    ACT_(tB[:], tB[:], AF.Exp, ["tB"], ["tB"], scale=CL)
                    bt_ = [self.nb(), self.nb()]
                    for hi_, (c0, cw) in enumerate(halves):
                        P.add("pe", lambda e, hi_=hi_, c0=c0, cw=cw, bt_=bt_: e.matmul(ps[bt_[hi_]][:, :cw], onesf[:], Ssg[:, c0:c0 + cw], start=True, stop=True),
                              reads=["Ssg", "onesf"], writes=[("ps", bt_[hi_])])
                        TT_("dve", tC[:, c0:c0 + cw], ps[bt_[hi_]][:, :cw], tC[:, c0:c0 + cw], ALU.subtract, [("ps", bt_[hi_]), "tC"], ["tC"])
                    ACT_(tC[:], tC[:], AF.Exp, ["tC"], ["tC"], scale=CL)
                    bgc = self.nb()
                    for h in range(12):
                        P.add("pe", lambda e, h=h, bgc=bgc: e.matmul(ps[bgc][:64, 2 * h:2 * h + 2], Ssg[:, h * 64:(h + 1) * 64], onesf[:, 0:2], start=True, stop=True),
                              reads=["Ssg", "onesf"], writes=[("ps", bgc)])
                    ACT_(gC[:], ps[bgc][:64, 0:24], AF.Exp, [("ps", bgc)], ["gC"], scale=CL)
                    TT_("pool", Rt[:], r_, E1[:], ALU.mult, ["zc", "E1"], ["Rt"])
                    TT_("pool", Kt[:], kd[:], E2[:], ALU.mult, ["kd", "E2"], ["Kt"])
                    TT_("pool", At[:], kk[:], tB[:], ALU.mult, ["kk", "tB"], ["At"])
                    P.add("dve", lambda e: e.scalar_tensor_tensor(Bt[:], akk[:], -1.0, E2[:], ALU.mult, ALU.mult), reads=["akk", "E2"], writes=["Bt"])
                    TT_("pool", Kcb[:], kd[:], tC[:], ALU.mult, ["kd", "tC"], ["Kcb"])
                    P.add("dve", lambda e: e.scalar_tensor_tensor(Bcb[:], akk[:], -1.0, tC[:], ALU.mult, ALU.mult), reads=["akk", "tC"], writes=["Bcb"])
                    ACT_(Vb[:], v_, AF.Copy, ["zc"], ["Vb"])
                    TT_("pool", tA[:], r_, kd[:], ALU.mult, ["zc", "kd"], ["tA"])
                    TT_("pool", tA[:], tA[:], rkbc[:], ALU.mult, ["tA", "rkbc"], ["tA"])
                    P.add("dve", lambda e: e.tensor_reduce(s12[:, 16:28], h3(tA[:]), AX.X, ALU.add), reads=["tA"], writes=["s12b"])
                    TT_("pool", h3(bon[:]), h3(v_), bc12(s12[:, 16:28]), ALU.mult, ["zc", "s12b"], ["bon"])
                    for (X0, X1, xk0, xk1, DST, dk_) in ((At, Rt, "At", "Rt", ART, "ART"), (Kt, Bt, "Kt", "Bt", KBT, "KBT")):
                        for j in range(3):
                            b = self.nb()
                            pv = ps[b][:].bitcast(BF16)
                            for hh in range(4):
                                h = 4 * j + hh
                                for wi, (X, xk) in enumerate(((X0, xk0), (X1, xk1))):
                                    off = (hh * 2 + wi) * 128
                                    P.add("pe", lambda e, pv=pv, off=off, X=X, h=h: e.transpose(pv[:64, off:off + 128], X[:, h * 64:(h + 1) * 64], idb[:]),
                                          reads=[xk, "idb"], writes=[("ps", b)])
                            dsl = DST[:64, 4 * j:4 * j + 4, :, :].rearrange("p h a t -> p (h a t)")
                            if j % 2 == 0:
                                ACT_(dsl, pv[:64, :], AF.Copy, [("ps", b)], [dk_])
                            else:
                                P.add("dve", lambda e, dsl=dsl, pv=pv: e.tensor_copy(dsl, pv[:64, :]), reads=[("ps", b)], writes=[dk_])
                    mk2 = MK2[d][:].rearrange("p h a t -> p (h a t)")
                    mn4 = MN4[d][:].rearrange("p h t -> p (h t)")
                    for (wi, DST, dk_) in ((0, AKRK, "AKRK"), (1, ABRB, "ABRB")):
                        for hp in range(6):
                            b = self.nb()
                            for hh in range(2):
                                h = 2 * hp + hh
                                P.add("pe", lambda e, b=b, hh=hh, h=h, wi=wi: e.matmul(
                                    ps[b][:, hh * 256:(hh + 1) * 256], KBT[:, h, wi, :], ART[:, h, :, :].rearrange("p a t -> p (a t)"),
                                    start=True, stop=True), reads=["KBT", "ART"], writes=[("ps", b)])
                            dsl = DST[:, 2 * hp:2 * hp + 2, :, :].rearrange("p h a t -> p (h a t)")
                            TT_("dve", dsl, ps[b][:], mk2, ALU.mult, [("ps", b), "MK2_%d" % d], [dk_])
                    for j in range(3):
                        b = self.nb()
                        for hh in range(4):
                            h = 4 * j + hh
                            P.add("pe", lambda e, b=b, hh=hh, h=h: e.matmul(
                                ps[b][:, hh * 128:(hh + 1) * 128], ART[:, h, 0, :], KBT[:, h, 1, :], start=True, stop=True),
                                reads=["KBT", "ART"], writes=[("ps", b)])
                        TT_("dve", Pn[0][:, 4 * j:4 * j + 4, :].rearrange("p h t -> p (h t)"), ps[b][:], mn4, ALU.mult,
                            [("ps", b), "MN4_%d" % d], ["Pn0"])
                    TT_("pool", TTb[0][:], ABRB[:, :, 0, :], idb[:].rearrange("p (o t) -> p o t", o=1).to_broadcast([128, 12, 128]), ALU.add,
                        ["ABRB", "idb"], ["TTb0"])
                    for k in range(6):
                        cur, nxt = k % 2, (k + 1) % 2
                        Pc = Pn[cur]
                        PTc = (lambda h: ABRB[:, h, 0, :]) if k == 0 else (lambda h, cur=cur: PnT[cur][:, h, :])
                        ptkey = "ABRB" if k == 0 else "PnT%d" % cur
                        for j in range(3):
                            b = self.nb()
                            for hh in range(4):
                                h = 4 * j + hh
                                P.add("pe", lambda e, b=b, hh=hh, h=h, Pc=Pc, PTc=PTc: e.matmul(
                                    ps[b][:, hh * 128:(hh + 1) * 128], PTc(h), Pc[:, h, :], start=True, stop=True),
                                    reads=[ptkey, "Pn%d" % cur], writes=[("ps", b)])
                            ACT_(Pn[nxt][:, 4 * j:4 * j + 4, :].rearrange("p h t -> p (h t)"), ps[b][:], AF.Copy, [("ps", b)], ["Pn%d" % nxt])
                        if k < 5:
                            for j in range(3):
                                b = self.nb()
                                for hh in range(4):
                                    h = 4 * j + hh
                                    P.add("pe", lambda e, b=b, hh=hh, h=h, Pc=Pc, PTc=PTc: e.matmul(
                                        ps[b][:, hh * 128:(hh + 1) * 128], Pc[:, h, :], PTc(h), start=True, stop=True),
                                        reads=[ptkey, "Pn%d" % cur], writes=[("ps", b)])
                                P.add("dve", lambda e, b=b, j=j, nxt=nxt: e.tensor_copy(
                                    PnT[nxt][:, 4 * j:4 * j + 4, :].rearrange("p h t -> p (h t)"), ps[b][:]),
                                    reads=[("ps", b)], writes=["PnT%d" % nxt])
                        for j in range(3):
                            b = self.nb()
                            for hh in range(4):
                                h = 4 * j + hh
                                P.add("pe", lambda e, b=b, hh=hh, h=h, nxt=nxt, cur=cur: e.matmul(
                                    ps[b][:, hh * 128:(hh + 1) * 128], Pn[nxt][:, h, :], TTb[cur][:, h, :], start=True, stop=True),
                                    reads=["Pn%d" % nxt, "TTb%d" % cur], writes=[("ps", b)])
                            TT_("dve", TTb[nxt][:, 4 * j:4 * j + 4, :].rearrange("p h t -> p (h t)"), ps[b][:],
                                TTb[cur][:, 4 * j:4 * j + 4, :].rearrange("p h t -> p (h t)"), ALU.add,
                                [("ps", b), "TTb%d" % cur], ["TTb%d" % nxt])
                    TTf = TTb[0]
                    hsl = lambda h: slice(h * 64, (h + 1) * 64)
                    bank_of = lambda bb, h: (bb[0], (h * 64)) if h < 8 else (bb[1], (h - 8) * 64)
                    bW = [self.nb(), self.nb()]
                    for h in range(12):
                        b, o = bank_of(bW, h)
                        P.add("pe", lambda e, b=b, o=o, h=h: e.matmul(ps[b][:, o:o + 64], ART[:, h, 0, :], Hb[:, h, :], start=True, stop=False),
                              reads=["ART", "Hb"], writes=[("ps", b)])
                        P.add("pe", lambda e, b=b, o=o, h=h: e.matmul(ps[b][:, o:o + 64], AKRK[:, h, 0, :], Vb[:, hsl(h)], start=False, stop=True),
                              reads=["AKRK", "Vb"], writes=[("ps", b)])
                    ACT_(Wb[:, 0:512], ps[bW[0]][:], AF.Copy, [("ps", bW[0])], ["Wb"])
                    P.add("dve", lambda e, bW=bW: e.tensor_copy(Wb[:, 512:768], ps[bW[1]][:, 0:256]), reads=[("ps", bW[1])], writes=["Wb"])
                    bU = [self.nb(), self.nb()]
                    for h in range(12):
                        b, o = bank_of(bU, h)
                        P.add("pe", lambda e, b=b, o=o, h=h: e.matmul(ps[b][:, o:o + 64], TTf[:, h, :], Wb[:, hsl(h)], start=True, stop=True),
                              reads=["TTb0", "Wb"], writes=[("ps", b)])
                    ACT_(Ub[:, 0:512], ps[bU[0]][:], AF.Copy, [("ps", bU[0])], ["Ub"])
                    P.add("dve", lambda e, bU=bU: e.tensor_copy(Ub[:, 512:768], ps[bU[1]][:, 0:256]), reads=[("ps", bU[1])], writes=["Ub"])
                    bY = [self.nb(), self.nb()]
                    for h in range(12):
                        b, o = bank_of(bY, h)
                        P.add("pe", lambda e, b=b, o=o, h=h: e.matmul(ps[b][:, o:o + 64], ART[:, h, 1, :], Hb[:, h, :], start=True, stop=False),
                              reads=["ART", "Hb"], writes=[("ps", b)])
                        P.add("pe", lambda e, b=b, o=o, h=h: e.matmul(ps[b][:, o:o + 64], AKRK[:, h, 1, :], Vb[:, hsl(h)], start=False, stop=False),
                              reads=["AKRK", "Vb"], writes=[("ps", b)])
                        P.add("pe", lambda e, b=b, o=o, h=h: e.matmul(ps[b][:, o:o + 64], ABRB[:, h, 1, :], Ub[:, hsl(h)], start=False, stop=True),
                              reads=["ABRB", "Ub"], writes=[("ps", b)])
                    bH = [self.nb(), self.nb()]
                    for h in range(12):
                        b, o = bank_of(bH, h)
                        P.add("pe", lambda e, b=b, o=o, h=h: e.matmul(ps[b][:64, o:o + 64], Kcb[:, hsl(h)], Vb[:, hsl(h)], start=True, stop=False),
                              reads=["Kcb", "Vb"], writes=[("ps", b)])
                        P.add("pe", lambda e, b=b, o=o, h=h: e.matmul(ps[b][:64, o:o + 64], Bcb[:, hsl(h)], Ub[:, hsl(h)], start=False, stop=True),
                              reads=["Bcb", "Ub"], writes=[("ps", b)])
                    for h in range(12):
                        b, o = bank_of(bH, h)
                        P.add("dve", lambda e, b=b, o=o, h=h: e.scalar_tensor_tensor(
                            Hf[:, h, :], Hf[:, h, :], gC[:, 2 * h:2 * h + 1], ps[b][:64, o:o + 64], ALU.mult, ALU.add),
                            reads=["Hf", "gC", ("ps", b)], writes=["Hf"])
                    ACT_(Hb[:64, :, :], Hf[:], AF.Copy, ["Hf"], ["Hb"])
                    if d == 0:
                        ACT_(y_[:, 0:512], ps[bY[0]][:], AF.Copy, [("ps", bY[0])], ["y_"])
                        P.add("dve", lambda e, bY=bY: e.tensor_copy(y_[:, 512:768], ps[bY[1]][:, 0:256]), reads=[("ps", bY[1])], writes=["y_"])
                        P.add("sp", lambda e, t0=t0: e.dma_start(out=S["yrw"][t0:t0 + 128, :], in_=y_[:]), reads=["y_"], dma=True, semkey="st_y")
                        P.add("sp", lambda e, t0=t0: e.dma_start(out=S["bon"][t0:t0 + 128, :], in_=bon[:]), reads=["bon"], dma=True, semkey="st_bon")
                    else:
                        ld("sp", yf[:], S["yrw"][t0:t0 + 128, :], "E1")
                        ld("sp", bf_[:], S["bon"][t0:t0 + 128, :], "E2")
                        TT_("dve", y_[:, 0:512], ps[bY[0]][:], yf[:, 0:512], ALU.add, [("ps", bY[0]), "E1"], ["y_"])
                        TT_("dve", y_[:, 512:768], ps[bY[1]][:, 0:256], yf[:, 512:768], ALU.add, [("ps", bY[1]), "E1"], ["y_"])
                        P.add("dve", lambda e: e.tensor_reduce(s12[:, 32:44], h3(y_[:]), AX.X, ALU.add), reads=["y_"], writes=["s12c"])
                        P.add("pool", lambda e: e.tensor_scalar(s12[:, 32:44], s12[:, 32:44], -1.0 / 64, None, ALU.mult), reads=["s12c"], writes=["s12c"])
                        TT_("pool", h3(y_[:]), h3(y_[:]), bc12(s12[:, 32:44]), ALU.add, ["y_", "s12c"], ["y_"])
                        TT_("pool", tA[:], y_[:], y_[:], ALU.mult, ["y_"], ["tA"])
                        P.add("dve", lambda e: e.tensor_reduce(s12[:, 48:60], h3(tA[:]), AX.X, ALU.add), reads=["tA"], writes=["s12d"])
                        ACT_(s12[:, 48:60], s12[:, 48:60], AF.Sqrt, ["s12d"], ["s12d"], bias=GN_EPS, scale=1.0 / 64)
                        P.add("dve", lambda e: e.reciprocal(s12[:, 48:60], s12[:, 48:60]), reads=["s12d"], writes=["s12d"])
                        TT_("pool", h3(y_[:]), h3(y_[:]), bc12(s12[:, 48:60]), ALU.mult, ["y_", "s12d"], ["y_"])
                        TT_("pool", y_[:], y_[:], lgbc[:], ALU.mult, ["y_", "lgbc"], ["y_"])
                        TT_("pool", y_[:], y_[:], lbbc[:], ALU.add, ["y_", "lbbc"], ["y_"])
                        TT_("pool", y_[:], y_[:], bon[:], ALU.add, ["y_", "bon"], ["y_"])
                        TT_("pool", y_[:], y_[:], bf_[:], ALU.add, ["y_", "E2"], ["y_"])
                        TT_("dve", ob[:], y_[:], g_[:], ALU.mult, ["y_", "g_"], ["ob"])
                        b = self.nb()
                        pv = ps[b][:].bitcast(BF16)
                        for c in range(6):
                            P.add("pe", lambda e, pv=pv, c=c: e.transpose(pv[:, c * 128:(c + 1) * 128], ob[:, c * 128:(c + 1) * 128], idb[:]),
                                  reads=["ob", "idb"], writes=[("ps", b)])
                        ACT_(ost[:].rearrange("p c t -> p (c t)"), pv[:, 0:768], AF.Copy, [("ps", b)], ["ost"])
                        dst = S["oT"][0:768, t0:t0 + 128].rearrange("(c p) t -> p c t", p=128)
                        P.add("sp", lambda e, dst=dst: e.dma_start(out=dst, in_=ost[:]), reads=["ost"], dma=True, semkey="st_o")
                P.flush()

    def phase_ffn(self, l, xin, xout, T):
        nc, P, S, C, W, ps = self.nc, self.P, self.S, self.C, self.W, self.ps
        NT = T // TT
        with contextlib.ExitStack() as st0:
            u0 = self.uid()
            sb0 = lambda name, shape, dt: st0.enter_context(nc.sbuf_tensor("E%d_" % u0 + name, shape, dt))
            x1 = sb0("x1", [128, 4, D], F32)
            x1T = sb0("x1T", [128, 16, TT], BF16)
            gates = sb0("gates", [128, 4, NE], F32)
            idf = sb0("idf", [128, 128], F32)
            P.add("sp", lambda e: e.dma_start(out=idf[:], in_=C["ident"]), writes=["idf"], dma=True, semkey="L_idf")
            P.flush()
            for ti in range(NT):
                t0 = ti * TT
                with contextlib.ExitStack() as st:
                    u = self.uid()
                    sb = lambda name, shape, dt: st.enter_context(nc.sbuf_tensor("E%d_" % u + name, shape, dt))
                    oT = sb("oT", [128, 16, TT], BF16)
                    mT = sb("mT", [128, 16, TT], BF16)
                    sg = [sb("sg%d" % i, [128, 3, 4, TT], BF16) for i in range(2)]
                    wt = [sb("wt%d" % i, [128, 16, 512], BF16) for i in range(2)]
                    gbc = sb("gbc", [128, D], F32)
                    bbc = sb("bbc", [128, D], F32)
                    ta = [sb("ta%d" % i, [128, TT], F32) for i in range(2)]
                    tb = [sb("tb%d" % i, [128, TT], F32) for i in range(2)]
                    tcc = [sb("tc%d" % i, [128, TT], F32) for i in range(2)]
                    ts1 = [sb("ts%d" % i, [128, TT], F32) for i in range(2)]
                    xf = [sb("xf%d" % i, [128, TT], F32) for i in range(2)]
                    rw = sb("rw", [128, 16, NE], F32)
                    rb = sb("rb", [128, NE], F32)
                    junk = sb("junk", [128, D], BF16)
                    st4 = sb("st4", [128, 8], F32)
                    rt_ = sb("rt_", [128, 64], F32)
                    ld = lambda eng, out, in_, key: P.add(eng, lambda e: e.dma_start(out=out, in_=in_), writes=[key], dma=True, semkey="L_" + key)
                    ld("sp", oT[:], S["oT"][:, t0:t0 + TT].rearrange("(c p) t -> p c t", p=128), "oT")
                    ld("sp", x1[:], xin[t0:t0 + TT, :].rearrange("(s p) d -> p s d", p=128), "x1")
                    ld("sp", gbc[:], W["ln1_g"][l].partition_broadcast(128), "gbc")
                    ld("sp", bbc[:], W["ln1_b"][l].partition_broadcast(128), "bbc")
                    ld("sp", rw[:], W["router_w"].rearrange("(c p) e -> p c e", p=128), "rw")
                    ld("sp", rb[:], W["router_bias"][0].partition_broadcast(128), "rb")
                    wcnt = 0
                    for nbk in range(4):
                        ws = wcnt % 2
                        wcnt += 1
                        for (nm, k0, nk) in (("w_up_rw", 0, 6), ("w_up_mla", 6, 4), ("w_up_df", 10, 6)):
                            P.add("pool", lambda e, ws=ws, nm=nm, k0=k0, nk=nk, nbk=nbk: e.dma_start(
                                out=wt[ws][:, k0:k0 + nk, :],
                                in_=self.WB[nm][l].rearrange("(c p) n -> p c n", p=128)[:, :, nbk * 512:(nbk + 1) * 512]),
                                writes=[("wt", ws)], dma=True, semkey="L_wt%d" % ws)
                        sgs = nbk % 2
                        for br in range(3):
                            P.add("sp", lambda e, sgs=sgs, br=br, nbk=nbk: e.dma_start(
                                out=sg[sgs][:, br, :, :],
                                in_=S["sgT"][br * D + nbk * 512:br * D + (nbk + 1) * 512, t0:t0 + TT].rearrange("(c p) t -> p c t", p=128)),
                                writes=[("sg", sgs)], dma=True, semkey="L_sg%d" % sgs)
                        for c in range(4):
                            n = nbk * 4 + c
                            q = n % 2
                            bks = []
                            for br, (k0, nk) in enumerate(((0, 6), (6, 4), (10, 6))):
                                b = self.nb()
                                bks.append(b)
                                for kk in range(nk):
                                    kc = k0 + kk
                                    P.add("pe", lambda e, b=b, ws=ws, kc=kc, c=c, kk=kk, nk=nk: e.matmul(
                                        ps[b][:], wt[ws][:, kc, c * 128:(c + 1) * 128], oT[:, kc, :],
                                        start=(kk == 0), stop=(kk == nk - 1)),
                                        reads=[("wt", ws), "oT"], writes=[("ps", b)])
                            P.add("dve", lambda e, q=q, b=bks[0], sgs=sgs, c=c: e.tensor_tensor(ta[q][:], ps[b][:], sg[sgs][:, 0, c, :], ALU.mult),
                                  reads=[("ps", bks[0]), ("sg", sgs)], writes=[("ta", q)])
                            P.add("dve", lambda e, q=q, b=bks[1], sgs=sgs, c=c: e.tensor_tensor(tb[q][:], ps[b][:], sg[sgs][:, 1, c, :], ALU.mult),
                                  reads=[("ps", bks[1]), ("sg", sgs)], writes=[("tb", q)])
                            P.add("dve", lambda e, q=q, b=bks[2], sgs=sgs, c=c: e.tensor_tensor(tcc[q][:], ps[b][:], sg[sgs][:, 2, c, :], ALU.mult),
                                  reads=[("ps", bks[2]), ("sg", sgs)], writes=[("tc", q)])
                            P.add("pool", lambda e, q=q: e.tensor_tensor(ts1[q][:], ta[q][:], tb[q][:], ALU.add),
                                  reads=[("ta", q), ("tb", q)], writes=[("ts", q)])
                            P.add("pool", lambda e, q=q, n=n: e.tensor_tensor(mT[:, n, :], ts1[q][:], tcc[q][:], ALU.add),
                                  reads=[("ts", q), ("tc", q)], writes=[("mT", n)])
                    mT_all = [("mT", n) for n in range(16)]
                    for nbk in range(4):
                        ws = wcnt % 2
                        wcnt += 1
                        P.add("pool", lambda e, ws=ws, nbk=nbk: e.dma_start(
                            out=wt[ws][:], in_=self.WB["w_o"][l].rearrange("(c p) n -> p c n", p=128)[:, :, nbk * 512:(nbk + 1) * 512]),
                            writes=[("wt", ws)], dma=True, semkey="L_wt%d" % ws)
                        for s_ in range(4):
                            b = self.nb()
                            for kc in range(16):
                                P.add("pe", lambda e, b=b, ws=ws, kc=kc, s_=s_: e.matmul(
                                    ps[b][:], mT[:, kc, s_ * 128:(s_ + 1) * 128], wt[ws][:, kc, :],
                                    start=(kc == 0), stop=(kc == 15)),
                                    reads=mT_all + [("wt", ws)], writes=[("ps", b)])
                            P.add("dve", lambda e, b=b, s_=s_, nbk=nbk: e.scalar_tensor_tensor(
                                x1[:, s_, nbk * 512:(nbk + 1) * 512], x1[:, s_, nbk * 512:(nbk + 1) * 512], float(ALPHA), ps[b][:],
                                ALU.mult, ALU.add), reads=[("ps", b), "x1"], writes=[("x1s", s_)])
                    for s_ in range(4):
                        self.layer_norm(x1[:, s_, :], ("x1s", s_), gbc, bbc, junk, st4, s_)
                    rbanks = [4, 5, 6, 7]
                    for kc in range(16):
                        b = kc % 4
                        q = kc % 2
                        for s_ in range(4):
                            P.add("pe", lambda e, b=b, kc=kc, s_=s_: e.transpose(
                                ps[b][:, s_ * 128:(s_ + 1) * 128], x1[:, s_, kc * 128:(kc + 1) * 128], idf[:]),
                                reads=[("x1s", s_), "idf"], writes=[("ps", b)])
                        P.add("act", lambda e, b=b, kc=kc: e.activation(x1T[:, kc, :], ps[b][:], AF.Copy),
                              reads=[("ps", b)], writes=[("x1T", kc)])
                        P.add("dve", lambda e, b=b, q=q: e.tensor_copy(xf[q][:], ps[b][:]), reads=[("ps", b)], writes=[("xf", q)])
                        for s_ in range(4):
                            P.add("pe", lambda e, kc=kc, s_=s_, q=q: e.matmul(
                                ps[rbanks[s_]][:, :NE], xf[q][:, s_ * 128:(s_ + 1) * 128], rw[:, kc, :],
                                start=(kc == 0), stop=(kc == 15)),
                                reads=[("xf", q), "rw"], writes=[("ps", rbanks[s_])])
                    for s_ in range(4):
                        self.routing(ps[rbanks[s_]][:, :NE], ("ps", rbanks[s_]), rb, rt_, gates[:, s_, :], s_)
                    P.flush()
                with contextlib.ExitStack() as st:
                    u = self.uid()
                    sb = lambda name, shape, dt: st.enter_context(nc.sbuf_tensor("E%d_" % u + name, shape, dt))
                    wsl = [sb("w%d" % i, [128, 16, 512], BF16) for i in range(3)]
                    hT = sb("hT", [128, 8, TT], BF16)
                    yacc = sb("yacc", [128, 4, D], F32)
                    sgt = [sb("sgt%d" % i, [128, TT], F32) for i in range(2)]
                    gbc = sb("gbc", [128, D], F32)
                    bbc = sb("bbc", [128, D], F32)
                    junk = sb("junk", [128, D], BF16)
                    st4 = sb("st4", [128, 8], F32)
                    ld = lambda eng, out, in_, key: P.add(eng, lambda e: e.dma_start(out=out, in_=in_), writes=[key], dma=True, semkey="L_" + key)
                    ld("sp", gbc[:], W["ln2_g"][l].partition_broadcast(128), "gbc")
                    ld("sp", bbc[:], W["ln2_b"][l].partition_broadcast(128), "bbc")
                    x1T_all = [("x1T", kc) for kc in range(16)]
                    wcnt = 0
                    hcnt = 0
                    for ex in range(NE):
                        for jb in range(2):
                            wsg = wcnt % 3
                            wsu = (wcnt + 1) % 3
                            wcnt += 2
                            for (nm, wsx) in (("ex_w_gate", wsg), ("ex_w_up", wsu)):
                                P.add("pool", lambda e, nm=nm, wsx=wsx, ex=ex, jb=jb: e.dma_start(
                                    out=wsl[wsx][:], in_=self.WB[nm][l, ex].rearrange("(c p) n -> p c n", p=128)[:, :, jb * 512:(jb + 1) * 512]),
                                    writes=[("w", wsx)], dma=True, semkey="L_w%d" % wsx)
                            for jj in range(4):
                                j = jb * 4 + jj
                                bg, bu = self.nb(), self.nb()
                                for kc in range(16):
                                    P.add("pe", lambda e, bg=bg, wsg=wsg, kc=kc, jj=jj: e.matmul(
                                        ps[bg][:], wsl[wsg][:, kc, jj * 128:(jj + 1) * 128], x1T[:, kc, :],
                                        start=(kc == 0), stop=(kc == 15)), reads=x1T_all + [("w", wsg)], writes=[("ps", bg)])
                                for kc in range(16):
                                    P.add("pe", lambda e, bu=bu, wsu=wsu, kc=kc, jj=jj: e.matmul(
                                        ps[bu][:], wsl[wsu][:, kc, jj * 128:(jj + 1) * 128], x1T[:, kc, :],
                                        start=(kc == 0), stop=(kc == 15)), reads=x1T_all + [("w", wsu)], writes=[("ps", bu)])
                                q = hcnt % 2
                                hcnt += 1
                                P.add("act", lambda e, bg=bg, q=q: e.activation(sgt[q][:], ps[bg][:], AF.Silu),
                                      reads=[("ps", bg)], writes=[("sgt", q)])
                                P.add("dve", lambda e, bu=bu, q=q, j=j: e.tensor_tensor(hT[:, j, :], ps[bu][:], sgt[q][:], ALU.mult),
                                      reads=[("ps", bu), ("sgt", q)], writes=[("hT", j)])
                        hT_all = [("hT", j) for j in range(8)]
                        for nbk in range(4):
                            wsd = wcnt % 3
                            wcnt += 1
                            P.add("pool", lambda e, wsd=wsd, ex=ex, nbk=nbk: e.dma_start(
                                out=wsl[wsd][:, 0:8, :],
                                in_=self.WB["ex_w_down"][l, ex].rearrange("(c p) n -> p c n", p=128)[:, :, nbk * 512:(nbk + 1) * 512]),
                                writes=[("w", wsd)], dma=True, semkey="L_w%d" % wsd)
                            for s_ in range(4):
                                b = self.nb()
                                for j in range(8):
                                    P.add("pe", lambda e, b=b, wsd=wsd, j=j, s_=s_: e.matmul(
                                        ps[b][:], hT[:, j, s_ * 128:(s_ + 1) * 128], wsl[wsd][:, j, :],
                                        start=(j == 0), stop=(j == 7)), reads=hT_all + [("w", wsd)], writes=[("ps", b)])
                                ysl = yacc[:, s_, nbk * 512:(nbk + 1) * 512]
                                if ex == 0:
                                    P.add("dve", lambda e, b=b, ysl=ysl, s_=s_, ex=ex: e.tensor_scalar(
                                        ysl, ps[b][:], gates[:, s_, ex:ex + 1], None, ALU.mult),
                                        reads=[("ps", b), "gates"], writes=[("y", s_, nbk)])
                                else:
                                    P.add("dve", lambda e, b=b, ysl=ysl, s_=s_, ex=ex: e.scalar_tensor_tensor(
                                        ysl, ps[b][:], gates[:, s_, ex:ex + 1], ysl, ALU.mult, ALU.add),
                                        reads=[("ps", b), "gates", ("y", s_, nbk)], writes=[("y", s_, nbk)])
                    for s_ in range(4):
                        yk = [("y", s_, nbk) for nbk in range(4)]
                        P.add("dve", lambda e, s_=s_: e.scalar_tensor_tensor(
                            yacc[:, s_, :], x1[:, s_, :], float(ALPHA), yacc[:, s_, :], ALU.mult, ALU.add),
                            reads=yk + [("x1s", s_)], writes=[("ys", s_)])
                        self.layer_norm(yacc[:, s_, :], ("ys", s_), gbc, bbc, junk, st4, s_)
                    dst = xout[t0:t0 + TT, :].rearrange("(s p) d -> p s d", p=128)
                    P.add("sp", lambda e, dst=dst: e.dma_start(out=dst, in_=yacc[:]),
                          reads=[("ys", s_) for s_ in range(4)], dma=True, semkey="st_y")
                    P.flush()

    def layer_norm(self, xap, key, gbc, bbc, junk, st4, s_):
        P = self.P
        k = "st4"
        P.add("dve", lambda e: e.tensor_reduce(st4[:, 0:1], xap, AX.X, ALU.add), reads=[key], writes=[k])
        P.add("pool", lambda e: e.tensor_scalar(st4[:, 1:2], st4[:, 0:1], -1.0 / D, None, ALU.mult), reads=[k], writes=[(k, 1)])
        P.add("act", lambda e: e.activation(xap, xap, AF.Identity, bias=st4[:, 1:2]), reads=[(k, 1), key], writes=[key])
        P.add("act", lambda e: e.activation(junk[:], xap, AF.Square, accum_out=st4[:, 2:3]), reads=[key], writes=["junk", (k, 2)])
        P.add("act", lambda e: e.activation(st4[:, 3:4], st4[:, 2:3], AF.Sqrt, bias=LN_EPS, scale=1.0 / D), reads=[(k, 2)], writes=[(k, 3)])
        P.add("dve", lambda e: e.reciprocal(st4[:, 4:5], st4[:, 3:4]), reads=[(k, 3)], writes=[(k, 4)])
        P.add("dve", lambda e: e.scalar_tensor_tensor(xap, xap, st4[:, 4:5], gbc[:], ALU.mult, ALU.mult),
              reads=[key, (k, 4), "gbc"], writes=[key])
        P.add("pool", lambda e: e.tensor_tensor(xap, xap, bbc[:], ALU.add), reads=[key, "bbc"], writes=[key])

    def routing(self, logits, lkey, rb, rt_, gout, s_):
        P = self.P
        sc, sel, M, N_ = rt_[:, 0:16], rt_[:, 16:32], rt_[:, 32:40], rt_[:, 40:48]
        sel3 = sel.rearrange("p (g e) -> p g e", e=4)
        M3 = M.rearrange("p (g e) -> p g e", e=2)
        N3 = N_.rearrange("p (g e) -> p g e", e=2)
        hi, lo, nn, gsc = rt_[:, 48:52], rt_[:, 52:56], rt_[:, 56:60], rt_[:, 60:64]
        k = "rt"
        seq = []
        A = lambda eng, fn: seq.append((eng, fn))
        A("act", lambda e: e.activation(sc, logits, AF.Sigmoid))
        A("dve", lambda e: e.tensor_tensor(sel, sc, rb[:], ALU.add))
        A("dve", lambda e: e.tensor_tensor(M3, sel3[:, :, 0:2], sel3[:, :, 2:4], ALU.max))
        A("dve", lambda e: e.tensor_tensor(N3, sel3[:, :, 0:2], sel3[:, :, 2:4], ALU.min))
        A("dve", lambda e: e.tensor_tensor(hi.rearrange("p (g o) -> p g o", o=1), M3[:, :, 0:1], M3[:, :, 1:2], ALU.max))
        A("dve", lambda e: e.tensor_tensor(lo.rearrange("p (g o) -> p g o", o=1), M3[:, :, 0:1], M3[:, :, 1:2], ALU.min))
        A("dve", lambda e: e.tensor_tensor(nn.rearrange("p (g o) -> p g o", o=1), N3[:, :, 0:1], N3[:, :, 1:2], ALU.max))
        A("dve", lambda e: e.tensor_tensor(lo, lo, nn, ALU.max))
        A("dve", lambda e: e.tensor_tensor(gsc, hi, lo, ALU.add))
        gm = M[:, 0:1]
        A("dve", lambda e: e.tensor_reduce(gm, gsc, AX.X, ALU.max))
        A("dve", lambda e: e.tensor_scalar(hi, gsc, gm, None, ALU.is_equal))
        A("dve", lambda e: e.tensor_scalar(hi, hi, -1.0, 1e30, ALU.add, ALU.mult))
        for ei in range(4):
            A("dve", lambda e, ei=ei: e.tensor_tensor(sel3[:, :, ei:ei + 1], sel3[:, :, ei:ei + 1],
                                                     hi.rearrange("p (g o) -> p g o", o=1), ALU.add))
        m1 = M[:, 1:2]
        eq = rt_[:, 32:48]
        A("dve", lambda e: e.tensor_reduce(m1, sel, AX.X, ALU.max))
        eq1 = rt_[:, 40:56]
        A("dve", lambda e: e.tensor_scalar(eq1, sel, m1, None, ALU.is_equal))
        A("dve", lambda e: e.scalar_tensor_tensor(sel, eq1, -1e30, sel, ALU.mult, ALU.add))
        m2 = M[:, 2:3]
        A("dve", lambda e: e.tensor_reduce(m2, sel, AX.X, ALU.max))
        A("dve", lambda e: e.scalar_tensor_tensor(sel, sel, m2, eq1, ALU.is_equal, ALU.add))
        A("dve", lambda e: e.tensor_tensor(sel, sel, sc, ALU.mult))
        den = M[:, 3:4]
        A("dve", lambda e: e.tensor_reduce(den, sel, AX.X, ALU.add))
        A("dve", lambda e: e.reciprocal(den, den))
        A("dve", lambda e: e.tensor_scalar(gout, sel, den, None, ALU.mult))
        for i, (eng, fn) in enumerate(seq):
            rd = [k, "rb"] + ([lkey] if i == 0 else [])
            wr = [k] + (["gates"] if i == len(seq) - 1 else [])
            P.add(eng, fn, reads=rd, writes=wr)


def host_weights(W):
    out = {}
    for k, shp in WEIGHT_SHAPES.items():
        out[k] = np.ascontiguousarray(np.asarray(W[k], dtype=np.float32).reshape(shp))
    return out


_CACHE = {}


def _get_program(seqs):
    key = tuple(seqs)
    if key not in _CACHE:
        b = Builder(seqs)
        nc = b.build()
        _CACHE[key] = (b, nc)
    return _CACHE[key]


def kernel(**inputs):
    x_prompt = np.asarray(inputs["x_prompt"], dtype=np.float32)
    x_sample = np.asarray(inputs["x_sample"], dtype=np.float32)
    NB_P, S_P = x_prompt.shape[0], x_prompt.shape[1]
    NB_S, S_S = x_sample.shape[0], x_sample.shape[1]
    n_cores = 8
    assert NB_S == n_cores and n_cores % NB_P == 0
    seqs = [S_S, S_P]
    b, nc = _get_program(seqs)
    hw = host_weights(inputs)
    consts = make_consts(max(seqs))
    base = {k: hw[k] for k in b.W.used()}
    base.update({k: consts[k] for k in b.C.used()})
    in_maps = []
    for c in range(n_cores):
        d = dict(base)
        d["xs0"] = np.ascontiguousarray(x_sample[c])
        d["xs1"] = np.ascontiguousarray(x_prompt[c % NB_P])
        in_maps.append(d)
    res = run_bass_kernel_spmd(nc, in_maps, core_ids=list(range(n_cores)))
    y_sample = np.stack([np.asarray(res.results[c]["ys0"], dtype=np.float32) for c in range(n_cores)], 0)
    y_prompt = np.stack([np.asarray(res.results[c]["ys1"], dtype=np.float32) for c in range(NB_P)], 0)
    return (y_prompt, y_sample)
```

```python
import contextlib
import numpy as np
import ml_dtypes
import concourse.bass as bass
import concourse.mybir as mybir
from concourse.bass_utils import run_bass_kernel_spmd

F32 = mybir.dt.float32
BF16 = mybir.dt.bfloat16
AF = mybir.ActivationFunctionType
ALU = mybir.AluOpType
AX = mybir.AxisListType

ENGS = ("pe", "act", "dve", "pool", "sp")

D = 2048
DEPTH = 2
IN_W = 11936
RW_IN, MLA_IN, DF_IN, GATE_IN = 2688, 800, 2304, 6144
C_RW = 768
NE, DE = 16, 1024
ALPHA = (2 * DEPTH) ** 0.25
LN_EPS = 1e-5
RMS_EPS = 1e-6
DF_EPS = 1e-5
GN_EPS = 64e-5
TT = 512
import os as _os
NOSELF = _os.environ.get("NOSELF", "0") == "1"


class _Op:
    __slots__ = ("eng", "fn", "reads", "writes", "dma", "semkey", "deps", "needed", "token")


class Prog:
    def __init__(self, nc):
        self.nc = nc
        self.ops = []
        self.st = contextlib.ExitStack()
        self.esem = {e: self.st.enter_context(nc.semaphore("s_" + e)) for e in ENGS}
        self.dsem = {}
        self.ecount = {e: 0 for e in ENGS}
        self.dcount = {}
        self.seen = {e: {} for e in ENGS}
        self.n_total = 0
        self.phase_keys = {}

    def add(self, eng, fn, reads=(), writes=(), dma=False, semkey=None):
        op = _Op()
        if eng != "pe":
            extra = [r for r in reads if isinstance(r, tuple) and r and r[0] == "ps" and r not in writes]
            if extra:
                writes = tuple(writes) + tuple(extra)
        op.eng, op.fn, op.reads, op.writes = eng, fn, tuple(reads), tuple(writes)
        op.dma, op.semkey = dma, semkey
        op.deps, op.needed, op.token = set(), False, None
        if dma:
            pk = (eng, semkey)
            if pk not in self.phase_keys:
                idx = (eng, sum(1 for k in self.phase_keys if k[0] == eng))
                self.phase_keys[pk] = idx
                if idx not in self.dsem:
                    self.dsem[idx] = self.st.enter_context(self.nc.semaphore("d_%s%d" % idx))
                    self.dcount[idx] = 0
            op.semkey = self.phase_keys[pk]
        self.ops.append(op)
        return op

    def barrier(self):
        for e in ENGS:
            self.add(e, None, reads=("__all__",))

    def _analyze(self):
        ops = self.ops
        last_w, readers, last_of_eng, last_dma = {}, {}, {}, {}
        for i, op in enumerate(ops):
            deps = set()
            if op.reads == ("__all__",):
                deps.update(last_of_eng.values())
                deps.update(last_dma.values())
            else:
                for r in op.reads:
                    if r in last_w:
                        deps.add(last_w[r])
                for w in op.writes:
                    if w in last_w:
                        deps.add(last_w[w])
                    deps.update(readers.get(w, ()))
                for r in op.reads:
                    readers.setdefault(r, []).append(i)
                for w in op.writes:
                    last_w[w] = i
                    readers[w] = []
            deps.discard(i)
            op.deps = deps
            if op.fn is not None:
                if op.dma:
                    last_dma[op.semkey] = i
                else:
                    last_of_eng[op.eng] = i
        for op in ops:
            for d in op.deps:
                p = ops[d]
                if p.dma:
                    continue
                if p.eng == op.eng and not op.dma and (p.eng == "pe" or NOSELF):
                    continue
                p.needed = True

    def flush(self):
        self.barrier()
        self._analyze()
        ops = self.ops
        waits = []
        for op in ops:
            w = {}
            for d in op.deps:
                p = ops[d]
                if p.dma:
                    s = ("d", p.semkey)
                    v = self.dcount[p.semkey]
                else:
                    if p.eng == op.eng and not op.dma and (p.eng == "pe" or NOSELF):
                        continue
                    s = ("e", p.eng)
                    v = p.token
                if v > w.get(s, 0):
                    w[s] = v
            wl = []
            for s, v in w.items():
                if self.seen[op.eng].get(s, 0) >= v:
                    continue
                self.seen[op.eng][s] = v
                wl.append((self.dsem[s[1]] if s[0] == "d" else self.esem[s[1]], v))
            waits.append(wl)
            if op.fn is not None:
                if op.dma:
                    self.dcount[op.semkey] += 16
                    op.token = self.dcount[op.semkey]
                elif op.needed:
                    self.ecount[op.eng] += 1
                    op.token = self.ecount[op.eng]
        dsem, esem = self.dsem, self.esem

        def run(ename, eng):
            for op, wl in zip(ops, waits):
                if op.eng != ename:
                    continue
                for s, v in wl:
                    eng.wait_ge(s, v)
                if op.fn is None:
                    continue
                ins = op.fn(eng)
                if op.dma:
                    ins.then_inc(dsem[op.semkey], 16)
                elif op.needed:
                    ins.then_inc(esem[op.eng], 1)

        with self.nc.Block() as block:
            @block.tensor
            def _(eng):
                run("pe", eng)

            @block.scalar
            def _(eng):
                run("act", eng)

            @block.vector
            def _(eng):
                run("dve", eng)

            @block.gpsimd
            def _(eng):
                run("pool", eng)

            @block.sync
            def _(eng):
                run("sp", eng)
        self.n_total += len(ops)
        self.ops = []
        self.phase_keys = {}

    def close(self):
        self.st.close()


def _rope_tables(d, S):
    half = d // 2
    inv = np.power(np.float32(10000.0), -np.arange(half, dtype=np.float32) * np.float32(2.0 / d)).astype(np.float32)
    ang = np.arange(S, dtype=np.float32)[None, :] * inv[:, None]
    return np.cos(ang).astype(np.float32), np.sin(ang).astype(np.float32)


def make_consts(Tmax):
    c = {}
    cos64, sin64 = _rope_tables(64, Tmax)
    cos32, sin32 = _rope_tables(32, Tmax)
    c["cosdf"] = np.concatenate([cos64, cos64, cos64, cos64], 0)
    c["sindf"] = np.concatenate([-sin64, sin64, -sin64, sin64], 0)
    perm = np.zeros((128, 128), np.float32)
    for m in range(128):
        blk, j = divmod(m, 64)
        perm[blk * 64 + (j + 32) % 64, m] = 1.0
    c["permdf"] = perm
    c["cos96"] = np.concatenate([np.ones((64, Tmax), np.float32), cos32, cos32], 0)
    c["sin96"] = np.concatenate([np.zeros((64, Tmax), np.float32), -sin32, sin32], 0)
    p96 = np.zeros((96, 96), np.float32)
    for m in range(96):
        if m < 64:
            p96[m, m] = 1.0
        else:
            j = m - 64
            p96[64 + (j + 16) % 32, m] = 1.0
    c["perm96"] = p96
    p32 = np.zeros((128, 32), np.float32)
    for m in range(32):
        p32[(m + 16) % 32, m] = 1.0
    c["perm32"] = p32
    permq = np.zeros((768, 128), np.float32)
    cosq = np.ones((768, Tmax), np.float32)
    sinq = np.zeros((768, Tmax), np.float32)
    for g in range(768):
        j, m = divmod(g, 128)
        h, d_ = divmod(g, 96)
        if d_ < 64:
            permq[j * 128 + m, m] = 1.0
        else:
            jj = d_ - 64
            g2 = h * 96 + 64 + (jj + 16) % 32
            assert g2 // 128 == j
            permq[j * 128 + (g2 - j * 128), m] = 1.0
            cosq[g] = cos32[jj % 16]
            sinq[g] = -sin32[jj] if jj < 16 else sin32[jj - 16]
    c["permq"], c["cosq"], c["sinq"] = permq, cosq, sinq
    permk = np.zeros((128, 128), np.float32)
    for m in range(32):
        permk[(m + 16) % 32, m] = 1.0
    c["permk"] = permk
    cosk = np.zeros((128, Tmax), np.float32)
    sink = np.zeros((128, Tmax), np.float32)
    cosk[:32] = np.concatenate([cos32, cos32], 0)
    sink[:32] = np.concatenate([-sin32, sin32], 0)
    c["cosk"], c["sink"] = cosk, sink
    c["ident"] = np.eye(128, dtype=np.float32)
    c["ones"] = np.ones((128, 128), np.float32)
    s = np.arange(128)[:, None]
    t = np.arange(128)[None, :]
    c["m_su"] = (s < t).astype(np.float32)
    c["m_iu"] = (s <= t).astype(np.float32)
    c["m_sl"] = (s > t).astype(np.float32)
    c["m_il"] = (s >= t).astype(np.float32)
    return c


CONST_SHAPES = lambda Tmax: {
    "cosdf": [128, Tmax], "sindf": [128, Tmax], "permdf": [128, 128],
    "cos96": [96, Tmax], "sin96": [96, Tmax], "perm96": [96, 96], "perm32": [128, 32],
    "ident": [128, 128], "ones": [128, 128],
    "permq": [768, 128], "cosq": [768, Tmax], "sinq": [768, Tmax], "permk": [128, 128], "cosk": [128, Tmax], "sink": [128, Tmax],
    "m_su": [128, 128], "m_iu": [128, 128], "m_sl": [128, 128], "m_il": [128, 128],
}

WEIGHT_SHAPES = {
    "w_in": [2, 2048, 11936], "shift_prev": [2, 2688], "shift_next": [2, 2688],
    "rw_w0": [2, 2, 768], "rw_w2": [2, 2, 64, 768], "rw_a0": [2, 2, 768], "rw_a2": [2, 2, 64, 768],
    "rw_g2": [2, 128, 768], "rw_k_k": [2, 768], "rw_k_a": [2, 768], "rw_r_k": [2, 768],
    "rw_lnx_g": [2, 768], "rw_lnx_b": [2, 768], "mla_q_norm": [2, 512], "mla_kv_norm": [2, 256],
    "mla_w_uq": [2, 512, 768], "mla_w_ukv": [2, 256, 1024], "df_lq1": [2, 64], "df_lk1": [2, 64],
    "df_lq2": [2, 64], "df_lk2": [2, 64], "df_subln": [2, 128], "w_up_rw": [2, 768, 2048],
    "w_up_mla": [2, 512, 2048], "w_up_df": [2, 768, 2048], "w_o": [2, 2048, 2048],
    "ln1_g": [2, 2048], "ln1_b": [2, 2048], "ln2_g": [2, 2048], "ln2_b": [2, 2048],
    "router_w": [2048, 16], "router_bias": [1, 16],
    "ex_w_gate": [2, 16, 2048, 1024], "ex_w_up": [2, 16, 2048, 1024], "ex_w_down": [2, 16, 1024, 2048],
}


class _Lazy:
    def __init__(self, nc, shapes):
        self.nc, self.shapes, self.d = nc, shapes, {}

    def __getitem__(self, k):
        if k not in self.d:
            self.d[k] = self.nc.dram_tensor(k, self.shapes[k], F32, kind="ExternalInput").ap()
        return self.d[k]

    def used(self):
        return list(self.d.keys())


class Builder:
    def __init__(self, seqs, dbg=(), n_layers=DEPTH, phases=None):
        self.seqs = list(seqs)
        self.Tmax = max(seqs)
        self.dbg = set(dbg)
        self.n_layers = n_layers
        self.phases = phases
        nc = self.nc = bass.Bass("TRN2", target_bir_lowering=False)
        self.P = Prog(nc)
        self.W = _Lazy(nc, WEIGHT_SHAPES)
        self.C = _Lazy(nc, CONST_SHAPES(self.Tmax))
        self.xin = [nc.dram_tensor("xs%d" % i, [T, D], F32, kind="ExternalInput").ap() for i, T in enumerate(seqs)]
        self.yout = [nc.dram_tensor("ys%d" % i, [T, D], F32, kind="ExternalOutput").ap() for i, T in enumerate(seqs)]
        Tm = self.Tmax
        self.S = {}
        for name, shape, dt in [
            ("xmid", [Tm, D], F32),
            ("zrw", [Tm, RW_IN], F32),
            ("cqT", [512, Tm], F32), ("ckvT", [256, Tm], F32), ("krT", [32, Tm], F32),
            ("dfqkT", [24 * 64, Tm], BF16),
            ("vdf", [Tm, 768], BF16),
            ("sgT", [GATE_IN, Tm], BF16),
            ("qmT", [8 * 96, Tm], BF16), ("kmT", [8 * 96, Tm], BF16), ("vm", [Tm, 512], BF16),
            ("oT", [D, Tm], BF16),
            ("yrw", [Tm, C_RW], F32),
            ("bon", [Tm, C_RW], F32),
        ]:
            kind = "ExternalOutput" if name in self.dbg else "Internal"
            self.S[name] = nc.dram_tensor("scr_" + name, shape, dt, kind=kind).ap()
        self._bank = 0
        self.WB = {}

    def precast_weights(self):
        nc, P, W = self.nc, self.P, self.W
        L = self.n_layers
        def mk(name, shape):
            self.WB[name] = nc.dram_tensor("wb_" + name, shape, BF16, kind="Internal").ap()
        mk("w_in", [2, 2048, IN_W]); mk("w_o", [2, 2048, 2048])
        mk("w_up_rw", [2, 768, 2048]); mk("w_up_mla", [2, 512, 2048]); mk("w_up_df", [2, 768, 2048])
        mk("ex_w_gate", [2, 16, 2048, 1024]); mk("ex_w_up", [2, 16, 2048, 1024]); mk("ex_w_down", [2, 16, 1024, 2048])
        n = 0
        def cp(dst, src):
            nonlocal n
            P.add("pool", lambda e: e.dma_start(out=dst, in_=src), dma=True, semkey="pc%d" % (n % 4))
            n += 1
        for l in range(L):
            for r0 in range(0, 2048, 256):
                cp(self.WB["w_in"][l, r0:r0 + 256, :], W["w_in"][l, r0:r0 + 256, :])
            for r0 in range(0, 2048, 512):
                cp(self.WB["w_o"][l, r0:r0 + 512, :], W["w_o"][l, r0:r0 + 512, :])
            for nm in ("w_up_rw", "w_up_mla", "w_up_df"):
                cp(self.WB[nm][l], W[nm][l])
            for ex in range(NE):
                for nm in ("ex_w_gate", "ex_w_up", "ex_w_down"):
                    rows = WEIGHT_SHAPES[nm][2]
                    for r0 in range(0, rows, 512):
                        cp(self.WB[nm][l, ex, r0:r0 + 512, :], W[nm][l, ex, r0:r0 + 512, :])
        P.flush()

    def uid(self):
        self._uid = getattr(self, "_uid", 0) + 1
        return self._uid

    def nb(self):
        b = self._bank
        self._bank = (self._bank + 1) % 8
        return b

    def build(self):
        nc, P = self.nc, self.P
        with contextlib.ExitStack() as gst:
            self.ps = [gst.enter_context(nc.psum_tensor("ps%d" % b, [128, 512], F32)) for b in range(8)]
            if self.phases is None or "A" in self.phases or "E" in self.phases:
                self.precast_weights()
            for si, T in enumerate(self.seqs):
                for l in range(self.n_layers):
                    xin = self.xin[si] if l == 0 else self.S["xmid"]
                    xout = self.yout[si] if l == self.n_layers - 1 else self.S["xmid"]
                    ph = self.phases
                    if ph is None or "A" in ph:
                        self.phase_inproj(l, xin, T)
                    if ph is None or "B" in ph:
                        self.phase_mla(l, T)
                    if ph is None or "C" in ph:
                        self.phase_diff(l, T)
                    if ph is None or "D" in ph:
                        self.phase_rwkv(l, T)
                    if ph is None or "E" in ph:
                        self.phase_ffn(l, xin, xout, T)
        P.close()
        return nc

    def phase_inproj(self, l, xin, T):
        nc, P, S, C, ps = self.nc, self.P, self.S, self.C, self.ps
        w_in = self.WB["w_in"][l].rearrange("(kc p) n -> p kc n", p=128)
        with contextlib.ExitStack() as st:
            sb = lambda name, shape, dt: st.enter_context(nc.sbuf_tensor("A%d_" % self.uid() + name, shape, dt))
            xt = sb("xt", [128, 4, D], BF16)
            xT = sb("xT", [128, 16, TT], BF16)
            wt = [sb("wt%d" % i, [128, 16, 512], BF16) for i in range(3)]
            stgb = [sb("stgb%d" % i, [128, 4, 512], BF16) for i in range(2)]
            stgf = [sb("stgf%d" % i, [128, 4, 512], F32) for i in range(2)]
            zb = [sb("zb%d" % i, [128, 512], BF16) for i in range(2)]
            t1 = [sb("t1_%d" % i, [128, 512], F32) for i in range(2)]
            t2 = [sb("t2_%d" % i, [128, 512], F32) for i in range(2)]
            idn = sb("idn", [128, 128], BF16)
            perm = sb("perm", [128, 128], BF16)
            cosb = sb("cos", [128, TT], F32)
            sinb = sb("sin", [128, TT], F32)
            P.add("pool", lambda e: e.dma_start(out=idn[:], in_=C["ident"]), writes=["idn"], dma=True, semkey="c0")
            P.add("pool", lambda e: e.dma_start(out=perm[:], in_=C["permdf"]), writes=["perm"], dma=True, semkey="c1")
            cnt = {"w": 0, "sb": 0, "sf": 0, "z": 0, "ev": 0}

            def evac_copy(out_ap, in_ap, reads, writes):
                cnt["ev"] += 1
                if cnt["ev"] % 2 == 0:
                    P.add("act", lambda e: e.activation(out_ap, in_ap, AF.Copy), reads=reads, writes=writes)
                else:
                    P.add("dve", lambda e: e.tensor_copy(out_ap, in_ap), reads=reads, writes=writes)

            blocks = []
            for c0 in range(0, RW_IN, 512):
                blocks.append((c0, min(512, RW_IN - c0), "rw"))
            blocks.append((RW_IN, 512, "cq"))
            blocks.append((RW_IN + 512, 288, "ckv"))
            base = RW_IN + MLA_IN
            for which in range(2):
                blocks.append((base + which * 768, 512, "dfqk"))
                blocks.append((base + which * 768 + 512, 256, "dfqk"))
            blocks.append((base + 1536, 512, "dfv"))
            blocks.append((base + 1536 + 512, 256, "dfv"))
            gbase = base + DF_IN
            for c0 in range(0, GATE_IN, 512):
                blocks.append((gbase + c0, 512, "gate"))

            for ti in range(T // TT):
                t0 = ti * TT
                P.add("pool", lambda e, t0=t0: e.dma_start(
                    out=xt[:], in_=xin[t0:t0 + TT, :].rearrange("(s p) d -> p s d", p=128)),
                    writes=["xt"], dma=True, semkey="xt")
                P.add("sp", lambda e, t0=t0: e.dma_start(out=cosb[:], in_=C["cosdf"][:, t0:t0 + TT]),
                      writes=["cos"], dma=True, semkey="cos")
                P.add("sp", lambda e, t0=t0: e.dma_start(out=sinb[:], in_=C["sindf"][:, t0:t0 + TT]),
                      writes=["sin"], dma=True, semkey="sin")
                for g in range(8):
                    b = self.nb()
                    pv = ps[b][:].bitcast(BF16)
                    for kk in range(2):
                        kc = 2 * g + kk
                        for s in range(4):
                            P.add("pe", lambda e, pv=pv, kk=kk, s=s, kc=kc: e.transpose(
                                pv[:, kk * 512 + s * 128: kk * 512 + (s + 1) * 128],
                                xt[:, s, kc * 128:(kc + 1) * 128], idn[:]),
                                reads=["xt", "idn"], writes=[("ps", b)])
                    evac_copy(xT[:, 2 * g:2 * g + 2, :], pv.rearrange("p (a t) -> p a t", a=2),
                              [("ps", b)], [("xT", g)])
                xT_all = [("xT", g) for g in range(8)]

                for (c0, w, kind) in blocks:
                    if getattr(self, "only_kinds", None) is not None and kind not in self.only_kinds:
                        continue
                    ws = cnt["w"] % 3
                    cnt["w"] += 1
                    P.add("pool", lambda e, ws=ws, c0=c0, w=w: e.dma_start(
                        out=wt[ws][:, :, :w], in_=w_in[:, :, c0:c0 + w]),
                        writes=[("wt", ws)], dma=True, semkey="wt%d" % ws)
                    if kind in ("rw", "dfv"):
                        if kind == "rw":
                            ss = cnt["sf"] % 2
                            cnt["sf"] += 1
                            stg, skey = stgf[ss], ("stgf", ss)
                        else:
                            ss = cnt["sb"] % 2
                            cnt["sb"] += 1
                            stg, skey = stgb[ss], ("stgb", ss)
                        for s in range(4):
                            b = self.nb()
                            for kc in range(16):
                                P.add("pe", lambda e, b=b, kc=kc, s=s, ws=ws, w=w: e.matmul(
                                    ps[b][:, :w], xT[:, kc, s * 128:(s + 1) * 128], wt[ws][:, kc, :w],
                                    start=(kc == 0), stop=(kc == 15)),
                                    reads=xT_all + [("wt", ws)], writes=[("ps", b)])
                            evac_copy(stg[:, s, :w], ps[b][:, :w], [("ps", b)], [skey])
                        if kind == "rw":
                            dst = S["zrw"][t0:t0 + TT, c0:c0 + w].rearrange("(s p) n -> p s n", p=128)
                        else:
                            cc = c0 - (base + 1536)
                            dst = S["vdf"][t0:t0 + TT, cc:cc + w].rearrange("(s p) n -> p s n", p=128)
                        P.add("sp", lambda e, dst=dst, stg=stg, w=w: e.dma_start(out=dst, in_=stg[:, :, :w]),
                              reads=[skey], dma=True, semkey="st_%s%d" % (skey[0], ss))
                    else:
                        if kind in ("cq", "ckv"):
                            ss = cnt["sf"] % 2
                            cnt["sf"] += 1
                            stg, skey = stgf[ss], ("stgf", ss)
                        else:
                            ss = cnt["sb"] % 2
                            cnt["sb"] += 1
                            stg, skey = stgb[ss], ("stgb", ss)
                        chunks = [(j, min(128, w - j)) for j in range(0, w, 128)]
                        for ci, (j, cw) in enumerate(chunks):
                            b = self.nb()
                            for kc in range(16):
                                P.add("pe", lambda e, b=b, kc=kc, j=j, cw=cw, ws=ws: e.matmul(
                                    ps[b][:cw, :], wt[ws][:, kc, j:j + cw], xT[:, kc, :],
                                    start=(kc == 0), stop=(kc == 15)),
                                    reads=xT_all + [("wt", ws)], writes=[("ps", b)])
                            if kind == "gate":
                                P.add("act", lambda e, b=b, ci=ci, stg=stg: e.activation(
                                    stg[:, ci, :], ps[b][:], AF.Sigmoid), reads=[("ps", b)], writes=[skey])
                            elif kind == "dfqk":
                                zs = cnt["z"] % 2
                                cnt["z"] += 1
                                b2 = self.nb()
                                P.add("dve", lambda e, b=b, zs=zs: e.tensor_copy(zb[zs][:], ps[b][:]),
                                      reads=[("ps", b)], writes=[("zb", zs)])
                                import os
                                _m = os.environ.get("DFQK_MODE", "")
                                if _m == "1":
                                    b2 = b
                                else:
                                    P.add("pe", lambda e, b2=b2, zs=zs: e.matmul(ps[b2][:], perm[:], zb[zs][:],
                                                                               start=True, stop=True),
                                          reads=[("zb", zs), "perm"], writes=[("ps", b2)])
                                P.add("dve", lambda e, b=b, zs=zs: e.tensor_tensor(t1[zs][:], ps[b][:], cosb[:], ALU.mult),
                                      reads=[("ps", b), "cos"], writes=[("t1", zs)])
                                P.add("dve", lambda e, b2=b2, zs=zs: e.tensor_tensor(t2[zs][:], ps[b2][:], sinb[:], ALU.mult),
                                      reads=[("ps", b2), "sin"], writes=[("t2", zs)])
                                P.add(os.environ.get("ADD_ENG", "pool"), lambda e, zs=zs, ci=ci, stg=stg: e.tensor_tensor(
                                    stg[:, ci, :], t1[zs][:], t2[zs][:], ALU.add),
                                    reads=[("t1", zs), ("t2", zs)], writes=[skey])
                            else:
                                evac_copy(stg[:cw, ci, :], ps[b][:cw, :], [("ps", b)], [skey])
                        stkey = "st_%s%d" % (skey[0], ss)
                        if kind == "gate":
                            r0 = c0 - gbase
                            dst = S["sgT"][r0:r0 + 512, t0:t0 + TT].rearrange("(c p) t -> p c t", p=128)
                            P.add("sp", lambda e, dst=dst, stg=stg: e.dma_start(out=dst, in_=stg[:]),
                                  reads=[skey], dma=True, semkey=stkey)
                        elif kind == "dfqk":
                            r0 = c0 - base
                            nch = len(chunks)
                            dst = S["dfqkT"][r0:r0 + w, t0:t0 + TT].rearrange("(c p) t -> p c t", p=128)
                            P.add("sp", lambda e, dst=dst, stg=stg, nch=nch: e.dma_start(out=dst, in_=stg[:, :nch, :]),
                                  reads=[skey], dma=True, semkey=stkey)
                        elif kind == "cq":
                            dst = S["cqT"][:, t0:t0 + TT].rearrange("(c p) t -> p c t", p=128)
                            P.add("sp", lambda e, dst=dst, stg=stg: e.dma_start(out=dst, in_=stg[:]),
                                  reads=[skey], dma=True, semkey=stkey)
                        else:
                            dst = S["ckvT"][:, t0:t0 + TT].rearrange("(c p) t -> p c t", p=128)
                            P.add("sp", lambda e, dst=dst, stg=stg: e.dma_start(out=dst, in_=stg[:, 0:2, :]),
                                  reads=[skey], dma=True, semkey=stkey)
                            dst2 = S["krT"][:, t0:t0 + TT]
                            P.add("sp", lambda e, dst2=dst2, stg=stg: e.dma_start(out=dst2, in_=stg[:32, 2, :]),
                                  reads=[skey], dma=True, semkey=stkey)
            P.flush()

    def phase_mla(self, l, T):
        nc, P, S, C, W, ps = self.nc, self.P, self.S, self.C, self.W, self.ps
        NT = T // TT
        scale = 96.0 ** -0.5
        with contextlib.ExitStack() as st:
            sb = lambda name, shape, dt: st.enter_context(nc.sbuf_tensor("B%d_" % self.uid() + name, shape, dt))
            onesb = sb("ones", [128, 128], BF16)
            pq = sb("pq", [128, 6, 128], BF16)
            pk = sb("pk", [128, 128], BF16)
            wkv = sb("wkv", [128, 2, 1024], BF16)
            KR = sb("KR", [128, TT], F32)
            cosq = sb("cosq", [128, 6, TT], F32)
            sinq = sb("sinq", [128, 6, TT], F32)
            cosk = sb("cosk", [128, TT], F32)
            sink = sb("sink", [128, TT], F32)
            wuq = sb("wuq", [128, 4, 768], BF16)
            wv = sb("wv", [128, 2, 8, 64], BF16)
            gq = sb("gq", [128, 4], F32)
            gkv = sb("gkv", [128, 2], F32)
            cq = sb("cq", [128, 4, TT], F32)
            ckv = sb("ckv", [128, 2, TT], F32)
            sq = sb("sq", [128, 4, TT], BF16)
            rt = sb("rt", [128, TT], F32)
            rstd = sb("rstd", [128, TT], F32)
            cqn = sb("cqn", [128, 4, TT], BF16)
            ckvn = sb("ckvn", [128, 2, TT], BF16)
            zb = [sb("zb%d" % i, [128, TT], BF16) for i in range(2)]
            for i in range(2):
                P.add("pool", lambda e, i=i: e.memset(zb[i][:], 0.0), writes=[("zb", i)])
            t1 = [sb("t1_%d" % i, [128, TT], F32) for i in range(2)]
            t2 = [sb("t2_%d" % i, [128, TT], F32) for i in range(2)]
            P.add("pool", lambda e: e.memset(KR[:], 0.0), writes=["kr"])
            qstg = [sb("qstg%d" % i, [128, 3, TT], BF16) for i in range(2)]
            kstg = [sb("kstg%d" % i, [128, 4, TT], BF16) for i in range(2)]
            krr = sb("krr", [128, TT], BF16)
            vstg = sb("vstg", [128, 4, 512], BF16)
            ld = lambda eng, out, in_, key: P.add(eng, lambda e: e.dma_start(out=out, in_=in_), writes=[key], dma=True, semkey="L_" + key)
            ld("pool", onesb[:], C["ones"], "ones")
            ld("pool", pq[:], C["permq"].rearrange("(j k) m -> k j m", k=128), "pq")
            ld("pool", pk[:], C["permk"], "pk")
            ld("pool", wkv[:], W["mla_w_ukv"][l].rearrange("(kc p) n -> p kc n", p=128), "wkv")
            ld("pool", wuq[:], W["mla_w_uq"][l].rearrange("(kc p) n -> p kc n", p=128), "wuq")
            wukv = W["mla_w_ukv"][l].rearrange("(kc p) (h c) -> kc p h c", p=128, c=128)
            for kc in range(2):
                P.add("pool", lambda e, kc=kc: e.dma_start(out=wv[:, kc, :, :], in_=wukv[kc, :, :, 64:128]),
                      writes=["wv"], dma=True, semkey="L_wv")
            for kc in range(4):
                P.add("sp", lambda e, kc=kc: e.dma_start(
                    out=gq[:, kc:kc + 1], in_=W["mla_q_norm"][l, kc * 128:(kc + 1) * 128].rearrange("(p o) -> p o", o=1)),
                    writes=["gq"], dma=True, semkey="L_gq")
            for kc in range(2):
                P.add("sp", lambda e, kc=kc: e.dma_start(
                    out=gkv[:, kc:kc + 1], in_=W["mla_kv_norm"][l, kc * 128:(kc + 1) * 128].rearrange("(p o) -> p o", o=1)),
                    writes=["gkv"], dma=True, semkey="L_gkv")
            cnt = {"z": 0, "q": 0, "k": 0}
            for ti in range(NT):
                t0 = ti * TT
                ld("sp", cq[:], S["cqT"][:, t0:t0 + TT].rearrange("(c p) t -> p c t", p=128), "cq")
                ld("sp", ckv[:], S["ckvT"][:, t0:t0 + TT].rearrange("(c p) t -> p c t", p=128), "ckv")
                ld("sp", KR[:32, :], S["krT"][:, t0:t0 + TT], "kr")
                ld("sp", cosq[:], C["cosq"][:, t0:t0 + TT].rearrange("(c p) t -> p c t", p=128), "cosq")
                ld("sp", sinq[:], C["sinq"][:, t0:t0 + TT].rearrange("(c p) t -> p c t", p=128), "sinq")
                ld("sp", cosk[:], C["cosk"][:, t0:t0 + TT], "cosk")
                ld("sp", sink[:], C["sink"][:, t0:t0 + TT], "sink")

                def rmsnorm(src, key, nk, g, dst, dkey, width):
                    b = self.nb()
                    P.add("act", lambda e: e.activation(sq[:, :nk, :], src[:], AF.Square), reads=[key], writes=["sq"])
                    for kc in range(nk):
                        P.add("pe", lambda e, kc=kc: e.matmul(ps[b][:], onesb[:], sq[:, kc, :], start=(kc == 0), stop=(kc == nk - 1)),
                              reads=["sq", "ones"], writes=[("ps", b)])
                    P.add("act", lambda e: e.activation(rt[:], ps[b][:], AF.Sqrt, bias=RMS_EPS, scale=1.0 / width),
                          reads=[("ps", b)], writes=["rt"])
                    P.add("dve", lambda e: e.reciprocal(rstd[:], rt[:]), reads=["rt"], writes=["rstd"])
                    for kc in range(nk):
                        P.add("dve" if kc % 2 == 0 else "pool", lambda e, kc=kc: (
                            e.scalar_tensor_tensor(dst[:, kc, :], src[:, kc, :], g[:, kc:kc + 1], rstd[:], ALU.mult, ALU.mult)
                            if False else e.scalar_tensor_tensor(dst[:, kc, :], src[:, kc, :], g[:, kc:kc + 1], rstd[:], ALU.mult, ALU.mult)),
                            reads=[key, "rstd", "gq", "gkv"], writes=[dkey]) if False else None
                        P.add("dve", lambda e, kc=kc: e.scalar_tensor_tensor(
                            dst[:, kc, :], src[:, kc, :], g[:, kc:kc + 1], rstd[:], ALU.mult, ALU.mult),
                            reads=[key, "rstd", "gq", "gkv"], writes=[dkey])

                rmsnorm(cq, "cq", 4, gq, cqn, "cqn", 512.0)
                rmsnorm(ckv, "ckv", 2, gkv, ckvn, "ckvn", 256.0)
                for j in range(6):
                    b = self.nb()
                    for kc in range(4):
                        P.add("pe", lambda e, b=b, kc=kc, j=j: e.matmul(
                            ps[b][:], wuq[:, kc, j * 128:(j + 1) * 128], cqn[:, kc, :], start=(kc == 0), stop=(kc == 3)),
                            reads=["wuq", "cqn"], writes=[("ps", b)])
                    zs = cnt["z"] % 2
                    cnt["z"] += 1
                    b2 = self.nb()
                    P.add("act", lambda e, b=b, zs=zs: e.activation(zb[zs][:], ps[b][:], AF.Copy),
                          reads=[("ps", b)], writes=[("zb", zs)])
                    P.add("pe", lambda e, b2=b2, zs=zs, j=j: e.matmul(ps[b2][:], pq[:, j, :], zb[zs][:], start=True, stop=True),
                          reads=[("zb", zs), "pq"], writes=[("ps", b2)])
                    P.add("dve", lambda e, b=b, zs=zs, j=j: e.tensor_tensor(t1[zs][:], ps[b][:], cosq[:, j, :], ALU.mult),
                          reads=[("ps", b), "cosq"], writes=[("t1", zs)])
                    P.add("dve", lambda e, b2=b2, zs=zs, j=j: e.tensor_tensor(t2[zs][:], ps[b2][:], sinq[:, j, :], ALU.mult),
                          reads=[("ps", b2), "sinq"], writes=[("t2", zs)])
                    qs = (cnt["q"] // 3) % 2
                    cnt["q"] += 1
                    P.add("pool", lambda e, zs=zs, qs=qs, j=j: e.tensor_tensor(qstg[qs][:, j % 3, :], t1[zs][:], t2[zs][:], ALU.add),
                          reads=[("t1", zs), ("t2", zs)], writes=[("qstg", qs)])
                    if j % 3 == 2:
                        j0 = j - 2
                        dst = S["qmT"][j0 * 128:(j0 + 3) * 128, t0:t0 + TT].rearrange("(c p) t -> p c t", p=128)
                        P.add("sp", lambda e, dst=dst, qs=qs: e.dma_start(out=dst, in_=qstg[qs][:]),
                              reads=[("qstg", qs)], dma=True, semkey="st_q%d" % qs)
                for h in range(8):
                    b = self.nb()
                    for kc in range(2):
                        P.add("pe", lambda e, b=b, kc=kc, h=h: e.matmul(
                            ps[b][:], wkv[:, kc, h * 128:(h + 1) * 128], ckvn[:, kc, :], start=(kc == 0), stop=(kc == 1)),
                            reads=["wkv", "ckvn"], writes=[("ps", b)])
                    ks = (cnt["k"] // 4) % 2
                    cnt["k"] += 1
                    if h % 2 == 0:
                        P.add("act", lambda e, b=b, ks=ks, h=h: e.activation(kstg[ks][:, h % 4, :], ps[b][:], AF.Copy),
                              reads=[("ps", b)], writes=[("kstg", ks)])
                    else:
                        P.add("dve", lambda e, b=b, ks=ks, h=h: e.tensor_copy(kstg[ks][:, h % 4, :], ps[b][:]),
                              reads=[("ps", b)], writes=[("kstg", ks)])
                    dst = S["kmT"][h * 96:h * 96 + 64, t0:t0 + TT]
                    P.add("sp", lambda e, dst=dst, ks=ks, h=h: e.dma_start(out=dst, in_=kstg[ks][:64, h % 4, :]),
                          reads=[("kstg", ks)], dma=True, semkey="st_k%d" % ks)
                zs = cnt["z"] % 2
                cnt["z"] += 1
                b2 = self.nb()
                P.add("act", lambda e, zs=zs: e.activation(zb[zs][:], KR[:], AF.Copy), reads=["kr"], writes=[("zb", zs)])
                P.add("pe", lambda e, b2=b2, zs=zs: e.matmul(ps[b2][:], pk[:], zb[zs][:], start=True, stop=True),
                      reads=[("zb", zs), "pk"], writes=[("ps", b2)])
                P.add("dve", lambda e, zs=zs: e.tensor_tensor(t1[zs][:], KR[:], cosk[:], ALU.mult),
                      reads=["kr", "cosk"], writes=[("t1", zs)])
                P.add("dve", lambda e, b2=b2, zs=zs: e.tensor_tensor(t2[zs][:], ps[b2][:], sink[:], ALU.mult),
                      reads=[("ps", b2), "sink"], writes=[("t2", zs)])
                P.add("pool", lambda e, zs=zs: e.tensor_tensor(krr[:], t1[zs][:], t2[zs][:], ALU.add),
                      reads=[("t1", zs), ("t2", zs)], writes=["krr"])
                for h in range(8):
                    dst = S["kmT"][h * 96 + 64:h * 96 + 96, t0:t0 + TT]
                    P.add("sp", lambda e, dst=dst: e.dma_start(out=dst, in_=krr[:32, :]), reads=["krr"], dma=True, semkey="st_kr")
                for s_ in range(4):
                    b = self.nb()
                    for kc in range(2):
                        P.add("pe", lambda e, b=b, kc=kc, s_=s_: e.matmul(
                            ps[b][:], ckvn[:, kc, s_ * 128:(s_ + 1) * 128], wv[:, kc, :, :].rearrange("p h c -> p (h c)"),
                            start=(kc == 0), stop=(kc == 1)), reads=["wv", "ckvn"], writes=[("ps", b)])
                    if s_ % 2 == 0:
                        P.add("act", lambda e, b=b, s_=s_: e.activation(vstg[:, s_, :], ps[b][:], AF.Copy),
                              reads=[("ps", b)], writes=["vstg"])
                    else:
                        P.add("dve", lambda e, b=b, s_=s_: e.tensor_copy(vstg[:, s_, :], ps[b][:]),
                              reads=[("ps", b)], writes=["vstg"])
                dst = S["vm"][t0:t0 + TT, :].rearrange("(s p) n -> p s n", p=128)
                P.add("sp", lambda e, dst=dst: e.dma_start(out=dst, in_=vstg[:]), reads=["vstg"], dma=True, semkey="st_v")
            P.flush()
        import os
        if "B2" in os.environ.get("SKIP", ""):
            return
        self.attention(T, nheads=8, parts=96, dv=64, scale=scale,
                       kT_rows=lambda h, c: (S["kmT"], h * 96), qT_rows=lambda h, c: (S["qmT"], h * 96),
                       v_src=lambda h: (S["vm"], h * 64), ncomp=1, out_row0=768, l=l)

    def attention(self, T, nheads, parts, dv, scale, kT_rows, qT_rows, v_src, ncomp, out_row0, l):
        nc, P, S, C, W, ps = self.nc, self.P, self.S, self.C, self.W, self.ps
        NT, NK = T // TT, T // 128
        lambda_init = 0.8 - 0.6 * np.exp(-0.3 * l)
        with contextlib.ExitStack() as st:
            sb = lambda name, shape, dt: st.enter_context(nc.sbuf_tensor("At%d_" % self.uid() + name, shape, dt))
            onesb = sb("ones", [128, 128], BF16)
            nrows = parts
            KT = [sb("KT%d" % i, [128, T], BF16) for i in range(2)]
            V = [sb("V%d" % i, [128, NK, dv], BF16) for i in range(2)]
            QT = [[sb("QT%d_%d" % (i, c), [128, TT], BF16) for c in range(ncomp)] for i in range(2)]
            for i in range(2):
                P.add("pool", lambda e, i=i: e.memset(KT[i][:], 0.0), writes=[("KT", i)])
                for c in range(ncomp):
                    P.add("pool", lambda e, i=i, c=c: e.memset(QT[i][c][:], 0.0), writes=[("QT", i, c)])
            pT = [sb("pT%d" % i, [128, TT], BF16) for i in range(3)]
            rD = [sb("rD%d" % i, [dv, TT], F32) for i in range(2)]
            ostg = [sb("ostg%d" % i, [dv, TT], BF16) for i in range(2)]
            P.add("pool", lambda e: e.dma_start(out=onesb[:], in_=C["ones"]), writes=["ones"], dma=True, semkey="L_ones")
            if ncomp == 2:
                lq = [sb("lq%d" % i, [128, 64], F32) for i in range(4)]
                lp = [sb("lp%d" % i, [128, 64], F32) for i in range(2)]
                lsum = sb("lsum", [128, 2], F32)
                lexp = sb("lexp", [128, 2], F32)
                neglam = sb("neglam", [128, 1], F32)
                gs0 = sb("gs0", [128, 1], F32)
                gs = sb("gs", [128, 1], F32)
                o1 = sb("o1", [128, TT], F32)
                o2 = sb("o2", [128, TT], F32)
                oo = sb("oo", [128, TT], F32)
                osq = sb("osq", [128, TT], BF16)
                ort = sb("ort", [128, TT], F32)
                orstd = sb("orstd", [128, TT], F32)
                for i, nm in enumerate(["df_lq1", "df_lk1", "df_lq2", "df_lk2"]):
                    P.add("sp", lambda e, i=i, nm=nm: e.dma_start(out=lq[i][:], in_=W[nm][l].partition_broadcast(128)),
                          writes=[("lq", i)], dma=True, semkey="L_lq%d" % i)
                P.add("sp", lambda e: e.dma_start(out=gs0[:], in_=W["df_subln"][l].rearrange("(p o) -> p o", o=1)),
                      writes=["gs0"], dma=True, semkey="L_gs0")
                for j in range(2):
                    P.add("dve", lambda e, j=j: e.tensor_tensor(lp[j][:], lq[2 * j][:], lq[2 * j + 1][:], ALU.mult),
                          reads=[("lq", 2 * j), ("lq", 2 * j + 1)], writes=[("lp", j)])
                    P.add("pool" if False else "dve", lambda e, j=j: e.tensor_reduce(lsum[:, j:j + 1], lp[j][:], AX.X, ALU.add),
                          reads=[("lp", j)], writes=["lsum"])
                P.add("act", lambda e: e.activation(lexp[:], lsum[:], AF.Exp), reads=["lsum"], writes=["lexp"])
                P.add("dve", lambda e: e.scalar_tensor_tensor(neglam[:], lexp[:, 1:2], -float(lambda_init), lexp[:, 0:1],
                                                              ALU.add, ALU.subtract), reads=["lexp"], writes=["neglam"])
                P.add("pool", lambda e: e.tensor_scalar(gs[:], gs0[:], float(1.0 - lambda_init), None, ALU.mult),
                      reads=["gs0"], writes=["gs"])
            cnt = {"p": 0, "o": 0}
            acc_banks = [3, 4, 5, 6] if ncomp == 2 else [3, 4, 5, 6]
            for h in range(nheads):
                hs = h % 2
                src, r0 = kT_rows(h, 0)
                nk_rows = nrows * ncomp
                P.add("sp", lambda e, hs=hs, src=src, r0=r0, nk_rows=nk_rows: e.dma_start(out=KT[hs][:nk_rows, :], in_=src[r0:r0 + nk_rows, 0:T]),
                      writes=[("KT", hs)], dma=True, semkey="L_KT%d" % hs)
                vsrc, c0 = v_src(h)
                P.add("sp", lambda e, hs=hs, vsrc=vsrc, c0=c0: e.dma_start(
                    out=V[hs][:], in_=vsrc[0:T, c0:c0 + dv].rearrange("(c p) d -> p c d", p=128)),
                    writes=[("V", hs)], dma=True, semkey="L_V%d" % hs)
                for qi in range(NT):
                    t0 = qi * TT
                    qs = (h * NT + qi) % 2
                    for c in range(ncomp):
                        src, r0 = qT_rows(h, c)
                        rr = r0 + c * nrows * (ncomp - 1)
                        pr = c * nrows * (ncomp - 1)
                        P.add("sp", lambda e, qs=qs, c=c, src=src, rr=rr, pr=pr, t0=t0: e.dma_start(
                            out=QT[qs][c][pr:pr + nrows, :], in_=src[rr:rr + nrows, t0:t0 + TT]),
                            writes=[("QT", qs, c)], dma=True, semkey="L_QT%d%d" % (qs, c))
                    accs = []
                    for c in range(ncomp):
                        if ncomp == 1:
                            bo, bd = acc_banks[2 * (cnt["o"] % 2)], acc_banks[2 * (cnt["o"] % 2) + 1]
                        else:
                            bo, bd = acc_banks[2 * c], acc_banks[2 * c + 1]
                        accs.append((bo, bd))
                        for kc in range(NK):
                            b = cnt["p"] % 3
                            pslot = cnt["p"] % 3
                            cnt["p"] += 1
                            P.add("pe", lambda e, b=b, hs=hs, c=c, kc=kc, qs=qs: e.matmul(
                                ps[b][:], KT[hs][:, kc * 128:(kc + 1) * 128], QT[qs][c][:], start=True, stop=True),
                                reads=[("KT", hs), ("QT", qs, c)], writes=[("ps", b)])
                            P.add("act", lambda e, b=b, pslot=pslot: e.activation(pT[pslot][:], ps[b][:], AF.Exp, scale=float(scale)),
                                  reads=[("ps", b)], writes=[("pT", pslot)])
                            P.add("pe", lambda e, bo=bo, hs=hs, kc=kc, pslot=pslot: e.matmul(
                                ps[bo][:dv, :], V[hs][:, kc, :], pT[pslot][:], start=(kc == 0), stop=(kc == NK - 1)),
                                reads=[("V", hs), ("pT", pslot)], writes=[("ps", bo)])
                            P.add("pe", lambda e, bd=bd, kc=kc, pslot=pslot: e.matmul(
                                ps[bd][:dv, :], onesb[:, :dv], pT[pslot][:], start=(kc == 0), stop=(kc == NK - 1)),
                                reads=["ones", ("pT", pslot)], writes=[("ps", bd)])
                    os_ = cnt["o"] % 2
                    cnt["o"] += 1
                    if ncomp == 1:
                        bo, bd = accs[0]
                        P.add("dve", lambda e, bd=bd, os_=os_: e.reciprocal(rD[os_][:], ps[bd][:dv, :]),
                              reads=[("ps", bd)], writes=[("rD", os_)])
                        P.add("dve", lambda e, bo=bo, os_=os_: e.tensor_tensor(ostg[os_][:], ps[bo][:dv, :], rD[os_][:], ALU.mult),
                              reads=[("ps", bo), ("rD", os_)], writes=[("ostg", os_)])
                    else:
                        (bo1, bd1), (bo2, bd2) = accs
                        P.add("dve", lambda e, bd1=bd1: e.reciprocal(rD[0][:], ps[bd1][:]), reads=[("ps", bd1)], writes=[("rD", 0)])
                        P.add("dve", lambda e, bd2=bd2: e.reciprocal(rD[1][:], ps[bd2][:]), reads=[("ps", bd2)], writes=[("rD", 1)])
                        P.add("dve", lambda e, bo1=bo1: e.tensor_tensor(o1[:], ps[bo1][:], rD[0][:], ALU.mult),
                              reads=[("ps", bo1), ("rD", 0)], writes=["o1"])
                        P.add("dve", lambda e, bo2=bo2: e.tensor_tensor(o2[:], ps[bo2][:], rD[1][:], ALU.mult),
                              reads=[("ps", bo2), ("rD", 1)], writes=["o2"])
                        P.add("dve", lambda e: e.scalar_tensor_tensor(oo[:], o2[:], neglam[:], o1[:], ALU.mult, ALU.add),
                              reads=["o1", "o2", "neglam"], writes=["oo"])
                        P.add("act", lambda e: e.activation(osq[:], oo[:], AF.Square), reads=["oo"], writes=["osq"])
                        bs = 7
                        P.add("pe", lambda e: e.matmul(ps[bs][:], onesb[:], osq[:], start=True, stop=True),
                              reads=["osq", "ones"], writes=[("ps", bs)])
                        P.add("act", lambda e: e.activation(ort[:], ps[bs][:], AF.Sqrt, bias=DF_EPS, scale=1.0 / 128.0),
                              reads=[("ps", bs)], writes=["ort"])
                        P.add("dve", lambda e: e.reciprocal(orstd[:], ort[:]), reads=["ort"], writes=["orstd"])
                        P.add("dve", lambda e, os_=os_: e.scalar_tensor_tensor(ostg[os_][:], oo[:], gs[:], orstd[:], ALU.mult, ALU.mult),
                              reads=["oo", "gs", "orstd"], writes=[("ostg", os_)])
                    dst = S["oT"][out_row0 + h * dv:out_row0 + (h + 1) * dv, t0:t0 + TT]
                    P.add("sp", lambda e, dst=dst, os_=os_: e.dma_start(out=dst, in_=ostg[os_][:]),
                          reads=[("ostg", os_)], dma=True, semkey="st_o%d" % os_)
            P.flush()

    def phase_diff(self, l, T):
        S = self.S
        self.attention(T, nheads=6, parts=64, dv=128, scale=64.0 ** -0.5,
                       kT_rows=lambda h, c: (S["dfqkT"], (6 + h) * 128), qT_rows=lambda h, c: (S["dfqkT"], h * 128),
                       v_src=lambda h: (S["vdf"], h * 128), ncomp=2, out_row0=1280, l=l)

    def phase_rwkv(self, l, T):
        nc, P, S, C, W, ps = self.nc, self.P, self.S, self.C, self.W, self.ps
        NCH = T // 128
        CL = -float(np.exp(-0.5))
        u = self.uid()
        with contextlib.ExitStack() as st:
            sb = lambda name, shape, dt: st.enter_context(nc.sbuf_tensor("D%d_" % u + name, shape, dt))
            f768 = lambda name: sb(name, [128, 768], F32)
            b768 = lambda name: sb(name, [128, 768], BF16)
            mup, mun = sb("mup", [128, RW_IN], F32), sb("mun", [128, RW_IN], F32)
            kkbc, kabc, omka, rkbc, lgbc, lbbc = [f768(n) for n in ("kkbc", "kabc", "omka", "rkbc", "lgbc", "lbbc")]
            _w0 = f768("w0bc"); w0bc = [_w0, _w0]
            _a0 = f768("a0bc"); a0bc = [_a0, _a0]
            _w2 = sb("w2b", [128, 768], BF16); w2b = [_w2, _w2]
            _a2 = sb("a2b", [128, 768], BF16); a2b = [_a2, _a2]
            g2b = sb("g2b", [128, 768], BF16)
            _mk = sb("MK2", [128, 2, 2, 128], F32); MK2 = [_mk, _mk]
            _mn = sb("MN4", [128, 4, 128], F32); MN4 = [_mn, _mn]
            _tr = sb("TRI", [128, 128], F32); TRI = [_tr, _tr]
            onesf = sb("onesf", [128, 128], F32)
            idb = sb("idb", [128, 128], BF16)
            zc, zp, zn = [sb(n, [128, RW_IN], F32) for n in ("zc", "zp", "zn")]
            lorab = sb("lorab", [128, 256], BF16)
            lT = sb("lT", [128, 384], BF16)
            Ssg, a_, g_, kk, akk, kd, tA, tB, tC, E1, E2, bon, y_ = [f768(n) for n in (
                "Ssg", "a_", "g_", "kk", "akk", "kd", "tA", "tB", "tC", "E1", "E2", "bon", "y_")]
            yf, bf_ = E1, E2
            s12 = sb("s12", [128, 64], F32)
            gC = sb("gC", [64, 24], F32)
            Rt, Kt, At, Bt, Kcb, Bcb, Vb, Wb, Ub, ob = [b768(n) for n in ("Rt", "Kt", "At", "Bt", "Kcb", "Bcb", "Vb", "Wb", "Ub", "ob")]
            ART = sb("ART", [128, 12, 2, 128], BF16)
            KBT = sb("KBT", [128, 12, 2, 128], BF16)
            AKRK = sb("AKRK", [128, 12, 2, 128], BF16)
            ABRB = sb("ABRB", [128, 12, 2, 128], BF16)
            Pn = [sb("Pn%d" % i, [128, 12, 128], BF16) for i in range(2)]
            PnT = [sb("PnT%d" % i, [128, 12, 128], BF16) for i in range(2)]
            TTb = [sb("TTb%d" % i, [128, 12, 128], BF16) for i in range(2)]
            Hf = sb("Hf", [64, 12, 64], F32)
            Hb = sb("Hb", [128, 12, 64], BF16)
            ost = sb("ost", [128, 6, 128], BF16)

            def ld(eng, out, in_, key):
                P.add(eng, lambda e: e.dma_start(out=out, in_=in_), writes=[key], dma=True, semkey="L_" + key)

            def TT_(eng, out, a, b, op, rd, wr):
                P.add(eng, lambda e: e.tensor_tensor(out, a, b, op), reads=rd, writes=wr)

            def ACT_(out, in_, func, rd, wr, **kw):
                P.add("act", lambda e: e.activation(out, in_, func, **kw), reads=rd, writes=wr)

            ld("sp", mup[:], W["shift_prev"][l].partition_broadcast(128), "mup")
            ld("sp", mun[:], W["shift_next"][l].partition_broadcast(128), "mun")
            ld("sp", kkbc[:], W["rw_k_k"][l].partition_broadcast(128), "kkbc")
            ld("sp", kabc[:], W["rw_k_a"][l].partition_broadcast(128), "kabc")
            ld("sp", rkbc[:], W["rw_r_k"][l].partition_broadcast(128), "rkbc")
            ld("sp", lgbc[:], W["rw_lnx_g"][l].partition_broadcast(128), "lgbc")
            ld("sp", lbbc[:], W["rw_lnx_b"][l].partition_broadcast(128), "lbbc")
            P.add("pool", lambda e: e.tensor_scalar(omka[:], kabc[:], -1.0, 1.0, ALU.mult, ALU.add), reads=["kabc"], writes=["omka"])
            def load_dir_consts(d):
                ld("sp", w0bc[d][:], W["rw_w0"][l, d].partition_broadcast(128), "w0bc%d" % d)
                ld("sp", a0bc[d][:], W["rw_a0"][l, d].partition_broadcast(128), "a0bc%d" % d)
                ld("pool", w2b[d][:64, :], W["rw_w2"][l, d], "w2b%d" % d)
                ld("pool", a2b[d][:64, :], W["rw_a2"][l, d], "a2b%d" % d)
                strict, incl, nmask = ("m_su", "m_iu", "m_sl") if d == 0 else ("m_sl", "m_il", "m_su")
                for hh in range(2):
                    ld("sp", MK2[d][:, hh, 0, :], C[strict], "MK2_%d" % d)
                    ld("sp", MK2[d][:, hh, 1, :], C[incl], "MK2_%d" % d)
                for hh in range(4):
                    ld("sp", MN4[d][:, hh, :], C[nmask], "MN4_%d" % d)
                ld("sp", TRI[d][:], C[incl], "TRI%d" % d)
            ld("pool", g2b[:], W["rw_g2"][l], "g2b")
            for (tl, nm) in ((_w2, "w2b0"), (_a2, "a2b0"), (lT, "lT"), (ART, "ART"), (KBT, "KBT")):
                P.add("pool", lambda e, tl=tl: e.memset(tl[:], 0.0), writes=[nm])
            P.flush()
            ld("sp", onesf[:], C["ones"], "onesf")
            ld("pool", idb[:], C["ident"], "idb")

            for d in range(2):
                load_dir_consts(d)
                P.add("pool", lambda e: e.memset(Hf[:], 0.0), writes=["Hf"])
                P.add("pool", lambda e: e.memset(Hb[:], 0.0), writes=["Hb"])
                order = list(range(NCH)) if d == 0 else list(range(NCH - 1, -1, -1))
                for ci in order:
                    t0 = ci * 128
                    ld("sp", zc[:], S["zrw"][t0:t0 + 128, :], "zc")
                    if t0 == 0:
                        P.add("pool", lambda e: e.memset(zp[:], 0.0), writes=["zp"])
                        ld("sp", zp[1:128, :], S["zrw"][0:127, :], "zp")
                    else:
                        ld("sp", zp[:], S["zrw"][t0 - 1:t0 + 127, :], "zp")
                    if t0 + 128 == T:
                        P.add("pool", lambda e: e.memset(zn[:], 0.0), writes=["zn"])
                        ld("sp", zn[0:127, :], S["zrw"][t0 + 1:t0 + 128, :], "zn")
                    else:
                        ld("sp", zn[:], S["zrw"][t0 + 1:t0 + 129, :], "zn")
                    TT_("pool", zp[:], zp[:], zc[:], ALU.subtract, ["zp", "zc"], ["zp"])
                    TT_("pool", zp[:], zp[:], mup[:], ALU.mult, ["zp", "mup"], ["zp"])
                    TT_("dve", zn[:], zn[:], zc[:], ALU.subtract, ["zn", "zc"], ["zn"])
                    TT_("dve", zn[:], zn[:], mun[:], ALU.mult, ["zn", "mun"], ["zn"])
                    TT_("dve", zc[:], zc[:], zp[:], ALU.add, ["zp", "zc"], ["zc"])
                    TT_("dve", zc[:], zc[:], zn[:], ALU.add, ["zn", "zc"], ["zc"])
                    r_, k_, v_ = zc[:, 0:768], zc[:, 768:1536], zc[:, 1536:2304]
                    wl = zc[:, 2304 + d * 64:2304 + (d + 1) * 64]
                    al = zc[:, 2432 + d * 64:2432 + (d + 1) * 64]
                    gl = zc[:, 2560:2688]
                    ACT_(lorab[:, 0:64], wl, AF.Tanh, ["zc"], ["lorab"])
                    ACT_(lorab[:, 64:128], al, AF.Copy, ["zc"], ["lorab"])
                    ACT_(lorab[:, 128:256], gl, AF.Sigmoid, ["zc"], ["lorab"])
                    bt = self.nb()
                    pv = ps[bt][:].bitcast(BF16)
                    P.add("pe", lambda e, pv=pv: e.transpose(pv[:64, 0:128], lorab[:, 0:64], idb[:]), reads=["lorab", "idb"], writes=[("ps", bt)])
                    P.add("pe", lambda e, pv=pv: e.transpose(pv[:64, 128:256], lorab[:, 64:128], idb[:]), reads=["lorab", "idb"], writes=[("ps", bt)])
                    P.add("pe", lambda e, pv=pv: e.transpose(pv[:, 256:384], lorab[:, 128:256], idb[:]), reads=["lorab", "idb"], writes=[("ps", bt)])
                    ACT_(lT[:64, 0:256], pv[:64, 0:256], AF.Copy, [("ps", bt)], ["lT"])
                    P.add("dve", lambda e, pv=pv: e.tensor_copy(lT[:, 256:384], pv[:, 256:384]), reads=[("ps", bt)], writes=["lT"])
                    halves = ((0, 512), (512, 256))
                    bw = [self.nb(), self.nb()]
                    ba = [self.nb(), self.nb()]
                    for hi_, (c0, cw) in enumerate(halves):
                        P.add("pe", lambda e, hi_=hi_, c0=c0, cw=cw, bw=bw, d=d: e.matmul(ps[bw[hi_]][:, :cw], lT[:, 0:128], w2b[d][:, c0:c0 + cw], start=True, stop=True),
                              reads=["lT", "w2b%d" % d], writes=[("ps", bw[hi_])])
                        P.add("pe", lambda e, hi_=hi_, c0=c0, cw=cw, ba=ba, d=d: e.matmul(ps[ba[hi_]][:, :cw], lT[:, 128:256], a2b[d][:, c0:c0 + cw], start=True, stop=True),
                              reads=["lT", "a2b%d" % d], writes=[("ps", ba[hi_])])
                    for hi_, (c0, cw) in enumerate(halves):
                        TT_("dve", Ssg[:, c0:c0 + cw], ps[bw[hi_]][:, :cw], w0bc[d][:, c0:c0 + cw], ALU.add, [("ps", bw[hi_]), "w0bc%d" % d], ["Ssg"])
                        TT_("dve", a_[:, c0:c0 + cw], ps[ba[hi_]][:, :cw], a0bc[d][:, c0:c0 + cw], ALU.add, [("ps", ba[hi_]), "a0bc%d" % d], ["a_"])
                    ACT_(Ssg[:], Ssg[:], AF.Sigmoid, ["Ssg"], ["Ssg"])
                    ACT_(a_[:], a_[:], AF.Sigmoid, ["a_"], ["a_"])
                    if d == 1:
                        bg = [self.nb(), self.nb()]
                        for hi_, (c0, cw) in enumerate(halves):
                            P.add("pe", lambda e, hi_=hi_, c0=c0, cw=cw, bg=bg: e.matmul(ps[bg[hi_]][:, :cw], lT[:, 256:384], g2b[:, c0:c0 + cw], start=True, stop=True),
                                  reads=["lT", "g2b"], writes=[("ps", bg[hi_])])
                            ACT_(g_[:, c0:c0 + cw], ps[bg[hi_]][:, :cw], AF.Copy, [("ps", bg[hi_])], ["g_"])
                    h3 = lambda ap: ap.rearrange("p (h n) -> p h n", n=64)
                    bc12 = lambda ap: ap.rearrange("p (h o) -> p h o", o=1).to_broadcast([128, 12, 64])
                    TT_("pool", kk[:], k_, kkbc[:], ALU.mult, ["zc", "kkbc"], ["kk"])
                    TT_("pool", tA[:], kk[:], kk[:], ALU.mult, ["kk"], ["tA"])
                    P.add("dve", lambda e: e.tensor_reduce(s12[:, 0:12], h3(tA[:]), AX.X, ALU.add), reads=["tA"], writes=["s12a"])
                    ACT_(s12[:, 0:12], s12[:, 0:12], AF.Sqrt, ["s12a"], ["s12a"], bias=1e-12)
                    P.add("dve", lambda e: e.reciprocal(s12[:, 0:12], s12[:, 0:12]), reads=["s12a"], writes=["s12a"])
                    TT_("pool", h3(kk[:]), h3(kk[:]), bc12(s12[:, 0:12]), ALU.mult, ["kk", "s12a"], ["kk"])
                    TT_("pool", akk[:], a_[:], kk[:], ALU.mult, ["a_", "kk"], ["akk"])
                    TT_("pool", tA[:], a_[:], kabc[:], ALU.mult, ["a_", "kabc"], ["tA"])
                    TT_("pool", tA[:], tA[:], omka[:], ALU.add, ["tA", "omka"], ["tA"])
                    TT_("pool", kd[:], k_, tA[:], ALU.mult, ["zc", "tA"], ["kd"])
                    bc_ = [self.nb(), self.nb()]
                    for hi_, (c0, cw) in enumerate(halves):
                        P.add("pe", lambda e, hi_=hi_, c0=c0, cw=cw, bc_=bc_, d=d: e.matmul(ps[bc_[hi_]][:, :cw], TRI[d][:], Ssg[:, c0:c0 + cw], start=True, stop=True),
                              reads=["Ssg", "TRI%d" % d], writes=[("ps", bc_[hi_])])
                    for hi_, (c0, cw) in enumerate(halves):
                        kps = ("ps", bc_[hi_])
                        ACT_(E1[:, c0:c0 + cw], ps[bc_[hi_]][:, :cw], AF.Exp, [kps], ["E1"], scale=CL)
                        ACT_(E2[:, c0:c0 + cw], ps[bc_[hi_]][:, :cw], AF.Exp, [kps], ["E2"], scale=-CL)
                        TT_("dve", tB[:, c0:c0 + cw], ps[bc_[hi_]][:, :cw], Ssg[:, c0:c0 + cw], ALU.subtract, [kps, "Ssg"], ["tB"])
                        P.add("dve", lambda e, hi_=hi_, c0=c0, cw=cw, bc_=bc_: e.tensor_copy(tC[:, c0:c0 + cw], ps[bc_[hi_]][:, :cw]), reads=[kps], writes=["tC"])
                    ACT_(tB[:], tB[:], AF.Exp, ["tB"], ["tB"], scale=CL)
                    bt_ = [self.nb(), self.nb()]
                    for hi_, (c0, cw) in enumerate(halves):
                        P.add("pe", lambda e, hi_=hi_, c0=c0, cw=cw, bt_=bt_: e.matmul(ps[bt_[hi_]][:, :cw], onesf[:], Ssg[:, c0:c0 + cw], start=True, stop=True),
                              reads=["Ssg", "onesf"], writes=[("ps", bt_[hi_])])
                        TT_("dve", tC[:, c0:c0 + cw], ps[bt_[hi_]][:, :cw], tC[:, c0:c0 + cw], ALU.subtract, [("ps", bt_[hi_]), "tC"], ["tC"])
                    ACT_(tC[:], tC[:], AF.Exp, ["tC"], ["tC"], scale=CL)
                    bgc = self.nb()
                    for h in range(12):
                        P.add("pe", lambda e, h=h, bgc=bgc: e.matmul(ps[bgc][:64, 2 * h:2 * h + 2], Ssg[:, h * 64:(h + 1) * 64], onesf[:, 0:2], start=True, stop=True),
                              reads=["Ssg", "onesf"], writes=[("ps", bgc)])
                    ACT_(gC[:], ps[bgc][:64, 0:24], AF.Exp, [("ps", bgc)], ["gC"], scale=CL)
                    TT_("pool", Rt[:], r_, E1[:], ALU.mult, ["zc", "E1"], ["Rt"])
                    TT_("pool", Kt[:], kd[:], E2[:], ALU.mult, ["kd", "E2"], ["Kt"])
                    TT_("pool", At[:], kk[:], tB[:], ALU.mult, ["kk", "tB"], ["At"])
                    P.add("dve", lambda e: e.scalar_tensor_tensor(Bt[:], akk[:], -1.0, E2[:], ALU.mult, ALU.mult), reads=["akk", "E2"], writes=["Bt"])
                    TT_("pool", Kcb[:], kd[:], tC[:], ALU.mult, ["kd", "tC"], ["Kcb"])
                    P.add("dve", lambda e: e.scalar_tensor_tensor(Bcb[:], akk[:], -1.0, tC[:], ALU.mult, ALU.mult), reads=["akk", "tC"], writes=["Bcb"])
                    ACT_(Vb[:], v_, AF.Copy, ["zc"], ["Vb"])
                    TT_("pool", tA[:], r_, kd[:], ALU.mult, ["zc", "kd"], ["tA"])
                    TT_("pool", tA[:], tA[:], rkbc[:], ALU.mult, ["tA", "rkbc"], ["tA"])
                    P.add("dve", lambda e: e.tensor_reduce(s12[:, 16:28], h3(tA[:]), AX.X, ALU.add), reads=["tA"], writes=["s12b"])
                    TT_("pool", h3(bon[:]), h3(v_), bc12(s12[:, 16:28]), ALU.mult, ["zc", "s12b"], ["bon"])
                    for (X0, X1, xk0, xk1, DST, dk_) in ((At, Rt, "At", "Rt", ART, "ART"), (Kt, Bt, "Kt", "Bt", KBT, "KBT")):
                        for j in range(3):
                            b = self.nb()
                            pv = ps[b][:].bitcast(BF16)
                            for hh in range(4):
                                h = 4 * j + hh
                                for wi, (X, xk) in enumerate(((X0, xk0), (X1, xk1))):
                                    off = (hh * 2 + wi) * 128
                                    P.add("pe", lambda e, pv=pv, off=off, X=X, h=h: e.transpose(pv[:64, off:off + 128], X[:, h * 64:(h + 1) * 64], idb[:]),
                                          reads=[xk, "idb"], writes=[("ps", b)])
                            dsl = DST[:64, 4 * j:4 * j + 4, :, :].rearrange("p h a t -> p (h a t)")
                            if j % 2 == 0:
                                ACT_(dsl, pv[:64, :], AF.Copy, [("ps", b)], [dk_])
                            else:
                                P.add("dve", lambda e, dsl=dsl, pv=pv: e.tensor_copy(dsl, pv[:64, :]), reads=[("ps", b)], writes=[dk_])
                    mk2 = MK2[d][:].rearrange("p h a t -> p (h a t)")
                    mn4 = MN4[d][:].rearrange("p h t -> p (h t)")
                    for (wi, DST, dk_) in ((0, AKRK, "AKRK"), (1, ABRB, "ABRB")):
                        for hp in range(6):
                            b = self.nb()
                            for hh in range(2):
                                h = 2 * hp + hh
                                P.add("pe", lambda e, b=b, hh=hh, h=h, wi=wi: e.matmul(
                                    ps[b][:, hh * 256:(hh + 1) * 256], KBT[:, h, wi, :], ART[:, h, :, :].rearrange("p a t -> p (a t)"),
                                    start=True, stop=True), reads=["KBT", "ART"], writes=[("ps", b)])
                            dsl = DST[:, 2 * hp:2 * hp + 2, :, :].rearrange("p h a t -> p (h a t)")
                            TT_("dve", dsl, ps[b][:], mk2, ALU.mult, [("ps", b), "MK2_%d" % d], [dk_])
                    for j in range(3):
                        b = self.nb()
                        for hh in range(4):
                            h = 4 * j + hh
                            P.add("pe", lambda e, b=b, hh=hh, h=h: e.matmul(
                                ps[b][:, hh * 128:(hh + 1) * 128], ART[:, h, 0, :], KBT[:, h, 1, :], start=True, stop=True),
                                reads=["KBT", "ART"], writes=[("ps", b)])
                        TT_("dve", Pn[0][:, 4 * j:4 * j + 4, :].rearrange("p h t -> p (h t)"), ps[b][:], mn4, ALU.mult,
                            [("ps", b), "MN4_%d" % d], ["Pn0"])
                    TT_("pool", TTb[0][:], ABRB[:, :, 0, :], idb[:].rearrange("p (o t) -> p o t", o=1).to_broadcast([128, 12, 128]), ALU.add,
                        ["ABRB", "idb"], ["TTb0"])
                    for k in range(6):
                        cur, nxt = k % 2, (k + 1) % 2
                        Pc = Pn[cur]
                        PTc = (lambda h: ABRB[:, h, 0, :]) if k == 0 else (lambda h, cur=cur: PnT[cur][:, h, :])
                        ptkey = "ABRB" if k == 0 else "PnT%d" % cur
                        for j in range(3):
                            b = self.nb()
                            for hh in range(4):
                                h = 4 * j + hh
                                P.add("pe", lambda e, b=b, hh=hh, h=h, Pc=Pc, PTc=PTc: e.matmul(
                                    ps[b][:, hh * 128:(hh + 1) * 128], PTc(h), Pc[:, h, :], start=True, stop=True),
                                    reads=[ptkey, "Pn%d" % cur], writes=[("ps", b)])
                            ACT_(Pn[nxt][:, 4 * j:4 * j + 4, :].rearrange("p h t -> p (h t)"), ps[b][:], AF.Copy, [("ps", b)], ["Pn%d" % nxt])
                        if k < 5:
                            for j in range(3):
                                b = self.nb()
                                for hh in range(4):
                                    h = 4 * j + hh
                                    P.add("pe", lambda e, b=b, hh=hh, h=h, Pc=Pc, PTc=PTc: e.matmul(
                                        ps[b][:, hh * 128:(hh + 1) * 128], Pc[:, h, :], PTc(h), start=True, stop=True),
                                        reads=[ptkey, "Pn%d" % cur], writes=[("ps", b)])
                                P.add("dve", lambda e, b=b, j=j, nxt=nxt: e.tensor_copy(
                                    PnT[nxt][:, 4 * j:4 * j + 4, :].rearrange("p h t -> p (h t)"), ps[b][:]),
                                    reads=[("ps", b)], writes=["PnT%d" % nxt])
                        for j in range(3):
                            b = self.nb()
                            for hh in range(4):
                                h = 4 * j + hh
                                P.add("pe", lambda e, b=b, hh=hh, h=h, nxt=nxt, cur=cur: e.matmul(
                                    ps[b][:, hh * 128:(hh + 1) * 128], Pn[nxt][:, h, :], TTb[cur][:, h, :], start=True, stop=True),
                                    reads=["Pn%d" % nxt, "TTb%d" % cur], writes=[("ps", b)])
                            TT_("dve", TTb[nxt][:, 4 * j:4 * j + 4, :].rearrange("p h t -> p (h t)"), ps[b][:],
                                TTb[cur][:, 4 * j:4 * j + 4, :].rearrange("p h t -> p (h t)"), ALU.add,
                                [("ps", b), "TTb%d" % cur], ["TTb%d" % nxt])
                    TTf = TTb[0]
                    hsl = lambda h: slice(h * 64, (h + 1) * 64)
                    bank_of = lambda bb, h: (bb[0], (h * 64)) if h < 8 else (bb[1], (h - 8) * 64)
                    bW = [self.nb(), self.nb()]
                    for h in range(12):
                        b, o = bank_of(bW, h)
                        P.add("pe", lambda e, b=b, o=o, h=h: e.matmul(ps[b][:, o:o + 64], ART[:, h, 0, :], Hb[:, h, :], start=True, stop=False),
                              reads=["ART", "Hb"], writes=[("ps", b)])
                        P.add("pe", lambda e, b=b, o=o, h=h: e.matmul(ps[b][:, o:o + 64], AKRK[:, h, 0, :], Vb[:, hsl(h)], start=False, stop=True),
                              reads=["AKRK", "Vb"], writes=[("ps", b)])
                    ACT_(Wb[:, 0:512], ps[bW[0]][:], AF.Copy, [("ps", bW[0])], ["Wb"])
                    P.add("dve", lambda e, bW=bW: e.tensor_copy(Wb[:, 512:768], ps[bW[1]][:, 0:256]), reads=[("ps", bW[1])], writes=["Wb"])
                    bU = [self.nb(), self.nb()]
                    for h in range(12):
                        b, o = bank_of(bU, h)
                        P.add("pe", lambda e, b=b, o=o, h=h: e.matmul(ps[b][:, o:o + 64], TTf[:, h, :], Wb[:, hsl(h)], start=True, stop=True),
                              reads=["TTb0", "Wb"], writes=[("ps", b)])
                    ACT_(Ub[:, 0:512], ps[bU[0]][:], AF.Copy, [("ps", bU[0])], ["Ub"])
                    P.add("dve", lambda e, bU=bU: e.tensor_copy(Ub[:, 512:768], ps[bU[1]][:, 0:256]), reads=[("ps", bU[1])], writes=["Ub"])
                    bY = [self.nb(), self.nb()]
                    for h in range(12):
                        b, o = bank_of(bY, h)
                        P.add("pe", lambda e, b=b, o=o, h=h: e.matmul(ps[b][:, o:o + 64], ART[:, h, 1, :], Hb[:, h, :], start=True, stop=False),
                              reads=["ART", "Hb"], writes=[("ps", b)])
                        P.add("pe", lambda e, b=b, o=o, h=h: e.matmul(ps[b][:, o:o + 64], AKRK[:, h, 1, :], Vb[:, hsl(h)], start=False, stop=False),
                              reads=["AKRK", "Vb"], writes=[("ps", b)])
                        P.add("pe", lambda e, b=b, o=o, h=h: e.matmul(ps[b][:, o:o + 64], ABRB[:, h, 1, :], Ub[:, hsl(h)], start=False, stop=True),
                              reads=["ABRB", "Ub"], writes=[("ps", b)])
                    bH = [self.nb(), self.nb()]
                    for h in range(12):
                        b, o = bank_of(bH, h)
                        P.add("pe", lambda e, b=b, o=o, h=h: e.matmul(ps[b][:64, o:o + 64], Kcb[:, hsl(h)], Vb[:, hsl(h)], start=True, stop=False),
                              reads=["Kcb", "Vb"], writes=[("ps", b)])
                        P.add("pe", lambda e, b=b, o=o, h=h: e.matmul(ps[b][:64, o:o + 64], Bcb[:, hsl(h)], Ub[:, hsl(h)], start=False, stop=True),
                              reads=["Bcb", "Ub"], writes=[("ps", b)])
                    for h in range(12):
                        b, o = bank_of(bH, h)
                        P.add("dve", lambda e, b=b, o=o, h=h: e.scalar_tensor_tensor(
                            Hf[:, h, :], Hf[:, h, :], gC[:, 2 * h:2 * h + 1], ps[b][:64, o:o + 64], ALU.mult, ALU.add),
                            reads=["Hf", "gC", ("ps", b)], writes=["Hf"])
                    ACT_(Hb[:64, :, :], Hf[:], AF.Copy, ["Hf"], ["Hb"])
                    if d == 0:
                        ACT_(y_[:, 0:512], ps[bY[0]][:], AF.Copy, [("ps", bY[0])], ["y_"])
                        P.add("dve", lambda e, bY=bY: e.tensor_copy(y_[:, 512:768], ps[bY[1]][:, 0:256]), reads=[("ps", bY[1])], writes=["y_"])
                        P.add("sp", lambda e, t0=t0: e.dma_start(out=S["yrw"][t0:t0 + 128, :], in_=y_[:]), reads=["y_"], dma=True, semkey="st_y")
                        P.add("sp", lambda e, t0=t0: e.dma_start(out=S["bon"][t0:t0 + 128, :], in_=bon[:]), reads=["bon"], dma=True, semkey="st_bon")
                    else:
                        ld("sp", yf[:], S["yrw"][t0:t0 + 128, :], "E1")
                        ld("sp", bf_[:], S["bon"][t0:t0 + 128, :], "E2")
                        TT_("dve", y_[:, 0:512], ps[bY[0]][:], yf[:, 0:512], ALU.add, [("ps", bY[0]), "E1"], ["y_"])
                        TT_("dve", y_[:, 512:768], ps[bY[1]][:, 0:256], yf[:, 512:768], ALU.add, [("ps", bY[1]), "E1"], ["y_"])
                        P.add("dve", lambda e: e.tensor_reduce(s12[:, 32:44], h3(y_[:]), AX.X, ALU.add), reads=["y_"], writes=["s12c"])
                        P.add("pool", lambda e: e.tensor_scalar(s12[:, 32:44], s12[:, 32:44], -1.0 / 64, None, ALU.mult), reads=["s12c"], writes=["s12c"])
                        TT_("pool", h3(y_[:]), h3(y_[:]), bc12(s12[:, 32:44]), ALU.add, ["y_", "s12c"], ["y_"])
                        TT_("pool", tA[:], y_[:], y_[:], ALU.mult, ["y_"], ["tA"])
                        P.add("dve", lambda e: e.tensor_reduce(s12[:, 48:60], h3(tA[:]), AX.X, ALU.add), reads=["tA"], writes=["s12d"])
                        ACT_(s12[:, 48:60], s12[:, 48:60], AF.Sqrt, ["s12d"], ["s12d"], bias=GN_EPS, scale=1.0 / 64)
                        P.add("dve", lambda e: e.reciprocal(s12[:, 48:60], s12[:, 48:60]), reads=["s12d"], writes=["s12d"])
                        TT_("pool", h3(y_[:]), h3(y_[:]), bc12(s12[:, 48:60]), ALU.mult, ["y_", "s12d"], ["y_"])
                        TT_("pool", y_[:], y_[:], lgbc[:], ALU.mult, ["y_", "lgbc"], ["y_"])
                        TT_("pool", y_[:], y_[:], lbbc[:], ALU.add, ["y_", "lbbc"], ["y_"])
                        TT_("pool", y_[:], y_[:], bon[:], ALU.add, ["y_", "bon"], ["y_"])
                        TT_("pool", y_[:], y_[:], bf_[:], ALU.add, ["y_", "E2"], ["y_"])
                        TT_("dve", ob[:], y_[:], g_[:], ALU.mult, ["y_", "g_"], ["ob"])
                        b = self.nb()
                        pv = ps[b][:].bitcast(BF16)
                        for c in range(6):
                            P.add("pe", lambda e, pv=pv, c=c: e.transpose(pv[:, c * 128:(c + 1) * 128], ob[:, c * 128:(c + 1) * 128], idb[:]),
                                  reads=["ob", "idb"], writes=[("ps", b)])
                        ACT_(ost[:].rearrange("p c t -> p (c t)"), pv[:, 0:768], AF.Copy, [("ps", b)], ["ost"])
                        dst = S["oT"][0:768, t0:t0 + 128].rearrange("(c p) t -> p c t", p=128)
                        P.add("sp", lambda e, dst=dst: e.dma_start(out=dst, in_=ost[:]), reads=["ost"], dma=True, semkey="st_o")
                P.flush()

    def phase_ffn(self, l, xin, xout, T):
        nc, P, S, C, W, ps = self.nc, self.P, self.S, self.C, self.W, self.ps
        NT = T // TT
        with contextlib.ExitStack() as st0:
            u0 = self.uid()
            sb0 = lambda name, shape, dt: st0.enter_context(nc.sbuf_tensor("E%d_" % u0 + name, shape, dt))
            x1 = sb0("x1", [128, 4, D], F32)
            x1T = sb0("x1T", [128, 16, TT], BF16)
            gates = sb0("gates", [128, 4, NE], F32)
            idf = sb0("idf", [128, 128], F32)
            P.add("sp", lambda e: e.dma_start(out=idf[:], in_=C["ident"]), writes=["idf"], dma=True, semkey="L_idf")
            P.flush()
            for ti in range(NT):
                t0 = ti * TT
                with contextlib.ExitStack() as st:
                    u = self.uid()
                    sb = lambda name, shape, dt: st.enter_context(nc.sbuf_tensor("E%d_" % u + name, shape, dt))
                    oT = sb("oT", [128, 16, TT], BF16)
                    mT = sb("mT", [128, 16, TT], BF16)
                    sg = [sb("sg%d" % i, [128, 3, 4, TT], BF16) for i in range(2)]
                    wt = [sb("wt%d" % i, [128, 16, 512], BF16) for i in range(3)]
                    gbc = sb("gbc", [128, D], F32)
                    bbc = sb("bbc", [128, D], F32)
                    ta = [sb("ta%d" % i, [128, TT], F32) for i in range(2)]
                    tb = [sb("tb%d" % i, [128, TT], F32) for i in range(2)]
                    tcc = [sb("tc%d" % i, [128, TT], F32) for i in range(2)]
                    ts1 = [sb("ts%d" % i, [128, TT], F32) for i in range(2)]
                    xf = [sb("xf%d" % i, [128, TT], F32) for i in range(2)]
                    rw = sb("rw", [128, 16, NE], F32)
                    rb = sb("rb", [128, NE], F32)
                    junk = sb("junk", [128, D], BF16)
                    st4 = sb("st4", [128, 8], F32)
                    rt_ = sb("rt_", [128, 64], F32)
                    ld = lambda eng, out, in_, key: P.add(eng, lambda e: e.dma_start(out=out, in_=in_), writes=[key], dma=True, semkey="L_" + key)
                    ld("sp", oT[:], S["oT"][:, t0:t0 + TT].rearrange("(c p) t -> p c t", p=128), "oT")
                    ld("sp", x1[:], xin[t0:t0 + TT, :].rearrange("(s p) d -> p s d", p=128), "x1")
                    ld("sp", gbc[:], W["ln1_g"][l].partition_broadcast(128), "gbc")
                    ld("sp", bbc[:], W["ln1_b"][l].partition_broadcast(128), "bbc")
                    ld("sp", rw[:], W["router_w"].rearrange("(c p) e -> p c e", p=128), "rw")
                    ld("sp", rb[:], W["router_bias"][0].partition_broadcast(128), "rb")
                    wcnt = 0
                    for nbk in range(4):
                        ws = wcnt % 3
                        wcnt += 1
                        for (nm, k0, nk) in (("w_up_rw", 0, 6), ("w_up_mla", 6, 4), ("w_up_df", 10, 6)):
                            P.add("pool", lambda e, ws=ws, nm=nm, k0=k0, nk=nk, nbk=nbk: e.dma_start(
                                out=wt[ws][:, k0:k0 + nk, :],
                                in_=self.WB[nm][l].rearrange("(c p) n -> p c n", p=128)[:, :, nbk * 512:(nbk + 1) * 512]),
                                writes=[("wt", ws)], dma=True, semkey="L_wt%d" % ws)
                        sgs = nbk % 2
                        for br in range(3):
                            P.add("sp", lambda e, sgs=sgs, br=br, nbk=nbk: e.dma_start(
                                out=sg[sgs][:, br, :, :],
                                in_=S["sgT"][br * D + nbk * 512:br * D + (nbk + 1) * 512, t0:t0 + TT].rearrange("(c p) t -> p c t", p=128)),
                                writes=[("sg", sgs)], dma=True, semkey="L_sg%d" % sgs)
                        for c in range(4):
                            n = nbk * 4 + c
                            q = n % 2
                            bks = []
                            for br, (k0, nk) in enumerate(((0, 6), (6, 4), (10, 6))):
                                b = self.nb()
                                bks.append(b)
                                for kk in range(nk):
                                    kc = k0 + kk
                                    P.add("pe", lambda e, b=b, ws=ws, kc=kc, c=c, kk=kk, nk=nk: e.matmul(
                                        ps[b][:], wt[ws][:, kc, c * 128:(c + 1) * 128], oT[:, kc, :],
                                        start=(kk == 0), stop=(kk == nk - 1)),
                                        reads=[("wt", ws), "oT"], writes=[("ps", b)])
                            P.add("dve", lambda e, q=q, b=bks[0], sgs=sgs, c=c: e.tensor_tensor(ta[q][:], ps[b][:], sg[sgs][:, 0, c, :], ALU.mult),
                                  reads=[("ps", bks[0]), ("sg", sgs)], writes=[("ta", q)])
                            P.add("dve", lambda e, q=q, b=bks[1], sgs=sgs, c=c: e.tensor_tensor(tb[q][:], ps[b][:], sg[sgs][:, 1, c, :], ALU.mult),
                                  reads=[("ps", bks[1]), ("sg", sgs)], writes=[("tb", q)])
                            P.add("dve", lambda e, q=q, b=bks[2], sgs=sgs, c=c: e.tensor_tensor(tcc[q][:], ps[b][:], sg[sgs][:, 2, c, :], ALU.mult),
                                  reads=[("ps", bks[2]), ("sg", sgs)], writes=[("tc", q)])
                            P.add("pool", lambda e, q=q: e.tensor_tensor(ts1[q][:], ta[q][:], tb[q][:], ALU.add),
                                  reads=[("ta", q), ("tb", q)], writes=[("ts", q)])
                            P.add("pool", lambda e, q=q, n=n: e.tensor_tensor(mT[:, n, :], ts1[q][:], tcc[q][:], ALU.add),
                                  reads=[("ts", q), ("tc", q)], writes=[("mT", n)])
                    mT_all = [("mT", n) for n in range(16)]
                    for nbk in range(4):
                        ws = wcnt % 3
                        wcnt += 1
                        P.add("pool", lambda e, ws=ws, nbk=nbk: e.dma_start(
                            out=wt[ws][:], in_=self.WB["w_o"][l].rearrange("(c p) n -> p c n", p=128)[:, :, nbk * 512:(nbk + 1) * 512]),
                            writes=[("wt", ws)], dma=True, semkey="L_wt%d" % ws)
                        for s_ in range(4):
                            b = self.nb()
                            for kc in range(16):
                                P.add("pe", lambda e, b=b, ws=ws, kc=kc, s_=s_: e.matmul(
                                    ps[b][:], mT[:, kc, s_ * 128:(s_ + 1) * 128], wt[ws][:, kc, :],
                                    start=(kc == 0), stop=(kc == 15)),
                                    reads=mT_all + [("wt", ws)], writes=[("ps", b)])
                            P.add("dve", lambda e, b=b, s_=s_, nbk=nbk: e.scalar_tensor_tensor(
                                x1[:, s_, nbk * 512:(nbk + 1) * 512], x1[:, s_, nbk * 512:(nbk + 1) * 512], float(ALPHA), ps[b][:],
                                ALU.mult, ALU.add), reads=[("ps", b), "x1"], writes=[("x1s", s_)])
                    for s_ in range(4):
                        self.layer_norm(x1[:, s_, :], ("x1s", s_), gbc, bbc, junk, st4, s_)
                    rbanks = [4, 5, 6, 7]
                    for kc in range(16):
                        b = kc % 4
                        q = kc % 2
                        for s_ in range(4):
                            P.add("pe", lambda e, b=b, kc=kc, s_=s_: e.transpose(
                                ps[b][:, s_ * 128:(s_ + 1) * 128], x1[:, s_, kc * 128:(kc + 1) * 128], idf[:]),
                                reads=[("x1s", s_), "idf"], writes=[("ps", b)])
                        P.add("act", lambda e, b=b, kc=kc: e.activation(x1T[:, kc, :], ps[b][:], AF.Copy),
                              reads=[("ps", b)], writes=[("x1T", kc)])
                        P.add("dve", lambda e, b=b, q=q: e.tensor_copy(xf[q][:], ps[b][:]), reads=[("ps", b)], writes=[("xf", q)])
                        for s_ in range(4):
                            P.add("pe", lambda e, kc=kc, s_=s_, q=q: e.matmul(
                                ps[rbanks[s_]][:, :NE], xf[q][:, s_ * 128:(s_ + 1) * 128], rw[:, kc, :],
                                start=(kc == 0), stop=(kc == 15)),
                                reads=[("xf", q), "rw"], writes=[("ps", rbanks[s_])])
                    for s_ in range(4):
                        self.routing(ps[rbanks[s_]][:, :NE], ("ps", rbanks[s_]), rb, rt_, gates[:, s_, :], s_)
                    P.flush()
                with contextlib.ExitStack() as st:
                    u = self.uid()
                    sb = lambda name, shape, dt: st.enter_context(nc.sbuf_tensor("E%d_" % u + name, shape, dt))
                    wsl = [sb("w%d" % i, [128, 16, 512], BF16) for i in range(5)]
                    hT = sb("hT", [128, 8, TT], BF16)
                    yacc = sb("yacc", [128, 4, D], F32)
                    sgt = [sb("sgt%d" % i, [128, TT], F32) for i in range(2)]
                    gbc = sb("gbc", [128, D], F32)
                    bbc = sb("bbc", [128, D], F32)
                    junk = sb("junk", [128, D], BF16)
                    st4 = sb("st4", [128, 8], F32)
                    ld = lambda eng, out, in_, key: P.add(eng, lambda e: e.dma_start(out=out, in_=in_), writes=[key], dma=True, semkey="L_" + key)
                    ld("sp", gbc[:], W["ln2_g"][l].partition_broadcast(128), "gbc")
                    ld("sp", bbc[:], W["ln2_b"][l].partition_broadcast(128), "bbc")
                    x1T_all = [("x1T", kc) for kc in range(16)]
                    wcnt = 0
                    hcnt = 0
                    for ex in range(NE):
                        for jb in range(2):
                            wsg = wcnt % 5
                            wsu = (wcnt + 1) % 5
                            wcnt += 2
                            for (nm, wsx) in (("ex_w_gate", wsg), ("ex_w_up", wsu)):
                                P.add("pool", lambda e, nm=nm, wsx=wsx, ex=ex, jb=jb: e.dma_start(
                                    out=wsl[wsx][:], in_=self.WB[nm][l, ex].rearrange("(c p) n -> p c n", p=128)[:, :, jb * 512:(jb + 1) * 512]),
                                    writes=[("w", wsx)], dma=True, semkey="L_w%d" % wsx)
                            for jj in range(4):
                                j = jb * 4 + jj
                                bg, bu = self.nb(), self.nb()
                                for kc in range(16):
                                    P.add("pe", lambda e, bg=bg, wsg=wsg, kc=kc, jj=jj: e.matmul(
                                        ps[bg][:], wsl[wsg][:, kc, jj * 128:(jj + 1) * 128], x1T[:, kc, :],
                                        start=(kc == 0), stop=(kc == 15)), reads=x1T_all + [("w", wsg)], writes=[("ps", bg)])
                                for kc in range(16):
                                    P.add("pe", lambda e, bu=bu, wsu=wsu, kc=kc, jj=jj: e.matmul(
                                        ps[bu][:], wsl[wsu][:, kc, jj * 128:(jj + 1) * 128], x1T[:, kc, :],
                                        start=(kc == 0), stop=(kc == 15)), reads=x1T_all + [("w", wsu)], writes=[("ps", bu)])
                                q = hcnt % 2
                                hcnt += 1
                                P.add("act", lambda e, bg=bg, q=q: e.activation(sgt[q][:], ps[bg][:], AF.Silu),
                                      reads=[("ps", bg)], writes=[("sgt", q)])
                                P.add("dve", lambda e, bu=bu, q=q, j=j: e.tensor_tensor(hT[:, j, :], ps[bu][:], sgt[q][:], ALU.mult),
                                      reads=[("ps", bu), ("sgt", q)], writes=[("hT", j)])
                        hT_all = [("hT", j) for j in range(8)]
                        for nbk in range(4):
                            wsd = wcnt % 5
                            wcnt += 1
                            P.add("pool", lambda e, wsd=wsd, ex=ex, nbk=nbk: e.dma_start(
                                out=wsl[wsd][:, 0:8, :],
                                in_=self.WB["ex_w_down"][l, ex].rearrange("(c p) n -> p c n", p=128)[:, :, nbk * 512:(nbk + 1) * 512]),
                                writes=[("w", wsd)], dma=True, semkey="L_w%d" % wsd)
                            for s_ in range(4):
                                b = self.nb()
                                for j in range(8):
                                    P.add("pe", lambda e, b=b, wsd=wsd, j=j, s_=s_: e.matmul(
                                        ps[b][:], hT[:, j, s_ * 128:(s_ + 1) * 128], wsl[wsd][:, j, :],
                                        start=(j == 0), stop=(j == 7)), reads=hT_all + [("w", wsd)], writes=[("ps", b)])
                                ysl = yacc[:, s_, nbk * 512:(nbk + 1) * 512]
                                if ex == 0:
                                    P.add("dve", lambda e, b=b, ysl=ysl, s_=s_, ex=ex: e.tensor_scalar(
                                        ysl, ps[b][:], gates[:, s_, ex:ex + 1], None, ALU.mult),
                                        reads=[("ps", b), "gates"], writes=[("y", s_, nbk)])
                                else:
                                    P.add("dve", lambda e, b=b, ysl=ysl, s_=s_, ex=ex: e.scalar_tensor_tensor(
                                        ysl, ps[b][:], gates[:, s_, ex:ex + 1], ysl, ALU.mult, ALU.add),
                                        reads=[("ps", b), "gates", ("y", s_, nbk)], writes=[("y", s_, nbk)])
                    for s_ in range(4):
                        yk = [("y", s_, nbk) for nbk in range(4)]
                        P.add("dve", lambda e, s_=s_: e.scalar_tensor_tensor(
                            yacc[:, s_, :], x1[:, s_, :], float(ALPHA), yacc[:, s_, :], ALU.mult, ALU.add),
                            reads=yk + [("x1s", s_)], writes=[("ys", s_)])
                        self.layer_norm(yacc[:, s_, :], ("ys", s_), gbc, bbc, junk, st4, s_)
                    dst = xout[t0:t0 + TT, :].rearrange("(s p) d -> p s d", p=128)
                    P.add("sp", lambda e, dst=dst: e.dma_start(out=dst, in_=yacc[:]),
                          reads=[("ys", s_) for s_ in range(4)], dma=True, semkey="st_y")
                    P.flush()

    def layer_norm(self, xap, key, gbc, bbc, junk, st4, s_):
        P = self.P
        k = "st4"
        P.add("dve", lambda e: e.tensor_reduce(st4[:, 0:1], xap, AX.X, ALU.add), reads=[key], writes=[k])
        P.add("pool", lambda e: e.tensor_scalar(st4[:, 1:2], st4[:, 0:1], -1.0 / D, None, ALU.mult), reads=[k], writes=[(k, 1)])
        P.add("act", lambda e: e.activation(xap, xap, AF.Identity, bias=st4[:, 1:2]), reads=[(k, 1), key], writes=[key])
        P.add("act", lambda e: e.activation(junk[:], xap, AF.Square, accum_out=st4[:, 2:3]), reads=[key], writes=["junk", (k, 2)])
        P.add("act", lambda e: e.activation(st4[:, 3:4], st4[:, 2:3], AF.Sqrt, bias=LN_EPS, scale=1.0 / D), reads=[(k, 2)], writes=[(k, 3)])
        P.add("dve", lambda e: e.reciprocal(st4[:, 4:5], st4[:, 3:4]), reads=[(k, 3)], writes=[(k, 4)])
        P.add("dve", lambda e: e.scalar_tensor_tensor(xap, xap, st4[:, 4:5], gbc[:], ALU.mult, ALU.mult),
              reads=[key, (k, 4), "gbc"], writes=[key])
        P.add("pool", lambda e: e.tensor_tensor(xap, xap, bbc[:], ALU.add), reads=[key, "bbc"], writes=[key])

    def routing(self, logits, lkey, rb, rt_, gout, s_):
        P = self.P
        sc, sel, M, N_ = rt_[:, 0:16], rt_[:, 16:32], rt_[:, 32:40], rt_[:, 40:48]
        sel3 = sel.rearrange("p (g e) -> p g e", e=4)
        M3 = M.rearrange("p (g e) -> p g e", e=2)
        N3 = N_.rearrange("p (g e) -> p g e", e=2)
        hi, lo, nn, gsc = rt_[:, 48:52], rt_[:, 52:56], rt_[:, 56:60], rt_[:, 60:64]
        k = "rt"
        seq = []
        A = lambda eng, fn: seq.append((eng, fn))
        A("act", lambda e: e.activation(sc, logits, AF.Sigmoid))
        A("dve", lambda e: e.tensor_tensor(sel, sc, rb[:], ALU.add))
        A("dve", lambda e: e.tensor_tensor(M3, sel3[:, :, 0:2], sel3[:, :, 2:4], ALU.max))
        A("dve", lambda e: e.tensor_tensor(N3, sel3[:, :, 0:2], sel3[:, :, 2:4], ALU.min))
        A("dve", lambda e: e.tensor_tensor(hi.rearrange("p (g o) -> p g o", o=1), M3[:, :, 0:1], M3[:, :, 1:2], ALU.max))
        A("dve", lambda e: e.tensor_tensor(lo.rearrange("p (g o) -> p g o", o=1), M3[:, :, 0:1], M3[:, :, 1:2], ALU.min))
        A("dve", lambda e: e.tensor_tensor(nn.rearrange("p (g o) -> p g o", o=1), N3[:, :, 0:1], N3[:, :, 1:2], ALU.max))
        A("dve", lambda e: e.tensor_tensor(lo, lo, nn, ALU.max))
        A("dve", lambda e: e.tensor_tensor(gsc, hi, lo, ALU.add))
        gm = M[:, 0:1]
        A("dve", lambda e: e.tensor_reduce(gm, gsc, AX.X, ALU.max))
        A("dve", lambda e: e.tensor_scalar(hi, gsc, gm, None, ALU.is_equal))
        A("dve", lambda e: e.tensor_scalar(hi, hi, -1.0, 1e30, ALU.add, ALU.mult))
        for ei in range(4):
            A("dve", lambda e, ei=ei: e.tensor_tensor(sel3[:, :, ei:ei + 1], sel3[:, :, ei:ei + 1],
                                                     hi.rearrange("p (g o) -> p g o", o=1), ALU.add))
        m1 = M[:, 1:2]
        eq = rt_[:, 32:48]
        A("dve", lambda e: e.tensor_reduce(m1, sel, AX.X, ALU.max))
        eq1 = rt_[:, 40:56]
        A("dve", lambda e: e.tensor_scalar(eq1, sel, m1, None, ALU.is_equal))
        A("dve", lambda e: e.scalar_tensor_tensor(sel, eq1, -1e30, sel, ALU.mult, ALU.add))
        m2 = M[:, 2:3]
        A("dve", lambda e: e.tensor_reduce(m2, sel, AX.X, ALU.max))
        A("dve", lambda e: e.scalar_tensor_tensor(sel, sel, m2, eq1, ALU.is_equal, ALU.add))
        A("dve", lambda e: e.tensor_tensor(sel, sel, sc, ALU.mult))
        den = M[:, 3:4]
        A("dve", lambda e: e.tensor_reduce(den, sel, AX.X, ALU.add))
        A("dve", lambda e: e.reciprocal(den, den))
        A("dve", lambda e: e.tensor_scalar(gout, sel, den, None, ALU.mult))
        for i, (eng, fn) in enumerate(seq):
            rd = [k, "rb"] + ([lkey] if i == 0 else [])
            wr = [k] + (["gates"] if i == len(seq) - 1 else [])
            P.add(eng, fn, reads=rd, writes=wr)


def host_weights(W):
    out = {}
    for k, shp in WEIGHT_SHAPES.items():
        out[k] = np.ascontiguousarray(np.asarray(W[k], dtype=np.float32).reshape(shp))
    return out


_CACHE = {}


def _get_program(seqs):
    key = tuple(seqs)
    if key not in _CACHE:
        b = Builder(seqs)
        nc = b.build()
        _CACHE[key] = (b, nc)
    return _CACHE[key]


def kernel(**inputs):
    x_prompt = np.asarray(inputs["x_prompt"], dtype=np.float32)
    x_sample = np.asarray(inputs["x_sample"], dtype=np.float32)
    NB_P, S_P = x_prompt.shape[0], x_prompt.shape[1]
    NB_S, S_S = x_sample.shape[0], x_sample.shape[1]
    n_cores = 8
    assert NB_S == n_cores and n_cores % NB_P == 0
    seqs = [S_S, S_P]
    b, nc = _get_program(seqs)
    hw = host_weights(inputs)
    consts = make_consts(max(seqs))
    base = {k: hw[k] for k in b.W.used()}
    base.update({k: consts[k] for k in b.C.used()})
    in_maps = []
    for c in range(n_cores):
        d = dict(base)
        d["xs0"] = np.ascontiguousarray(x_sample[c])
        d["xs1"] = np.ascontiguousarray(x_prompt[c % NB_P])
        in_maps.append(d)
    res = run_bass_kernel_spmd(nc, in_maps, core_ids=list(range(n_cores)))
    y_sample = np.stack([np.asarray(res.results[c]["ys0"], dtype=np.float32) for c in range(n_cores)], 0)
    y_prompt = np.stack([np.asarray(res.results[c]["ys1"], dtype=np.float32) for c in range(NB_P)], 0)
    return (y_prompt, y_sample)
```

```python
import contextlib
import numpy as np
import ml_dtypes
import concourse.bass as bass
import concourse.mybir as mybir
from concourse.bass_utils import run_bass_kernel_spmd

F32 = mybir.dt.float32
BF16 = mybir.dt.bfloat16
AF = mybir.ActivationFunctionType
ALU = mybir.AluOpType
AX = mybir.AxisListType

ENGS = ("pe", "act", "dve", "pool", "sp")

D = 2048
DEPTH = 2
IN_W = 11936
RW_IN, MLA_IN, DF_IN, GATE_IN = 2688, 800, 2304, 6144
C_RW = 768
NE, DE = 16, 1024
ALPHA = (2 * DEPTH) ** 0.25
LN_EPS = 1e-5
RMS_EPS = 1e-6
DF_EPS = 1e-5
GN_EPS = 64e-5
TT = 512
import os as _os
NOSELF = _os.environ.get("NOSELF", "0") == "1"


class _Op:
    __slots__ = ("eng", "fn", "reads", "writes", "dma", "semkey", "deps", "needed", "token")


class Prog:
    def __init__(self, nc):
        self.nc = nc
        self.ops = []
        self.st = contextlib.ExitStack()
        self.esem = {e: self.st.enter_context(nc.semaphore("s_" + e)) for e in ENGS}
        self.dsem = {}
        self.ecount = {e: 0 for e in ENGS}
        self.dcount = {}
        self.seen = {e: {} for e in ENGS}
        self.n_total = 0
        self.phase_keys = {}

    def add(self, eng, fn, reads=(), writes=(), dma=False, semkey=None):
        op = _Op()
        if eng != "pe":
            extra = [r for r in reads if isinstance(r, tuple) and r and r[0] == "ps" and r not in writes]
            if extra:
                writes = tuple(writes) + tuple(extra)
        op.eng, op.fn, op.reads, op.writes = eng, fn, tuple(reads), tuple(writes)
        op.dma, op.semkey = dma, semkey
        op.deps, op.needed, op.token = set(), False, None
        if dma:
            pk = (eng, semkey)
            if pk not in self.phase_keys:
                idx = (eng, sum(1 for k in self.phase_keys if k[0] == eng))
                self.phase_keys[pk] = idx
                if idx not in self.dsem:
                    self.dsem[idx] = self.st.enter_context(self.nc.semaphore("d_%s%d" % idx))
                    self.dcount[idx] = 0
            op.semkey = self.phase_keys[pk]
        self.ops.append(op)
        return op

    def barrier(self):
        for e in ENGS:
            self.add(e, None, reads=("__all__",))

    def _analyze(self):
        ops = self.ops
        last_w, readers, last_of_eng, last_dma = {}, {}, {}, {}
        for i, op in enumerate(ops):
            deps = set()
            if op.reads == ("__all__",):
                deps.update(last_of_eng.values())
                deps.update(last_dma.values())
            else:
                for r in op.reads:
                    if r in last_w:
                        deps.add(last_w[r])
                for w in op.writes:
                    if w in last_w:
                        deps.add(last_w[w])
                    deps.update(readers.get(w, ()))
                for r in op.reads:
                    readers.setdefault(r, []).append(i)
                for w in op.writes:
                    last_w[w] = i
                    readers[w] = []
            deps.discard(i)
            op.deps = deps
            if op.fn is not None:
                if op.dma:
                    last_dma[op.semkey] = i
                else:
                    last_of_eng[op.eng] = i
        for op in ops:
            for d in op.deps:
                p = ops[d]
                if p.dma:
                    continue
                if p.eng == op.eng and not op.dma and (p.eng == "pe" or NOSELF):
                    continue
                p.needed = True

    def flush(self):
        self.barrier()
        self._analyze()
        ops = self.ops
        waits = []
        for op in ops:
            w = {}
            for d in op.deps:
                p = ops[d]
                if p.dma:
                    s = ("d", p.semkey)
                    v = self.dcount[p.semkey]
                else:
                    if p.eng == op.eng and not op.dma and (p.eng == "pe" or NOSELF):
                        continue
                    s = ("e", p.eng)
                    v = p.token
                if v > w.get(s, 0):
                    w[s] = v
            wl = []
            for s, v in w.items():
                if self.seen[op.eng].get(s, 0) >= v:
                    continue
                self.seen[op.eng][s] = v
                wl.append((self.dsem[s[1]] if s[0] == "d" else self.esem[s[1]], v))
            waits.append(wl)
            if op.fn is not None:
                if op.dma:
                    self.dcount[op.semkey] += 16
                    op.token = self.dcount[op.semkey]
                elif op.needed:
                    self.ecount[op.eng] += 1
                    op.token = self.ecount[op.eng]
        dsem, esem = self.dsem, self.esem

        def run(ename, eng):
            for op, wl in zip(ops, waits):
                if op.eng != ename:
                    continue
                for s, v in wl:
                    eng.wait_ge(s, v)
                if op.fn is None:
                    continue
                ins = op.fn(eng)
                if op.dma:
                    ins.then_inc(dsem[op.semkey], 16)
                elif op.needed:
                    ins.then_inc(esem[op.eng], 1)

        with self.nc.Block() as block:
            @block.tensor
            def _(eng):
                run("pe", eng)

            @block.scalar
            def _(eng):
                run("act", eng)

            @block.vector
            def _(eng):
                run("dve", eng)

            @block.gpsimd
            def _(eng):
                run("pool", eng)

            @block.sync
            def _(eng):
                run("sp", eng)
        self.n_total += len(ops)
        self.ops = []
        self.phase_keys = {}

    def close(self):
        self.st.close()


def _rope_tables(d, S):
    half = d // 2
    inv = np.power(np.float32(10000.0), -np.arange(half, dtype=np.float32) * np.float32(2.0 / d)).astype(np.float32)
    ang = np.arange(S, dtype=np.float32)[None, :] * inv[:, None]
    return np.cos(ang).astype(np.float32), np.sin(ang).astype(np.float32)


def make_consts(Tmax):
    c = {}
    cos64, sin64 = _rope_tables(64, Tmax)
    cos32, sin32 = _rope_tables(32, Tmax)
    c["cosdf"] = np.concatenate([cos64, cos64, cos64, cos64], 0)
    c["sindf"] = np.concatenate([-sin64, sin64, -sin64, sin64], 0)
    perm = np.zeros((128, 128), np.float32)
    for m in range(128):
        blk, j = divmod(m, 64)
        perm[blk * 64 + (j + 32) % 64, m] = 1.0
    c["permdf"] = perm
    c["cos96"] = np.concatenate([np.ones((64, Tmax), np.float32), cos32, cos32], 0)
    c["sin96"] = np.concatenate([np.zeros((64, Tmax), np.float32), -sin32, sin32], 0)
    p96 = np.zeros((96, 96), np.float32)
    for m in range(96):
        if m < 64:
            p96[m, m] = 1.0
        else:
            j = m - 64
            p96[64 + (j + 16) % 32, m] = 1.0
    c["perm96"] = p96
    p32 = np.zeros((128, 32), np.float32)
    for m in range(32):
        p32[(m + 16) % 32, m] = 1.0
    c["perm32"] = p32
    permq = np.zeros((768, 128), np.float32)
    cosq = np.ones((768, Tmax), np.float32)
    sinq = np.zeros((768, Tmax), np.float32)
    for g in range(768):
        j, m = divmod(g, 128)
        h, d_ = divmod(g, 96)
        if d_ < 64:
            permq[j * 128 + m, m] = 1.0
        else:
            jj = d_ - 64
            g2 = h * 96 + 64 + (jj + 16) % 32
            assert g2 // 128 == j
            permq[j * 128 + (g2 - j * 128), m] = 1.0
            cosq[g] = cos32[jj % 16]
            sinq[g] = -sin32[jj] if jj < 16 else sin32[jj - 16]
    c["permq"], c["cosq"], c["sinq"] = permq, cosq, sinq
    permk = np.zeros((128, 128), np.float32)
    for m in range(32):
        permk[(m + 16) % 32, m] = 1.0
    c["permk"] = permk
    cosk = np.zeros((128, Tmax), np.float32)
    sink = np.zeros((128, Tmax), np.float32)
    cosk[:32] = np.concatenate([cos32, cos32], 0)
    sink[:32] = np.concatenate([-sin32, sin32], 0)
    c["cosk"], c["sink"] = cosk, sink
    c["ident"] = np.eye(128, dtype=np.float32)
    c["ones"] = np.ones((128, 128), np.float32)
    s = np.arange(128)[:, None]
    t = np.arange(128)[None, :]
    c["m_su"] = (s < t).astype(np.float32)
    c["m_iu"] = (s <= t).astype(np.float32)
    c["m_sl"] = (s > t).astype(np.float32)
    c["m_il"] = (s >= t).astype(np.float32)
    return c


CONST_SHAPES = lambda Tmax: {
    "cosdf": [128, Tmax], "sindf": [128, Tmax], "permdf": [128, 128],
    "cos96": [96, Tmax], "sin96": [96, Tmax], "perm96": [96, 96], "perm32": [128, 32],
    "ident": [128, 128], "ones": [128, 128],
    "permq": [768, 128], "cosq": [768, Tmax], "sinq": [768, Tmax], "permk": [128, 128], "cosk": [128, Tmax], "sink": [128, Tmax],
    "m_su": [128, 128], "m_iu": [128, 128], "m_sl": [128, 128], "m_il": [128, 128],
}

WEIGHT_SHAPES = {
    "w_in": [2, 2048, 11936], "shift_prev": [2, 2688], "shift_next": [2, 2688],
    "rw_w0": [2, 2, 768], "rw_w2": [2, 2, 64, 768], "rw_a0": [2, 2, 768], "rw_a2": [2, 2, 64, 768],
    "rw_g2": [2, 128, 768], "rw_k_k": [2, 768], "rw_k_a": [2, 768], "rw_r_k": [2, 768],
    "rw_lnx_g": [2, 768], "rw_lnx_b": [2, 768], "mla_q_norm": [2, 512], "mla_kv_norm": [2, 256],
    "mla_w_uq": [2, 512, 768], "mla_w_ukv": [2, 256, 1024], "df_lq1": [2, 64], "df_lk1": [2, 64],
    "df_lq2": [2, 64], "df_lk2": [2, 64], "df_subln": [2, 128], "w_up_rw": [2, 768, 2048],
    "w_up_mla": [2, 512, 2048], "w_up_df": [2, 768, 2048], "w_o": [2, 2048, 2048],
    "ln1_g": [2, 2048], "ln1_b": [2, 2048], "ln2_g": [2, 2048], "ln2_b": [2, 2048],
    "router_w": [2048, 16], "router_bias": [1, 16],
    "ex_w_gate": [2, 16, 2048, 1024], "ex_w_up": [2, 16, 2048, 1024], "ex_w_down": [2, 16, 1024, 2048],
}


class _Lazy:
    def __init__(self, nc, shapes):
        self.nc, self.shapes, self.d = nc, shapes, {}

    def __getitem__(self, k):
        if k not in self.d:
            self.d[k] = self.nc.dram_tensor(k, self.shapes[k], F32, kind="ExternalInput").ap()
        return self.d[k]

    def used(self):
        return list(self.d.keys())


class Builder:
    def __init__(self, seqs, dbg=(), n_layers=DEPTH, phases=None):
        self.seqs = list(seqs)
        self.Tmax = max(seqs)
        self.dbg = set(dbg)
        self.n_layers = n_layers
        self.phases = phases
        nc = self.nc = bass.Bass("TRN2", target_bir_lowering=False)
        self.P = Prog(nc)
        self.W = _Lazy(nc, WEIGHT_SHAPES)
        self.C = _Lazy(nc, CONST_SHAPES(self.Tmax))
        self.xin = [nc.dram_tensor("xs%d" % i, [T, D], F32, kind="ExternalInput").ap() for i, T in enumerate(seqs)]
        self.yout = [nc.dram_tensor("ys%d" % i, [T, D], F32, kind="ExternalOutput").ap() for i, T in enumerate(seqs)]
        Tm = self.Tmax
        self.S = {}
        for name, shape, dt in [
            ("xmid", [Tm, D], F32),
            ("zrw", [Tm, RW_IN], F32),
            ("cqT", [512, Tm], F32), ("ckvT", [256, Tm], F32), ("krT", [32, Tm], F32),
            ("dfqkT", [24 * 64, Tm], BF16),
            ("vdf", [Tm, 768], BF16),
            ("sgT", [GATE_IN, Tm], BF16),
            ("qmT", [8 * 96, Tm], BF16), ("kmT", [8 * 96, Tm], BF16), ("vm", [Tm, 512], BF16),
            ("oT", [D, Tm], BF16),
            ("yrw", [Tm, C_RW], F32),
            ("bon", [Tm, C_RW], F32),
        ]:
            kind = "ExternalOutput" if name in self.dbg else "Internal"
            self.S[name] = nc.dram_tensor("scr_" + name, shape, dt, kind=kind).ap()
        self._bank = 0
        self.WB = {}

    def precast_weights(self):
        nc, P, W = self.nc, self.P, self.W
        L = self.n_layers
        def mk(name, shape):
            self.WB[name] = nc.dram_tensor("wb_" + name, shape, BF16, kind="Internal").ap()
        mk("w_in", [2, 2048, IN_W]); mk("w_o", [2, 2048, 2048])
        mk("w_up_rw", [2, 768, 2048]); mk("w_up_mla", [2, 512, 2048]); mk("w_up_df", [2, 768, 2048])
        mk("ex_w_gate", [2, 16, 2048, 1024]); mk("ex_w_up", [2, 16, 2048, 1024]); mk("ex_w_down", [2, 16, 1024, 2048])
        n = 0
        def cp(dst, src):
            nonlocal n
            P.add("pool", lambda e: e.dma_start(out=dst, in_=src), dma=True, semkey="pc%d" % (n % 4))
            n += 1
        for l in range(L):
            for r0 in range(0, 2048, 256):
                cp(self.WB["w_in"][l, r0:r0 + 256, :], W["w_in"][l, r0:r0 + 256, :])
            for r0 in range(0, 2048, 512):
                cp(self.WB["w_o"][l, r0:r0 + 512, :], W["w_o"][l, r0:r0 + 512, :])
            for nm in ("w_up_rw", "w_up_mla", "w_up_df"):
                cp(self.WB[nm][l], W[nm][l])
            for ex in range(NE):
                for nm in ("ex_w_gate", "ex_w_up", "ex_w_down"):
                    rows = WEIGHT_SHAPES[nm][2]
                    for r0 in range(0, rows, 512):
                        cp(self.WB[nm][l, ex, r0:r0 + 512, :], W[nm][l, ex, r0:r0 + 512, :])
        P.flush()

    def uid(self):
        self._uid = getattr(self, "_uid", 0) + 1
        return self._uid

    def nb(self):
        b = self._bank
        self._bank = (self._bank + 1) % 8
        return b

    def build(self):
        nc, P = self.nc, self.P
        with contextlib.ExitStack() as gst:
            self.ps = [gst.enter_context(nc.psum_tensor("ps%d" % b, [128, 512], F32)) for b in range(8)]
            if self.phases is None or "A" in self.phases or "E" in self.phases:
                self.precast_weights()
            for si, T in enumerate(self.seqs):
                for l in range(self.n_layers):
                    xin = self.xin[si] if l == 0 else self.S["xmid"]
                    xout = self.yout[si] if l == self.n_layers - 1 else self.S["xmid"]
                    ph = self.phases
                    if ph is None or "A" in ph:
                        self.phase_inproj(l, xin, T)
                    if ph is None or "B" in ph:
                        self.phase_mla(l, T)
                    if ph is None or "C" in ph:
                        self.phase_diff(l, T)
                    if ph is None or "D" in ph:
                        self.phase_rwkv(l, T)
                    if ph is None or "E" in ph:
                        self.phase_ffn(l, xin, xout, T)
        P.close()
        return nc

    def phase_inproj(self, l, xin, T):
        nc, P, S, C, ps = self.nc, self.P, self.S, self.C, self.ps
        w_in = self.WB["w_in"][l].rearrange("(kc p) n -> p kc n", p=128)
        with contextlib.ExitStack() as st:
            sb = lambda name, shape, dt: st.enter_context(nc.sbuf_tensor("A%d_" % self.uid() + name, shape, dt))
            xt = sb("xt", [128, 4, D], BF16)
            xT = sb("xT", [128, 16, TT], BF16)
            wt = [sb("wt%d" % i, [128, 16, 512], BF16) for i in range(3)]
            stgb = [sb("stgb%d" % i, [128, 4, 512], BF16) for i in range(2)]
            stgf = [sb("stgf%d" % i, [128, 4, 512], F32) for i in range(2)]
            zb = [sb("zb%d" % i, [128, 512], BF16) for i in range(2)]
            t1 = [sb("t1_%d" % i, [128, 512], F32) for i in range(2)]
            t2 = [sb("t2_%d" % i, [128, 512], F32) for i in range(2)]
            idn = sb("idn", [128, 128], BF16)
            perm = sb("perm", [128, 128], BF16)
            cosb = sb("cos", [128, TT], F32)
            sinb = sb("sin", [128, TT], F32)
            P.add("pool", lambda e: e.dma_start(out=idn[:], in_=C["ident"]), writes=["idn"], dma=True, semkey="c0")
            P.add("pool", lambda e: e.dma_start(out=perm[:], in_=C["permdf"]), writes=["perm"], dma=True, semkey="c1")
            cnt = {"w": 0, "sb": 0, "sf": 0, "z": 0, "ev": 0}

            def evac_copy(out_ap, in_ap, reads, writes):
                cnt["ev"] += 1
                if cnt["ev"] % 2 == 0:
                    P.add("act", lambda e: e.activation(out_ap, in_ap, AF.Copy), reads=reads, writes=writes)
                else:
                    P.add("dve", lambda e: e.tensor_copy(out_ap, in_ap), reads=reads, writes=writes)

            blocks = []
            for c0 in range(0, RW_IN, 512):
                blocks.append((c0, min(512, RW_IN - c0), "rw"))
            blocks.append((RW_IN, 512, "cq"))
            blocks.append((RW_IN + 512, 288, "ckv"))
            base = RW_IN + MLA_IN
            for which in range(2):
                blocks.append((base + which * 768, 512, "dfqk"))
                blocks.append((base + which * 768 + 512, 256, "dfqk"))
            blocks.append((base + 1536, 512, "dfv"))
            blocks.append((base + 1536 + 512, 256, "dfv"))
            gbase = base + DF_IN
            for c0 in range(0, GATE_IN, 512):
                blocks.append((gbase + c0, 512, "gate"))

            for ti in range(T // TT):
                t0 = ti * TT
                P.add("pool", lambda e, t0=t0: e.dma_start(
                    out=xt[:], in_=xin[t0:t0 + TT, :].rearrange("(s p) d -> p s d", p=128)),
                    writes=["xt"], dma=True, semkey="xt")
                P.add("sp", lambda e, t0=t0: e.dma_start(out=cosb[:], in_=C["cosdf"][:, t0:t0 + TT]),
                      writes=["cos"], dma=True, semkey="cos")
                P.add("sp", lambda e, t0=t0: e.dma_start(out=sinb[:], in_=C["sindf"][:, t0:t0 + TT]),
                      writes=["sin"], dma=True, semkey="sin")
                for g in range(8):
                    b = self.nb()
                    pv = ps[b][:].bitcast(BF16)
                    for kk in range(2):
                        kc = 2 * g + kk
                        for s in range(4):
                            P.add("pe", lambda e, pv=pv, kk=kk, s=s, kc=kc: e.transpose(
                                pv[:, kk * 512 + s * 128: kk * 512 + (s + 1) * 128],
                                xt[:, s, kc * 128:(kc + 1) * 128], idn[:]),
                                reads=["xt", "idn"], writes=[("ps", b)])
                    evac_copy(xT[:, 2 * g:2 * g + 2, :], pv.rearrange("p (a t) -> p a t", a=2),
                              [("ps", b)], [("xT", g)])
                xT_all = [("xT", g) for g in range(8)]

                for (c0, w, kind) in blocks:
                    if getattr(self, "only_kinds", None) is not None and kind not in self.only_kinds:
                        continue
                    ws = cnt["w"] % 3
                    cnt["w"] += 1
                    P.add("pool", lambda e, ws=ws, c0=c0, w=w: e.dma_start(
                        out=wt[ws][:, :, :w], in_=w_in[:, :, c0:c0 + w]),
                        writes=[("wt", ws)], dma=True, semkey="wt%d" % ws)
                    if kind in ("rw", "dfv"):
                        if kind == "rw":
                            ss = cnt["sf"] % 2
                            cnt["sf"] += 1
                            stg, skey = stgf[ss], ("stgf", ss)
                        else:
                            ss = cnt["sb"] % 2
                            cnt["sb"] += 1
                            stg, skey = stgb[ss], ("stgb", ss)
                        for s in range(4):
                            b = self.nb()
                            for kc in range(16):
                                P.add("pe", lambda e, b=b, kc=kc, s=s, ws=ws, w=w: e.matmul(
                                    ps[b][:, :w], xT[:, kc, s * 128:(s + 1) * 128], wt[ws][:, kc, :w],
                                    start=(kc == 0), stop=(kc == 15)),
                                    reads=xT_all + [("wt", ws)], writes=[("ps", b)])
                            evac_copy(stg[:, s, :w], ps[b][:, :w], [("ps", b)], [skey])
                        if kind == "rw":
                            dst = S["zrw"][t0:t0 + TT, c0:c0 + w].rearrange("(s p) n -> p s n", p=128)
                        else:
                            cc = c0 - (base + 1536)
                            dst = S["vdf"][t0:t0 + TT, cc:cc + w].rearrange("(s p) n -> p s n", p=128)
                        P.add("sp", lambda e, dst=dst, stg=stg, w=w: e.dma_start(out=dst, in_=stg[:, :, :w]),
                              reads=[skey], dma=True, semkey="st_%s%d" % (skey[0], ss))
                    else:
                        if kind in ("cq", "ckv"):
                            ss = cnt["sf"] % 2
                            cnt["sf"] += 1
                            stg, skey = stgf[ss], ("stgf", ss)
                        else:
                            ss = cnt["sb"] % 2
                            cnt["sb"] += 1
                            stg, skey = stgb[ss], ("stgb", ss)
                        chunks = [(j, min(128, w - j)) for j in range(0, w, 128)]
                        for ci, (j, cw) in enumerate(chunks):
                            b = self.nb()
                            for kc in range(16):
                                P.add("pe", lambda e, b=b, kc=kc, j=j, cw=cw, ws=ws: e.matmul(
                                    ps[b][:cw, :], wt[ws][:, kc, j:j + cw], xT[:, kc, :],
                                    start=(kc == 0), stop=(kc == 15)),
                                    reads=xT_all + [("wt", ws)], writes=[("ps", b)])
                            if kind == "gate":
                                P.add("act", lambda e, b=b, ci=ci, stg=stg: e.activation(
                                    stg[:, ci, :], ps[b][:], AF.Sigmoid), reads=[("ps", b)], writes=[skey])
                            elif kind == "dfqk":
                                zs = cnt["z"] % 2
                                cnt["z"] += 1
                                b2 = self.nb()
                                P.add("dve", lambda e, b=b, zs=zs: e.tensor_copy(zb[zs][:], ps[b][:]),
                                      reads=[("ps", b)], writes=[("zb", zs)])
                                import os
                                _m = os.environ.get("DFQK_MODE", "")
                                if _m == "1":
                                    b2 = b
                                else:
                                    P.add("pe", lambda e, b2=b2, zs=zs: e.matmul(ps[b2][:], perm[:], zb[zs][:],
                                                                               start=True, stop=True),
                                          reads=[("zb", zs), "perm"], writes=[("ps", b2)])
                                P.add("dve", lambda e, b=b, zs=zs: e.tensor_tensor(t1[zs][:], ps[b][:], cosb[:], ALU.mult),
                                      reads=[("ps", b), "cos"], writes=[("t1", zs)])
                                P.add("dve", lambda e, b2=b2, zs=zs: e.tensor_tensor(t2[zs][:], ps[b2][:], sinb[:], ALU.mult),
                                      reads=[("ps", b2), "sin"], writes=[("t2", zs)])
                                P.add(os.environ.get("ADD_ENG", "pool"), lambda e, zs=zs, ci=ci, stg=stg: e.tensor_tensor(
                                    stg[:, ci, :], t1[zs][:], t2[zs][:], ALU.add),
                                    reads=[("t1", zs), ("t2", zs)], writes=[skey])
                            else:
                                evac_copy(stg[:cw, ci, :], ps[b][:cw, :], [("ps", b)], [skey])
                        stkey = "st_%s%d" % (skey[0], ss)
                        if kind == "gate":
                            r0 = c0 - gbase
                            dst = S["sgT"][r0:r0 + 512, t0:t0 + TT].rearrange("(c p) t -> p c t", p=128)
                            P.add("sp", lambda e, dst=dst, stg=stg: e.dma_start(out=dst, in_=stg[:]),
                                  reads=[skey], dma=True, semkey=stkey)
                        elif kind == "dfqk":
                            r0 = c0 - base
                            nch = len(chunks)
                            dst = S["dfqkT"][r0:r0 + w, t0:t0 + TT].rearrange("(c p) t -> p c t", p=128)
                            P.add("sp", lambda e, dst=dst, stg=stg, nch=nch: e.dma_start(out=dst, in_=stg[:, :nch, :]),
                                  reads=[skey], dma=True, semkey=stkey)
                        elif kind == "cq":
                            dst = S["cqT"][:, t0:t0 + TT].rearrange("(c p) t -> p c t", p=128)
                            P.add("sp", lambda e, dst=dst, stg=stg: e.dma_start(out=dst, in_=stg[:]),
                                  reads=[skey], dma=True, semkey=stkey)
                        else:
                            dst = S["ckvT"][:, t0:t0 + TT].rearrange("(c p) t -> p c t", p=128)
                            P.add("sp", lambda e, dst=dst, stg=stg: e.dma_start(out=dst, in_=stg[:, 0:2, :]),
                                  reads=[skey], dma=True, semkey=stkey)
                            dst2 = S["krT"][:, t0:t0 + TT]
                            P.add("sp", lambda e, dst2=dst2, stg=stg: e.dma_start(out=dst2, in_=stg[:32, 2, :]),
                                  reads=[skey], dma=True, semkey=stkey)
            P.flush()

    def phase_mla(self, l, T):
        nc, P, S, C, W, ps = self.nc, self.P, self.S, self.C, self.W, self.ps
        NT = T // TT
        scale = 96.0 ** -0.5
        with contextlib.ExitStack() as st:
            sb = lambda name, shape, dt: st.enter_context(nc.sbuf_tensor("B%d_" % self.uid() + name, shape, dt))
            onesb = sb("ones", [128, 128], BF16)
            pq = sb("pq", [128, 6, 128], BF16)
            pk = sb("pk", [128, 128], BF16)
            wkv = sb("wkv", [128, 2, 1024], BF16)
            KR = sb("KR", [128, TT], F32)
            cosq = sb("cosq", [128, 6, TT], F32)
            sinq = sb("sinq", [128, 6, TT], F32)
            cosk = sb("cosk", [128, TT], F32)
            sink = sb("sink", [128, TT], F32)
            wuq = sb("wuq", [128, 4, 768], BF16)
            wv = sb("wv", [128, 2, 8, 64], BF16)
            gq = sb("gq", [128, 4], F32)
            gkv = sb("gkv", [128, 2], F32)
            cq = sb("cq", [128, 4, TT], F32)
            ckv = sb("ckv", [128, 2, TT], F32)
            sq = sb("sq", [128, 4, TT], BF16)
            rt = sb("rt", [128, TT], F32)
            rstd = sb("rstd", [128, TT], F32)
            cqn = sb("cqn", [128, 4, TT], BF16)
            ckvn = sb("ckvn", [128, 2, TT], BF16)
            zb = [sb("zb%d" % i, [128, TT], BF16) for i in range(2)]
            for i in range(2):
                P.add("pool", lambda e, i=i: e.memset(zb[i][:], 0.0), writes=[("zb", i)])
            t1 = [sb("t1_%d" % i, [128, TT], F32) for i in range(2)]
            t2 = [sb("t2_%d" % i, [128, TT], F32) for i in range(2)]
            P.add("pool", lambda e: e.memset(KR[:], 0.0), writes=["kr"])
            qstg = [sb("qstg%d" % i, [128, 3, TT], BF16) for i in range(2)]
            kstg = [sb("kstg%d" % i, [128, 4, TT], BF16) for i in range(2)]
            krr = sb("krr", [128, TT], BF16)
            vstg = sb("vstg", [128, 4, 512], BF16)
            ld = lambda eng, out, in_, key: P.add(eng, lambda e: e.dma_start(out=out, in_=in_), writes=[key], dma=True, semkey="L_" + key)
            ld("pool", onesb[:], C["ones"], "ones")
            ld("pool", pq[:], C["permq"].rearrange("(j k) m -> k j m", k=128), "pq")
            ld("pool", pk[:], C["permk"], "pk")
            ld("pool", wkv[:], W["mla_w_ukv"][l].rearrange("(kc p) n -> p kc n", p=128), "wkv")
            ld("pool", wuq[:], W["mla_w_uq"][l].rearrange("(kc p) n -> p kc n", p=128), "wuq")
            wukv = W["mla_w_ukv"][l].rearrange("(kc p) (h c) -> kc p h c", p=128, c=128)
            for kc in range(2):
                P.add("pool", lambda e, kc=kc: e.dma_start(out=wv[:, kc, :, :], in_=wukv[kc, :, :, 64:128]),
                      writes=["wv"], dma=True, semkey="L_wv")
            for kc in range(4):
                P.add("sp", lambda e, kc=kc: e.dma_start(
                    out=gq[:, kc:kc + 1], in_=W["mla_q_norm"][l, kc * 128:(kc + 1) * 128].rearrange("(p o) -> p o", o=1)),
                    writes=["gq"], dma=True, semkey="L_gq")
            for kc in range(2):
                P.add("sp", lambda e, kc=kc: e.dma_start(
                    out=gkv[:, kc:kc + 1], in_=W["mla_kv_norm"][l, kc * 128:(kc + 1) * 128].rearrange("(p o) -> p o", o=1)),
                    writes=["gkv"], dma=True, semkey="L_gkv")
            cnt = {"z": 0, "q": 0, "k": 0}
            for ti in range(NT):
                t0 = ti * TT
                ld("sp", cq[:], S["cqT"][:, t0:t0 + TT].rearrange("(c p) t -> p c t", p=128), "cq")
                ld("sp", ckv[:], S["ckvT"][:, t0:t0 + TT].rearrange("(c p) t -> p c t", p=128), "ckv")
                ld("sp", KR[:32, :], S["krT"][:, t0:t0 + TT], "kr")
                ld("sp", cosq[:], C["cosq"][:, t0:t0 + TT].rearrange("(c p) t -> p c t", p=128), "cosq")
                ld("sp", sinq[:], C["sinq"][:, t0:t0 + TT].rearrange("(c p) t -> p c t", p=128), "sinq")
                ld("sp", cosk[:], C["cosk"][:, t0:t0 + TT], "cosk")
                ld("sp", sink[:], C["sink"][:, t0:t0 + TT], "sink")

                def rmsnorm(src, key, nk, g, dst, dkey, width):
                    b = self.nb()
                    P.add("act", lambda e: e.activation(sq[:, :nk, :], src[:], AF.Square), reads=[key], writes=["sq"])
                    for kc in range(nk):
                        P.add("pe", lambda e, kc=kc: e.matmul(ps[b][:], onesb[:], sq[:, kc, :], start=(kc == 0), stop=(kc == nk - 1)),
                              reads=["sq", "ones"], writes=[("ps", b)])
                    P.add("act", lambda e: e.activation(rt[:], ps[b][:], AF.Sqrt, bias=RMS_EPS, scale=1.0 / width),
                          reads=[("ps", b)], writes=["rt"])
                    P.add("dve", lambda e: e.reciprocal(rstd[:], rt[:]), reads=["rt"], writes=["rstd"])
                    for kc in range(nk):
                        P.add("dve" if kc % 2 == 0 else "pool", lambda e, kc=kc: (
                            e.scalar_tensor_tensor(dst[:, kc, :], src[:, kc, :], g[:, kc:kc + 1], rstd[:], ALU.mult, ALU.mult)
                            if False else e.scalar_tensor_tensor(dst[:, kc, :], src[:, kc, :], g[:, kc:kc + 1], rstd[:], ALU.mult, ALU.mult)),
                            reads=[key, "rstd", "gq", "gkv"], writes=[dkey]) if False else None
                        P.add("dve", lambda e, kc=kc: e.scalar_tensor_tensor(
                            dst[:, kc, :], src[:, kc, :], g[:, kc:kc + 1], rstd[:], ALU.mult, ALU.mult),
                            reads=[key, "rstd", "gq", "gkv"], writes=[dkey])

                rmsnorm(cq, "cq", 4, gq, cqn, "cqn", 512.0)
                rmsnorm(ckv, "ckv", 2, gkv, ckvn, "ckvn", 256.0)
                for j in range(6):
                    b = self.nb()
                    for kc in range(4):
                        P.add("pe", lambda e, b=b, kc=kc, j=j: e.matmul(
                            ps[b][:], wuq[:, kc, j * 128:(j + 1) * 128], cqn[:, kc, :], start=(kc == 0), stop=(kc == 3)),
                            reads=["wuq", "cqn"], writes=[("ps", b)])
                    zs = cnt["z"] % 2
                    cnt["z"] += 1
                    b2 = self.nb()
                    P.add("act", lambda e, b=b, zs=zs: e.activation(zb[zs][:], ps[b][:], AF.Copy),
                          reads=[("ps", b)], writes=[("zb", zs)])
                    P.add("pe", lambda e, b2=b2, zs=zs, j=j: e.matmul(ps[b2][:], pq[:, j, :], zb[zs][:], start=True, stop=True),
                          reads=[("zb", zs), "pq"], writes=[("ps", b2)])
                    P.add("dve", lambda e, b=b, zs=zs, j=j: e.tensor_tensor(t1[zs][:], ps[b][:], cosq[:, j, :], ALU.mult),
                          reads=[("ps", b), "cosq"], writes=[("t1", zs)])
                    P.add("dve", lambda e, b2=b2, zs=zs, j=j: e.tensor_tensor(t2[zs][:], ps[b2][:], sinq[:, j, :], ALU.mult),
                          reads=[("ps", b2), "sinq"], writes=[("t2", zs)])
                    qs = (cnt["q"] // 3) % 2
                    cnt["q"] += 1
                    P.add("pool", lambda e, zs=zs, qs=qs, j=j: e.tensor_tensor(qstg[qs][:, j % 3, :], t1[zs][:], t2[zs][:], ALU.add),
                          reads=[("t1", zs), ("t2", zs)], writes=[("qstg", qs)])
                    if j % 3 == 2:
                        j0 = j - 2
                        dst = S["qmT"][j0 * 128:(j0 + 3) * 128, t0:t0 + TT].rearrange("(c p) t -> p c t", p=128)
                        P.add("sp", lambda e, dst=dst, qs=qs: e.dma_start(out=dst, in_=qstg[qs][:]),
                              reads=[("qstg", qs)], dma=True, semkey="st_q%d" % qs)
                for h in range(8):
                    b = self.nb()
                    for kc in range(2):
                        P.add("pe", lambda e, b=b, kc=kc, h=h: e.matmul(
                            ps[b][:], wkv[:, kc, h * 128:(h + 1) * 128], ckvn[:, kc, :], start=(kc == 0), stop=(kc == 1)),
                            reads=["wkv", "ckvn"], writes=[("ps", b)])
                    ks = (cnt["k"] // 4) % 2
                    cnt["k"] += 1
                    if h % 2 == 0:
                        P.add("act", lambda e, b=b, ks=ks, h=h: e.activation(kstg[ks][:, h % 4, :], ps[b][:], AF.Copy),
                              reads=[("ps", b)], writes=[("kstg", ks)])
                    else:
                        P.add("dve", lambda e, b=b, ks=ks, h=h: e.tensor_copy(kstg[ks][:, h % 4, :], ps[b][:]),
                              reads=[("ps", b)], writes=[("kstg", ks)])
                    dst = S["kmT"][h * 96:h * 96 + 64, t0:t0 + TT]
                    P.add("sp", lambda e, dst=dst, ks=ks, h=h: e.dma_start(out=dst, in_=kstg[ks][:64, h % 4, :]),
                          reads=[("kstg", ks)], dma=True, semkey="st_k%d" % ks)
                zs = cnt["z"] % 2
                cnt["z"] += 1
                b2 = self.nb()
                P.add("act", lambda e, zs=zs: e.activation(zb[zs][:], KR[:], AF.Copy), reads=["kr"], writes=[("zb", zs)])
                P.add("pe", lambda e, b2=b2, zs=zs: e.matmul(ps[b2][:], pk[:], zb[zs][:], start=True, stop=True),
                      reads=[("zb", zs), "pk"], writes=[("ps", b2)])
                P.add("dve", lambda e, zs=zs: e.tensor_tensor(t1[zs][:], KR[:], cosk[:], ALU.mult),
                      reads=["kr", "cosk"], writes=[("t1", zs)])
                P.add("dve", lambda e, b2=b2, zs=zs: e.tensor_tensor(t2[zs][:], ps[b2][:], sink[:], ALU.mult),
                      reads=[("ps", b2), "sink"], writes=[("t2", zs)])
                P.add("pool", lambda e, zs=zs: e.tensor_tensor(krr[:], t1[zs][:], t2[zs][:], ALU.add),
                      reads=[("t1", zs), ("t2", zs)], writes=["krr"])
                for h in range(8):
                    dst = S["kmT"][h * 96 + 64:h * 96 + 96, t0:t0 + TT]
                    P.add("sp", lambda e, dst=dst: e.dma_start(out=dst, in_=krr[:32, :]), reads=["krr"], dma=True, semkey="st_kr")
                for s_ in range(4):
                    b = self.nb()
                    for kc in range(2):
                        P.add("pe", lambda e, b=b, kc=kc, s_=s_: e.matmul(
                            ps[b][:], ckvn[:, kc, s_ * 128:(s_ + 1) * 128], wv[:, kc, :, :].rearrange("p h c -> p (h c)"),
                            start=(kc == 0), stop=(kc == 1)), reads=["wv", "ckvn"], writes=[("ps", b)])
                    if s_ % 2 == 0:
                        P.add("act", lambda e, b=b, s_=s_: e.activation(vstg[:, s_, :], ps[b][:], AF.Copy),
                              reads=[("ps", b)], writes=["vstg"])
                    else:
                        P.add("dve", lambda e, b=b, s_=s_: e.tensor_copy(vstg[:, s_, :], ps[b][:]),
                              reads=[("ps", b)], writes=["vstg"])
                dst = S["vm"][t0:t0 + TT, :].rearrange("(s p) n -> p s n", p=128)
                P.add("sp", lambda e, dst=dst: e.dma_start(out=dst, in_=vstg[:]), reads=["vstg"], dma=True, semkey="st_v")
            P.flush()
        import os
        if "B2" in os.environ.get("SKIP", ""):
            return
        self.attention(T, nheads=8, parts=96, dv=64, scale=scale,
                       kT_rows=lambda h, c: (S["kmT"], h * 96), qT_rows=lambda h, c: (S["qmT"], h * 96),
                       v_src=lambda h: (S["vm"], h * 64), ncomp=1, out_row0=768, l=l)

    def attention(self, T, nheads, parts, dv, scale, kT_rows, qT_rows, v_src, ncomp, out_row0, l):
        nc, P, S, C, W, ps = self.nc, self.P, self.S, self.C, self.W, self.ps
        NT, NK = T // TT, T // 128
        lambda_init = 0.8 - 0.6 * np.exp(-0.3 * l)
        with contextlib.ExitStack() as st:
            sb = lambda name, shape, dt: st.enter_context(nc.sbuf_tensor("At%d_" % self.uid() + name, shape, dt))
            onesb = sb("ones", [128, 128], BF16)
            nrows = parts
            KT = [sb("KT%d" % i, [128, T], BF16) for i in range(2)]
            V = [sb("V%d" % i, [128, NK, dv], BF16) for i in range(2)]
            QT = [[sb("QT%d_%d" % (i, c), [128, TT], BF16) for c in range(ncomp)] for i in range(2)]
            for i in range(2):
                P.add("pool", lambda e, i=i: e.memset(KT[i][:], 0.0), writes=[("KT", i)])
                for c in range(ncomp):
                    P.add("pool", lambda e, i=i, c=c: e.memset(QT[i][c][:], 0.0), writes=[("QT", i, c)])
            pT = [sb("pT%d" % i, [128, TT], BF16) for i in range(3)]
            rD = [sb("rD%d" % i, [dv, TT], F32) for i in range(2)]
            ostg = [sb("ostg%d" % i, [dv, TT], BF16) for i in range(2)]
            P.add("pool", lambda e: e.dma_start(out=onesb[:], in_=C["ones"]), writes=["ones"], dma=True, semkey="L_ones")
            if ncomp == 2:
                lq = [sb("lq%d" % i, [128, 64], F32) for i in range(4)]
                lp = [sb("lp%d" % i, [128, 64], F32) for i in range(2)]
                lsum = sb("lsum", [128, 2], F32)
                lexp = sb("lexp", [128, 2], F32)
                neglam = sb("neglam", [128, 1], F32)
                gs0 = sb("gs0", [128, 1], F32)
                gs = sb("gs", [128, 1], F32)
                o1 = sb("o1", [128, TT], F32)
                o2 = sb("o2", [128, TT], F32)
                oo = sb("oo", [128, TT], F32)
                osq = sb("osq", [128, TT], BF16)
                ort = sb("ort", [128, TT], F32)
                orstd = sb("orstd", [128, TT], F32)
                for i, nm in enumerate(["df_lq1", "df_lk1", "df_lq2", "df_lk2"]):
                    P.add("sp", lambda e, i=i, nm=nm: e.dma_start(out=lq[i][:], in_=W[nm][l].partition_broadcast(128)),
                          writes=[("lq", i)], dma=True, semkey="L_lq%d" % i)
                P.add("sp", lambda e: e.dma_start(out=gs0[:], in_=W["df_subln"][l].rearrange("(p o) -> p o", o=1)),
                      writes=["gs0"], dma=True, semkey="L_gs0")
                for j in range(2):
                    P.add("dve", lambda e, j=j: e.tensor_tensor(lp[j][:], lq[2 * j][:], lq[2 * j + 1][:], ALU.mult),
                          reads=[("lq", 2 * j), ("lq", 2 * j + 1)], writes=[("lp", j)])
                    P.add("pool" if False else "dve", lambda e, j=j: e.tensor_reduce(lsum[:, j:j + 1], lp[j][:], AX.X, ALU.add),
                          reads=[("lp", j)], writes=["lsum"])
                P.add("act", lambda e: e.activation(lexp[:], lsum[:], AF.Exp), reads=["lsum"], writes=["lexp"])
                P.add("dve", lambda e: e.scalar_tensor_tensor(neglam[:], lexp[:, 1:2], -float(lambda_init), lexp[:, 0:1],
                                                              ALU.add, ALU.subtract), reads=["lexp"], writes=["neglam"])
                P.add("pool", lambda e: e.tensor_scalar(gs[:], gs0[:], float(1.0 - lambda_init), None, ALU.mult),
                      reads=["gs0"], writes=["gs"])
            cnt = {"p": 0, "o": 0}
            acc_banks = [3, 4, 5, 6] if ncomp == 2 else [3, 4, 5, 6]
            for h in range(nheads):
                hs = h % 2
                src, r0 = kT_rows(h, 0)
                nk_rows = nrows * ncomp
                P.add("sp", lambda e, hs=hs, src=src, r0=r0, nk_rows=nk_rows: e.dma_start(out=KT[hs][:nk_rows, :], in_=src[r0:r0 + nk_rows, 0:T]),
                      writes=[("KT", hs)], dma=True, semkey="L_KT%d" % hs)
                vsrc, c0 = v_src(h)
                P.add("sp", lambda e, hs=hs, vsrc=vsrc, c0=c0: e.dma_start(
                    out=V[hs][:], in_=vsrc[0:T, c0:c0 + dv].rearrange("(c p) d -> p c d", p=128)),
                    writes=[("V", hs)], dma=True, semkey="L_V%d" % hs)
                for qi in range(NT):
                    t0 = qi * TT
                    qs = (h * NT + qi) % 2
                    for c in range(ncomp):
                        src, r0 = qT_rows(h, c)
                        rr = r0 + c * nrows * (ncomp - 1)
                        pr = c * nrows * (ncomp - 1)
                        P.add("sp", lambda e, qs=qs, c=c, src=src, rr=rr, pr=pr, t0=t0: e.dma_start(
                            out=QT[qs][c][pr:pr + nrows, :], in_=src[rr:rr + nrows, t0:t0 + TT]),
                            writes=[("QT", qs, c)], dma=True, semkey="L_QT%d%d" % (qs, c))
                    accs = []
                    for c in range(ncomp):
                        if ncomp == 1:
                            bo, bd = acc_banks[2 * (cnt["o"] % 2)], acc_banks[2 * (cnt["o"] % 2) + 1]
                        else:
                            bo, bd = acc_banks[2 * c], acc_banks[2 * c + 1]
                        accs.append((bo, bd))
                        slots = []
                        for kc in range(NK):
                            slots.append(cnt["p"] % 3)
                            cnt["p"] += 1

                        def score(kc, hs=hs, c=c, qs=qs):
                            b = slots[kc]
                            P.add("pe", lambda e, b=b, hs=hs, c=c, kc=kc, qs=qs: e.matmul(
                                ps[b][:], KT[hs][:, kc * 128:(kc + 1) * 128], QT[qs][c][:], start=True, stop=True),
                                reads=[("KT", hs), ("QT", qs, c)], writes=[("ps", b)])

                        score(0)
                        for kc in range(NK):
                            b = slots[kc]
                            pslot = slots[kc]
                            P.add("act", lambda e, b=b, pslot=pslot: e.activation(pT[pslot][:], ps[b][:], AF.Exp, scale=float(scale)),
                                  reads=[("ps", b)], writes=[("pT", pslot)])
                            if kc + 1 < NK:
                                score(kc + 1)
                            P.add("pe", lambda e, bo=bo, hs=hs, kc=kc, pslot=pslot: e.matmul(
                                ps[bo][:dv, :], V[hs][:, kc, :], pT[pslot][:], start=(kc == 0), stop=(kc == NK - 1)),
                                reads=[("V", hs), ("pT", pslot)], writes=[("ps", bo)])
                            P.add("pe", lambda e, bd=bd, kc=kc, pslot=pslot: e.matmul(
                                ps[bd][:dv, :], onesb[:, :dv], pT[pslot][:], start=(kc == 0), stop=(kc == NK - 1)),
                                reads=["ones", ("pT", pslot)], writes=[("ps", bd)])
                    os_ = cnt["o"] % 2
                    cnt["o"] += 1
                    if ncomp == 1:
                        bo, bd = accs[0]
                        P.add("dve", lambda e, bd=bd, os_=os_: e.reciprocal(rD[os_][:], ps[bd][:dv, :]),
                              reads=[("ps", bd)], writes=[("rD", os_)])
                        P.add("dve", lambda e, bo=bo, os_=os_: e.tensor_tensor(ostg[os_][:], ps[bo][:dv, :], rD[os_][:], ALU.mult),
                              reads=[("ps", bo), ("rD", os_)], writes=[("ostg", os_)])
                    else:
                        (bo1, bd1), (bo2, bd2) = accs
                        P.add("dve", lambda e, bd1=bd1: e.reciprocal(rD[0][:], ps[bd1][:]), reads=[("ps", bd1)], writes=[("rD", 0)])
                        P.add("dve", lambda e, bd2=bd2: e.reciprocal(rD[1][:], ps[bd2][:]), reads=[("ps", bd2)], writes=[("rD", 1)])
                        P.add("dve", lambda e, bo1=bo1: e.tensor_tensor(o1[:], ps[bo1][:], rD[0][:], ALU.mult),
                              reads=[("ps", bo1), ("rD", 0)], writes=["o1"])
                        P.add("dve", lambda e, bo2=bo2: e.tensor_tensor(o2[:], ps[bo2][:], rD[1][:], ALU.mult),
                              reads=[("ps", bo2), ("rD", 1)], writes=["o2"])
                        P.add("dve", lambda e: e.scalar_tensor_tensor(oo[:], o2[:], neglam[:], o1[:], ALU.mult, ALU.add),
                              reads=["o1", "o2", "neglam"], writes=["oo"])
                        P.add("act", lambda e: e.activation(osq[:], oo[:], AF.Square), reads=["oo"], writes=["osq"])
                        bs = 7
                        P.add("pe", lambda e: e.matmul(ps[bs][:], onesb[:], osq[:], start=True, stop=True),
                              reads=["osq", "ones"], writes=[("ps", bs)])
                        P.add("act", lambda e: e.activation(ort[:], ps[bs][:], AF.Sqrt, bias=DF_EPS, scale=1.0 / 128.0),
                              reads=[("ps", bs)], writes=["ort"])
                        P.add("dve", lambda e: e.reciprocal(orstd[:], ort[:]), reads=["ort"], writes=["orstd"])
                        P.add("dve", lambda e, os_=os_: e.scalar_tensor_tensor(ostg[os_][:], oo[:], gs[:], orstd[:], ALU.mult, ALU.mult),
                              reads=["oo", "gs", "orstd"], writes=[("ostg", os_)])
                    dst = S["oT"][out_row0 + h * dv:out_row0 + (h + 1) * dv, t0:t0 + TT]
                    P.add("sp", lambda e, dst=dst, os_=os_: e.dma_start(out=dst, in_=ostg[os_][:]),
                          reads=[("ostg", os_)], dma=True, semkey="st_o%d" % os_)
            P.flush()

    def phase_diff(self, l, T):
        S = self.S
        self.attention(T, nheads=6, parts=64, dv=128, scale=64.0 ** -0.5,
                       kT_rows=lambda h, c: (S["dfqkT"], (6 + h) * 128), qT_rows=lambda h, c: (S["dfqkT"], h * 128),
                       v_src=lambda h: (S["vdf"], h * 128), ncomp=2, out_row0=1280, l=l)

    def phase_rwkv(self, l, T):
        nc, P, S, C, W, ps = self.nc, self.P, self.S, self.C, self.W, self.ps
        NCH = T // 128
        CL = -float(np.exp(-0.5))
        u = self.uid()
        with contextlib.ExitStack() as st:
            sb = lambda name, shape, dt: st.enter_context(nc.sbuf_tensor("D%d_" % u + name, shape, dt))
            f768 = lambda name: sb(name, [128, 768], F32)
            b768 = lambda name: sb(name, [128, 768], BF16)
            mup, mun = sb("mup", [128, RW_IN], F32), sb("mun", [128, RW_IN], F32)
            kkbc, kabc, omka, rkbc, lgbc, lbbc = [f768(n) for n in ("kkbc", "kabc", "omka", "rkbc", "lgbc", "lbbc")]
            _w0 = f768("w0bc"); w0bc = [_w0, _w0]
            _a0 = f768("a0bc"); a0bc = [_a0, _a0]
            _w2 = sb("w2b", [128, 768], BF16); w2b = [_w2, _w2]
            _a2 = sb("a2b", [128, 768], BF16); a2b = [_a2, _a2]
            g2b = sb("g2b", [128, 768], BF16)
            _mk = sb("MK2", [128, 2, 2, 128], F32); MK2 = [_mk, _mk]
            _mn = sb("MN4", [128, 4, 128], F32); MN4 = [_mn, _mn]
            _tr = sb("TRI", [128, 128], F32); TRI = [_tr, _tr]
            onesf = sb("onesf", [128, 128], F32)
            idb = sb("idb", [128, 128], BF16)
            zc, zp, zn = [sb(n, [128, RW_IN], F32) for n in ("zc", "zp", "zn")]
            lorab = sb("lorab", [128, 256], BF16)
            lT = sb("lT", [128, 384], BF16)
            Ssg, a_, g_, kk, akk, kd, tA, tB, tC, E1, E2, bon, y_ = [f768(n) for n in (
                "Ssg", "a_", "g_", "kk", "akk", "kd", "tA", "tB", "tC", "E1", "E2", "bon", "y_")]
            yf, bf_ = E1, E2
            s12 = sb("s12", [128, 64], F32)
            gC = sb("gC", [64, 24], F32)
            Rt, Kt, At, Bt, Kcb, Bcb, Vb, Wb, Ub, ob = [b768(n) for n in ("Rt", "Kt", "At", "Bt", "Kcb", "Bcb", "Vb", "Wb", "Ub", "ob")]
            ART = sb("ART", [128, 12, 2, 128], BF16)
            KBT = sb("KBT", [128, 12, 2, 128], BF16)
            AKRK = sb("AKRK", [128, 12, 2, 128], BF16)
            ABRB = sb("ABRB", [128, 12, 2, 128], BF16)
            Pn = [sb("Pn%d" % i, [128, 12, 128], BF16) for i in range(2)]
            PnT = [sb("PnT%d" % i, [128, 12, 128], BF16) for i in range(2)]
            TTb = [sb("TTb%d" % i, [128, 12, 128], BF16) for i in range(2)]
            Hf = sb("Hf", [64, 12, 64], F32)
            Hb = sb("Hb", [128, 12, 64], BF16)
            ost = sb("ost", [128, 6, 128], BF16)

            def ld(eng, out, in_, key):
                P.add(eng, lambda e: e.dma_start(out=out, in_=in_), writes=[key], dma=True, semkey="L_" + key)

            def TT_(eng, out, a, b, op, rd, wr):
                P.add(eng, lambda e: e.tensor_tensor(out, a, b, op), reads=rd, writes=wr)

            def ACT_(out, in_, func, rd, wr, **kw):
                P.add("act", lambda e: e.activation(out, in_, func, **kw), reads=rd, writes=wr)

            ld("sp", mup[:], W["shift_prev"][l].partition_broadcast(128), "mup")
            ld("sp", mun[:], W["shift_next"][l].partition_broadcast(128), "mun")
            ld("sp", kkbc[:], W["rw_k_k"][l].partition_broadcast(128), "kkbc")
            ld("sp", kabc[:], W["rw_k_a"][l].partition_broadcast(128), "kabc")
            ld("sp", rkbc[:], W["rw_r_k"][l].partition_broadcast(128), "rkbc")
            ld("sp", lgbc[:], W["rw_lnx_g"][l].partition_broadcast(128), "lgbc")
            ld("sp", lbbc[:], W["rw_lnx_b"][l].partition_broadcast(128), "lbbc")
            P.add("pool", lambda e: e.tensor_scalar(omka[:], kabc[:], -1.0, 1.0, ALU.mult, ALU.add), reads=["kabc"], writes=["omka"])
            def load_dir_consts(d):
                ld("sp", w0bc[d][:], W["rw_w0"][l, d].partition_broadcast(128), "w0bc%d" % d)
                ld("sp", a0bc[d][:], W["rw_a0"][l, d].partition_broadcast(128), "a0bc%d" % d)
                ld("pool", w2b[d][:64, :], W["rw_w2"][l, d], "w2b%d" % d)
                ld("pool", a2b[d][:64, :], W["rw_a2"][l, d], "a2b%d" % d)
                strict, incl, nmask = ("m_su", "m_iu", "m_sl") if d == 0 else ("m_sl", "m_il", "m_su")
                for hh in range(2):
                    ld("sp", MK2[d][:, hh, 0, :], C[strict], "MK2_%d" % d)
                    ld("sp", MK2[d][:, hh, 1, :], C[incl], "MK2_%d" % d)
                for hh in range(4):
                    ld("sp", MN4[d][:, hh, :], C[nmask], "MN4_%d" % d)
                ld("sp", TRI[d][:], C[incl], "TRI%d" % d)
            ld("pool", g2b[:], W["rw_g2"][l], "g2b")
            for (tl, nm) in ((_w2, "w2b0"), (_a2, "a2b0"), (lT, "lT"), (ART, "ART"), (KBT, "KBT")):
                P.add("pool", lambda e, tl=tl: e.memset(tl[:], 0.0), writes=[nm])
            P.flush()
            ld("sp", onesf[:], C["ones"], "onesf")
            ld("pool", idb[:], C["ident"], "idb")

            for d in range(2):
                load_dir_consts(d)
                P.add("pool", lambda e: e.memset(Hf[:], 0.0), writes=["Hf"])
                P.add("pool", lambda e: e.memset(Hb[:], 0.0), writes=["Hb"])
                order = list(range(NCH)) if d == 0 else list(range(NCH - 1, -1, -1))
                for ci in order:
                    t0 = ci * 128
                    ld("sp", zc[:], S["zrw"][t0:t0 + 128, :], "zc")
                    if t0 == 0:
                        P.add("pool", lambda e: e.memset(zp[:], 0.0), writes=["zp"])
                        ld("sp", zp[1:128, :], S["zrw"][0:127, :], "zp")
                    else:
                        ld("sp", zp[:], S["zrw"][t0 - 1:t0 + 127, :], "zp")
                    if t0 + 128 == T:
                        P.add("pool", lambda e: e.memset(zn[:], 0.0), writes=["zn"])
                        ld("sp", zn[0:127, :], S["zrw"][t0 + 1:t0 + 128, :], "zn")
                    else:
                        ld("sp", zn[:], S["zrw"][t0 + 1:t0 + 129, :], "zn")
                    TT_("pool", zp[:], zp[:], zc[:], ALU.subtract, ["zp", "zc"], ["zp"])
                    TT_("pool", zp[:], zp[:], mup[:], ALU.mult, ["zp", "mup"], ["zp"])
                    TT_("dve", zn[:], zn[:], zc[:], ALU.subtract, ["zn", "zc"], ["zn"])
                    TT_("dve", zn[:], zn[:], mun[:], ALU.mult, ["zn", "mun"], ["zn"])
                    TT_("dve", zc[:], zc[:], zp[:], ALU.add, ["zp", "zc"], ["zc"])
                    TT_("dve", zc[:], zc[:], zn[:], ALU.add, ["zn", "zc"], ["zc"])
                    r_, k_, v_ = zc[:, 0:768], zc[:, 768:1536], zc[:, 1536:2304]
                    wl = zc[:, 2304 + d * 64:2304 + (d + 1) * 64]
                    al = zc[:, 2432 + d * 64:2432 + (d + 1) * 64]
                    gl = zc[:, 2560:2688]
                    ACT_(lorab[:, 0:64], wl, AF.Tanh, ["zc"], ["lorab"])
                    ACT_(lorab[:, 64:128], al, AF.Copy, ["zc"], ["lorab"])
                    ACT_(lorab[:, 128:256], gl, AF.Sigmoid, ["zc"], ["lorab"])
                    bt = self.nb()
                    pv = ps[bt][:].bitcast(BF16)
                    P.add("pe", lambda e, pv=pv: e.transpose(pv[:64, 0:128], lorab[:, 0:64], idb[:]), reads=["lorab", "idb"], writes=[("ps", bt)])
                    P.add("pe", lambda e, pv=pv: e.transpose(pv[:64, 128:256], lorab[:, 64:128], idb[:]), reads=["lorab", "idb"], writes=[("ps", bt)])
                    P.add("pe", lambda e, pv=pv: e.transpose(pv[:, 256:384], lorab[:, 128:256], idb[:]), reads=["lorab", "idb"], writes=[("ps", bt)])
                    ACT_(lT[:64, 0:256], pv[:64, 0:256], AF.Copy, [("ps", bt)], ["lT"])
                    P.add("dve", lambda e, pv=pv: e.tensor_copy(lT[:, 256:384], pv[:, 256:384]), reads=[("ps", bt)], writes=["lT"])
                    halves = ((0, 512), (512, 256))
                    bw = [self.nb(), self.nb()]
                    ba = [self.nb(), self.nb()]
                    for hi_, (c0, cw) in enumerate(halves):
                        P.add("pe", lambda e, hi_=hi_, c0=c0, cw=cw, bw=bw, d=d: e.matmul(ps[bw[hi_]][:, :cw], lT[:, 0:128], w2b[d][:, c0:c0 + cw], start=True, stop=True),
                              reads=["lT", "w2b%d" % d], writes=[("ps", bw[hi_])])
                        P.add("pe", lambda e, hi_=hi_, c0=c0, cw=cw, ba=ba, d=d: e.matmul(ps[ba[hi_]][:, :cw], lT[:, 128:256], a2b[d][:, c0:c0 + cw], start=True, stop=True),
                              reads=["lT", "a2b%d" % d], writes=[("ps", ba[hi_])])
                    for hi_, (c0, cw) in enumerate(halves):
                        TT_("dve", Ssg[:, c0:c0 + cw], ps[bw[hi_]][:, :cw], w0bc[d][:, c0:c0 + cw], ALU.add, [("ps", bw[hi_]), "w0bc%d" % d], ["Ssg"])
                        TT_("dve", a_[:, c0:c0 + cw], ps[ba[hi_]][:, :cw], a0bc[d][:, c0:c0 + cw], ALU.add, [("ps", ba[hi_]), "a0bc%d" % d], ["a_"])
                    ACT_(Ssg[:], Ssg[:], AF.Sigmoid, ["Ssg"], ["Ssg"])
                    ACT_(a_[:], a_[:], AF.Sigmoid, ["a_"], ["a_"])
                    if d == 1:
                        bg = [self.nb(), self.nb()]
                        for hi_, (c0, cw) in enumerate(halves):
                            P.add("pe", lambda e, hi_=hi_, c0=c0, cw=cw, bg=bg: e.matmul(ps[bg[hi_]][:, :cw], lT[:, 256:384], g2b[:, c0:c0 + cw], start=True, stop=True),
                                  reads=["lT", "g2b"], writes=[("ps", bg[hi_])])
                            ACT_(g_[:, c0:c0 + cw], ps[bg[hi_]][:, :cw], AF.Copy, [("ps", bg[hi_])], ["g_"])
                    h3 = lambda ap: ap.rearrange("p (h n) -> p h n", n=64)
                    bc12 = lambda ap: ap.rearrange("p (h o) -> p h o", o=1).to_broadcast([128, 12, 64])
                    TT_("pool", kk[:], k_, kkbc[:], ALU.mult, ["zc", "kkbc"], ["kk"])
                    TT_("pool", tA[:], kk[:], kk[:], ALU.mult, ["kk"], ["tA"])
                    P.add("dve", lambda e: e.tensor_reduce(s12[:, 0:12], h3(tA[:]), AX.X, ALU.add), reads=["tA"], writes=["s12a"])
                    ACT_(s12[:, 0:12], s12[:, 0:12], AF.Sqrt, ["s12a"], ["s12a"], bias=1e-12)
                    P.add("dve", lambda e: e.reciprocal(s12[:, 0:12], s12[:, 0:12]), reads=["s12a"], writes=["s12a"])
                    TT_("pool", h3(kk[:]), h3(kk[:]), bc12(s12[:, 0:12]), ALU.mult, ["kk", "s12a"], ["kk"])
                    TT_("pool", akk[:], a_[:], kk[:], ALU.mult, ["a_", "kk"], ["akk"])
                    TT_("pool", tA[:], a_[:], kabc[:], ALU.mult, ["a_", "kabc"], ["tA"])
                    TT_("pool", tA[:], tA[:], omka[:], ALU.add, ["tA", "omka"], ["tA"])
                    TT_("pool", kd[:], k_, tA[:], ALU.mult, ["zc", "tA"], ["kd"])
                    bc_ = [self.nb(), self.nb()]
                    for hi_, (c0, cw) in enumerate(halves):
                        P.add("pe", lambda e, hi_=hi_, c0=c0, cw=cw, bc_=bc_, d=d: e.matmul(ps[bc_[hi_]][:, :cw], TRI[d][:], Ssg[:, c0:c0 + cw], start=True, stop=True),
                              reads=["Ssg", "TRI%d" % d], writes=[("ps", bc_[hi_])])
                    for hi_, (c0, cw) in enumerate(halves):
                        kps = ("ps", bc_[hi_])
                        ACT_(E1[:, c0:c0 + cw], ps[bc_[hi_]][:, :cw], AF.Exp, [kps], ["E1"], scale=CL)
                        ACT_(E2[:, c0:c0 + cw], ps[bc_[hi_]][:, :cw], AF.Exp, [kps], ["E2"], scale=-CL)
                        TT_("dve", tB[:, c0:c0 + cw], ps[bc_[hi_]][:, :cw], Ssg[:, c0:c0 + cw], ALU.subtract, [kps, "Ssg"], ["tB"])
                        P.add("dve", lambda e, hi_=hi_, c0=c0, cw=cw, bc_=bc_: e.tensor_copy(tC[:, c0:c0 + cw], ps[bc_[hi_]][:, :cw]), reads=[kps], writes=["tC"])
                    ACT_(tB[:], tB[:], AF.Exp, ["tB"], ["tB"], scale=CL)
                    bt_ = [self.nb(), self.nb()]
                    for hi_, (c0, cw) in enumerate(halves):
                        P.add("pe", lambda e, hi_=hi_, c0=c0, cw=cw, bt_=bt_: e.matmul(ps[bt_[hi_]][:, :cw], onesf[:], Ssg[:, c0:c0 + cw], start=True, stop=True),
                              reads=["Ssg", "onesf"], writes=[("ps", bt_[hi_])])
                        TT_("dve", tC[:, c0:c0 + cw], ps[bt_[hi_]][:, :cw], tC[:, c0:c0 + cw], ALU.subtract, [("ps", bt_[hi_]), "tC"], ["tC"])
                    ACT_(tC[:], tC[:], AF.Exp, ["tC"], ["tC"], scale=CL)
                    bgc = self.nb()
                    for h in range(12):
                        P.add("pe", lambda e, h=h, bgc=bgc: e.matmul(ps[bgc][:64, 2 * h:2 * h + 2], Ssg[:, h * 64:(h + 1) * 64], onesf[:, 0:2], start=True, stop=True),
                              reads=["Ssg", "onesf"], writes=[("ps", bgc)])
                    ACT_(gC[:], ps[bgc][:64, 0:24], AF.Exp, [("ps", bgc)], ["gC"], scale=CL)
                    TT_("pool", Rt[:], r_, E1[:], ALU.mult, ["zc", "E1"], ["Rt"])
                    TT_("pool", Kt[:], kd[:], E2[:], ALU.mult, ["kd", "E2"], ["Kt"])
                    TT_("pool", At[:], kk[:], tB[:], ALU.mult, ["kk", "tB"], ["At"])
                    P.add("dve", lambda e: e.scalar_tensor_tensor(Bt[:], akk[:], -1.0, E2[:], ALU.mult, ALU.mult), reads=["akk", "E2"], writes=["Bt"])
                    TT_("pool", Kcb[:], kd[:], tC[:], ALU.mult, ["kd", "tC"], ["Kcb"])
                    P.add("dve", lambda e: e.scalar_tensor_tensor(Bcb[:], akk[:], -1.0, tC[:], ALU.mult, ALU.mult), reads=["akk", "tC"], writes=["Bcb"])
                    ACT_(Vb[:], v_, AF.Copy, ["zc"], ["Vb"])
                    TT_("pool", tA[:], r_, kd[:], ALU.mult, ["zc", "kd"], ["tA"])
                    TT_("pool", tA[:], tA[:], rkbc[:], ALU.mult, ["tA", "rkbc"], ["tA"])
                    P.add("dve", lambda e: e.tensor_reduce(s12[:, 16:28], h3(tA[:]), AX.X, ALU.add), reads=["tA"], writes=["s12b"])
                    TT_("pool", h3(bon[:]), h3(v_), bc12(s12[:, 16:28]), ALU.mult, ["zc", "s12b"], ["bon"])
                    for (X0, X1, xk0, xk1, DST, dk_) in ((At, Rt, "At", "Rt", ART, "ART"), (Kt, Bt, "Kt", "Bt", KBT, "KBT")):
                        for j in range(3):
                            b = self.nb()
                            pv = ps[b][:].bitcast(BF16)
                            for hh in range(4):
                                h = 4 * j + hh
                                for wi, (X, xk) in enumerate(((X0, xk0), (X1, xk1))):
                                    off = (hh * 2 + wi) * 128
                                    P.add("pe", lambda e, pv=pv, off=off, X=X, h=h: e.transpose(pv[:64, off:off + 128], X[:, h * 64:(h + 1) * 64], idb[:]),
                                          reads=[xk, "idb"], writes=[("ps", b)])
                            dsl = DST[:64, 4 * j:4 * j + 4, :, :].rearrange("p h a t -> p (h a t)")
                            if j % 2 == 0:
                                ACT_(dsl, pv[:64, :], AF.Copy, [("ps", b)], [dk_])
                            else:
                                P.add("dve", lambda e, dsl=dsl, pv=pv: e.tensor_copy(dsl, pv[:64, :]), reads=[("ps", b)], writes=[dk_])
                    mk2 = MK2[d][:].rearrange("p h a t -> p (h a t)")
                    mn4 = MN4[d][:].rearrange("p h t -> p (h t)")
                    for (wi, DST, dk_) in ((0, AKRK, "AKRK"), (1, ABRB, "ABRB")):
                        for hp in range(6):
                            b = self.nb()
                            for hh in range(2):
                                h = 2 * hp + hh
                                P.add("pe", lambda e, b=b, hh=hh, h=h, wi=wi: e.matmul(
                                    ps[b][:, hh * 256:(hh + 1) * 256], KBT[:, h, wi, :], ART[:, h, :, :].rearrange("p a t -> p (a t)"),
                                    start=True, stop=True), reads=["KBT", "ART"], writes=[("ps", b)])
                            dsl = DST[:, 2 * hp:2 * hp + 2, :, :].rearrange("p h a t -> p (h a t)")
                            TT_("dve", dsl, ps[b][:], mk2, ALU.mult, [("ps", b), "MK2_%d" % d], [dk_])
                    for j in range(3):
                        b = self.nb()
                        for hh in range(4):
                            h = 4 * j + hh
                            P.add("pe", lambda e, b=b, hh=hh, h=h: e.matmul(
                                ps[b][:, hh * 128:(hh + 1) * 128], ART[:, h, 0, :], KBT[:, h, 1, :], start=True, stop=True),
                                reads=["KBT", "ART"], writes=[("ps", b)])
                        TT_("dve", Pn[0][:, 4 * j:4 * j + 4, :].rearrange("p h t -> p (h t)"), ps[b][:], mn4, ALU.mult,
                            [("ps", b), "MN4_%d" % d], ["Pn0"])
                    TT_("pool", TTb[0][:], ABRB[:, :, 0, :], idb[:].rearrange("p (o t) -> p o t", o=1).to_broadcast([128, 12, 128]), ALU.add,
                        ["ABRB", "idb"], ["TTb0"])
                    for k in range(6):
                        cur, nxt = k % 2, (k + 1) % 2
                        Pc = Pn[cur]
                        PTc = (lambda h: ABRB[:, h, 0, :]) if k == 0 else (lambda h, cur=cur: PnT[cur][:, h, :])
                        ptkey = "ABRB" if k == 0 else "PnT%d" % cur
                        for j in range(3):
                            b = self.nb()
                            for hh in range(4):
                                h = 4 * j + hh
                                P.add("pe", lambda e, b=b, hh=hh, h=h, Pc=Pc, PTc=PTc: e.matmul(
                                    ps[b][:, hh * 128:(hh + 1) * 128], PTc(h), Pc[:, h, :], start=True, stop=True),
                                    reads=[ptkey, "Pn%d" % cur], writes=[("ps", b)])
                            ACT_(Pn[nxt][:, 4 * j:4 * j + 4, :].rearrange("p h t -> p (h t)"), ps[b][:], AF.Copy, [("ps", b)], ["Pn%d" % nxt])
                        if k < 5:
                            for j in range(3):
                                b = self.nb()
                                for hh in range(4):
                                    h = 4 * j + hh
                                    P.add("pe", lambda e, b=b, hh=hh, h=h, Pc=Pc, PTc=PTc: e.matmul(
                                        ps[b][:, hh * 128:(hh + 1) * 128], Pc[:, h, :], PTc(h), start=True, stop=True),
                                        reads=[ptkey, "Pn%d" % cur], writes=[("ps", b)])
                                P.add("dve", lambda e, b=b, j=j, nxt=nxt: e.tensor_copy(
                                    PnT[nxt][:, 4 * j:4 * j + 4, :].rearrange("p h t -> p (h t)"), ps[b][:]),
                                    reads=[("ps", b)], writes=["PnT%d" % nxt])
                        for j in range(3):
                            b = self.nb()
                            for hh in range(4):
                                h = 4 * j + hh
                                P.add("pe", lambda e, b=b, hh=hh, h=h, nxt=nxt, cur=cur: e.matmul(
                                    ps[b][:, hh * 128:(hh + 1) * 128], Pn[nxt][:, h, :], TTb[cur][:, h, :], start=True, stop=True),
                                    reads=["Pn%d" % nxt, "TTb%d" % cur], writes=[("ps", b)])
                            TT_("dve", TTb[nxt][:, 4 * j:4 * j + 4, :].rearrange("p h t -> p (h t)"), ps[b][:],
                                TTb[cur][:, 4 * j:4 * j + 4, :].rearrange("p h t -> p (h t)"), ALU.add,
                                [("ps", b), "TTb%d" % cur], ["TTb%d" % nxt])
                    TTf = TTb[0]
                    hsl = lambda h: slice(h * 64, (h + 1) * 64)
                    bank_of = lambda bb, h: (bb[0], (h * 64)) if h < 8 else (bb[1], (h - 8) * 64)
                    bW = [self.nb(), self.nb()]
                    for h in range(12):
                        b, o = bank_of(bW, h)
                        P.add("pe", lambda e, b=b, o=o, h=h: e.matmul(ps[b][:, o:o + 64], ART[:, h, 0, :], Hb[:, h, :], start=True, stop=False),
                              reads=["ART", "Hb"], writes=[("ps", b)])
                        P.add("pe", lambda e, b=b, o=o, h=h: e.matmul(ps[b][:, o:o + 64], AKRK[:, h, 0, :], Vb[:, hsl(h)], start=False, stop=True),
                              reads=["AKRK", "Vb"], writes=[("ps", b)])
                    ACT_(Wb[:, 0:512], ps[bW[0]][:], AF.Copy, [("ps", bW[0])], ["Wb"])
                    P.add("dve", lambda e, bW=bW: e.tensor_copy(Wb[:, 512:768], ps[bW[1]][:, 0:256]), reads=[("ps", bW[1])], writes=["Wb"])
                    bU = [self.nb(), self.nb()]
                    for h in range(12):
                        b, o = bank_of(bU, h)
                        P.add("pe", lambda e, b=b, o=o, h=h: e.matmul(ps[b][:, o:o + 64], TTf[:, h, :], Wb[:, hsl(h)], start=True, stop=True),
                              reads=["TTb0", "Wb"], writes=[("ps", b)])
                    ACT_(Ub[:, 0:512], ps[bU[0]][:], AF.Copy, [("ps", bU[0])], ["Ub"])
                    P.add("dve", lambda e, bU=bU: e.tensor_copy(Ub[:, 512:768], ps[bU[1]][:, 0:256]), reads=[("ps", bU[1])], writes=["Ub"])
                    bY = [self.nb(), self.nb()]
                    for h in range(12):
                        b, o = bank_of(bY, h)
                        P.add("pe", lambda e, b=b, o=o, h=h: e.matmul(ps[b][:, o:o + 64], ART[:, h, 1, :], Hb[:, h, :], start=True, stop=False),
                              reads=["ART", "Hb"], writes=[("ps", b)])
                        P.add("pe", lambda e, b=b, o=o, h=h: e.matmul(ps[b][:, o:o + 64], AKRK[:, h, 1, :], Vb[:, hsl(h)], start=False, stop=False),
                              reads=["AKRK", "Vb"], writes=[("ps", b)])
                        P.add("pe", lambda e, b=b, o=o, h=h: e.matmul(ps[b][:, o:o + 64], ABRB[:, h, 1, :], Ub[:, hsl(h)], start=False, stop=True),
                              reads=["ABRB", "Ub"], writes=[("ps", b)])
                    bH = [self.nb(), self.nb()]
                    for h in range(12):
                        b, o = bank_of(bH, h)
                        P.add("pe", lambda e, b=b, o=o, h=h: e.matmul(ps[b][:64, o:o + 64], Kcb[:, hsl(h)], Vb[:, hsl(h)], start=True, stop=False),
                              reads=["Kcb", "Vb"], writes=[("ps", b)])
                        P.add("pe", lambda e, b=b, o=o, h=h: e.matmul(ps[b][:64, o:o + 64], Bcb[:, hsl(h)], Ub[:, hsl(h)], start=False, stop=True),
                              reads=["Bcb", "Ub"], writes=[("ps", b)])
                    for h in range(12):
                        b, o = bank_of(bH, h)
                        P.add("dve", lambda e, b=b, o=o, h=h: e.scalar_tensor_tensor(
                            Hf[:, h, :], Hf[:, h, :], gC[:, 2 * h:2 * h + 1], ps[b][:64, o:o + 64], ALU.mult, ALU.add),
                            reads=["Hf", "gC", ("ps", b)], writes=["Hf"])
                    ACT_(Hb[:64, :, :], Hf[:], AF.Copy, ["Hf"], ["Hb"])
                    if d == 0:
                        ACT_(y_[:, 0:512], ps[bY[0]][:], AF.Copy, [("ps", bY[0])], ["y_"])
                        P.add("dve", lambda e, bY=bY: e.tensor_copy(y_[:, 512:768], ps[bY[1]][:, 0:256]), reads=[("ps", bY[1])], writes=["y_"])
                        P.add("sp", lambda e, t0=t0: e.dma_start(out=S["yrw"][t0:t0 + 128, :], in_=y_[:]), reads=["y_"], dma=True, semkey="st_y")
                        P.add("sp", lambda e, t0=t0: e.dma_start(out=S["bon"][t0:t0 + 128, :], in_=bon[:]), reads=["bon"], dma=True, semkey="st_bon")
                    else:
                        ld("sp", yf[:], S["yrw"][t0:t0 + 128, :], "E1")
                        ld("sp", bf_[:], S["bon"][t0:t0 + 128, :], "E2")
                        TT_("dve", y_[:, 0:512], ps[bY[0]][:], yf[:, 0:512], ALU.add, [("ps", bY[0]), "E1"], ["y_"])
                        TT_("dve", y_[:, 512:768], ps[bY[1]][:, 0:256], yf[:, 512:768], ALU.add, [("ps", bY[1]), "E1"], ["y_"])
                        P.add("dve", lambda e: e.tensor_reduce(s12[:, 32:44], h3(y_[:]), AX.X, ALU.add), reads=["y_"], writes=["s12c"])
                        P.add("pool", lambda e: e.tensor_scalar(s12[:, 32:44], s12[:, 32:44], -1.0 / 64, None, ALU.mult), reads=["s12c"], writes=["s12c"])
                        TT_("pool", h3(y_[:]), h3(y_[:]), bc12(s12[:, 32:44]), ALU.add, ["y_", "s12c"], ["y_"])
                        TT_("pool", tA[:], y_[:], y_[:], ALU.mult, ["y_"], ["tA"])
                        P.add("dve", lambda e: e.tensor_reduce(s12[:, 48:60], h3(tA[:]), AX.X, ALU.add), reads=["tA"], writes=["s12d"])
                        ACT_(s12[:, 48:60], s12[:, 48:60], AF.Sqrt, ["s12d"], ["s12d"], bias=GN_EPS, scale=1.0 / 64)
                        P.add("dve", lambda e: e.reciprocal(s12[:, 48:60], s12[:, 48:60]), reads=["s12d"], writes=["s12d"])
                        TT_("pool", h3(y_[:]), h3(y_[:]), bc12(s12[:, 48:60]), ALU.mult, ["y_", "s12d"], ["y_"])
                        TT_("pool", y_[:], y_[:], lgbc[:], ALU.mult, ["y_", "lgbc"], ["y_"])
                        TT_("pool", y_[:], y_[:], lbbc[:], ALU.add, ["y_", "lbbc"], ["y_"])
                        TT_("pool", y_[:], y_[:], bon[:], ALU.add, ["y_", "bon"], ["y_"])
                        TT_("pool", y_[:], y_[:], bf_[:], ALU.add, ["y_", "E2"], ["y_"])
                        TT_("dve", ob[:], y_[:], g_[:], ALU.mult, ["y_", "g_"], ["ob"])
                        b = self.nb()
                        pv = ps[b][:].bitcast(BF16)
                        for c in range(6):
                            P.add("pe", lambda e, pv=pv, c=c: e.transpose(pv[:, c * 128:(c + 1) * 128], ob[:, c * 128:(c + 1) * 128], idb[:]),
                                  reads=["ob", "idb"], writes=[("ps", b)])
                        ACT_(ost[:].rearrange("p c t -> p (c t)"), pv[:, 0:768], AF.Copy, [("ps", b)], ["ost"])
                        dst = S["oT"][0:768, t0:t0 + 128].rearrange("(c p) t -> p c t", p=128)
                        P.add("sp", lambda e, dst=dst: e.dma_start(out=dst, in_=ost[:]), reads=["ost"], dma=True, semkey="st_o")
                P.flush()

    def phase_ffn(self, l, xin, xout, T):
        nc, P, S, C, W, ps = self.nc, self.P, self.S, self.C, self.W, self.ps
        NT = T // TT
        with contextlib.ExitStack() as st0:
            u0 = self.uid()
            sb0 = lambda name, shape, dt: st0.enter_context(nc.sbuf_tensor("E%d_" % u0 + name, shape, dt))
            x1 = sb0("x1", [128, 4, D], F32)
            x1T = sb0("x1T", [128, 16, TT], BF16)
            gates = sb0("gates", [128, 4, NE], F32)
            idf = sb0("idf", [128, 128], F32)
            P.add("sp", lambda e: e.dma_start(out=idf[:], in_=C["ident"]), writes=["idf"], dma=True, semkey="L_idf")
            P.flush()
            for ti in range(NT):
                t0 = ti * TT
                with contextlib.ExitStack() as st:
                    u = self.uid()
                    sb = lambda name, shape, dt: st.enter_context(nc.sbuf_tensor("E%d_" % u + name, shape, dt))
                    oT = sb("oT", [128, 16, TT], BF16)
                    mT = sb("mT", [128, 16, TT], BF16)
                    sg = [sb("sg%d" % i, [128, 3, 4, TT], BF16) for i in range(2)]
                    wt = [sb("wt%d" % i, [128, 16, 512], BF16) for i in range(3)]
                    gbc = sb("gbc", [128, D], F32)
                    bbc = sb("bbc", [128, D], F32)
                    ta = [sb("ta%d" % i, [128, TT], F32) for i in range(2)]
                    tb = [sb("tb%d" % i, [128, TT], F32) for i in range(2)]
                    tcc = [sb("tc%d" % i, [128, TT], F32) for i in range(2)]
                    ts1 = [sb("ts%d" % i, [128, TT], F32) for i in range(2)]
                    xf = [sb("xf%d" % i, [128, TT], F32) for i in range(2)]
                    rw = sb("rw", [128, 16, NE], F32)
                    rb = sb("rb", [128, NE], F32)
                    junk = sb("junk", [128, D], BF16)
                    st4 = sb("st4", [128, 8], F32)
                    rt_ = sb("rt_", [128, 64], F32)
                    ld = lambda eng, out, in_, key: P.add(eng, lambda e: e.dma_start(out=out, in_=in_), writes=[key], dma=True, semkey="L_" + key)
                    ld("sp", oT[:], S["oT"][:, t0:t0 + TT].rearrange("(c p) t -> p c t", p=128), "oT")
                    ld("sp", x1[:], xin[t0:t0 + TT, :].rearrange("(s p) d -> p s d", p=128), "x1")
                    ld("sp", gbc[:], W["ln1_g"][l].partition_broadcast(128), "gbc")
                    ld("sp", bbc[:], W["ln1_b"][l].partition_broadcast(128), "bbc")
                    ld("sp", rw[:], W["router_w"].rearrange("(c p) e -> p c e", p=128), "rw")
                    ld("sp", rb[:], W["router_bias"][0].partition_broadcast(128), "rb")
                    wcnt = 0
                    for nbk in range(4):
                        ws = wcnt % 3
                        wcnt += 1
                        for (nm, k0, nk) in (("w_up_rw", 0, 6), ("w_up_mla", 6, 4), ("w_up_df", 10, 6)):
                            P.add("pool", lambda e, ws=ws, nm=nm, k0=k0, nk=nk, nbk=nbk: e.dma_start(
                                out=wt[ws][:, k0:k0 + nk, :],
                                in_=self.WB[nm][l].rearrange("(c p) n -> p c n", p=128)[:, :, nbk * 512:(nbk + 1) * 512]),
                                writes=[("wt", ws)], dma=True, semkey="L_wt%d" % ws)
                        sgs = nbk % 2
                        for br in range(3):
                            P.add("sp", lambda e, sgs=sgs, br=br, nbk=nbk: e.dma_start(
                                out=sg[sgs][:, br, :, :],
                                in_=S["sgT"][br * D + nbk * 512:br * D + (nbk + 1) * 512, t0:t0 + TT].rearrange("(c p) t -> p c t", p=128)),
                                writes=[("sg", sgs)], dma=True, semkey="L_sg%d" % sgs)
                        for c in range(4):
                            n = nbk * 4 + c
                            q = n % 2
                            bks = []
                            for br, (k0, nk) in enumerate(((0, 6), (6, 4), (10, 6))):
                                b = self.nb()
                                bks.append(b)
                                for kk in range(nk):
                                    kc = k0 + kk
                                    P.add("pe", lambda e, b=b, ws=ws, kc=kc, c=c, kk=kk, nk=nk: e.matmul(
                                        ps[b][:], wt[ws][:, kc, c * 128:(c + 1) * 128], oT[:, kc, :],
                                        start=(kk == 0), stop=(kk == nk - 1)),
                                        reads=[("wt", ws), "oT"], writes=[("ps", b)])
                            P.add("dve", lambda e, q=q, b=bks[0], sgs=sgs, c=c: e.tensor_tensor(ta[q][:], ps[b][:], sg[sgs][:, 0, c, :], ALU.mult),
                                  reads=[("ps", bks[0]), ("sg", sgs)], writes=[("ta", q)])
                            P.add("dve", lambda e, q=q, b=bks[1], sgs=sgs, c=c: e.tensor_tensor(tb[q][:], ps[b][:], sg[sgs][:, 1, c, :], ALU.mult),
                                  reads=[("ps", bks[1]), ("sg", sgs)], writes=[("tb", q)])
                            P.add("dve", lambda e, q=q, b=bks[2], sgs=sgs, c=c: e.tensor_tensor(tcc[q][:], ps[b][:], sg[sgs][:, 2, c, :], ALU.mult),
                                  reads=[("ps", bks[2]), ("sg", sgs)], writes=[("tc", q)])
                            P.add("pool", lambda e, q=q: e.tensor_tensor(ts1[q][:], ta[q][:], tb[q][:], ALU.add),
                                  reads=[("ta", q), ("tb", q)], writes=[("ts", q)])
                            P.add("pool", lambda e, q=q, n=n: e.tensor_tensor(mT[:, n, :], ts1[q][:], tcc[q][:], ALU.add),
                                  reads=[("ts", q), ("tc", q)], writes=[("mT", n)])
                    mT_all = [("mT", n) for n in range(16)]
                    for nbk in range(4):
                        ws = wcnt % 3
                        wcnt += 1
                        P.add("pool", lambda e, ws=ws, nbk=nbk: e.dma_start(
                            out=wt[ws][:], in_=self.WB["w_o"][l].rearrange("(c p) n -> p c n", p=128)[:, :, nbk * 512:(nbk + 1) * 512]),
                            writes=[("wt", ws)], dma=True, semkey="L_wt%d" % ws)
                        for s_ in range(4):
                            b = self.nb()
                            for kc in range(16):
                                P.add("pe", lambda e, b=b, ws=ws, kc=kc, s_=s_: e.matmul(
                                    ps[b][:], mT[:, kc, s_ * 128:(s_ + 1) * 128], wt[ws][:, kc, :],
                                    start=(kc == 0), stop=(kc == 15)),
                                    reads=mT_all + [("wt", ws)], writes=[("ps", b)])
                            P.add("dve", lambda e, b=b, s_=s_, nbk=nbk: e.scalar_tensor_tensor(
                                x1[:, s_, nbk * 512:(nbk + 1) * 512], x1[:, s_, nbk * 512:(nbk + 1) * 512], float(ALPHA), ps[b][:],
                                ALU.mult, ALU.add), reads=[("ps", b), "x1"], writes=[("x1s", s_)])
                    for s_ in range(4):
                        self.layer_norm(x1[:, s_, :], ("x1s", s_), gbc, bbc, junk, st4, s_)
                    rbanks = [4, 5, 6, 7]
                    for kc in range(16):
                        b = kc % 4
                        q = kc % 2
                        for s_ in range(4):
                            P.add("pe", lambda e, b=b, kc=kc, s_=s_: e.transpose(
                                ps[b][:, s_ * 128:(s_ + 1) * 128], x1[:, s_, kc * 128:(kc + 1) * 128], idf[:]),
                                reads=[("x1s", s_), "idf"], writes=[("ps", b)])
                        P.add("act", lambda e, b=b, kc=kc: e.activation(x1T[:, kc, :], ps[b][:], AF.Copy),
                              reads=[("ps", b)], writes=[("x1T", kc)])
                        P.add("dve", lambda e, b=b, q=q: e.tensor_copy(xf[q][:], ps[b][:]), reads=[("ps", b)], writes=[("xf", q)])
                        for s_ in range(4):
                            P.add("pe", lambda e, kc=kc, s_=s_, q=q: e.matmul(
                                ps[rbanks[s_]][:, :NE], xf[q][:, s_ * 128:(s_ + 1) * 128], rw[:, kc, :],
                                start=(kc == 0), stop=(kc == 15)),
                                reads=[("xf", q), "rw"], writes=[("ps", rbanks[s_])])
                    for s_ in range(4):
                        self.routing(ps[rbanks[s_]][:, :NE], ("ps", rbanks[s_]), rb, rt_, gates[:, s_, :], s_)
                    P.flush()
                with contextlib.ExitStack() as st:
                    u = self.uid()
                    sb = lambda name, shape, dt: st.enter_context(nc.sbuf_tensor("E%d_" % u + name, shape, dt))
                    wsl = [sb("w%d" % i, [128, 16, 512], BF16) for i in range(5)]
                    hT = sb("hT", [128, 8, TT], BF16)
                    yacc = sb("yacc", [128, 4, D], F32)
                    sgt = [sb("sgt%d" % i, [128, TT], F32) for i in range(2)]
                    gbc = sb("gbc", [128, D], F32)
                    bbc = sb("bbc", [128, D], F32)
                    junk = sb("junk", [128, D], BF16)
                    st4 = sb("st4", [128, 8], F32)
                    ld = lambda eng, out, in_, key: P.add(eng, lambda e: e.dma_start(out=out, in_=in_), writes=[key], dma=True, semkey="L_" + key)
                    ld("sp", gbc[:], W["ln2_g"][l].partition_broadcast(128), "gbc")
                    ld("sp", bbc[:], W["ln2_b"][l].partition_broadcast(128), "bbc")
                    x1T_all = [("x1T", kc) for kc in range(16)]
                    wcnt = 0
                    hcnt = 0
                    for ex in range(NE):
                        for jb in range(2):
                            wsg = wcnt % 5
                            wsu = (wcnt + 1) % 5
                            wcnt += 2
                            for (nm, wsx) in (("ex_w_gate", wsg), ("ex_w_up", wsu)):
                                P.add("pool", lambda e, nm=nm, wsx=wsx, ex=ex, jb=jb: e.dma_start(
                                    out=wsl[wsx][:], in_=self.WB[nm][l, ex].rearrange("(c p) n -> p c n", p=128)[:, :, jb * 512:(jb + 1) * 512]),
                                    writes=[("w", wsx)], dma=True, semkey="L_w%d" % wsx)
                            for jj in range(4):
                                j = jb * 4 + jj
                                bg, bu = self.nb(), self.nb()
                                for kc in range(16):
                                    P.add("pe", lambda e, bg=bg, wsg=wsg, kc=kc, jj=jj: e.matmul(
                                        ps[bg][:], wsl[wsg][:, kc, jj * 128:(jj + 1) * 128], x1T[:, kc, :],
                                        start=(kc == 0), stop=(kc == 15)), reads=x1T_all + [("w", wsg)], writes=[("ps", bg)])
                                for kc in range(16):
                                    P.add("pe", lambda e, bu=bu, wsu=wsu, kc=kc, jj=jj: e.matmul(
                                        ps[bu][:], wsl[wsu][:, kc, jj * 128:(jj + 1) * 128], x1T[:, kc, :],
                                        start=(kc == 0), stop=(kc == 15)), reads=x1T_all + [("w", wsu)], writes=[("ps", bu)])
                                q = hcnt % 2
                                hcnt += 1
                                P.add("act", lambda e, bg=bg, q=q: e.activation(sgt[q][:], ps[bg][:], AF.Silu),
                                      reads=[("ps", bg)], writes=[("sgt", q)])
                                P.add("dve", lambda e, bu=bu, q=q, j=j: e.tensor_tensor(hT[:, j, :], ps[bu][:], sgt[q][:], ALU.mult),
                                      reads=[("ps", bu), ("sgt", q)], writes=[("hT", j)])
                        hT_all = [("hT", j) for j in range(8)]
                        for nbk in range(4):
                            wsd = wcnt % 5
                            wcnt += 1
                            P.add("pool", lambda e, wsd=wsd, ex=ex, nbk=nbk: e.dma_start(
                                out=wsl[wsd][:, 0:8, :],
                                in_=self.WB["ex_w_down"][l, ex].rearrange("(c p) n -> p c n", p=128)[:, :, nbk * 512:(nbk + 1) * 512]),
                                writes=[("w", wsd)], dma=True, semkey="L_w%d" % wsd)
                            for s_ in range(4):
                                b = self.nb()
                                for j in range(8):
                                    P.add("pe", lambda e, b=b, wsd=wsd, j=j, s_=s_: e.matmul(
                                        ps[b][:], hT[:, j, s_ * 128:(s_ + 1) * 128], wsl[wsd][:, j, :],
                                        start=(j == 0), stop=(j == 7)), reads=hT_all + [("w", wsd)], writes=[("ps", b)])
                                ysl = yacc[:, s_, nbk * 512:(nbk + 1) * 512]
                                if ex == 0:
                                    P.add("dve", lambda e, b=b, ysl=ysl, s_=s_, ex=ex: e.tensor_scalar(
                                        ysl, ps[b][:], gates[:, s_, ex:ex + 1], None, ALU.mult),
                                        reads=[("ps", b), "gates"], writes=[("y", s_, nbk)])
                                else:
                                    P.add("dve", lambda e, b=b, ysl=ysl, s_=s_, ex=ex: e.scalar_tensor_tensor(
                                        ysl, ps[b][:], gates[:, s_, ex:ex + 1], ysl, ALU.mult, ALU.add),
                                        reads=[("ps", b), "gates", ("y", s_, nbk)], writes=[("y", s_, nbk)])
                    for s_ in range(4):
                        yk = [("y", s_, nbk) for nbk in range(4)]
                        P.add("dve", lambda e, s_=s_: e.scalar_tensor_tensor(
                            yacc[:, s_, :], x1[:, s_, :], float(ALPHA), yacc[:, s_, :], ALU.mult, ALU.add),
                            reads=yk + [("x1s", s_)], writes=[("ys", s_)])
                        self.layer_norm(yacc[:, s_, :], ("ys", s_), gbc, bbc, junk, st4, s_)
                    dst = xout[t0:t0 + TT, :].rearrange("(s p) d -> p s d", p=128)
                    P.add("sp", lambda e, dst=dst: e.dma_start(out=dst, in_=yacc[:]),
                          reads=[("ys", s_) for s_ in range(4)], dma=True, semkey="st_y")
                    P.flush()

    def layer_norm(self, xap, key, gbc, bbc, junk, st4, s_):
        P = self.P
        k = "st4"
        P.add("dve", lambda e: e.tensor_reduce(st4[:, 0:1], xap, AX.X, ALU.add), reads=[key], writes=[k])
        P.add("pool", lambda e: e.tensor_scalar(st4[:, 1:2], st4[:, 0:1], -1.0 / D, None, ALU.mult), reads=[k], writes=[(k, 1)])
        P.add("act", lambda e: e.activation(xap, xap, AF.Identity, bias=st4[:, 1:2]), reads=[(k, 1), key], writes=[key])
        P.add("act", lambda e: e.activation(junk[:], xap, AF.Square, accum_out=st4[:, 2:3]), reads=[key], writes=["junk", (k, 2)])
        P.add("act", lambda e: e.activation(st4[:, 3:4], st4[:, 2:3], AF.Sqrt, bias=LN_EPS, scale=1.0 / D), reads=[(k, 2)], writes=[(k, 3)])
        P.add("dve", lambda e: e.reciprocal(st4[:, 4:5], st4[:, 3:4]), reads=[(k, 3)], writes=[(k, 4)])
        P.add("dve", lambda e: e.scalar_tensor_tensor(xap, xap, st4[:, 4:5], gbc[:], ALU.mult, ALU.mult),
              reads=[key, (k, 4), "gbc"], writes=[key])
        P.add("pool", lambda e: e.tensor_tensor(xap, xap, bbc[:], ALU.add), reads=[key, "bbc"], writes=[key])

    def routing(self, logits, lkey, rb, rt_, gout, s_):
        P = self.P
        sc, sel, M, N_ = rt_[:, 0:16], rt_[:, 16:32], rt_[:, 32:40], rt_[:, 40:48]
        sel3 = sel.rearrange("p (g e) -> p g e", e=4)
        M3 = M.rearrange("p (g e) -> p g e", e=2)
        N3 = N_.rearrange("p (g e) -> p g e", e=2)
        hi, lo, nn, gsc = rt_[:, 48:52], rt_[:, 52:56], rt_[:, 56:60], rt_[:, 60:64]
        k = "rt"
        seq = []
        A = lambda eng, fn: seq.append((eng, fn))
        A("act", lambda e: e.activation(sc, logits, AF.Sigmoid))
        A("dve", lambda e: e.tensor_tensor(sel, sc, rb[:], ALU.add))
        A("dve", lambda e: e.tensor_tensor(M3, sel3[:, :, 0:2], sel3[:, :, 2:4], ALU.max))
        A("dve", lambda e: e.tensor_tensor(N3, sel3[:, :, 0:2], sel3[:, :, 2:4], ALU.min))
        A("dve", lambda e: e.tensor_tensor(hi.rearrange("p (g o) -> p g o", o=1), M3[:, :, 0:1], M3[:, :, 1:2], ALU.max))
        A("dve", lambda e: e.tensor_tensor(lo.rearrange("p (g o) -> p g o", o=1), M3[:, :, 0:1], M3[:, :, 1:2], ALU.min))
        A("dve", lambda e: e.tensor_tensor(nn.rearrange("p (g o) -> p g o", o=1), N3[:, :, 0:1], N3[:, :, 1:2], ALU.max))
        A("dve", lambda e: e.tensor_tensor(lo, lo, nn, ALU.max))
        A("dve", lambda e: e.tensor_tensor(gsc, hi, lo, ALU.add))
        gm = M[:, 0:1]
        A("dve", lambda e: e.tensor_reduce(gm, gsc, AX.X, ALU.max))
        A("dve", lambda e: e.tensor_scalar(hi, gsc, gm, None, ALU.is_equal))
        A("dve", lambda e: e.tensor_scalar(hi, hi, -1.0, 1e30, ALU.add, ALU.mult))
        for ei in range(4):
            A("dve", lambda e, ei=ei: e.tensor_tensor(sel3[:, :, ei:ei + 1], sel3[:, :, ei:ei + 1],
                                                     hi.rearrange("p (g o) -> p g o", o=1), ALU.add))
        m1 = M[:, 1:2]
        eq = rt_[:, 32:48]
        A("dve", lambda e: e.tensor_reduce(m1, sel, AX.X, ALU.max))
        eq1 = rt_[:, 40:56]
        A("dve", lambda e: e.tensor_scalar(eq1, sel, m1, None, ALU.is_equal))
        A("dve", lambda e: e.scalar_tensor_tensor(sel, eq1, -1e30, sel, ALU.mult, ALU.add))
        m2 = M[:, 2:3]
        A("dve", lambda e: e.tensor_reduce(m2, sel, AX.X, ALU.max))
        A("dve", lambda e: e.scalar_tensor_tensor(sel, sel, m2, eq1, ALU.is_equal, ALU.add))
        A("dve", lambda e: e.tensor_tensor(sel, sel, sc, ALU.mult))
        den = M[:, 3:4]
        A("dve", lambda e: e.tensor_reduce(den, sel, AX.X, ALU.add))
        A("dve", lambda e: e.reciprocal(den, den))
        A("dve", lambda e: e.tensor_scalar(gout, sel, den, None, ALU.mult))
        for i, (eng, fn) in enumerate(seq):
            rd = [k, "rb"] + ([lkey] if i == 0 else [])
            wr = [k] + (["gates"] if i == len(seq) - 1 else [])
            P.add(eng, fn, reads=rd, writes=wr)


def host_weights(W):
    out = {}
    for k, shp in WEIGHT_SHAPES.items():
        out[k] = np.ascontiguousarray(np.asarray(W[k], dtype=np.float32).reshape(shp))
    return out


_CACHE = {}


def _get_program(seqs):
    key = tuple(seqs)
    if key not in _CACHE:
        b = Builder(seqs)
        nc = b.build()
        _CACHE[key] = (b, nc)
    return _CACHE[key]


def kernel(**inputs):
    x_prompt = np.asarray(inputs["x_prompt"], dtype=np.float32)
    x_sample = np.asarray(inputs["x_sample"], dtype=np.float32)
    NB_P, S_P = x_prompt.shape[0], x_prompt.shape[1]
    NB_S, S_S = x_sample.shape[0], x_sample.shape[1]
    n_cores = 8
    assert NB_S == n_cores and n_cores % NB_P == 0
    seqs = [S_S, S_P]
    b, nc = _get_program(seqs)
    hw = host_weights(inputs)
    consts = make_consts(max(seqs))
    base = {k: hw[k] for k in b.W.used()}
    base.update({k: consts[k] for k in b.C.used()})
    in_maps = []
    for c in range(n_cores):
        d = dict(base)
        d["xs0"] = np.ascontiguousarray(x_sample[c])
        d["xs1"] = np.ascontiguousarray(x_prompt[c % NB_P])
        in_maps.append(d)
    res = run_bass_kernel_spmd(nc, in_maps, core_ids=list(range(n_cores)))
    y_sample = np.stack([np.asarray(res.results[c]["ys0"], dtype=np.float32) for c in range(n_cores)], 0)
    y_prompt = np.stack([np.asarray(res.results[c]["ys1"], dtype=np.float32) for c in range(NB_P)], 0)
    return (y_prompt, y_sample)
```
